# Optimizing a Trainium2 kernel written in Bass

```python
import jax
import jax.numpy as jnp
from jax import lax
import numpy as np

D_MODEL = 1024
BATCH = 8
SEQ = 4096
DEPTH = 2

HEAD_DIM = 64
N_HEADS = 4
MIX_WIDTH = N_HEADS * HEAD_DIM
N_BRANCH = 4
NORM_EPS = 1e-6
MASK_VALUE = -1e30

SB_QBLOCK = 128

RWKV_DECAY_LORA = 64
RWKV_AAA_LORA = 64
RWKV_MV_LORA = 32
RWKV_GATE_LORA = 128
RWKV_GN_EPS = 64e-5

MOBA_BLOCK = 256
MOBA_TOPK = 3
MOBA_QCHUNK = 32
ROPE_THETA = 500000.0
ROPE_DIM = HEAD_DIM // 4

HGRN_CHUNK = 16

D_FF = -(-(8 * D_MODEL) // (3 * 256)) * 256

SB_COLS = 3 * MIX_WIDTH
RWKV_COLS = 3 * MIX_WIDTH + RWKV_DECAY_LORA + RWKV_AAA_LORA + RWKV_GATE_LORA
MOBA_COLS = 3 * MIX_WIDTH
HGRN_COLS = 4 * MIX_WIDTH
SB_OFF = 0
RWKV_OFF = SB_OFF + SB_COLS
MOBA_OFF = RWKV_OFF + RWKV_COLS
HGRN_OFF = MOBA_OFF + MOBA_COLS
GATE_OFF = HGRN_OFF + HGRN_COLS
IN_COLS = GATE_OFF + N_BRANCH * D_MODEL

kernel_name = 'hybrid_gated_four_mixer_trunk'


def rms_norm(x, g):
    xf = x.astype(jnp.float32)
    y = xf * lax.rsqrt(jnp.mean(xf * xf, axis=-1, keepdims=True) + NORM_EPS)
    return (y * g.astype(jnp.float32)).astype(x.dtype)


def split_heads(t):
    b, s, _ = t.shape
    return t.reshape(b, s, N_HEADS, HEAD_DIM).transpose(0, 2, 1, 3)


def merge_heads(t):
    b, h, s, d = t.shape
    return t.transpose(0, 2, 1, 3).reshape(b, s, h * d)


def stick_breaking_attention(q, k, v):
    seq = q.shape[2]
    scale = HEAD_DIM ** -0.5
    qf = q.astype(jnp.float32)
    kf = k.astype(jnp.float32)
    vf = v.astype(jnp.float32)
    outs = []
    for blk in range(seq // SB_QBLOCK):
        t0 = blk * SB_QBLOCK
        kv_len = t0 + SB_QBLOCK
        z = jnp.einsum('bhtd,bhsd->bhts', qf[:, :, t0:kv_len], kf[:, :, :kv_len]) * scale
        t_pos = t0 + jnp.arange(SB_QBLOCK)[:, None]
        s_pos = jnp.arange(kv_len)[None, :]
        past = s_pos < t_pos
        log_keep = jnp.where(past, jax.nn.log_sigmoid(-z), 0.0)
        log_between = lax.cumsum(log_keep, axis=3, reverse=True) - log_keep
        att = jnp.where(past, jnp.exp(jax.nn.log_sigmoid(z) + log_between), 0.0)
        outs.append(jnp.einsum('bhts,bhsd->bhtd', att, vf[:, :, :kv_len]))
    return jnp.concatenate(outs, axis=2).astype(v.dtype)


def token_shift(p, mu):
    prev = jnp.pad(p, ((0, 0), (1, 0), (0, 0)))[:, :-1]
    return p + (prev - p) * mu


def rwkv7_recurrence(r, w, k, v, a, b):
    bsz, _, nh, nd = r.shape

    def step(state, inp):
        r_t, w_t, k_t, v_t, a_t, b_t = inp
        sa = jnp.einsum('bhvk,bhk->bhv', state, a_t)
        state = (state * w_t[:, :, None, :] + sa[..., None] * b_t[:, :, None, :]
                 + v_t[..., None] * k_t[:, :, None, :])
        return state, jnp.einsum('bhvk,bhk->bhv', state, r_t)

    xs = (jnp.moveaxis(r, 1, 0), jnp.moveaxis(w, 1, 0), jnp.moveaxis(k, 1, 0),
          jnp.moveaxis(v, 1, 0), jnp.moveaxis(a, 1, 0), jnp.moveaxis(b, 1, 0))
    init = jnp.zeros((bsz, nh, nd, nd), jnp.float32)
    _, y = lax.scan(step, init, xs)
    return jnp.moveaxis(y, 0, 1)


def rwkv7_time_mix(p, mu, w0, w2, a0, a2, g2, k_k, k_a, r_k, gn_w, gn_b, v_first, v0, v1, v2):
    bsz, seq, _ = p.shape
    p = token_shift(p, mu).astype(jnp.float32)
    c = MIX_WIDTH
    r = p[..., :c]
    k = p[..., c:2 * c]
    v = p[..., 2 * c:3 * c]
    o = 3 * c
    wl = p[..., o:o + RWKV_DECAY_LORA]
    o += RWKV_DECAY_LORA
    al = p[..., o:o + RWKV_AAA_LORA]
    o += RWKV_AAA_LORA
    gl = p[..., o:o + RWKV_GATE_LORA]
    w_log = -jax.nn.softplus(-(w0 + jnp.tanh(wl) @ w2)) - 0.5
    decay = jnp.exp(-jnp.exp(w_log))
    if v_first is None:
        v_first = v
    else:
        v = v + (v_first - v) * jax.nn.sigmoid(v0 + (v @ v1) @ v2)
    a = jax.nn.sigmoid(a0 + al @ a2)
    g = jax.nn.sigmoid(gl) @ g2

    def hs(t):
        return t.reshape(bsz, seq, N_HEADS, HEAD_DIM)

    kk = hs(k * k_k)
    kk = kk / jnp.maximum(jnp.sqrt(jnp.sum(kk * kk, axis=-1, keepdims=True)), 1e-12)
    k = k * (1.0 + (a - 1.0) * k_a)
    rh, kh, vh, ah = hs(r), hs(k), hs(v), hs(a)
    y = rwkv7_recurrence(rh, hs(decay), kh, vh, -kk, kk * ah)
    mean = jnp.mean(y, axis=-1, keepdims=True)
    var = jnp.mean(jnp.square(y - mean), axis=-1, keepdims=True)
    y = ((y - mean) * lax.rsqrt(var + RWKV_GN_EPS)).reshape(bsz, seq, c) * gn_w + gn_b
    bonus = jnp.sum(rh * kh * r_k, axis=-1, keepdims=True) * vh
    y = (y + bonus.reshape(bsz, seq, c)) * g
    return y, v_first


def rotary_tables(seq):
    inv_freq = ROPE_THETA ** (-jnp.arange(0, ROPE_DIM, 2, dtype=jnp.float32) / ROPE_DIM)
    ang = jnp.arange(seq, dtype=jnp.float32)[:, None] * inv_freq[None, :]
    return jnp.cos(ang), jnp.sin(ang)


def partial_rotary(t, cos, sin):
    half = ROPE_DIM // 2
    x1 = t[..., :half]
    x2 = t[..., half:ROPE_DIM]
    return jnp.concatenate([x1 * cos - x2 * sin, x2 * cos + x1 * sin, t[..., ROPE_DIM:]], axis=-1)


def moba_attention(q, k, v):
    bsz, nh, seq, hd = q.shape
    dt = v.dtype
    seq_pad = -(-seq // MOBA_BLOCK) * MOBA_BLOCK
    pad = ((0, 0), (0, 0), (0, seq_pad - seq), (0, 0))
    q = jnp.pad(q.astype(jnp.float32), pad)
    k = jnp.pad(k.astype(jnp.float32), pad)
    v = jnp.pad(v.astype(jnp.float32), pad)
    n_blk = seq_pad // MOBA_BLOCK
    k_blocks = k.reshape(bsz, nh, n_blk, MOBA_BLOCK, hd)
    v_blocks = v.reshape(bsz, nh, n_blk, MOBA_BLOCK, hd)
    k_mean = jnp.mean(k_blocks, axis=3)
    gate = jnp.einsum('bhtd,bhnd->bhtn', q, k_mean)
    q_blk = jnp.arange(seq_pad) // MOBA_BLOCK
    fully_past = jnp.arange(n_blk)[None, :] < q_blk[:, None]
    gate = jnp.where(fully_past, gate, MASK_VALUE)
    n_sel = min(MOBA_TOPK, n_blk)
    _, sel_idx = lax.top_k(gate, n_sel)
    sel_valid = sel_idx < q_blk[None, None, :, None]
    bi = jnp.arange(bsz)[:, None, None, None]
    hi = jnp.arange(nh)[None, :, None, None]
    scale = hd ** -0.5
    n_gathered = n_sel * MOBA_BLOCK

    def attend_chunk(t0):
        qc = lax.dynamic_slice_in_dim(q, t0, MOBA_QCHUNK, axis=2)
        idx = lax.dynamic_slice_in_dim(sel_idx, t0, MOBA_QCHUNK, axis=2)
        valid = lax.dynamic_slice_in_dim(sel_valid, t0, MOBA_QCHUNK, axis=2)
        k_sel = k_blocks[bi, hi, idx]
        v_sel = v_blocks[bi, hi, idx]
        s_sel = jnp.einsum('bhqd,bhqnkd->bhqnk', qc, k_sel) * scale
        s_sel = jnp.where(valid[..., None], s_sel, MASK_VALUE)
        own = t0 // MOBA_BLOCK
        k_own = lax.dynamic_index_in_dim(k_blocks, own, axis=2, keepdims=False)
        v_own = lax.dynamic_index_in_dim(v_blocks, own, axis=2, keepdims=False)
        s_own = jnp.einsum('bhqd,bhkd->bhqk', qc, k_own) * scale
        q_pos = t0 + jnp.arange(MOBA_QCHUNK)
        k_pos = own * MOBA_BLOCK + jnp.arange(MOBA_BLOCK)
        s_own = jnp.where(k_pos[None, :] <= q_pos[:, None], s_own, MASK_VALUE)
        scores = jnp.concatenate([s_sel.reshape(bsz, nh, MOBA_QCHUNK, n_gathered), s_own], axis=-1)
        probs = jax.nn.softmax(scores, axis=-1)
        p_sel = probs[..., :n_gathered].reshape(bsz, nh, MOBA_QCHUNK, n_sel, MOBA_BLOCK)
        p_own = probs[..., n_gathered:]
        return (jnp.einsum('bhqnk,bhqnkd->bhqd', p_sel, v_sel)
                + jnp.einsum('bhqk,bhkd->bhqd', p_own, v_own))

    starts = jnp.arange(seq_pad // MOBA_QCHUNK) * MOBA_QCHUNK
    out = lax.map(attend_chunk, starts)
    out = out.transpose(1, 2, 0, 3, 4).reshape(bsz, nh, seq_pad, hd)[:, :, :seq]
    return out.astype(dt)


def hgrn2_mixer(q, f_logit, i, g, lb, gn_g):
    bsz, seq, _ = q.shape
    n_chunk = seq // HGRN_CHUNK
    q = q.astype(jnp.float32)
    i = i.astype(jnp.float32)
    f_logit = f_logit.astype(jnp.float32)
    lb = lb.astype(jnp.float32)
    sig = jax.nn.sigmoid(f_logit)
    log_f = jnp.log(lb + (1.0 - lb) * sig)
    key = (1.0 - lb) * (1.0 - sig)

    def chunks(t):
        return t.reshape(bsz, n_chunk, HGRN_CHUNK, N_HEADS, HEAD_DIM).transpose(0, 3, 1, 2, 4)

    qc, kc, vc, lc = chunks(q), chunks(key), chunks(i), chunks(log_f)
    b = jnp.cumsum(lc, axis=3)
    b_end = b[:, :, :, -1:, :]
    causal = jnp.tril(jnp.ones((HGRN_CHUNK, HGRN_CHUNK), dtype=bool))
    diff = b[:, :, :, :, None, :] - b[:, :, :, None, :, :]
    pair_decay = jnp.exp(jnp.where(causal[:, :, None], diff, 0.0))
    scores = jnp.einsum('bhnck,bhnsk,bhncsk->bhncs', qc, kc, pair_decay)
    scores = jnp.where(causal, scores, 0.0)
    o = jnp.einsum('bhncs,bhnsv->bhncv', scores, vc)
    q_dec = qc * jnp.exp(b)
    k_end = kc * jnp.exp(b_end - b)
    chunk_kv = jnp.einsum('bhnsk,bhnsv->bhnkv', k_end, vc)
    chunk_decay = jnp.exp(b_end[:, :, :, 0, :])

    def step(state, inp):
        dec, kv = inp
        return dec[..., None] * state + kv, state

    init = jnp.zeros((bsz, N_HEADS, HEAD_DIM, HEAD_DIM), jnp.float32)
    _, prev = lax.scan(step, init, (jnp.moveaxis(chunk_decay, 2, 0), jnp.moveaxis(chunk_kv, 2, 0)))
    prev = jnp.moveaxis(prev, 0, 2)
    o = o + jnp.einsum('bhnck,bhnkv->bhncv', q_dec, prev)
    o = o.transpose(0, 2, 3, 1, 4).reshape(bsz, seq, N_HEADS, HEAD_DIM)
    o = o * lax.rsqrt(jnp.mean(o * o, axis=-1, keepdims=True) + NORM_EPS)
    return o.reshape(bsz, seq, MIX_WIDTH) * gn_g * jax.nn.silu(g.astype(jnp.float32))


def setup_inputs(seed: int = 0) -> dict:
    key = jax.random.key(seed)
    ks = jax.random.split(key, 32)
    L, D, W = DEPTH, D_MODEL, MIX_WIDTH
    LV = max(DEPTH - 1, 0)

    def nrm(k, shape, scale):
        return jax.random.normal(k, shape, jnp.float32) * scale

    return {
        'x': nrm(ks[0], (BATCH, SEQ, D), 1.0),
        'norm1_g': 1.0 + nrm(ks[1], (L, D), 0.02),
        'w_in': nrm(ks[2], (L, D, IN_COLS), D ** -0.5),
        'rwkv_mu': jax.random.uniform(ks[3], (L, RWKV_COLS), jnp.float32),
        'rwkv_w0': nrm(ks[4], (L, W), 0.5),
        'rwkv_w2': nrm(ks[5], (L, RWKV_DECAY_LORA, W), 0.1),
        'rwkv_a0': nrm(ks[6], (L, W), 0.1),
        'rwkv_a2': nrm(ks[7], (L, RWKV_AAA_LORA, W), 0.1),
        'rwkv_g2': nrm(ks[8], (L, RWKV_GATE_LORA, W), RWKV_GATE_LORA ** -0.5),
        'rwkv_k_k': 1.0 + nrm(ks[9], (L, W), 0.1),
        'rwkv_k_a': 1.0 + nrm(ks[10], (L, W), 0.1),
        'rwkv_r_k': nrm(ks[11], (L, N_HEADS, HEAD_DIM), 0.1),
        'rwkv_gn_w': 1.0 + nrm(ks[12], (L, W), 0.02),
        'rwkv_gn_b': nrm(ks[13], (L, W), 0.02),
        'rwkv_v0': nrm(ks[14], (LV, W), 0.1),
        'rwkv_v1': nrm(ks[15], (LV, W, RWKV_MV_LORA), W ** -0.5),
        'rwkv_v2': nrm(ks[16], (LV, RWKV_MV_LORA, W), 0.1),
        'hgrn_lb_logits': nrm(ks[17], (L, W), 1.0),
        'hgrn_gn_g': 1.0 + nrm(ks[18], (L, W), 0.02),
        'w_branch': nrm(ks[19], (L, N_BRANCH, W, D), W ** -0.5),
        'w_out': nrm(ks[20], (L, D, D), D ** -0.5),
        'norm2_g': 1.0 + nrm(ks[21], (L, D), 0.02),
        'w_ffn_gate': nrm(ks[22], (L, D, D_FF), D ** -0.5),
        'w_ffn_up': nrm(ks[23], (L, D, D_FF), D ** -0.5),
        'w_ffn_down': nrm(ks[24], (L, D_FF, D), D_FF ** -0.5),
        'final_g': 1.0 + nrm(ks[25], (D,), 0.02),
    }


def reference(x, norm1_g, w_in, rwkv_mu, rwkv_w0, rwkv_w2, rwkv_a0, rwkv_a2, rwkv_g2,
              rwkv_k_k, rwkv_k_a, rwkv_r_k, rwkv_gn_w, rwkv_gn_b, rwkv_v0, rwkv_v1, rwkv_v2,
              hgrn_lb_logits, hgrn_gn_g, w_branch, w_out, norm2_g, w_ffn_gate, w_ffn_up,
              w_ffn_down, final_g):
    seq = x.shape[1]
    W = MIX_WIDTH
    cos, sin = rotary_tables(seq)
    lb_w = jax.nn.softmax(hgrn_lb_logits.astype(jnp.float32), axis=0)
    lower_bounds = jnp.cumsum(lb_w, axis=0) - lb_w[0]
    v_first = None
    for layer in range(DEPTH):
        h = rms_norm(x, norm1_g[layer])
        w_l = w_in[layer]

        def cols(lo, width):
            return h @ w_l[:, lo:lo + width]

        sb = cols(SB_OFF, SB_COLS)
        y_sb = merge_heads(stick_breaking_attention(
            split_heads(sb[..., :W]), split_heads(sb[..., W:2 * W]), split_heads(sb[..., 2 * W:])))
        if layer == 0:
            v0 = v1 = v2 = None
        else:
            v0, v1, v2 = rwkv_v0[layer - 1], rwkv_v1[layer - 1], rwkv_v2[layer - 1]
        y_rwkv, v_first = rwkv7_time_mix(
            cols(RWKV_OFF, RWKV_COLS), rwkv_mu[layer], rwkv_w0[layer], rwkv_w2[layer],
            rwkv_a0[layer], rwkv_a2[layer], rwkv_g2[layer], rwkv_k_k[layer], rwkv_k_a[layer],
            rwkv_r_k[layer], rwkv_gn_w[layer], rwkv_gn_b[layer], v_first, v0, v1, v2)
        mc = cols(MOBA_OFF, MOBA_COLS)
        q_m = partial_rotary(split_heads(mc[..., :W]), cos, sin)
        k_m = partial_rotary(split_heads(mc[..., W:2 * W]), cos, sin)
        y_moba = merge_heads(moba_attention(q_m, k_m, split_heads(mc[..., 2 * W:])))
        hc = cols(HGRN_OFF, HGRN_COLS)
        y_hgrn = hgrn2_mixer(hc[..., :W], hc[..., W:2 * W], hc[..., 2 * W:3 * W], hc[..., 3 * W:],
                             lower_bounds[layer], hgrn_gn_g[layer])
        merged = None
        for n, y_n in enumerate((y_sb, y_rwkv, y_moba, y_hgrn)):
            gate = jax.nn.sigmoid(cols(GATE_OFF + n * D_MODEL, D_MODEL))
            term = gate * (y_n.astype(x.dtype) @ w_branch[layer, n])
            merged = term if merged is None else merged + term
        x = x + merged @ w_out[layer]
        h2 = rms_norm(x, norm2_g[layer])
        x = x + (jax.nn.silu(h2 @ w_ffn_gate[layer]) * (h2 @ w_ffn_up[layer])) @ w_ffn_down[layer]
    return rms_norm(x, final_g)
```

```python
import numpy as np
import ml_dtypes
from contextlib import ExitStack
import concourse.bass as bass
import concourse.mybir as mybir
from concourse.bass_utils import run_bass_kernel_spmd

F32 = mybir.dt.float32
BF16 = mybir.dt.bfloat16
AF = mybir.ActivationFunctionType
ALU = mybir.AluOpType
AX = mybir.AxisListType

D = 1024
T = 4096
DEPTH = 2
HD = 64
NH = 4
W = 256
DFF = 2816
SB_OFF = 0
RWKV_OFF = 768
MOBA_OFF = 1792
HGRN_OFF = 2560
GATE_OFF = 3584
IN_COLS = 7680
NEG_BIG = -30000.0
NORM_EPS = 1e-6
GN_EPS = 64e-5
NCORES = 8


class Buf:
    __slots__ = ("w", "r", "name", "psum")

    def __init__(self, name=""):
        self.w = None
        self.r = []
        self.name = name
        self.psum = False


class Tl:
    def __init__(self, t, name=""):
        self.t = t
        self.b = Buf(name)

    def __getitem__(self, idx):
        return self.t[idx]


def _bufs(lst):
    out = []
    for x in lst:
        if x is None:
            continue
        out.append(x.b if isinstance(x, Tl) else x)
    return out


class SemEpoch:
    def __init__(self, kb, name):
        self.sem = kb.es.enter_context(kb.nc.semaphore(name))
        self.cnt = 0


class Eng:
    def __init__(self, kb, name, eng):
        self.kb = kb
        self.name = name
        self.eng = eng
        self.n_ep = 0
        self.ep = SemEpoch(kb, "s_%s_0" % name)
        self.seen = {}
        self.total = 0

    @property
    def sem(self):
        return self.ep.sem

    @property
    def cnt(self):
        return self.ep.cnt

    def new_epoch(self):
        self.n_ep += 1
        self.ep = SemEpoch(self.kb, "s_%s_%d" % (self.name, self.n_ep))

    def wait(self, src, val):
        if self.seen.get(id(src), 0) >= val:
            return
        self.eng.wait_ge(src.sem, val)
        self.seen[id(src)] = val


class DmaSlot:
    def __init__(self, kb, i):
        self.sem = kb.es.enter_context(kb.nc.semaphore("s_dma%d" % i))
        self.cnt = 0


class KB:
    def __init__(self, nc, es, ndma=24):
        self.nc = nc
        self.es = es
        self.PE = Eng(self, "pe", nc.tensor)
        self.ACT = Eng(self, "act", nc.scalar)
        self.DVE = Eng(self, "dve", nc.vector)
        self.POOL = Eng(self, "pool", nc.gpsimd)
        self.SP = Eng(self, "sp", nc.sync)
        self.slots = [DmaSlot(self, i) for i in range(ndma)]
        self.slot_i = 0
        self.n_ins = 0

    def _deps(self, E, R, Wr):
        deps = []
        mine = E.ep
        for b in R:
            if b.w is not None:
                deps.append(b.w)
            if b.psum:
                for ev in b.r:
                    if ev[0] is not mine:
                        deps.append(ev)
        for b in Wr:
            if b.w is not None and (b.w[0] is not mine or E is not self.PE):
                deps.append(b.w)
            for ev in b.r:
                if ev[0] is not mine:
                    deps.append(ev)
        for src, val in deps:
            E.wait(src, val)

    def op(self, E, fn, R=(), Wr=()):
        R = _bufs(R)
        Wr = _bufs(Wr)
        self._deps(E, R, Wr)
        ins = fn()
        E.ep.cnt += 1
        E.total += 1
        ins.then_inc(E.ep.sem, 1)
        ev = (E.ep, E.ep.cnt)
        for b in R:
            b.r.append(ev)
        for b in Wr:
            b.w = ev
            b.r = []
        self.n_ins += 1
        return ins

    def dma(self, out, in_, R=(), Wr=(), q=None):
        q = q or self.SP
        R = _bufs(R)
        Wr = _bufs(Wr)
        self._deps(q, R, Wr)
        slot = self.slots[self.slot_i]
        self.slot_i = (self.slot_i + 1) % len(self.slots)
        if slot.cnt:
            q.wait(slot, slot.cnt)
        ins = q.eng.dma_start(out=out, in_=in_)
        slot.cnt += 16
        ins.then_inc(slot.sem, 16)
        ev = (slot, slot.cnt)
        for b in R:
            b.r.append(ev)
        for b in Wr:
            b.w = ev
            b.r = []
        self.n_ins += 1
        return ev

    def _uniq(self, name):
        self.n_names = getattr(self, "n_names", 0) + 1
        return "%s_%d" % (name, self.n_names)

    def sb(self, name, shape, dt, es=None):
        es = es or self.es
        name = self._uniq(name)
        return Tl(es.enter_context(self.nc.sbuf_tensor(name, list(shape), dt)), name)

    def ps(self, name, shape, dt=F32, es=None):
        es = es or self.es
        name = self._uniq(name)
        t_ = Tl(es.enter_context(self.nc.psum_tensor(name, list(shape), dt)), name)
        t_.b.psum = True
        return t_

    def barrier(self):
        engs = [self.PE, self.ACT, self.DVE, self.POOL, self.SP]
        for E in engs:
            for F in engs:
                if F is not E and F.ep.cnt:
                    E.wait(F.ep, F.ep.cnt)
            for sl in self.slots:
                if sl.cnt:
                    E.wait(sl, sl.cnt)
        for E in engs:
            if E.ep.cnt > 12000:
                E.new_epoch()

    def finish(self, bufs):
        for b in _bufs(bufs):
            if b.w is not None:
                self.SP.wait(b.w[0], b.w[1])
        self.barrier()


def pipeline(units, stages):
    n = len(units)
    ns = len(stages)
    for step in range(n + ns - 1):
        for s, st in enumerate(stages):
            i = step - s
            if 0 <= i < n:
                st(units[i], i)


def host_consts():
    c = {}
    s = np.arange(128)[:, None]
    t = np.arange(512)[None, :]
    sbm = np.stack([((128 * o + s) < t) for o in range(4)]).astype(np.float32)
    c["c_mask_lt"] = sbm
    c["c_mask_le"] = np.stack([((128 * o + s) <= t) for o in range(4)]).astype(np.float32)
    a = np.arange(128)
    c["c_tri_ge"] = (a[:, None] >= a[None, :]).astype(np.float32)
    c["c_ones"] = np.ones((128, 128), np.float32)
    c["c_ident"] = np.eye(128, dtype=np.float32)
    c["c_blk64"] = (a[:, None] // 64 == a[None, :] // 64).astype(np.float32)
    half = 8
    inv_freq = (np.float32(500000.0) ** (-np.arange(0, 16, 2, dtype=np.float32) / np.float32(16))).astype(np.float32)
    ang = (np.arange(T, dtype=np.float32)[:, None] * inv_freq[None, :]).astype(np.float32)
    cos, sin = np.cos(ang).astype(np.float32), np.sin(ang).astype(np.float32)
    cosF = np.ones((64, T), np.float32)
    sinF = np.zeros((64, T), np.float32)
    cosF[0:8] = cos.T
    cosF[8:16] = cos.T
    sinF[0:8] = -sin.T
    sinF[8:16] = sin.T
    c["c_cosF"] = np.concatenate([cosF, cosF], 0)
    c["c_sinF"] = np.concatenate([sinF, sinF], 0)
    n = np.arange(16)
    qb = np.arange(16)[:, None]
    past = (n[None, :] < qb)
    own = (n[None, :] == qb)
    BIG = -NEG_BIG
    def rep(m):
        return np.ascontiguousarray(np.broadcast_to(m[None, :, None, :], (128, 16, 4, 16))).astype(np.float32)
    c["c_pastneg"] = rep(np.where(past, 0.0, -1e30))
    c["c_pastbig"] = rep(np.where(past, BIG, 0.0))
    c["c_ownm"] = rep(np.where(own, 0.0, -BIG))
    E = np.zeros((128, 16, 128), np.float32)
    for h in range(2):
        for i in range(16):
            E[64 * h + i, i, :] = 1.0
    c["c_E_b"] = E.astype(ml_dtypes.bfloat16)
    c["c_hgmask"] = ((a[:, None] // 32 == a[None, :] // 32) & (a[:, None] <= a[None, :])).astype(np.float32)
    c["c_scanmask"] = (np.broadcast_to((np.arange(512) % 32 != 0)[None, :], (128, 512))).astype(np.float32).copy()
    c["c_rw_low"] = (a[:, None] > a[None, :]).astype(np.float32)
    c["c_rw_upS"] = (a[:, None] < a[None, :]).astype(np.float32)
    c["c_rw_upI"] = (a[:, None] <= a[None, :]).astype(np.float32)
    c["c_scanmask128"] = (np.broadcast_to((np.arange(512) % 128 != 0)[None, :], (128, 512))).astype(np.float32).copy()
    c["c_mask_le_b"] = c["c_mask_le"].astype(ml_dtypes.bfloat16)
    c["c_mask_lt_b"] = c["c_mask_lt"].astype(ml_dtypes.bfloat16)
    return c


CONST_SHAPES = {
    "c_mask_lt": [4, 128, 512],
    "c_mask_le": [4, 128, 512],
    "c_tri_ge": [128, 128],
    "c_ones": [128, 128],
    "c_ident": [128, 128],
    "c_blk64": [128, 128],
    "c_cosF": [128, 4096],
    "c_sinF": [128, 4096],
    "c_pastneg": [128, 16, 4, 16],
    "c_pastbig": [128, 16, 4, 16],
    "c_ownm": [128, 16, 4, 16],
    "c_E_b": [128, 16, 128],
    "c_hgmask": [128, 128],
    "c_scanmask": [128, 512],
    "c_rw_low": [128, 128],
    "c_rw_upS": [128, 128],
    "c_rw_upI": [128, 128],
    "c_scanmask128": [128, 512],
    "c_mask_le_b": [4, 128, 512],
    "c_mask_lt_b": [4, 128, 512],
}


def const_dtype(k):
    return BF16 if k.endswith("_b") else F32


WEIGHT_SHAPES = {
    "norm1_g": [2, 1024], "w_in": [2, 1024, 7680], "rwkv_mu": [2, 1024], "rwkv_w0": [2, 256],
    "rwkv_w2": [2, 64, 256], "rwkv_a0": [2, 256], "rwkv_a2": [2, 64, 256], "rwkv_g2": [2, 128, 256],
    "rwkv_k_k": [2, 256], "rwkv_k_a": [2, 256], "rwkv_r_k": [2, 4, 64], "rwkv_gn_w": [2, 256],
    "rwkv_gn_b": [2, 256], "rwkv_v0": [1, 256], "rwkv_v1": [1, 256, 32], "rwkv_v2": [1, 32, 256],
    "hgrn_lb_logits": [2, 256], "hgrn_gn_g": [2, 256], "w_branch": [2, 4, 256, 1024],
    "w_out": [2, 1024, 1024], "norm2_g": [2, 1024], "w_ffn_gate": [2, 1024, 2816],
    "w_ffn_up": [2, 1024, 2816], "w_ffn_down": [2, 2816, 1024], "final_g": [1024],
}


class DT:
    def __init__(self, ap):
        self.ap = ap
        self.bufs = {}

    def buf(self, i):
        if i not in self.bufs:
            self.bufs[i] = Buf()
        return self.bufs[i]

    def all(self):
        return list(self.bufs.values())


class Ctx:
    def __init__(self):
        self.dram = {}

    def xbuf(self, dt, i):
        return dt.buf(i)


def load_consts(kb, cx):
    nc = kb.nc
    cx.ones_f = kb.sb("ones_f", [128, 128], F32)
    cx.tri_f = kb.sb("tri_f", [128, 128], F32)
    cx.ident_f = kb.sb("ident_f", [128, 128], F32)
    cx.blk_f = kb.sb("blk_f", [128, 128], F32)
    cx.ones_b = kb.sb("ones_b", [128, 128], BF16)
    cx.ident_b = kb.sb("ident_b", [128, 128], BF16)
    kb.dma(cx.ones_f[:], cx.dram["c_ones"][:, :], Wr=[cx.ones_f])
    kb.dma(cx.tri_f[:], cx.dram["c_tri_ge"][:, :], Wr=[cx.tri_f])
    kb.dma(cx.ident_f[:], cx.dram["c_ident"][:, :], Wr=[cx.ident_f])
    kb.dma(cx.blk_f[:], cx.dram["c_blk64"][:, :], Wr=[cx.blk_f])
    cx.eps_norm = kb.sb("eps_norm", [128, 1], F32)
    cx.one_c = kb.sb("one_c", [128, 1], F32)
    kb.op(kb.POOL, lambda: nc.gpsimd.memset(cx.eps_norm[:], NORM_EPS), Wr=[cx.eps_norm])
    kb.op(kb.POOL, lambda: nc.gpsimd.memset(cx.one_c[:], 1.0), Wr=[cx.one_c])
    kb.op(kb.DVE, lambda: nc.vector.tensor_copy(out=cx.ones_b[:], in_=cx.ones_f[:]), R=[cx.ones_f], Wr=[cx.ones_b])
    kb.op(kb.DVE, lambda: nc.vector.tensor_copy(out=cx.ident_b[:], in_=cx.ident_f[:]), R=[cx.ident_f], Wr=[cx.ident_b])


class WStream:
    def __init__(self, kb, es, name, kch, ncols, nbuf=2, alloc_wb=True):
        self.kb = kb
        self.kch = kch
        self.ncols = ncols
        self.stg = [kb.sb("%s_stg%d" % (name, i), [128, kch, ncols], F32, es) for i in range(nbuf)]
        self.wb = [kb.sb("%s_wb%d" % (name, i), [128, kch, ncols], BF16, es) for i in range(nbuf)] if alloc_wb else []
        self.i = 0

    def load(self, src_ap, eng=None, kch=None, ncols=None):
        kb = self.kb
        nc = kb.nc
        kch = kch or self.kch
        ncols = ncols or self.ncols
        i = self.i
        self.i = (self.i + 1) % len(self.stg)
        stg, wb = self.stg[i], self.wb[i]
        kb.dma(stg[:, :kch, :ncols], src_ap, Wr=[stg])
        eng = eng or kb.DVE
        kb.op(eng, lambda: eng.eng.tensor_copy(out=wb[:, :kch, :ncols], in_=stg[:, :kch, :ncols]), R=[stg], Wr=[wb])
        return wb


def wload(kb, ws, dst, src_ap, eng=None):
    nc = kb.nc
    i = ws.i
    ws.i = (ws.i + 1) % len(ws.stg)
    stg = ws.stg[i]
    kch, ncols = dst.t.shape[1], dst.t.shape[2]
    kb.dma(stg[:, :kch, :ncols], src_ap, Wr=[stg])
    eng = eng or kb.DVE
    kb.op(eng, lambda: eng.eng.tensor_copy(out=dst[:], in_=stg[:, :kch, :ncols]), R=[stg], Wr=[dst])
    return dst


def wcols(w2d, c0, ncols):
    return w2d.rearrange("(kc p) f -> p kc f", p=128)[:, :, c0:c0 + ncols]


def phase_norm(kb, cx, xT, g_ap, hT, out_dram=None):
    nc = kb.nc
    with ExitStack() as es:
        gT = kb.sb("n_gT", [128, 8], F32, es)
        kb.dma(gT[:], g_ap.rearrange("(kc p) -> p kc", p=128), Wr=[gT])
        xt = [kb.sb("n_x%d" % i, [128, 8, 512], F32, es) for i in range(2)]
        sq = [kb.sb("n_sq%d" % i, [128, 8, 512], F32, es) for i in range(2)]
        rs = [kb.sb("n_rs%d" % i, [128, 512], F32, es) for i in range(2)]
        ob = [kb.sb("n_ob%d" % i, [128, 8, 512], F32, es) for i in range(2)] if out_dram is not None else None
        pss = [kb.ps("n_ps%d" % i, [128, 512], F32, es) for i in range(2)]
        xv = xT.ap.rearrange("(kc p) t -> p kc t", p=128)
        for tg in range(T // 512):
            i = tg % 2
            x_, sq_, rs_, ps_ = xt[i], sq[i], rs[i], pss[i]
            kb.dma(x_[:], xv[:, :, tg * 512:(tg + 1) * 512], R=[cx.xbuf(xT, tg)], Wr=[x_])
            kb.op(kb.ACT, lambda: nc.scalar.activation(out=sq_[:], in_=x_[:], func=AF.Square), R=[x_], Wr=[sq_])
            for kc in range(8):
                kb.op(kb.PE, lambda: nc.tensor.matmul(ps_[:], lhsT=cx.ones_f[:], rhs=sq_[:, kc, :],
                                                      start=(kc == 0), stop=(kc == 7)),
                      R=[cx.ones_f, sq_], Wr=[ps_])
            kb.op(kb.ACT, lambda: nc.scalar.activation(out=rs_[:], in_=ps_[:], func=AF.Sqrt, scale=1.0 / D,
                                                       bias=cx.eps_norm[:, 0:1]),
                  R=[ps_, cx.eps_norm], Wr=[rs_])
            kb.op(kb.DVE, lambda: nc.vector.reciprocal(out=rs_[:], in_=rs_[:]), R=[rs_], Wr=[rs_])
            for kc in range(8):
                if out_dram is None:
                    o_ap = hT[:, kc, tg * 512:(tg + 1) * 512]
                    wr = [hT]
                else:
                    o_ap = ob[i][:, kc, :]
                    wr = [ob[i]]
                kb.op(kb.DVE, lambda: nc.vector.scalar_tensor_tensor(out=o_ap, in0=x_[:, kc, :], scalar=gT[:, kc:kc + 1],
                                                                     in1=rs_[:], op0=ALU.mult, op1=ALU.mult),
                      R=[x_, gT, rs_], Wr=wr)
            if out_dram is not None:
                kb.dma(out_dram.ap.rearrange("(kc p) t -> p kc t", p=128)[:, :, tg * 512:(tg + 1) * 512], ob[i][:],
                       R=[ob[i]], Wr=[cx.xbuf(out_dram, tg)])
        kb.barrier()


def phase_sb(kb, cx, layer, hT, yT, dbg=None):
    nc = kb.nc
    w2d = cx.dram["w_in"][layer]
    with ExitStack() as es:
        QT = kb.sb("sb_QT", [128, 2, T], BF16, es)
        NQ = kb.sb("sb_NQ", [128, 2, T], BF16, es)
        KT = kb.sb("sb_KT", [128, 2, T], BF16, es)
        V = kb.sb("sb_V", [128, 32, 256], BF16, es)
        mlt_f = kb.sb("sb_mltf", [128, 4, 512], F32, es)
        mlt_b = kb.sb("sb_mltb", [128, 4, 512], BF16, es)
        kb.dma(mlt_f[:], cx.dram["c_mask_lt"].rearrange("o s t -> s o t"), Wr=[mlt_f])
        kb.dma(mlt_b[:], cx.dram["c_mask_lt_b"].rearrange("o s t -> s o t"), Wr=[mlt_b])
        if dbg == "p0":
            kb.dma(yT.ap[0:128, 0:2048], mlt_b[:].rearrange("p o t -> p (o t)"), R=[mlt_b], Wr=[cx.xbuf(yT, 0)])
            return
        with ExitStack() as es2:
            ws = WStream(kb, es2, "sb_w", 8, 256, nbuf=2)
            if dbg == "p1":
                wb = ws.load(wcols(w2d, SB_OFF, 256))
                kb.dma(yT.ap[0:128, 0:2048], wb[:].rearrange("p o t -> p (o t)"), R=[wb], Wr=[cx.xbuf(yT, 0)])
                return
            pp = [kb.ps("sb_pp%d" % i, [128, 512], F32, es2) for i in range(2)]
            n = 0
            for which in range(2 if dbg != "p3" else 0):
                wb = ws.load(wcols(w2d, SB_OFF + which * 256, 256))
                for fb in range(2):
                    for tg in range(8):
                        ps_ = pp[n % 2]
                        n += 1
                        for kc in range(8):
                            kb.op(kb.PE, lambda: nc.tensor.matmul(ps_[:], lhsT=wb[:, kc, fb * 128:(fb + 1) * 128],
                                                                  rhs=hT[:, kc, tg * 512:(tg + 1) * 512],
                                                                  start=(kc == 0), stop=(kc == 7)),
                                  R=[wb, hT], Wr=[ps_])
                        sl = slice(tg * 512, (tg + 1) * 512)
                        if which == 0:
                            kb.op(kb.ACT, lambda: nc.scalar.activation(out=QT[:, fb, sl], in_=ps_[:], func=AF.Copy, scale=0.125),
                                  R=[ps_], Wr=[QT])
                            kb.op(kb.DVE, lambda: nc.vector.tensor_scalar(out=NQ[:, fb, sl], in0=QT[:, fb, sl], scalar1=-1.0, scalar2=None,
                                                                          op0=ALU.mult),
                                  R=[QT], Wr=[NQ])
                        else:
                            kb.op(kb.ACT, lambda: nc.scalar.activation(out=KT[:, fb, sl], in_=ps_[:], func=AF.Copy),
                                  R=[ps_], Wr=[KT])
            wb = ws.load(wcols(w2d, SB_OFF + 512, 256))
            for tt in range(32 if dbg not in ("p2", "p2a") else 0):
                ps_ = pp[n % 2]
                n += 1
                for kc in range(8):
                    kb.op(kb.PE, lambda: nc.tensor.matmul(ps_[:, 0:256], lhsT=hT[:, kc, tt * 128:(tt + 1) * 128],
                                                          rhs=wb[:, kc, :], start=(kc == 0), stop=(kc == 7)),
                          R=[wb, hT], Wr=[ps_])
                kb.op(kb.ACT, lambda: nc.scalar.activation(out=V[:, tt, :], in_=ps_[:, 0:256], func=AF.Copy), R=[ps_], Wr=[V])
            kb.barrier()
        if dbg == "p3":
            kb.dma(yT.ap[0:128, :], V[:, 0:16, :].rearrange("p o t -> p (o t)"), R=[V], Wr=[cx.xbuf(yT, 0)])
            return
        if dbg in ("proj", "p2", "p2a"):
            for b2 in range(2):
                kb.dma(yT.ap[b2 * 128:(b2 + 1) * 128, :], KT[:, b2, :], R=[KT], Wr=[cx.xbuf(yT, 0)])
            return
        with ExitStack() as es2:
            NB = 3
            pz = [kb.ps("sb_pz%d" % i, [128, 512], F32, es2) for i in range(2)]
            pc = [kb.ps("sb_pc%d" % i, [128, 512], F32, es2) for i in range(2)]
            po = [kb.ps("sb_po%d" % i, [64, 512], F32, es2) for i in range(2)]
            ee = [kb.sb("sb_e%d" % i, [128, 512], F32, es2) for i in range(NB)]
            lk = [kb.sb("sb_lk%d" % i, [128, 512], F32, es2) for i in range(NB)]
            at = [kb.sb("sb_at%d" % i, [128, 512], BF16, es2) for i in range(NB)]
            arun = [kb.sb("sb_ar%d" % i, [128, 512], F32, es2) for i in range(2)]
            yst = [kb.sb("sb_y%d" % i, [64, 512], BF16, es2) for i in range(2)]
            units = []
            gi = 0
            for h in range(NH):
                for g in range(8):
                    for j in range(4 * g + 3, -1, -1):
                        units.append((h, g, j, gi))
                    gi += 1

            def stA(u, i):
                h, g, j, gi = u
                blk, po_ = h // 2, (h % 2) * 64
                pz_, e_, lk_ = pz[i % 2], ee[i % NB], lk[i % NB]
                kb.op(kb.PE, lambda: nc.tensor.matmul(pz_[:], lhsT=KT[po_:po_ + 64, blk, j * 128:(j + 1) * 128],
                                                      rhs=QT[po_:po_ + 64, blk, g * 512:(g + 1) * 512], start=True, stop=True),
                      R=[KT, QT], Wr=[pz_])
                kb.op(kb.ACT, lambda: nc.scalar.activation(out=e_[:], in_=pz_[:], func=AF.Exp), R=[pz_], Wr=[e_])
                kb.op(kb.ACT, lambda: nc.scalar.activation(out=lk_[:], in_=e_[:], func=AF.Ln, bias=cx.one_c[:, 0:1]),
                      R=[e_, cx.one_c], Wr=[lk_])
                o = j - 4 * g
                if o >= 0:
                    kb.op(kb.POOL, lambda: nc.gpsimd.tensor_tensor(out=lk_[:], in0=lk_[:], in1=mlt_f[:, o, :], op=ALU.mult),
                          R=[lk_, mlt_f], Wr=[lk_])

            def stB(u, i):
                h, g, j, gi = u
                blk, po_ = h // 2, (h % 2) * 64
                pc_, lk_, at_, ar_ = pc[i % 2], lk[i % NB], at[i % NB], arun[gi % 2]
                first = (j == 4 * g + 3)
                kb.op(kb.PE, lambda: nc.tensor.matmul(pc_[:], lhsT=cx.tri_f[:], rhs=lk_[:], start=True, stop=False),
                      R=[cx.tri_f, lk_], Wr=[pc_])
                if not first:
                    kb.op(kb.PE, lambda: nc.tensor.matmul(pc_[:], lhsT=cx.ones_f[:], rhs=ar_[:], start=False, stop=False),
                          R=[cx.ones_f, ar_], Wr=[pc_])
                kb.op(kb.PE, lambda: nc.tensor.matmul(pc_[:], lhsT=KT[po_:po_ + 64, blk, j * 128:(j + 1) * 128],
                                                      rhs=NQ[po_:po_ + 64, blk, g * 512:(g + 1) * 512], start=False, stop=True),
                      R=[KT, NQ], Wr=[pc_])
                kb.op(kb.ACT, lambda: nc.scalar.activation(out=at_[:], in_=pc_[:], func=AF.Exp, scale=-1.0), R=[pc_], Wr=[at_])
                o = j - 4 * g
                if o >= 0:
                    kb.op(kb.POOL, lambda: nc.gpsimd.tensor_tensor(out=at_[:], in0=at_[:], in1=mlt_b[:, o, :], op=ALU.mult),
                          R=[at_, mlt_b], Wr=[at_])
                if j > 0:
                    if first:
                        kb.op(kb.DVE, lambda: nc.vector.tensor_copy(out=ar_[:], in_=lk_[:]), R=[lk_], Wr=[ar_])
                    else:
                        kb.op(kb.DVE, lambda: nc.vector.tensor_tensor(out=ar_[:], in0=ar_[:], in1=lk_[:], op=ALU.add),
                              R=[lk_, ar_], Wr=[ar_])

            def stC(u, i):
                h, g, j, gi = u
                at_, po_t = at[i % NB], po[gi % 2]
                first = (j == 4 * g + 3)
                kb.op(kb.PE, lambda: nc.tensor.matmul(po_t[:], lhsT=V[:, j, h * 64:(h + 1) * 64], rhs=at_[:],
                                                      start=first, stop=(j == 0)),
                      R=[V, at_], Wr=[po_t])
                if j == 0:
                    y_ = yst[gi % 2]
                    kb.op(kb.DVE, lambda: nc.vector.tensor_copy(out=y_[:], in_=po_t[:]), R=[po_t], Wr=[y_])
                    kb.dma(yT.ap[h * 64:(h + 1) * 64, g * 512:(g + 1) * 512], y_[:], R=[y_], Wr=[cx.xbuf(yT, g)])

            if dbg is not None and dbg.startswith("n"):
                units = units[:int(dbg[1:])]
            pipeline(units, [stA, stB, stC])
            kb.barrier()


def proj_fm(kb, hT, wb, ncols, pp, cnt, epi):
    nc = kb.nc
    for fb in range(ncols // 128):
        for tg in range(T // 512):
            ps_ = pp[cnt[0] % len(pp)]
            cnt[0] += 1
            for kc in range(8):
                kb.op(kb.PE, lambda: nc.tensor.matmul(ps_[:], lhsT=wb[:, kc, fb * 128:(fb + 1) * 128],
                                                      rhs=hT[:, kc, tg * 512:(tg + 1) * 512],
                                                      start=(kc == 0), stop=(kc == 7)),
                      R=[wb, hT], Wr=[ps_])
            epi(fb, tg, ps_)


def proj_tm(kb, hT, wb, ncols, pp, cnt, epi):
    nc = kb.nc
    for tt in range(T // 128):
        ps_ = pp[cnt[0] % len(pp)]
        cnt[0] += 1
        for kc in range(8):
            kb.op(kb.PE, lambda: nc.tensor.matmul(ps_[:, 0:ncols], lhsT=hT[:, kc, tt * 128:(tt + 1) * 128],
                                                  rhs=wb[:, kc, 0:ncols], start=(kc == 0), stop=(kc == 7)),
                  R=[wb, hT], Wr=[ps_])
        epi(tt, ps_)


def phase_moba(kb, cx, layer, hT, yT, dbg=None):
    nc = kb.nc
    w2d = cx.dram["w_in"][layer]
    with ExitStack() as es:
        QT = kb.sb("mb_QT", [128, 2, T], BF16, es)
        KT = kb.sb("mb_KT", [128, 2, T], BF16, es)
        V = kb.sb("mb_V", [128, 32, 256], BF16, es)
        selbT = kb.sb("mb_selbT", [128, 2, T], BF16, es)
        mle_b = kb.sb("mb_mleb", [128, 4, 512], BF16, es)
        E_b = kb.sb("mb_Eb", [128, 16, 128], BF16, es)
        kb.op(kb.POOL, lambda: nc.gpsimd.memset(selbT[:], 0.0), Wr=[selbT])
        kmT = kb.sb("mb_kmT", [128, 2, 16], BF16, es)
        kb.dma(mle_b[:], cx.dram["c_mask_le_b"].rearrange("o s t -> s o t"), Wr=[mle_b])
        kb.dma(E_b[:], cx.dram["c_E_b"][:, :, :], Wr=[E_b])
        with ExitStack() as es2:
            cosS = [kb.sb("mb_cos%d" % i, [128, 512], F32, es2) for i in range(2)]
            sinS = [kb.sb("mb_sin%d" % i, [128, 512], F32, es2) for i in range(2)]
            ws = WStream(kb, es2, "mb_w", 8, 256, nbuf=2)
            wp = kb.sb("mb_wp", [128, 8, 256], BF16, es2)
            t1 = [kb.sb("mb_t1%d" % i, [128, 512], F32, es2) for i in range(2)]
            t2 = [kb.sb("mb_t2%d" % i, [128, 512], F32, es2) for i in range(2)]
            pp = [kb.ps("mb_pp%d" % i, [128, 512], F32, es2) for i in range(2)]
            pq = [kb.ps("mb_pq%d" % i, [128, 512], F32, es2) for i in range(2)]
            cnt = [0]
            cnt2 = [0]
            for which in range(2):
                wb = ws.load(wcols(w2d, MOBA_OFF + which * 256, 256))
                wb4 = wb[:].rearrange("p k (h d) -> p k h d", h=4)
                wp4 = wp[:].rearrange("p k (h d) -> p k h d", h=4)
                kb.op(kb.POOL, lambda: nc.gpsimd.tensor_copy(out=wp4[:, :, :, 0:8], in_=wb4[:, :, :, 8:16]), R=[wb], Wr=[wp])
                kb.op(kb.POOL, lambda: nc.gpsimd.tensor_copy(out=wp4[:, :, :, 8:16], in_=wb4[:, :, :, 0:8]), R=[wb], Wr=[wp])
                kb.op(kb.POOL, lambda: nc.gpsimd.tensor_copy(out=wp4[:, :, :, 16:64], in_=wb4[:, :, :, 16:64]), R=[wb], Wr=[wp])
                dst = QT if which == 0 else KT
                sc = 0.125 if which == 0 else 1.0
                for fb in range(2):
                    for tg in range(8):
                        sl = slice(tg * 512, (tg + 1) * 512)
                        pa = pp[cnt[0] % 2]
                        pb = pq[cnt[0] % 2]
                        a1, a2 = t1[cnt[0] % 2], t2[cnt[0] % 2]
                        cosF, sinF = cosS[cnt[0] % 2], sinS[cnt[0] % 2]
                        cnt[0] += 1
                        kb.dma(cosF[:], cx.dram["c_cosF"][:, sl], Wr=[cosF])
                        kb.dma(sinF[:], cx.dram["c_sinF"][:, sl], Wr=[sinF])
                        for kc in range(8):
                            kb.op(kb.PE, lambda: nc.tensor.matmul(pa[:], lhsT=wb[:, kc, fb * 128:(fb + 1) * 128], rhs=hT[:, kc, sl],
                                                                  start=(kc == 0), stop=(kc == 7)), R=[wb, hT], Wr=[pa])
                        for kc in range(8):
                            kb.op(kb.PE, lambda: nc.tensor.matmul(pb[:], lhsT=wp[:, kc, fb * 128:(fb + 1) * 128], rhs=hT[:, kc, sl],
                                                                  start=(kc == 0), stop=(kc == 7)), R=[wp, hT], Wr=[pb])
                        kb.op(kb.DVE, lambda: nc.vector.tensor_tensor(out=a1[:], in0=pa[:], in1=cosF[:], op=ALU.mult),
                              R=[pa, cosF], Wr=[a1])
                        kb.op(kb.DVE, lambda: nc.vector.tensor_tensor(out=a2[:], in0=pb[:], in1=sinF[:], op=ALU.mult),
                              R=[pb, sinF], Wr=[a2])
                        if sc == 1.0:
                            kb.op(kb.POOL, lambda: nc.gpsimd.tensor_tensor(out=dst[:, fb, sl], in0=a1[:], in1=a2[:], op=ALU.add),
                                  R=[a1, a2], Wr=[dst])
                        else:
                            kb.op(kb.POOL, lambda: nc.gpsimd.tensor_tensor(out=a1[:], in0=a1[:], in1=a2[:], op=ALU.add),
                                  R=[a1, a2], Wr=[a1])
                        if sc != 1.0:
                            kb.op(kb.ACT, lambda: nc.scalar.activation(out=dst[:, fb, sl], in_=a1[:], func=AF.Copy, scale=sc),
                                  R=[a1], Wr=[dst])
            wb = ws.load(wcols(w2d, MOBA_OFF + 512, 256))
            proj_tm(kb, hT, wb, 256, pp, cnt2,
                    lambda tt, ps_: kb.op(kb.ACT, lambda: nc.scalar.activation(out=V[:, tt, :], in_=ps_[:, 0:256], func=AF.Copy),
                                          R=[ps_], Wr=[V]))
            kb.barrier()
        if dbg == "mproj":
            for b2 in range(2):
                kb.dma(yT.ap[b2 * 128:(b2 + 1) * 128, :], KT[:, b2, :], R=[KT], Wr=[cx.xbuf(yT, 0)])
            return
        with ExitStack() as es2:
            kmf = kb.sb("mb_kmf", [128, 2, 16], F32, es2)
            cpn = kb.sb("mb_cpn", [128, 16, 4, 16], F32, es2)
            cpb = kb.sb("mb_cpb", [128, 16, 4, 16], F32, es2)
            com = kb.sb("mb_com", [128, 16, 4, 16], F32, es2)
            kb.dma(cpn[:], cx.dram["c_pastneg"][:, :, :, :], Wr=[cpn])
            kb.dma(cpb[:], cx.dram["c_pastbig"][:, :, :, :], Wr=[cpb])
            kb.dma(com[:], cx.dram["c_ownm"][:, :, :, :], Wr=[com])
            for b2 in range(2):
                kb.op(kb.DVE, lambda: nc.vector.tensor_reduce(out=kmf[:, b2, :], in_=KT[:, b2, :].rearrange("p (n s) -> p n s", s=256),
                                                              axis=AX.X, op=ALU.add), R=[KT], Wr=[kmf])
            kb.op(kb.DVE, lambda: nc.vector.tensor_scalar(out=kmT[:], in0=kmf[:], scalar1=1.0 / 256, scalar2=None, op0=ALU.mult),
                  R=[kmf], Wr=[kmT])
            pg = [kb.ps("mb_pg%d" % i, [128, 512], F32, es2) for i in range(4)]
            pt = [kb.ps("mb_pt%d" % i, [128, 2, 128], BF16, es2) for i in range(2)]

            gm = [kb.sb("mb_gm%d" % i, [128, 4, 16], F32, es2) for i in range(2)]
            m8 = [kb.sb("mb_m8%d" % i, [128, 4, 8], F32, es2) for i in range(2)]
            sel = [kb.sb("mb_sel%d" % i, [128, 4, 16], F32, es2) for i in range(2)]
            selb = [kb.sb("mb_selb%d" % i, [128, 4, 16], BF16, es2) for i in range(2)]
            lvl = int(dbg[3:]) if (dbg or "").startswith("sel") and len(dbg) > 3 else 99
            for tt in range(32 if lvl > 0 else 0):
                i = tt % 2
                qb = tt // 2
                pt_, gm_, m8_, sel_, selb_ = pt[i], gm[i], m8[i], sel[i], selb[i]
                for r in range(2):
                    pg_ = pg[2 * i + r]
                    for q in range(2):
                        kb.op(kb.PE, lambda: nc.tensor.matmul(pg_[:, q * 16:(q + 1) * 16], lhsT=QT[r * 64:r * 64 + 64, q, tt * 128:(tt + 1) * 128],
                                                              rhs=kmT[r * 64:r * 64 + 64, q, :], start=True, stop=True),
                              R=[QT, kmT], Wr=[pg_])
                    kb.op(kb.DVE, lambda: nc.vector.tensor_tensor(out=gm_[:, 2 * r:2 * r + 2, :],
                                                                  in0=pg_[:, 0:32].rearrange("p (q n) -> p q n", q=2),
                                                                  in1=cpn[:, qb, 2 * r:2 * r + 2, :], op=ALU.add),
                          R=[pg_, cpn], Wr=[gm_])
                if lvl < 2:
                    continue
                for h in range(4):
                    kb.op(kb.DVE, lambda: nc.vector.max(out=m8_[:, h, :], in_=gm_[:, h, :]), R=[gm_], Wr=[m8_])
                if lvl < 3:
                    continue
                for h in range(4):
                    kb.op(kb.DVE, lambda: nc.vector.tensor_scalar(out=sel_[:, h, :], in0=gm_[:, h, :], scalar1=m8_[:, h, 2:3], scalar2=None,
                                                                  op0=ALU.is_ge), R=[gm_, m8_], Wr=[sel_])
                kb.op(kb.DVE, lambda: nc.vector.tensor_tensor(out=sel_[:], in0=sel_[:], in1=cpb[:, qb, :, :], op=ALU.mult),
                      R=[sel_, cpb], Wr=[sel_])
                kb.op(kb.DVE, lambda: nc.vector.tensor_tensor(out=selb_[:], in0=sel_[:], in1=com[:, qb, :, :], op=ALU.add),
                      R=[sel_, com], Wr=[selb_])
                if lvl < 4:
                    continue
                for h in range(4):
                    hh = (h % 2) * 2 + h // 2
                    kb.op(kb.PE, lambda: nc.tensor.transpose(pt_[64 * (h % 2):64 * (h % 2) + 16, h // 2, :], selb_[:, hh, :], cx.ident_b[:]),
                          R=[selb_, cx.ident_b], Wr=[pt_])
                if lvl < 5:
                    continue
                for pb_ in (0, 64):
                    kb.op(kb.ACT, lambda: nc.scalar.activation(out=selbT[pb_:pb_ + 16, :, tt * 128:(tt + 1) * 128],
                                                               in_=pt_[pb_:pb_ + 16, :, :], func=AF.Copy),
                          R=[pt_], Wr=[selbT])
            kb.barrier()
        if (dbg or "").startswith("sel"):
            kb.dma(yT.ap[0:128, :], selbT[:, 0, :], R=[selbT], Wr=[cx.xbuf(yT, 0)])
            kb.dma(yT.ap[128:256, :], selbT[:, 1, :], R=[selbT], Wr=[cx.xbuf(yT, 0)])
            return
        with ExitStack() as es2:
            NB = 3
            pz = [kb.ps("mb_pz%d" % i, [128, 512], F32, es2) for i in range(2)]
            po = [kb.ps("mb_po%d" % i, [64, 512], F32, es2) for i in range(2)]
            pd = [kb.ps("mb_pd%d" % i, [64, 512], F32, es2) for i in range(2)]
            pT = [kb.sb("mb_pT%d" % i, [128, 512], BF16, es2) for i in range(NB)]
            rec = [kb.sb("mb_rec%d" % i, [64, 512], F32, es2) for i in range(2)]
            yst = [kb.sb("mb_y%d" % i, [64, 512], BF16, es2) for i in range(2)]
            units = []
            gi = 0
            for h in range(NH):
                for g in range(8):
                    for j in range(4 * g + 4):
                        units.append((h, g, j, gi))
                    gi += 1
            if dbg is not None and dbg.startswith("n"):
                units = units[:int(dbg[1:])]

            def stA(u, i):
                h, g, j, gi = u
                blk, po_ = h // 2, (h % 2) * 64
                pz_, p_ = pz[i % 2], pT[i % NB]
                kb.op(kb.PE, lambda: nc.tensor.matmul(pz_[:], lhsT=KT[po_:po_ + 64, blk, j * 128:(j + 1) * 128],
                                                      rhs=QT[po_:po_ + 64, blk, g * 512:(g + 1) * 512], start=True, stop=False),
                      R=[KT, QT], Wr=[pz_])
                kb.op(kb.PE, lambda: nc.tensor.matmul(pz_[:], lhsT=E_b[po_:po_ + 64, j // 2, :],
                                                      rhs=selbT[po_:po_ + 64, h // 2, g * 512:(g + 1) * 512],
                                                      start=False, stop=True), R=[E_b, selbT], Wr=[pz_])
                kb.op(kb.ACT, lambda: nc.scalar.activation(out=p_[:], in_=pz_[:], func=AF.Exp), R=[pz_], Wr=[p_])
                o = j - 4 * g
                if o >= 0:
                    kb.op(kb.POOL, lambda: nc.gpsimd.tensor_tensor(out=p_[:], in0=p_[:], in1=mle_b[:, o, :], op=ALU.mult),
                          R=[p_, mle_b], Wr=[p_])

            def stB(u, i):
                h, g, j, gi = u
                p_, po_t, pd_t = pT[i % NB], po[gi % 2], pd[gi % 2]
                first, last = (j == 0), (j == 4 * g + 3)
                kb.op(kb.PE, lambda: nc.tensor.matmul(po_t[:], lhsT=V[:, j, h * 64:(h + 1) * 64], rhs=p_[:], start=first, stop=last),
                      R=[V, p_], Wr=[po_t])
                kb.op(kb.PE, lambda: nc.tensor.matmul(pd_t[:], lhsT=cx.ones_b[:, 0:64], rhs=p_[:], start=first, stop=last),
                      R=[cx.ones_b, p_], Wr=[pd_t])
                if last:
                    r_, y_ = rec[gi % 2], yst[gi % 2]
                    kb.op(kb.DVE, lambda: nc.vector.reciprocal(out=r_[:], in_=pd_t[:]), R=[pd_t], Wr=[r_])
                    kb.op(kb.DVE, lambda: nc.vector.tensor_tensor(out=y_[:], in0=po_t[:], in1=r_[:], op=ALU.mult), R=[po_t, r_], Wr=[y_])
                    kb.dma(yT.ap[h * 64:(h + 1) * 64, g * 512:(g + 1) * 512], y_[:], R=[y_], Wr=[cx.xbuf(yT, g)])

            pipeline(units, [stA, stB])
            kb.barrier()


def phase_hgrn(kb, cx, layer, hT, yT, dbg=None):
    nc = kb.nc
    w2d = cx.dram["w_in"][layer]
    with ExitStack() as es:
        qd = kb.sb("hg_qd", [128, 2, T], BF16, es)
        kt = kb.sb("hg_kt", [128, 2, T], BF16, es)
        ke = kb.sb("hg_ke", [128, 2, T], BF16, es)
        sg = kb.sb("hg_sg", [128, 2, T], BF16, es)
        iv = kb.sb("hg_iv", [128, 32, 256], BF16, es)
        dec = kb.sb("hg_dec", [128, 2, 128], F32, es)
        lbl = kb.sb("hg_lbl", [128, 2, 2], F32, es)
        lb = kb.sb("hg_lb", [128, 2], F32, es)
        oml = kb.sb("hg_oml", [128, 2], F32, es)
        noml = kb.sb("hg_noml", [128, 2], F32, es)
        gng = kb.sb("hg_gng", [128, 2], F32, es)
        hgm = kb.sb("hg_mask", [128, 128], F32, es)
        scm = kb.sb("hg_scm", [128, 512], F32, es)
        eps_c = kb.sb("hg_eps", [128, 1], F32, es)
        kb.op(kb.POOL, lambda: nc.gpsimd.memset(eps_c[:], NORM_EPS), Wr=[eps_c])
        kb.dma(hgm[:], cx.dram["c_hgmask"][:, :], Wr=[hgm])
        kb.dma(scm[:], cx.dram["c_scanmask"][:, :], Wr=[scm])
        kb.dma(lbl[:], cx.dram["hgrn_lb_logits"].rearrange("l (b p) -> p l b", p=128), Wr=[lbl])
        kb.dma(gng[:], cx.dram["hgrn_gn_g"][layer].rearrange("(b p) -> p b", p=128), Wr=[gng])
        if layer == 0:
            kb.op(kb.DVE, lambda: nc.vector.memset(lb[:], 0.0), Wr=[lb])
        else:
            kb.op(kb.DVE, lambda: nc.vector.tensor_tensor(out=lb[:], in0=lbl[:, 1, :], in1=lbl[:, 0, :], op=ALU.subtract),
                  R=[lbl], Wr=[lb])
            kb.op(kb.ACT, lambda: nc.scalar.activation(out=lb[:], in_=lb[:], func=AF.Sigmoid), R=[lb], Wr=[lb])
        kb.op(kb.DVE, lambda: nc.vector.tensor_scalar(out=oml[:], in0=lb[:], scalar1=-1.0, scalar2=1.0, op0=ALU.mult, op1=ALU.add),
              R=[lb], Wr=[oml])
        kb.op(kb.DVE, lambda: nc.vector.tensor_scalar(out=noml[:], in0=oml[:], scalar1=-1.0, scalar2=None, op0=ALU.mult),
              R=[oml], Wr=[noml])
        with ExitStack() as es2:
            ws = WStream(kb, es2, "hg_w", 8, 256, nbuf=2, alloc_wb=False)
            wq = kb.sb("hg_wq", [128, 8, 256], BF16, es2)
            wf = kb.sb("hg_wf", [128, 8, 256], BF16, es2)
            wg = kb.sb("hg_wg", [128, 8, 256], BF16, es2)
            wload(kb, ws, wq, wcols(w2d, HGRN_OFF, 256))
            wload(kb, ws, wf, wcols(w2d, HGRN_OFF + 256, 256))
            wload(kb, ws, wg, wcols(w2d, HGRN_OFF + 768, 256))
            pq = [kb.ps("hg_pq%d" % i, [128, 512], F32, es2) for i in range(2)]
            pf = [kb.ps("hg_pf%d" % i, [128, 512], F32, es2) for i in range(2)]
            pg = [kb.ps("hg_pg%d" % i, [128, 512], F32, es2) for i in range(2)]
            NW = 2
            sig = [kb.sb("hg_sig%d" % i, [128, 512], F32, es2) for i in range(NW)]
            fg = [kb.sb("hg_fg%d" % i, [128, 512], F32, es2) for i in range(NW)]
            key = [kb.sb("hg_key%d" % i, [128, 512], F32, es2) for i in range(NW)]
            bb = [kb.sb("hg_bb%d" % i, [128, 512], F32, es2) for i in range(NW)]
            eb = [kb.sb("hg_eb%d" % i, [128, 512], F32, es2) for i in range(NW)]
            enb = [kb.sb("hg_enb%d" % i, [128, 512], F32, es2) for i in range(NW)]
            dd = [kb.sb("hg_dd%d" % i, [128, 512], F32, es2) for i in range(NW)]
            n = 0
            for b2 in range(2):
                for tg in range(8):
                    i = n % 2
                    n += 1
                    sl = slice(tg * 512, (tg + 1) * 512)
                    fsl = slice(b2 * 128, (b2 + 1) * 128)
                    for (wt_, ps_) in ((wq, pq[i]), (wf, pf[i]), (wg, pg[i])):
                        for kc in range(8):
                            kb.op(kb.PE, lambda: nc.tensor.matmul(ps_[:], lhsT=wt_[:, kc, fsl], rhs=hT[:, kc, sl],
                                                                  start=(kc == 0), stop=(kc == 7)), R=[wt_, hT], Wr=[ps_])
                    sig_, fg_, key_, bb_, eb_, enb_, dd_ = sig[i], fg[i], key[i], bb[i], eb[i], enb[i], dd[i]
                    kb.op(kb.ACT, lambda: nc.scalar.activation(out=sig_[:], in_=pf[i][:], func=AF.Sigmoid), R=[pf[i]], Wr=[sig_])
                    kb.op(kb.ACT, lambda: nc.scalar.activation(out=sg[:, b2, sl], in_=pg[i][:], func=AF.Silu), R=[pg[i]], Wr=[sg])
                    kb.op(kb.DVE, lambda: nc.vector.tensor_scalar(out=fg_[:], in0=sig_[:], scalar1=oml[:, b2:b2 + 1], scalar2=lb[:, b2:b2 + 1],
                                                                  op0=ALU.mult, op1=ALU.add), R=[sig_, oml, lb], Wr=[fg_])
                    kb.op(kb.ACT, lambda: nc.scalar.activation(out=fg_[:], in_=fg_[:], func=AF.Ln), R=[fg_], Wr=[fg_])
                    kb.op(kb.DVE, lambda: nc.vector.tensor_scalar(out=key_[:], in0=sig_[:], scalar1=-1.0, scalar2=noml[:, b2:b2 + 1],
                                                                  op0=ALU.add, op1=ALU.mult), R=[sig_, noml], Wr=[key_])
                    kb.op(kb.DVE, lambda: nc.vector.tensor_tensor_scan(out=bb_[:], data0=scm[:], data1=fg_[:], initial=0.0,
                                                                       op0=ALU.mult, op1=ALU.add), R=[scm, fg_], Wr=[bb_])
                    kb.op(kb.ACT, lambda: nc.scalar.activation(out=eb_[:], in_=bb_[:], func=AF.Exp), R=[bb_], Wr=[eb_])
                    kb.op(kb.ACT, lambda: nc.scalar.activation(out=enb_[:], in_=bb_[:], func=AF.Exp, scale=-1.0), R=[bb_], Wr=[enb_])
                    b3 = bb_[:].rearrange("p (n c) -> p n c", c=32)
                    kb.op(kb.DVE, lambda: nc.vector.tensor_tensor(out=dd_[:].rearrange("p (n c) -> p n c", c=32),
                                                                  in0=b3[:, :, 31:32].to_broadcast([128, 16, 32]), in1=b3, op=ALU.subtract),
                          R=[bb_], Wr=[dd_])
                    kb.op(kb.ACT, lambda: nc.scalar.activation(out=dd_[:], in_=dd_[:], func=AF.Exp), R=[dd_], Wr=[dd_])
                    kb.op(kb.DVE, lambda: nc.vector.tensor_tensor(out=qd[:, b2, sl], in0=pq[i][:], in1=eb_[:], op=ALU.mult),
                          R=[pq[i], eb_], Wr=[qd])
                    kb.op(kb.POOL, lambda: nc.gpsimd.tensor_tensor(out=kt[:, b2, sl], in0=key_[:], in1=enb_[:], op=ALU.mult),
                          R=[key_, enb_], Wr=[kt])
                    kb.op(kb.POOL, lambda: nc.gpsimd.tensor_tensor(out=ke[:, b2, sl], in0=key_[:], in1=dd_[:], op=ALU.mult),
                          R=[key_, dd_], Wr=[ke])
                    kb.op(kb.POOL, lambda: nc.gpsimd.tensor_copy(out=dec[:, b2, tg * 16:(tg + 1) * 16],
                                                                 in_=eb_[:].rearrange("p (n c) -> p n c", c=32)[:, :, 31]),
                          R=[eb_], Wr=[dec])
            wi = wq
            wload(kb, ws, wi, wcols(w2d, HGRN_OFF + 512, 256))
            cnt = [0]
            proj_tm(kb, hT, wi, 256, pq, cnt,
                    lambda tt, ps_: kb.op(kb.ACT, lambda: nc.scalar.activation(out=iv[:, tt, :], in_=ps_[:, 0:256], func=AF.Copy),
                                          R=[ps_], Wr=[iv]))
            kb.barrier()
        if dbg == "hproj":
            for b2 in range(2):
                kb.dma(yT.ap[b2 * 128:(b2 + 1) * 128, :], ke[:, b2, :], R=[ke], Wr=[cx.xbuf(yT, 0)])
            return
        with ExitStack() as es2:
            ketok = kb.sb("hg_ketok", [128, 32, 256], BF16, es2)
            ketok3 = kb.sb("hg_ketok3", [128, 32, 256], BF16, es2)
            m3 = kb.sb("hg_m3", [128, 1], F32, es2)
            kb.op(kb.POOL, lambda: nc.gpsimd.memset(m3[:], 1.0), Wr=[m3])
            kb.op(kb.POOL, lambda: nc.gpsimd.memset(m3[64:96, :], 0.0), Wr=[m3])
            with ExitStack() as es3:
                ptr = [kb.ps("hg_ptr%d" % i, [128, 4, 128], BF16, es3) for i in range(2)]
                n = 0
                for b2 in range(2):
                    for t4 in range(8):
                        p_ = ptr[n % 2]
                        n += 1
                        for q in range(4):
                            tt = t4 * 4 + q
                            kb.op(kb.PE, lambda: nc.tensor.transpose(p_[:, q, :], ke[:, b2, tt * 128:(tt + 1) * 128], cx.ident_b[:]),
                                  R=[ke, cx.ident_b], Wr=[p_])
                        kb.op(kb.ACT, lambda: nc.scalar.activation(out=ketok[:, t4 * 4:(t4 + 1) * 4, b2 * 128:(b2 + 1) * 128], in_=p_[:], func=AF.Copy),
                              R=[p_], Wr=[ketok])
                        kb.op(kb.ACT, lambda: nc.scalar.activation(out=ketok3[64:128, t4 * 4:(t4 + 1) * 4, b2 * 128:(b2 + 1) * 128], in_=p_[64:128, :, :],
                                                                   func=AF.Copy, scale=m3[64:128, 0:1]),
                              R=[p_, m3], Wr=[ketok3])
                kb.barrier()
            psc = [kb.ps("hg_psc%d" % i, [128, 512], F32, es2) for i in range(2)]
            pkv = [kb.ps("hg_pkv%d" % i, [128, 512], F32, es2) for i in range(4)]
            pO = [kb.ps("hg_pO%d" % i, [128, 512], F32, es2) for i in range(2)]
            S = [kb.sb("hg_S%d" % i, [128, 128], F32, es2) for i in range(2)]
            Sb = [kb.sb("hg_Sb%d" % i, [128, 128], BF16, es2) for i in range(2)]
            scT = [[kb.sb("hg_scT%d%d" % (i, r), [128, 128], BF16, es2) for r in range(2)] for i in range(2)]
            osb = [kb.sb("hg_osb%d" % i, [128, 512], F32, es2) for i in range(2)]
            osq = [kb.sb("hg_osq%d" % i, [128, 512], F32, es2) for i in range(2)]
            rt = [kb.sb("hg_rt%d" % i, [128, 512], F32, es2) for i in range(2)]
            yst = [kb.sb("hg_y%d" % i, [128, 512], BF16, es2) for i in range(2)]
            for b2 in range(2):
                kb.op(kb.DVE, lambda: nc.vector.memset(S[b2][:], 0.0), Wr=[S[b2]])
                kb.op(kb.DVE, lambda: nc.vector.memset(Sb[b2][:], 0.0), Wr=[Sb[b2]])
            ntt = 32 if not (dbg or "").startswith("ht") else int(dbg[2:])
            for tt in range(ntt):
                tg, q4 = tt // 4, tt % 4
                for b2 in range(2):
                    O_ = pO[b2]
                    csl = slice(q4 * 128, (q4 + 1) * 128)
                    tsl = slice(tt * 128, (tt + 1) * 128)
                    for r in range(2):
                        kb.op(kb.PE, lambda: nc.tensor.matmul(psc[r][:, 0:128], lhsT=kt[r * 64:(r + 1) * 64, b2, tsl],
                                                              rhs=qd[r * 64:(r + 1) * 64, b2, tsl], start=True, stop=True),
                              R=[kt, qd], Wr=[psc[r]])
                        sc_ = scT[b2][r]
                        kb.op(kb.DVE, lambda: nc.vector.tensor_tensor(out=sc_[:], in0=psc[r][:, 0:128], in1=hgm[:], op=ALU.mult),
                              R=[psc[r], hgm], Wr=[sc_])
                    for r in range(2):
                        sc_ = scT[b2][r]
                        h = b2 * 2 + r
                        kb.op(kb.PE, lambda: nc.tensor.matmul(O_[r * 64:(r + 1) * 64, csl], lhsT=iv[:, tt, h * 64:(h + 1) * 64], rhs=sc_[:],
                                                              start=True, stop=False, skip_group_check=True),
                              R=[iv, sc_], Wr=[O_])
                    for c4 in range(4):
                        nchunk = tt * 4 + c4
                        c0 = tt * 128 + c4 * 32
                        kb.op(kb.PE, lambda: nc.tensor.matmul(O_[:, q4 * 128 + c4 * 32:q4 * 128 + (c4 + 1) * 32], lhsT=Sb[b2][:],
                                                              rhs=qd[:, b2, c0:c0 + 32], start=False, stop=(c4 == 3), skip_group_check=True),
                              R=[Sb[b2], qd], Wr=[O_])
                        kv_ = pkv[c4]
                        if c4 < 3:
                            kb.op(kb.PE, lambda: nc.tensor.matmul(kv_[:, 0:128], lhsT=ketok[c4 * 32:(c4 + 1) * 32, tt, b2 * 128:(b2 + 1) * 128],
                                                                  rhs=iv[c4 * 32:(c4 + 1) * 32, tt, b2 * 128:(b2 + 1) * 128], start=True, stop=True),
                                  R=[ketok, iv], Wr=[kv_])
                        else:
                            kb.op(kb.PE, lambda: nc.tensor.matmul(kv_[:, 0:128], lhsT=ketok3[64:128, tt, b2 * 128:(b2 + 1) * 128],
                                                                  rhs=iv[64:128, tt, b2 * 128:(b2 + 1) * 128], start=True, stop=True),
                                  R=[ketok3, iv], Wr=[kv_])
                        kb.op(kb.DVE, lambda: nc.vector.scalar_tensor_tensor(out=S[b2][:], in0=S[b2][:], scalar=dec[:, b2, nchunk:nchunk + 1],
                                                                             in1=kv_[:, 0:128], op0=ALU.mult, op1=ALU.add),
                              R=[S[b2], dec, kv_], Wr=[S[b2]])
                        kb.op(kb.POOL, lambda: nc.gpsimd.tensor_tensor(out=Sb[b2][:], in0=S[b2][:], in1=cx.blk_f[:], op=ALU.mult),
                              R=[S[b2], cx.blk_f], Wr=[Sb[b2]])
                    if q4 == 3:
                        sl = slice(tg * 512, (tg + 1) * 512)
                        i = b2
                        kb.op(kb.ACT, lambda: nc.scalar.activation(out=osb[i][:], in_=O_[:], func=AF.Copy), R=[O_], Wr=[osb[i]])
                        kb.op(kb.ACT, lambda: nc.scalar.activation(out=osq[i][:], in_=O_[:], func=AF.Square), R=[O_], Wr=[osq[i]])
                        kb.op(kb.PE, lambda: nc.tensor.matmul(psc[0][:], lhsT=cx.blk_f[:], rhs=osq[i][:], start=True, stop=True),
                              R=[cx.blk_f, osq[i]], Wr=[psc[0]])
                        kb.op(kb.ACT, lambda: nc.scalar.activation(out=rt[i][:], in_=psc[0][:], func=AF.Sqrt, scale=1.0 / 64, bias=eps_c[:, 0:1]),
                              R=[psc[0], eps_c], Wr=[rt[i]])
                        kb.op(kb.DVE, lambda: nc.vector.reciprocal(out=rt[i][:], in_=rt[i][:]), R=[rt[i]], Wr=[rt[i]])
                        kb.op(kb.DVE, lambda: nc.vector.tensor_tensor(out=osb[i][:], in0=osb[i][:], in1=rt[i][:], op=ALU.mult),
                              R=[osb[i], rt[i]], Wr=[osb[i]])
                        kb.op(kb.DVE, lambda: nc.vector.scalar_tensor_tensor(out=yst[i][:], in0=osb[i][:], scalar=gng[:, b2:b2 + 1], in1=sg[:, b2, sl],
                                                                             op0=ALU.mult, op1=ALU.mult), R=[osb[i], gng, sg], Wr=[yst[i]])
                        kb.dma(yT.ap[b2 * 128:(b2 + 1) * 128, sl], yst[i][:], R=[yst[i]], Wr=[cx.xbuf(yT, tg)])
            kb.barrier()


C0 = 0.6065306597126334


class OpsH:
    def __init__(self, kb):
        self.kb = kb
        self.nc = kb.nc

    def act(self, out, in_, func, R, Wr, scale=1.0, bias=None):
        kb, nc = self.kb, self.nc
        if bias is None:
            return kb.op(kb.ACT, lambda: nc.scalar.activation(out=out, in_=in_, func=func, scale=scale), R=R, Wr=Wr)
        return kb.op(kb.ACT, lambda: nc.scalar.activation(out=out, in_=in_, func=func, scale=scale, bias=bias), R=R, Wr=Wr)

    def tt(self, E, out, a, b, op, R, Wr):
        kb = self.kb
        return kb.op(E, lambda: E.eng.tensor_tensor(out=out, in0=a, in1=b, op=op), R=R, Wr=Wr)

    def ts(self, E, out, a, s1, s2, op0, op1, R, Wr):
        kb = self.kb
        if s2 is None:
            return kb.op(E, lambda: E.eng.tensor_scalar(out=out, in0=a, scalar1=s1, scalar2=None, op0=op0), R=R, Wr=Wr)
        return kb.op(E, lambda: E.eng.tensor_scalar(out=out, in0=a, scalar1=s1, scalar2=s2, op0=op0, op1=op1), R=R, Wr=Wr)

    def stt(self, out, a, sc, b, op0, op1, R, Wr):
        kb, nc = self.kb, self.nc
        return kb.op(kb.DVE, lambda: nc.vector.scalar_tensor_tensor(out=out, in0=a, scalar=sc, in1=b, op0=op0, op1=op1), R=R, Wr=Wr)

    def mm(self, out, lhsT, rhs, R, Wr, start=True, stop=True, skip=False):
        kb, nc = self.kb, self.nc
        return kb.op(kb.PE, lambda: nc.tensor.matmul(out, lhsT=lhsT, rhs=rhs, start=start, stop=stop, skip_group_check=skip), R=R, Wr=Wr)


def phase_rwkv(kb, cx, layer, hT, yT, dbg=None):
    nc = kb.nc
    o = OpsH(kb)
    w2d = cx.dram["w_in"][layer]
    MUL, ADD, SUB = ALU.mult, ALU.add, ALU.subtract
    DVE, POOL = kb.DVE, kb.POOL
    with ExitStack() as es:
        def col2(name, src, es_=es):
            t_ = kb.sb(name, [128, 2], F32, es_)
            kb.dma(t_[:], src.rearrange("(b p) -> p b", p=128), Wr=[t_])
            return t_
        mu = kb.sb("rw_mu", [128, 8], F32, es)
        kb.dma(mu[:], cx.dram["rwkv_mu"][layer].rearrange("(b p) -> p b", p=128), Wr=[mu])
        w0 = col2("rw_w0", cx.dram["rwkv_w0"][layer])
        a0 = col2("rw_a0", cx.dram["rwkv_a0"][layer])
        k_k = col2("rw_kk", cx.dram["rwkv_k_k"][layer])
        k_a = col2("rw_ka", cx.dram["rwkv_k_a"][layer])
        gnw = col2("rw_gnw", cx.dram["rwkv_gn_w"][layer])
        gnb = col2("rw_gnb", cx.dram["rwkv_gn_b"][layer])
        r_k = col2("rw_rk", cx.dram["rwkv_r_k"][layer].rearrange("h d -> (h d)"))
        gneps = kb.sb("rw_gneps", [128, 1], F32, es)
        kb.op(POOL, lambda: nc.gpsimd.memset(gneps[:], GN_EPS), Wr=[gneps])
        if layer > 0:
            v0 = col2("rw_v0", cx.dram["rwkv_v0"][layer - 1])
        stg = kb.sb("rw_stg", [128, 256], F32, es)

        def small_w(name, src_ap, rows, r0=0, ncols=256):
            t_ = kb.sb(name, [128, ncols], BF16, es)
            kb.dma(stg[r0:r0 + rows, 0:ncols], src_ap, Wr=[stg])
            kb.op(DVE, lambda: nc.vector.tensor_copy(out=t_[r0:r0 + rows, :], in_=stg[r0:r0 + rows, 0:ncols]), R=[stg], Wr=[t_])
            return t_
        w2b = small_w("rw_w2b", cx.dram["rwkv_w2"][layer], 64, 0)
        a2b = small_w("rw_a2b", cx.dram["rwkv_a2"][layer], 64, 64)
        g2b = small_w("rw_g2b", cx.dram["rwkv_g2"][layer], 128, 0)
        if layer > 0:
            v2b = small_w("rw_v2b", cx.dram["rwkv_v2"][layer - 1], 32, 0)
            v1b = kb.sb("rw_v1b", [128, 2, 32], BF16, es)
            for b2 in range(2):
                kb.dma(stg[:, 0:32], cx.dram["rwkv_v1"][layer - 1][b2 * 128:(b2 + 1) * 128, :], Wr=[stg])
                kb.op(DVE, lambda: nc.vector.tensor_copy(out=v1b[:, b2, :], in_=stg[:, 0:32]), R=[stg], Wr=[v1b])
        scm = kb.sb("rw_scm", [128, 512], F32, es)
        kb.dma(scm[:], cx.dram["c_scanmask128"][:, :], Wr=[scm])
        LT = kb.sb("rw_LT", [128, T], BF16, es)
        SG = kb.sb("rw_SG", [128, T], BF16, es)
        VL = kb.sb("rw_VL", [32, T], BF16, es) if layer > 0 else None

        class Shifter:
            def __init__(self, name, es_, d=None):
                self.pb = [kb.sb("%s_pb%d" % (name, i), [128, 513], F32, es_) for i in range(2)]
                self.d = d if d is not None else kb.sb("%s_d" % name, [128, 512], F32, es_)
                self.n = 0

            def run(self, ps_, mucol, out, out_tl=None):
                cur = self.pb[self.n % 2]
                prv = self.pb[(self.n + 1) % 2]
                first = (self.n % 8 == 0)
                self.n += 1
                o.act(cur[:, 1:513], ps_[:], AF.Copy, [ps_], [cur])
                if first:
                    kb.op(DVE, lambda: nc.vector.memset(cur[:, 0:1], 0.0), Wr=[cur])
                else:
                    kb.op(DVE, lambda: nc.vector.tensor_copy(out=cur[:, 0:1], in_=prv[:, 512:513]), R=[prv], Wr=[cur])
                o.tt(DVE, self.d[:], cur[:, 0:512], cur[:, 1:513], SUB, [cur], [self.d])
                o.stt(out, self.d[:], mucol, cur[:, 1:513], MUL, ADD, [self.d, cur, mu], [out_tl])

        with ExitStack() as es2:
            ws = WStream(kb, es2, "rw0_w", 8, 128, nbuf=2, alloc_wb=False)
            wl_ = kb.sb("rw0_wl", [128, 8, 128], BF16, es2)
            wg_ = kb.sb("rw0_wg", [128, 8, 128], BF16, es2)
            wload(kb, ws, wl_, wcols(w2d, RWKV_OFF + 768, 128))
            wload(kb, ws, wg_, wcols(w2d, RWKV_OFF + 896, 128))
            nv = 2 if layer > 0 else 0
            wv_ = [kb.sb("rw0_wv%d" % i, [128, 8, 128], BF16, es2) for i in range(nv)]
            for i in range(nv):
                wload(kb, ws, wv_[i], wcols(w2d, RWKV_OFF + 512 + i * 128, 128))
            pp = [kb.ps("rw0_pp%d" % i, [128, 512], F32, es2) for i in range(4)]
            pvl = kb.ps("rw0_pvl", [128, 512], F32, es2)
            shl = Shifter("rw0_sl", es2)
            shg = Shifter("rw0_sg", es2)
            shv = [Shifter("rw0_sv%d" % i, es2) for i in range(nv)]
            tl = kb.sb("rw0_tl", [128, 512], F32, es2)
            tg_ = kb.sb("rw0_tg", [128, 512], F32, es2)
            tv = [kb.sb("rw0_tv%d" % i, [128, 512], F32, es2) for i in range(nv)]
            tvb = [kb.sb("rw0_tvb%d" % i, [128, 512], BF16, es2) for i in range(nv)]
            for tg in range(8):
                sl = slice(tg * 512, (tg + 1) * 512)
                for (wt_, ps_) in [(wl_, pp[0]), (wg_, pp[1])] + [(wv_[i], pp[2 + i]) for i in range(nv)]:
                    for kc in range(8):
                        o.mm(ps_[:], wt_[:, kc, :], hT[:, kc, sl], [wt_, hT], [ps_], start=(kc == 0), stop=(kc == 7))
                shl.run(pp[0], mu[:, 6:7], tl[:], tl)
                o.act(LT[0:64, sl], tl[0:64, :], AF.Tanh, [tl], [LT])
                o.act(LT[64:128, sl], tl[64:128, :], AF.Copy, [tl], [LT])
                shg.run(pp[1], mu[:, 7:8], tg_[:], tg_)
                o.act(SG[:, sl], tg_[:], AF.Sigmoid, [tg_], [SG])
                for i in range(nv):
                    shv[i].run(pp[2 + i], mu[:, 4 + i:5 + i], tv[i][:], tv[i])
                    kb.op(DVE, lambda: nc.vector.tensor_copy(out=tvb[i][:], in_=tv[i][:]), R=[tv[i]], Wr=[tvb[i]])
                if nv:
                    for i in range(2):
                        o.mm(pvl[0:32, :], v1b[:, i, :], tvb[i][:], [v1b, tvb[i]], [pvl], start=(i == 0), stop=(i == 1))
                    o.act(VL[0:32, sl], pvl[0:32, :], AF.Copy, [pvl], [VL])
            kb.barrier()
        if dbg == "r0":
            kb.dma(yT.ap[0:128, :], LT[:, :], R=[LT], Wr=[cx.xbuf(yT, 0)])
            kb.dma(yT.ap[128:256, :], SG[:, :], R=[SG], Wr=[cx.xbuf(yT, 0)])
            return

        for blk in range(2):
            with ExitStack() as esb:
                at = kb.sb("rw_at", [128, T], BF16, esb)
                bt = kb.sb("rw_bt", [128, T], BF16, esb)
                ktl = kb.sb("rw_ktl", [128, T], BF16, esb)
                rt = kb.sb("rw_rt", [128, T], BF16, esb)
                bh = kb.sb("rw_bh", [128, T], BF16, esb)
                kh = kb.sb("rw_kh", [128, T], BF16, esb)
                vT = kb.sb("rw_vT", [128, T], BF16, esb)
                gend = kb.sb("rw_gend", [128, 32], F32, esb)
                with ExitStack() as es2:
                    ws = WStream(kb, es2, "rwp_w", 8, 128, nbuf=2, alloc_wb=False)
                    wr_ = kb.sb("rwp_wr", [128, 8, 128], BF16, es2)
                    wk_ = kb.sb("rwp_wk", [128, 8, 128], BF16, es2)
                    wv_ = kb.sb("rwp_wv", [128, 8, 128], BF16, es2)
                    wload(kb, ws, wr_, wcols(w2d, RWKV_OFF + blk * 128, 128))
                    wload(kb, ws, wk_, wcols(w2d, RWKV_OFF + 256 + blk * 128, 128))
                    wload(kb, ws, wv_, wcols(w2d, RWKV_OFF + 512 + blk * 128, 128))
                    pr = kb.ps("rwp_pr", [128, 512], F32, es2)
                    pk = kb.ps("rwp_pk", [128, 512], F32, es2)
                    pv = kb.ps("rwp_pv", [128, 512], F32, es2)
                    pl = kb.ps("rwp_pl", [128, 512], F32, es2)
                    pa = kb.ps("rwp_pa", [128, 512], F32, es2)
                    pg2 = kb.ps("rwp_pg2", [128, 512], F32, es2)
                    pss = kb.ps("rwp_pss", [128, 512], F32, es2)
                    pvm = kb.ps("rwp_pvm", [128, 512], F32, es2)
                    shr = Shifter("rwp_sr", es2)
                    shk, shv = Shifter("rwp_sk", es2, shr.d), Shifter("rwp_sv", es2, shr.d)
                    names = ["rs", "ks", "vs", "sgw", "av", "cum", "e1", "e2", "e3", "e4", "kk", "t1", "t2"]
                    wt = {n_: kb.sb("rwp_" + n_, [128, 512], F32, es2) for n_ in names}
                    wt["kkn"] = wt["sgw"]
                    wt["kmod"] = wt["cum"]
                    gst = kb.sb("rwp_gst", [128, 512], BF16, es2)
                    bc = slice(blk * 128, (blk + 1) * 128)
                    b1 = slice(blk, blk + 1)
                    for tg in range(8):
                        sl = slice(tg * 512, (tg + 1) * 512)
                        for (wt_, ps_) in ((wr_, pr), (wk_, pk), (wv_, pv)):
                            for kc in range(8):
                                o.mm(ps_[:], wt_[:, kc, :], hT[:, kc, sl], [wt_, hT], [ps_], start=(kc == 0), stop=(kc == 7))
                        rs, ks, vs = wt["rs"], wt["ks"], wt["vs"]
                        for (sh_, ps_, mc, dst) in ((shr, pr, blk, rs), (shk, pk, 2 + blk, ks), (shv, pv, 4 + blk, vs)):
                            sh_.run(ps_, mu[:, mc:mc + 1], dst[:], dst)
                        t1, t2 = wt["t1"], wt["t2"]
                        if layer == 0:
                            kb.dma(cx.vfirst.ap[bc, sl], vs[:], R=[vs], Wr=[cx.vfirst.buf((blk, tg))])
                        else:
                            o.mm(pvm[:], v2b[0:32, bc], VL[0:32, sl], [v2b, VL], [pvm])
                            o.act(t1[:], pvm[:], AF.Sigmoid, [pvm, v0], [t1], bias=v0[:, b1])
                            kb.dma(t2[:], cx.vfirst.ap[bc, sl], R=[cx.vfirst.buf((blk, tg))], Wr=[t2])
                            o.tt(DVE, t2[:], t2[:], vs[:], SUB, [t2, vs], [t2])
                            o.tt(DVE, t2[:], t2[:], t1[:], MUL, [t2, t1], [t2])
                            o.tt(DVE, vs[:], vs[:], t2[:], ADD, [vs, t2], [vs])
                        sgw, av, cum = wt["sgw"], wt["av"], wt["cum"]
                        o.mm(pl[:], w2b[0:64, bc], LT[0:64, sl], [w2b, LT], [pl])
                        o.act(sgw[:], pl[:], AF.Sigmoid, [pl, w0], [sgw], bias=w0[:, b1])
                        o.mm(pa[:], a2b[64:128, bc], LT[64:128, sl], [a2b, LT], [pa])
                        o.act(av[:], pa[:], AF.Sigmoid, [pa, a0], [av], bias=a0[:, b1])
                        o.mm(pg2[:], g2b[:, bc], SG[:, sl], [g2b, SG], [pg2])
                        o.act(gst[:], pg2[:], AF.Copy, [pg2], [gst])
                        kb.dma(cx.gscr.ap[bc, sl], gst[:], R=[gst], Wr=[cx.gscr.buf((blk, tg))])
                        kb.op(DVE, lambda: nc.vector.tensor_tensor_scan(out=cum[:], data0=scm[:], data1=sgw[:], initial=0.0, op0=MUL, op1=ADD),
                              R=[scm, sgw], Wr=[cum])
                        e1, e2, e3, e4 = wt["e1"], wt["e2"], wt["e3"], wt["e4"]
                        o.act(e1[:], cum[:], AF.Exp, [cum], [e1], scale=-C0)
                        o.act(e2[:], cum[:], AF.Exp, [cum], [e2], scale=C0)
                        c3 = cum[:].rearrange("p (n c) -> p n c", c=128)
                        o.tt(DVE, e3[:].rearrange("p (n c) -> p n c", c=128), c3[:, :, 127:128].to_broadcast([128, 4, 128]), c3, SUB, [cum], [e3])
                        o.act(e3[:], e3[:], AF.Exp, [e3], [e3], scale=-C0)
                        o.tt(DVE, e4[:], cum[:], sgw[:], SUB, [cum, sgw], [e4])
                        o.act(e4[:], e4[:], AF.Exp, [e4], [e4], scale=-C0)
                        kb.op(POOL, lambda: nc.gpsimd.tensor_copy(out=gend[:, tg * 4:(tg + 1) * 4], in_=e1[:].rearrange("p (n c) -> p n c", c=128)[:, :, 127]),
                              R=[e1], Wr=[gend])
                        kk, kkn = wt["kk"], wt["kkn"]
                        o.ts(DVE, kk[:], ks[:], k_k[:, b1], None, MUL, None, [ks, k_k], [kk])
                        o.act(t1[:], kk[:], AF.Square, [kk], [t1])
                        o.mm(pss[:], cx.blk_f[:], t1[:], [cx.blk_f, t1], [pss])
                        o.act(t1[:], pss[:], AF.Sqrt, [pss], [t1])
                        o.ts(DVE, t1[:], t1[:], 1e-12, None, ALU.max, None, [t1], [t1])
                        kb.op(DVE, lambda: nc.vector.reciprocal(out=t1[:], in_=t1[:]), R=[t1], Wr=[t1])
                        o.tt(DVE, kkn[:], kk[:], t1[:], MUL, [kk, t1], [kkn])
                        kmod, ka = wt["kmod"], wt["kk"]
                        o.ts(DVE, t2[:], av[:], -1.0, k_a[:, b1], ADD, MUL, [av, k_a], [t2])
                        o.stt(kmod[:], t2[:], 1.0, ks[:], ADD, MUL, [t2, ks], [kmod])
                        o.stt(t2[:], rs[:], r_k[:, b1], kmod[:], MUL, MUL, [rs, r_k, kmod], [t2])
                        o.mm(pss[:], cx.blk_f[:], t2[:], [cx.blk_f, t2], [pss])
                        o.tt(DVE, t1[:], pss[:], vs[:], MUL, [pss, vs], [t1])
                        kb.dma(cx.bscr.ap[bc, sl], t1[:], R=[t1], Wr=[cx.bscr.buf((blk, tg))])
                        o.tt(POOL, ka[:], kkn[:], av[:], MUL, [kkn, av], [ka])
                        o.stt(at[:, sl], kkn[:], -1.0, e4[:], MUL, MUL, [kkn, e4], [at])
                        o.tt(POOL, bt[:, sl], ka[:], e2[:], MUL, [ka, e2], [bt])
                        o.tt(POOL, ktl[:, sl], kmod[:], e2[:], MUL, [kmod, e2], [ktl])
                        o.tt(POOL, rt[:, sl], rs[:], e1[:], MUL, [rs, e1], [rt])
                        o.tt(POOL, bh[:, sl], ka[:], e3[:], MUL, [ka, e3], [bh])
                        o.tt(POOL, kh[:, sl], kmod[:], e3[:], MUL, [kmod, e3], [kh])
                        o.act(vT[:, sl], vs[:], AF.Copy, [vs], [vT])
                    kb.barrier()
                if dbg == "prep":
                    kb.dma(yT.ap[0:128, :], at[:, :], R=[at], Wr=[cx.xbuf(yT, 0)])
                    kb.dma(yT.ap[128:256, :], kh[:, :], R=[kh], Wr=[cx.xbuf(yT, 0)])
                    return
                with ExitStack() as es2:
                    rw_recur(kb, cx, o, es2, layer, blk, at, bt, ktl, rt, bh, kh, vT, gend, gnw, gnb, gneps, yT, dbg)
                    kb.barrier()


def rw_recur(kb, cx, o, es, layer, blk, at, bt, ktl, rt, bh, kh, vT, gend, gnw, gnb, gneps, yT, dbg):
    nc = kb.nc
    MUL, ADD, SUB = ALU.mult, ALU.add, ALU.subtract
    DVE, POOL = kb.DVE, kb.POOL
    bc = slice(blk * 128, (blk + 1) * 128)
    b1 = slice(blk, blk + 1)

    def cst(name, key):
        t_ = kb.sb(name, [128, 128], F32, es)
        kb.dma(t_[:], cx.dram[key][:, :], Wr=[t_])
        return t_
    mlow, mupS, mupI = cst("rr_mlow", "c_rw_low"), cst("rr_mupS", "c_rw_upS"), cst("rr_mupI", "c_rw_upI")
    A = [kb.ps("rr_A%d" % r, [128, 512], F32, es) for r in range(2)]
    B = [kb.ps("rr_B%d" % r, [128, 512], F32, es) for r in range(2)]
    N = [kb.ps("rr_N%d" % r, [128, 512], F32, es) for r in range(2)]
    YS = kb.ps("rr_YS", [128, 512], F32, es)
    TR = kb.ps("rr_TR", [128, 4, 128], BF16, es)
    Ab = [[A[r].b] * 4 for r in range(2)]
    Bb = [[B[r].b] * 4 for r in range(2)]
    Nb = [[N[r].b] * 3 for r in range(2)]
    Yb = Ub_ = Mb_ = YS.b
    tok = [kb.sb("rr_tok%d" % i, [128, 4, 128], BF16, es) for i in range(2)]
    tokA = [kb.sb("rr_tokA%d" % i, [128, 128], F32, es) for i in range(2)]
    AakT = [[kb.sb("rr_aak%d%d" % (i, r), [128, 128], BF16, es) for r in range(2)] for i in range(2)]
    ArbT = [[kb.sb("rr_arb%d%d" % (i, r), [128, 128], BF16, es) for r in range(2)] for i in range(2)]
    ArkT = [[kb.sb("rr_ark%d%d" % (i, r), [128, 128], BF16, es) for r in range(2)] for i in range(2)]
    Pa = [[kb.sb("rr_P%d%d" % (r, i), [128, 128], F32, es) for i in range(2)] for r in range(2)]
    PTa = [[kb.sb("rr_PT%d%d" % (r, i), [128, 128], F32, es) for i in range(2)] for r in range(2)]
    G = [[kb.sb("rr_G%d%d" % (r, i), [128, 128], F32, es) for i in range(2)] for r in range(2)]
    X0sb = [kb.sb("rr_X0%d" % r, [128, 64], F32, es) for r in range(2)]
    Zb = [kb.sb("rr_Z%d" % i, [128, 128], F32, es) for i in range(2)]
    WmT = [kb.sb("rr_WmT%d" % i, [128, 128], BF16, es) for i in range(2)]
    M = kb.sb("rr_M", [128, 128], F32, es)
    Mb = kb.sb("rr_Mb", [128, 128], BF16, es)
    Ub = kb.sb("rr_Ub", [128, 128], BF16, es)
    ysb = kb.sb("rr_ysb", [128, 256], F32, es)
    yc = kb.sb("rr_yc", [128, 256], F32, es)
    sq = kb.sb("rr_sq", [128, 256], F32, es)
    bon = kb.sb("rr_bon", [128, 256], F32, es)
    gin = kb.sb("rr_gin", [128, 256], BF16, es)
    yo = kb.sb("rr_yo", [128, 256], BF16, es)
    kb.op(DVE, lambda: nc.vector.memset(M[:], 0.0), Wr=[M])
    kb.op(DVE, lambda: nc.vector.memset(Mb[:], 0.0), Wr=[Mb])
    ntile = 32 if not (dbg or "").startswith("rt") else int(dbg[2:])

    def preA(tt):
        tp = tt % 2
        cs = slice(tt * 128, (tt + 1) * 128)
        for i, src in enumerate((vT, at, bh, kh)):
            kb.op(kb.PE, lambda: nc.tensor.transpose(TR[:, i, :], src[:, cs], cx.ident_b[:]), R=[src, cx.ident_b], Wr=[TR])
        o.act(tok[tp][:], TR[:], AF.Copy, [TR], [tok[tp]])
        o.act(tokA[tp][:], TR[:, 1, :], AF.Copy, [TR], [tokA[tp]])
        for r in range(2):
            ps_ = slice(r * 64, (r + 1) * 64)
            jobs = ((A[r][:, 0:128], Ab[r][0], at, bt, Pa[r][0], mlow),
                    (A[r][:, 128:256], Ab[r][1], bt, at, PTa[r][0], mupS),
                    (A[r][:, 256:384], Ab[r][2], ktl, at, AakT[tp][r], mupS),
                    (A[r][:, 384:512], Ab[r][3], bt, rt, ArbT[tp][r], mupI),
                    (B[r][:, 0:128], Bb[r][0], ktl, rt, ArkT[tp][r], mupI))
            for (out_, ob_, l_, r_, dst, msk) in jobs:
                o.mm(out_, l_[ps_, cs], r_[ps_, cs], [l_, r_], [ob_])
            for (out_, ob_, l_, r_, dst, msk) in jobs:
                o.tt(DVE, dst[:], out_, msk[:], MUL, [ob_, msk], [dst])
            o.mm(B[r][:, 128:192], AakT[tp][r][:], tok[tp][:, 0, r * 64:(r + 1) * 64], [AakT[tp][r], tok[tp]], [Bb[r][1]])
            o.act(X0sb[r][:], B[r][:, 128:192], AF.Copy, [Bb[r][1]], [X0sb[r]])
            o.tt(POOL, G[r][0][:], PTa[r][0][:], cx.ident_f[:], ADD, [PTa[r][0], cx.ident_f], [G[r][0]])
        levels(0, 3)

    def levels(k0, k1):
        for k in range(k0, k1):
            cur, nxt = k % 2, (k + 1) % 2
            for r in range(2):
                o.mm(N[r][:, 0:128], PTa[r][cur][:], Pa[r][cur][:], [PTa[r][cur], Pa[r][cur]], [Nb[r][0]])
                if k < 5:
                    o.mm(N[r][:, 128:256], Pa[r][cur][:], PTa[r][cur][:], [PTa[r][cur], Pa[r][cur]], [Nb[r][1]])
                o.act(Pa[r][nxt][:], N[r][:, 0:128], AF.Copy, [Nb[r][0]], [Pa[r][nxt]])
                if k < 5:
                    o.act(PTa[r][nxt][:], N[r][:, 128:256], AF.Copy, [Nb[r][1]], [PTa[r][nxt]])
            for r in range(2):
                o.mm(N[r][:, 256:384], Pa[r][nxt][:], G[r][cur][:], [Pa[r][nxt], G[r][cur]], [Nb[r][2]])
                o.tt(DVE, G[r][nxt][:], N[r][:, 256:384], G[r][cur][:], ADD, [Nb[r][2], G[r][cur]], [G[r][nxt]])

    def preB(tt):
        tp = tt % 2
        levels(3, 6)
        for r in range(2):
            ps_ = slice(r * 64, (r + 1) * 64)
            Gf = G[r][0]
            o.mm(B[r][ps_, 256:384], tokA[tp][:, r * 64:(r + 1) * 64], Gf[:], [tokA[tp], Gf], [Bb[r][3]])
            o.act(WmT[tp][ps_, :], B[r][ps_, 256:384], AF.Copy, [Bb[r][3]], [WmT[tp]])
            o.mm(B[r][:, 192:256], Gf[:], X0sb[r][:], [Gf, X0sb[r]], [Bb[r][2]])
            o.act(Zb[tp][:, r * 64:(r + 1) * 64], B[r][:, 192:256], AF.Copy, [Bb[r][2]], [Zb[tp]])

    def seqU(tt):
        tp = tt % 2
        o.mm(YS[:, 256:384], WmT[tp][:], Mb[:], [WmT[tp], Mb], [Ub_])
        o.tt(DVE, Ub[:], YS[:, 256:384], Zb[tp][:], ADD, [Ub_, Zb[tp]], [Ub])

    def seqY(tt):
        tp = tt % 2
        cs = slice(tt * 128, (tt + 1) * 128)
        yc_ = slice((tt % 2) * 128, (tt % 2) * 128 + 128)
        o.mm(YS[:, yc_], Mb[:], rt[:, cs], [Mb, rt], [Yb], start=True, stop=False, skip=True)
        for r in range(2):
            ps_ = slice(r * 64, (r + 1) * 64)
            o.mm(YS[ps_, yc_], Ub[:, r * 64:(r + 1) * 64], ArbT[tp][r][:], [Ub, ArbT[tp][r]], [Yb], start=False, stop=False, skip=True)
            o.mm(YS[ps_, yc_], tok[tp][:, 0, r * 64:(r + 1) * 64], ArkT[tp][r][:], [tok[tp], ArkT[tp][r]], [Yb], start=False, stop=(r == 1), skip=True)

    def seqM(tt):
        tp = tt % 2
        o.mm(YS[:, 384:512], tok[tp][:, 2, :], Ub[:], [tok[tp], Ub], [Mb_], start=True, stop=False)
        o.mm(YS[:, 384:512], tok[tp][:, 3, :], tok[tp][:, 0, :], [tok[tp]], [Mb_], start=False, stop=True)
        o.stt(M[:], M[:], gend[:, tt:tt + 1], YS[:, 384:512], MUL, ADD, [M, gend, Mb_], [M])
        o.tt(POOL, Mb[:], M[:], cx.blk_f[:], MUL, [M, cx.blk_f], [Mb])

    def outp(tt):
        t2 = tt // 2
        sl = slice(t2 * 256, (t2 + 1) * 256)
        kb.op(DVE, lambda: nc.vector.tensor_copy(out=ysb[:], in_=YS[:, 0:256]), R=[Yb], Wr=[ysb])
        kb.dma(bon[:], cx.bscr.ap[bc, sl], R=[cx.bscr.buf((blk, t2 // 2))], Wr=[bon])
        kb.dma(gin[:], cx.gscr.ap[bc, sl], R=[cx.gscr.buf((blk, t2 // 2))], Wr=[gin])
        o.mm(N[0][:, 0:256], cx.blk_f[:], ysb[:], [cx.blk_f, ysb], [Nb[0][0], Nb[0][1]])
        o.stt(yc[:], N[0][:, 0:256], -1.0 / 64, ysb[:], MUL, ADD, [Nb[0][0], Nb[0][1], ysb], [yc])
        o.act(sq[:], yc[:], AF.Square, [yc], [sq])
        o.mm(N[0][:, 0:256], cx.blk_f[:], sq[:], [cx.blk_f, sq], [Nb[0][0], Nb[0][1]])
        o.act(sq[:], N[0][:, 0:256], AF.Sqrt, [Nb[0][0], Nb[0][1], gneps], [sq], scale=1.0 / 64, bias=gneps[:, 0:1])
        kb.op(DVE, lambda: nc.vector.reciprocal(out=sq[:], in_=sq[:]), R=[sq], Wr=[sq])
        o.tt(DVE, yc[:], yc[:], sq[:], MUL, [yc, sq], [yc])
        o.ts(DVE, yc[:], yc[:], gnw[:, b1], gnb[:, b1], MUL, ADD, [yc, gnw, gnb], [yc])
        o.tt(DVE, yc[:], yc[:], bon[:], ADD, [yc, bon], [yc])
        o.tt(DVE, yo[:], yc[:], gin[:], MUL, [yc, gin], [yo])
        kb.dma(yT.ap[bc, sl], yo[:], R=[yo], Wr=[cx.xbuf(yT, (blk, t2))])

    preA(0)
    preB(0)
    for tt in range(ntile):
        seqU(tt)
        if tt + 1 < ntile:
            preA(tt + 1)
        seqY(tt)
        seqM(tt)
        if tt + 1 < ntile:
            preB(tt + 1)
        if tt % 2 == 1:
            outp(tt)


def phase_merge(kb, cx, layer, hT, ys, mergedT):
    nc = kb.nc
    o = OpsH(kb)
    w2d = cx.dram["w_in"][layer]
    with ExitStack() as es:
        yS = [kb.sb("mg_y%d" % n, [128, 2, T], BF16, es) for n in range(4)]
        for n in range(4):
            kb.dma(yS[n][:], ys[n].ap.rearrange("(c p) t -> p c t", p=128), Wr=[yS[n]])
        ws = WStream(kb, es, "mg_w", 8, 128, nbuf=2, alloc_wb=False)
        wg = [[kb.sb("mg_wg%d%d" % (i, n), [128, 8, 128], BF16, es) for n in range(4)] for i in range(2)]
        wbr = [[kb.sb("mg_wb%d%d" % (i, n), [128, 2, 128], BF16, es) for n in range(4)] for i in range(2)]
        pg = [kb.ps("mg_pg%d" % i, [128, 512], F32, es) for i in range(3)]
        pu = [kb.ps("mg_pu%d" % i, [128, 512], F32, es) for i in range(3)]
        sg = [kb.sb("mg_sg%d" % i, [128, 512], F32, es) for i in range(3)]
        acc = [kb.sb("mg_acc%d" % i, [128, 512], F32, es) for i in range(2)]
        mst = [kb.sb("mg_mst%d" % i, [128, 512], BF16, es) for i in range(2)]
        cnt = 0
        for fb in range(8):
            i = fb % 2
            for n in range(4):
                wload(kb, ws, wg[i][n], wcols(w2d, GATE_OFF + n * 1024 + fb * 128, 128))
                wload(kb, ws, wbr[i][n], cx.dram["w_branch"][layer, n].rearrange("(c p) f -> p c f", p=128)[:, :, fb * 128:(fb + 1) * 128])
            for tg in range(8):
                sl = slice(tg * 512, (tg + 1) * 512)
                a_ = acc[(fb * 8 + tg) % 2]
                for n in range(4):
                    j = cnt % 3
                    cnt += 1
                    for kc in range(8):
                        o.mm(pg[j][:], wg[i][n][:, kc, :], hT[:, kc, sl], [wg[i][n], hT], [pg[j]], start=(kc == 0), stop=(kc == 7))
                    for c2 in range(2):
                        o.mm(pu[j][:], wbr[i][n][:, c2, :], yS[n][:, c2, sl], [wbr[i][n], yS[n]], [pu[j]], start=(c2 == 0), stop=(c2 == 1))
                    o.act(sg[j][:], pg[j][:], AF.Sigmoid, [pg[j]], [sg[j]])
                    if n == 0:
                        o.tt(kb.DVE, a_[:], pu[j][:], sg[j][:], ALU.mult, [pu[j], sg[j]], [a_])
                    else:
                        o.tt(kb.DVE, sg[j][:], pu[j][:], sg[j][:], ALU.mult, [pu[j], sg[j]], [sg[j]])
                        if n < 3:
                            o.tt(kb.POOL, a_[:], a_[:], sg[j][:], ALU.add, [a_, sg[j]], [a_])
                        else:
                            m_ = mst[(fb * 8 + tg) % 2]
                            o.tt(kb.POOL, m_[:], a_[:], sg[j][:], ALU.add, [a_, sg[j]], [m_])
                            kb.dma(mergedT.ap[fb * 128:(fb + 1) * 128, sl], m_[:], R=[m_])
        kb.barrier()


def phase_outproj(kb, cx, w2d, kch, inT, x_in, x_out):
    nc = kb.nc
    o = OpsH(kb)
    with ExitStack() as es:
        act = kb.sb("op_in", [128, kch, T], BF16, es)
        kb.dma(act[:], inT.ap.rearrange("(c p) t -> p c t", p=128), Wr=[act])
        ws = WStream(kb, es, "op_w", kch, 128, nbuf=2, alloc_wb=True)
        pp = [kb.ps("op_pp%d" % i, [128, 512], F32, es) for i in range(3)]
        xt = [kb.sb("op_x%d" % i, [128, 512], F32, es) for i in range(3)]
        n = 0
        for fb in range(8):
            wb = ws.load(wcols(w2d, fb * 128, 128))
            for tg in range(8):
                sl = slice(tg * 512, (tg + 1) * 512)
                j = n % 3
                n += 1
                kb.dma(xt[j][:], x_in.ap[fb * 128:(fb + 1) * 128, sl], Wr=[xt[j]])
                for kc in range(kch):
                    o.mm(pp[j][:], wb[:, kc, :], act[:, kc, sl], [wb, act], [pp[j]], start=(kc == 0), stop=(kc == kch - 1))
                o.tt(kb.DVE, xt[j][:], pp[j][:], xt[j][:], ALU.add, [pp[j], xt[j]], [xt[j]])
                kb.dma(x_out.ap[fb * 128:(fb + 1) * 128, sl], xt[j][:], R=[xt[j]])
        kb.barrier()


def phase_ffn_up(kb, cx, layer, hT, actT):
    nc = kb.nc
    o = OpsH(kb)
    wg2d = cx.dram["w_ffn_gate"][layer]
    wu2d = cx.dram["w_ffn_up"][layer]
    with ExitStack() as es:
        ws = WStream(kb, es, "fu_w", 8, 128, nbuf=2, alloc_wb=False)
        wg = [kb.sb("fu_wg%d" % i, [128, 8, 128], BF16, es) for i in range(2)]
        wu = [kb.sb("fu_wu%d" % i, [128, 8, 128], BF16, es) for i in range(2)]
        pg = [kb.ps("fu_pg%d" % i, [128, 512], F32, es) for i in range(3)]
        pu = [kb.ps("fu_pu%d" % i, [128, 512], F32, es) for i in range(3)]
        sg = [kb.sb("fu_sg%d" % i, [128, 512], F32, es) for i in range(3)]
        ast = [kb.sb("fu_a%d" % i, [128, 512], BF16, es) for i in range(3)]
        n = 0
        for fb in range(DFF // 128):
            i = fb % 2
            wload(kb, ws, wg[i], wcols(wg2d, fb * 128, 128))
            wload(kb, ws, wu[i], wcols(wu2d, fb * 128, 128))
            for tg in range(8):
                sl = slice(tg * 512, (tg + 1) * 512)
                j = n % 3
                n += 1
                for kc in range(8):
                    o.mm(pg[j][:], wg[i][:, kc, :], hT[:, kc, sl], [wg[i], hT], [pg[j]], start=(kc == 0), stop=(kc == 7))
                for kc in range(8):
                    o.mm(pu[j][:], wu[i][:, kc, :], hT[:, kc, sl], [wu[i], hT], [pu[j]], start=(kc == 0), stop=(kc == 7))
                o.act(sg[j][:], pg[j][:], AF.Silu, [pg[j]], [sg[j]])
                o.tt(kb.DVE, ast[j][:], pu[j][:], sg[j][:], ALU.mult, [pu[j], sg[j]], [ast[j]])
                kb.dma(actT.ap[fb * 128:(fb + 1) * 128, sl], ast[j][:], R=[ast[j]])
        kb.barrier()


def phase_ffn_down(kb, cx, layer, actT, x_in, x_out):
    nc = kb.nc
    o = OpsH(kb)
    w2d = cx.dram["w_ffn_down"][layer]
    KC = DFF // 128
    with ExitStack() as es:
        wd = kb.sb("fd_wd", [128, KC, 1024], BF16, es)
        stg = [kb.sb("fd_stg%d" % i, [128, 1024], F32, es) for i in range(2)]
        wv = w2d.rearrange("(c p) f -> p c f", p=128)
        for c in range(KC):
            s_ = stg[c % 2]
            kb.dma(s_[:], wv[:, c, :], Wr=[s_])
            kb.op(kb.DVE if c % 2 == 0 else kb.POOL,
                  lambda: (nc.vector if c % 2 == 0 else nc.gpsimd).tensor_copy(out=wd[:, c, :], in_=s_[:]), R=[s_], Wr=[wd])
        ain = [kb.sb("fd_a%d" % i, [128, KC, 512], BF16, es) for i in range(2)]
        pp = [kb.ps("fd_pp%d" % i, [128, 512], F32, es) for i in range(3)]
        xt = [kb.sb("fd_x%d" % i, [128, 512], F32, es) for i in range(3)]
        n = 0
        av = actT.ap.rearrange("(c p) t -> p c t", p=128)
        for tg in range(8):
            sl = slice(tg * 512, (tg + 1) * 512)
            a_ = ain[tg % 2]
            kb.dma(a_[:], av[:, :, sl], Wr=[a_])
            for fb in range(8):
                j = n % 3
                n += 1
                kb.dma(xt[j][:], x_in.ap[fb * 128:(fb + 1) * 128, sl], Wr=[xt[j]])
                for kc in range(KC):
                    o.mm(pp[j][:], wd[:, kc, fb * 128:(fb + 1) * 128], a_[:, kc, :], [wd, a_], [pp[j]], start=(kc == 0), stop=(kc == KC - 1))
                o.tt(kb.DVE, xt[j][:], pp[j][:], xt[j][:], ALU.add, [pp[j], xt[j]], [xt[j]])
                kb.dma(x_out.ap[fb * 128:(fb + 1) * 128, sl], xt[j][:], R=[xt[j]])
        kb.barrier()


def build_program(first_layer=0, n_layers=DEPTH, final=True):
    nc = bass.Bass("TRN2", target_bir_lowering=False)
    cx = Ctx()
    for k, shp in CONST_SHAPES.items():
        cx.dram[k] = nc.dram_tensor(k, shp, const_dtype(k), kind="ExternalInput").ap()
    for k, shp in WEIGHT_SHAPES.items():
        cx.dram[k] = nc.dram_tensor(k, shp, F32, kind="ExternalInput").ap()
    xT = DT(nc.dram_tensor("xT", [D, T], F32, kind="ExternalInput").ap())
    outT = DT(nc.dram_tensor("outT", [D, T], F32, kind="ExternalOutput").ap())
    xa = DT(nc.dram_tensor("x_a", [D, T], F32).ap())
    xb = DT(nc.dram_tensor("x_b", [D, T], F32).ap())
    ys = [DT(nc.dram_tensor("y_mix%d" % n, [W, T], BF16).ap()) for n in range(4)]
    mergedT = DT(nc.dram_tensor("mergedT", [D, T], BF16).ap())
    actT = DT(nc.dram_tensor("actT", [DFF, T], BF16).ap())
    cx.vfirst = DT(nc.dram_tensor("vfirst", [W, T], F32).ap())
    cx.gscr = DT(nc.dram_tensor("gscr", [W, T], BF16).ap())
    cx.bscr = DT(nc.dram_tensor("bscr", [W, T], F32).ap())
    with ExitStack() as es:
        es.enter_context(nc.allow_non_contiguous_dma(reason="small parameter loads"))
        kb = KB(nc, es)
        load_consts(kb, cx)
        kb.barrier()
        x_cur = xT
        for layer in range(first_layer, first_layer + n_layers):
            with ExitStack() as esl:
                hT = kb.sb("hT", [128, 8, T], BF16, esl)
                phase_norm(kb, cx, x_cur, cx.dram["norm1_g"][layer], hT)
                phase_sb(kb, cx, layer, hT, ys[0])
                phase_rwkv(kb, cx, layer, hT, ys[1])
                phase_moba(kb, cx, layer, hT, ys[2])
                phase_hgrn(kb, cx, layer, hT, ys[3])
                phase_merge(kb, cx, layer, hT, ys, mergedT)
                kb.barrier()
            phase_outproj(kb, cx, cx.dram["w_out"][layer], 8, mergedT, x_cur, xa)
            with ExitStack() as esl:
                hT = kb.sb("h2T", [128, 8, T], BF16, esl)
                phase_norm(kb, cx, xa, cx.dram["norm2_g"][layer], hT)
                phase_ffn_up(kb, cx, layer, hT, actT)
                kb.barrier()
            phase_ffn_down(kb, cx, layer, actT, xa, xb)
            x_cur = xb
        if final:
            phase_norm(kb, cx, x_cur, cx.dram["final_g"], None, out_dram=outT)
        kb.barrier()
        print("program: n_ins", kb.n_ins, {e.name: (e.total, e.n_ep) for e in [kb.PE, kb.ACT, kb.DVE, kb.POOL]})
    return nc


_CACHE = {}


def kernel(**inputs):
    x = np.asarray(inputs["x"], dtype=np.float32)
    consts = host_consts()
    if "nc" not in _CACHE:
        _CACHE["nc"] = build_program()
    nc = _CACHE["nc"]
    shared = {k: np.ascontiguousarray(np.asarray(inputs[k], dtype=np.float32)) for k in WEIGHT_SHAPES}
    shared.update(consts)
    in_maps = []
    for b in range(NCORES):
        m = dict(shared)
        m["xT"] = np.ascontiguousarray(x[b].T)
        in_maps.append(m)
    res = run_bass_kernel_spmd(nc, in_maps, core_ids=list(range(NCORES)))
    out = np.stack([np.asarray(res.results[b]["outT"], dtype=np.float32).T for b in range(NCORES)], axis=0)
    return np.ascontiguousarray(out)
```

```python
import numpy as np
import ml_dtypes
from contextlib import ExitStack
import concourse.bass as bass
import concourse.mybir as mybir
from concourse.bass_utils import run_bass_kernel_spmd

F32 = mybir.dt.float32
BF16 = mybir.dt.bfloat16
AF = mybir.ActivationFunctionType
ALU = mybir.AluOpType
AX = mybir.AxisListType

D = 1024
T = 4096
DEPTH = 2
HD = 64
NH = 4
W = 256
DFF = 2816
SB_OFF = 0
RWKV_OFF = 768
MOBA_OFF = 1792
HGRN_OFF = 2560
GATE_OFF = 3584
IN_COLS = 7680
NEG_BIG = -30000.0
NORM_EPS = 1e-6
GN_EPS = 64e-5
NCORES = 8


class Buf:
    __slots__ = ("w", "r", "name", "psum")

    def __init__(self, name=""):
        self.w = None
        self.r = []
        self.name = name
        self.psum = False


class Tl:
    def __init__(self, t, name=""):
        self.t = t
        self.b = Buf(name)

    def __getitem__(self, idx):
        return self.t[idx]


def _bufs(lst):
    out = []
    for x in lst:
        if x is None:
            continue
        out.append(x.b if isinstance(x, Tl) else x)
    return out


class SemEpoch:
    def __init__(self, kb, name):
        self.sem = kb.es.enter_context(kb.nc.semaphore(name))
        self.cnt = 0


class Eng:
    def __init__(self, kb, name, eng):
        self.kb = kb
        self.name = name
        self.eng = eng
        self.n_ep = 0
        self.ep = SemEpoch(kb, "s_%s_0" % name)
        self.seen = {}
        self.total = 0

    @property
    def sem(self):
        return self.ep.sem

    @property
    def cnt(self):
        return self.ep.cnt

    def new_epoch(self):
        self.n_ep += 1
        self.ep = SemEpoch(self.kb, "s_%s_%d" % (self.name, self.n_ep))

    def wait(self, src, val):
        if self.seen.get(id(src), 0) >= val:
            return
        self.eng.wait_ge(src.sem, val)
        self.seen[id(src)] = val


class DmaSlot:
    def __init__(self, kb, i):
        self.sem = kb.es.enter_context(kb.nc.semaphore("s_dma%d" % i))
        self.cnt = 0


class KB:
    def __init__(self, nc, es, ndma=24):
        self.nc = nc
        self.es = es
        self.PE = Eng(self, "pe", nc.tensor)
        self.ACT = Eng(self, "act", nc.scalar)
        self.DVE = Eng(self, "dve", nc.vector)
        self.POOL = Eng(self, "pool", nc.gpsimd)
        self.SP = Eng(self, "sp", nc.sync)
        self.slots = [DmaSlot(self, i) for i in range(ndma)]
        self.slot_i = 0
        self.n_ins = 0

    def _deps(self, E, R, Wr):
        deps = []
        mine = E.ep
        for b in R:
            if b.w is not None:
                deps.append(b.w)
            if b.psum:
                for ev in b.r:
                    if ev[0] is not mine:
                        deps.append(ev)
        for b in Wr:
            if b.w is not None and (b.w[0] is not mine or E is not self.PE):
                deps.append(b.w)
            for ev in b.r:
                if ev[0] is not mine:
                    deps.append(ev)
        for src, val in deps:
            E.wait(src, val)

    def op(self, E, fn, R=(), Wr=()):
        R = _bufs(R)
        Wr = _bufs(Wr)
        self._deps(E, R, Wr)
        ins = fn()
        E.ep.cnt += 1
        E.total += 1
        ins.then_inc(E.ep.sem, 1)
        ev = (E.ep, E.ep.cnt)
        for b in R:
            b.r.append(ev)
        for b in Wr:
            b.w = ev
            b.r = []
        self.n_ins += 1
        return ins

    def dma(self, out, in_, R=(), Wr=(), q=None):
        q = q or self.SP
        R = _bufs(R)
        Wr = _bufs(Wr)
        self._deps(q, R, Wr)
        slot = self.slots[self.slot_i]
        self.slot_i = (self.slot_i + 1) % len(self.slots)
        if slot.cnt:
            q.wait(slot, slot.cnt)
        ins = q.eng.dma_start(out=out, in_=in_)
        slot.cnt += 16
        ins.then_inc(slot.sem, 16)
        ev = (slot, slot.cnt)
        for b in R:
            b.r.append(ev)
        for b in Wr:
            b.w = ev
            b.r = []
        self.n_ins += 1
        return ev

    def _uniq(self, name):
        self.n_names = getattr(self, "n_names", 0) + 1
        return "%s_%d" % (name, self.n_names)

    def sb(self, name, shape, dt, es=None):
        es = es or self.es
        name = self._uniq(name)
        return Tl(es.enter_context(self.nc.sbuf_tensor(name, list(shape), dt)), name)

    def ps(self, name, shape, dt=F32, es=None):
        es = es or self.es
        name = self._uniq(name)
        t_ = Tl(es.enter_context(self.nc.psum_tensor(name, list(shape), dt)), name)
        t_.b.psum = True
        return t_

    def barrier(self):
        engs = [self.PE, self.ACT, self.DVE, self.POOL, self.SP]
        for E in engs:
            for F in engs:
                if F is not E and F.ep.cnt:
                    E.wait(F.ep, F.ep.cnt)
            for sl in self.slots:
                if sl.cnt:
                    E.wait(sl, sl.cnt)
        for E in engs:
            if E.ep.cnt > 12000:
                E.new_epoch()

    def finish(self, bufs):
        for b in _bufs(bufs):
            if b.w is not None:
                self.SP.wait(b.w[0], b.w[1])
        self.barrier()


def pipeline(units, stages):
    n = len(units)
    ns = len(stages)
    for step in range(n + ns - 1):
        for s, st in enumerate(stages):
            i = step - s
            if 0 <= i < n:
                st(units[i], i)


def host_consts():
    c = {}
    s = np.arange(128)[:, None]
    t = np.arange(512)[None, :]
    sbm = np.stack([((128 * o + s) < t) for o in range(4)]).astype(np.float32)
    c["c_mask_lt"] = sbm
    c["c_mask_le"] = np.stack([((128 * o + s) <= t) for o in range(4)]).astype(np.float32)
    a = np.arange(128)
    c["c_tri_ge"] = (a[:, None] >= a[None, :]).astype(np.float32)
    c["c_ones"] = np.ones((128, 128), np.float32)
    c["c_ident"] = np.eye(128, dtype=np.float32)
    c["c_blk64"] = (a[:, None] // 64 == a[None, :] // 64).astype(np.float32)
    half = 8
    inv_freq = (np.float32(500000.0) ** (-np.arange(0, 16, 2, dtype=np.float32) / np.float32(16))).astype(np.float32)
    ang = (np.arange(T, dtype=np.float32)[:, None] * inv_freq[None, :]).astype(np.float32)
    cos, sin = np.cos(ang).astype(np.float32), np.sin(ang).astype(np.float32)
    cosF = np.ones((64, T), np.float32)
    sinF = np.zeros((64, T), np.float32)
    cosF[0:8] = cos.T
    cosF[8:16] = cos.T
    sinF[0:8] = -sin.T
    sinF[8:16] = sin.T
    c["c_cosF"] = np.concatenate([cosF, cosF], 0)
    c["c_sinF"] = np.concatenate([sinF, sinF], 0)
    n = np.arange(16)
    qb = np.arange(16)[:, None]
    past = (n[None, :] < qb)
    own = (n[None, :] == qb)
    BIG = -NEG_BIG
    def rep(m):
        return np.ascontiguousarray(np.broadcast_to(m[None, :, None, :], (128, 16, 4, 16))).astype(np.float32)
    c["c_pastneg"] = rep(np.where(past, 0.0, -1e30))
    c["c_pastbig"] = rep(np.where(past, BIG, 0.0))
    c["c_ownm"] = rep(np.where(own, 0.0, -BIG))
    E = np.zeros((128, 16, 128), np.float32)
    for h in range(2):
        for i in range(16):
            E[64 * h + i, i, :] = 1.0
    c["c_E_b"] = E.astype(ml_dtypes.bfloat16)
    c["c_hgmask"] = ((a[:, None] // 32 == a[None, :] // 32) & (a[:, None] <= a[None, :])).astype(np.float32)
    c["c_scanmask"] = (np.broadcast_to((np.arange(512) % 32 != 0)[None, :], (128, 512))).astype(np.float32).copy()
    c["c_rw_low"] = (a[:, None] > a[None, :]).astype(np.float32)
    c["c_rw_upS"] = (a[:, None] < a[None, :]).astype(np.float32)
    c["c_rw_upI"] = (a[:, None] <= a[None, :]).astype(np.float32)
    c["c_scanmask128"] = (np.broadcast_to((np.arange(512) % 128 != 0)[None, :], (128, 512))).astype(np.float32).copy()
    c["c_mask_le_b"] = c["c_mask_le"].astype(ml_dtypes.bfloat16)
    c["c_mask_lt_b"] = c["c_mask_lt"].astype(ml_dtypes.bfloat16)
    return c


CONST_SHAPES = {
    "c_mask_lt": [4, 128, 512],
    "c_mask_le": [4, 128, 512],
    "c_tri_ge": [128, 128],
    "c_ones": [128, 128],
    "c_ident": [128, 128],
    "c_blk64": [128, 128],
    "c_cosF": [128, 4096],
    "c_sinF": [128, 4096],
    "c_pastneg": [128, 16, 4, 16],
    "c_pastbig": [128, 16, 4, 16],
    "c_ownm": [128, 16, 4, 16],
    "c_E_b": [128, 16, 128],
    "c_hgmask": [128, 128],
    "c_scanmask": [128, 512],
    "c_rw_low": [128, 128],
    "c_rw_upS": [128, 128],
    "c_rw_upI": [128, 128],
    "c_scanmask128": [128, 512],
    "c_mask_le_b": [4, 128, 512],
    "c_mask_lt_b": [4, 128, 512],
}


def const_dtype(k):
    return BF16 if k.endswith("_b") else F32


WEIGHT_SHAPES = {
    "norm1_g": [2, 1024], "w_in": [2, 1024, 7680], "rwkv_mu": [2, 1024], "rwkv_w0": [2, 256],
    "rwkv_w2": [2, 64, 256], "rwkv_a0": [2, 256], "rwkv_a2": [2, 64, 256], "rwkv_g2": [2, 128, 256],
    "rwkv_k_k": [2, 256], "rwkv_k_a": [2, 256], "rwkv_r_k": [2, 4, 64], "rwkv_gn_w": [2, 256],
    "rwkv_gn_b": [2, 256], "rwkv_v0": [1, 256], "rwkv_v1": [1, 256, 32], "rwkv_v2": [1, 32, 256],
    "hgrn_lb_logits": [2, 256], "hgrn_gn_g": [2, 256], "w_branch": [2, 4, 256, 1024],
    "w_out": [2, 1024, 1024], "norm2_g": [2, 1024], "w_ffn_gate": [2, 1024, 2816],
    "w_ffn_up": [2, 1024, 2816], "w_ffn_down": [2, 2816, 1024], "final_g": [1024],
}


class DT:
    def __init__(self, ap):
        self.ap = ap
        self.bufs = {}

    def buf(self, i):
        if i not in self.bufs:
            self.bufs[i] = Buf()
        return self.bufs[i]

    def all(self):
        return list(self.bufs.values())


class Ctx:
    def __init__(self):
        self.dram = {}

    def xbuf(self, dt, i):
        return dt.buf(i)


def load_consts(kb, cx):
    nc = kb.nc
    cx.ones_f = kb.sb("ones_f", [128, 128], F32)
    cx.tri_f = kb.sb("tri_f", [128, 128], F32)
    cx.ident_f = kb.sb("ident_f", [128, 128], F32)
    cx.blk_f = kb.sb("blk_f", [128, 128], F32)
    cx.ones_b = kb.sb("ones_b", [128, 128], BF16)
    cx.ident_b = kb.sb("ident_b", [128, 128], BF16)
    kb.dma(cx.ones_f[:], cx.dram["c_ones"][:, :], Wr=[cx.ones_f])
    kb.dma(cx.tri_f[:], cx.dram["c_tri_ge"][:, :], Wr=[cx.tri_f])
    kb.dma(cx.ident_f[:], cx.dram["c_ident"][:, :], Wr=[cx.ident_f])
    kb.dma(cx.blk_f[:], cx.dram["c_blk64"][:, :], Wr=[cx.blk_f])
    cx.eps_norm = kb.sb("eps_norm", [128, 1], F32)
    cx.one_c = kb.sb("one_c", [128, 1], F32)
    kb.op(kb.POOL, lambda: nc.gpsimd.memset(cx.eps_norm[:], NORM_EPS), Wr=[cx.eps_norm])
    kb.op(kb.POOL, lambda: nc.gpsimd.memset(cx.one_c[:], 1.0), Wr=[cx.one_c])
    kb.op(kb.DVE, lambda: nc.vector.tensor_copy(out=cx.ones_b[:], in_=cx.ones_f[:]), R=[cx.ones_f], Wr=[cx.ones_b])
    kb.op(kb.DVE, lambda: nc.vector.tensor_copy(out=cx.ident_b[:], in_=cx.ident_f[:]), R=[cx.ident_f], Wr=[cx.ident_b])


class WStream:
    def __init__(self, kb, es, name, kch, ncols, nbuf=2, alloc_wb=True):
        self.kb = kb
        self.kch = kch
        self.ncols = ncols
        self.stg = [kb.sb("%s_stg%d" % (name, i), [128, kch, ncols], F32, es) for i in range(nbuf)]
        self.wb = [kb.sb("%s_wb%d" % (name, i), [128, kch, ncols], BF16, es) for i in range(nbuf)] if alloc_wb else []
        self.i = 0

    def load(self, src_ap, eng=None, kch=None, ncols=None):
        kb = self.kb
        nc = kb.nc
        kch = kch or self.kch
        ncols = ncols or self.ncols
        i = self.i
        self.i = (self.i + 1) % len(self.stg)
        stg, wb = self.stg[i], self.wb[i]
        kb.dma(stg[:, :kch, :ncols], src_ap, Wr=[stg])
        eng = eng or kb.DVE
        kb.op(eng, lambda: eng.eng.tensor_copy(out=wb[:, :kch, :ncols], in_=stg[:, :kch, :ncols]), R=[stg], Wr=[wb])
        return wb


def wload(kb, ws, dst, src_ap, eng=None):
    nc = kb.nc
    i = ws.i
    ws.i = (ws.i + 1) % len(ws.stg)
    stg = ws.stg[i]
    kch, ncols = dst.t.shape[1], dst.t.shape[2]
    kb.dma(stg[:, :kch, :ncols], src_ap, Wr=[stg])
    eng = eng or kb.DVE
    kb.op(eng, lambda: eng.eng.tensor_copy(out=dst[:], in_=stg[:, :kch, :ncols]), R=[stg], Wr=[dst])
    return dst


def wcols(w2d, c0, ncols):
    return w2d.rearrange("(kc p) f -> p kc f", p=128)[:, :, c0:c0 + ncols]


def phase_norm(kb, cx, xT, g_ap, hT, out_dram=None, tgw=512, nbuf=2):
    nc = kb.nc
    with ExitStack() as es:
        gT = kb.sb("n_gT", [128, 8], F32, es)
        kb.dma(gT[:], g_ap.rearrange("(kc p) -> p kc", p=128), Wr=[gT])
        xt = [kb.sb("n_x%d" % i, [128, 8, tgw], F32, es) for i in range(nbuf)]
        sq = [kb.sb("n_sq%d" % i, [128, 8, tgw], F32, es) for i in range(nbuf)]
        rs = [kb.sb("n_rs%d" % i, [128, tgw], F32, es) for i in range(nbuf)]
        ob = [kb.sb("n_ob%d" % i, [128, 8, tgw], F32, es) for i in range(nbuf)] if out_dram is not None else None
        pss = [kb.ps("n_ps%d" % i, [128, tgw], F32, es) for i in range(nbuf)]
        xv = xT.ap.rearrange("(kc p) t -> p kc t", p=128)
        for tg in range(T // tgw):
            i = tg % nbuf
            x_, sq_, rs_, ps_ = xt[i], sq[i], rs[i], pss[i]
            kb.dma(x_[:], xv[:, :, tg * tgw:(tg + 1) * tgw], R=[cx.xbuf(xT, tg)], Wr=[x_])
            kb.op(kb.ACT, lambda: nc.scalar.activation(out=sq_[:], in_=x_[:], func=AF.Square), R=[x_], Wr=[sq_])
            for kc in range(8):
                kb.op(kb.PE, lambda: nc.tensor.matmul(ps_[:], lhsT=cx.ones_f[:], rhs=sq_[:, kc, :],
                                                      start=(kc == 0), stop=(kc == 7)),
                      R=[cx.ones_f, sq_], Wr=[ps_])
            kb.op(kb.ACT, lambda: nc.scalar.activation(out=rs_[:], in_=ps_[:], func=AF.Sqrt, scale=1.0 / D,
                                                       bias=cx.eps_norm[:, 0:1]),
                  R=[ps_, cx.eps_norm], Wr=[rs_])
            kb.op(kb.DVE, lambda: nc.vector.reciprocal(out=rs_[:], in_=rs_[:]), R=[rs_], Wr=[rs_])
            for kc in range(8):
                if out_dram is None:
                    o_ap = hT[:, kc, tg * tgw:(tg + 1) * tgw]
                    wr = [hT]
                else:
                    o_ap = ob[i][:, kc, :]
                    wr = [ob[i]]
                kb.op(kb.DVE, lambda: nc.vector.scalar_tensor_tensor(out=o_ap, in0=x_[:, kc, :], scalar=gT[:, kc:kc + 1],
                                                                     in1=rs_[:], op0=ALU.mult, op1=ALU.mult),
                      R=[x_, gT, rs_], Wr=wr)
            if out_dram is not None:
                kb.dma(out_dram.ap.rearrange("(kc p) t -> p kc t", p=128)[:, :, tg * tgw:(tg + 1) * tgw], ob[i][:],
                       R=[ob[i]], Wr=[cx.xbuf(out_dram, tg)])
        kb.barrier()


def phase_sb(kb, cx, layer, hT, yT, dbg=None, defer_es=None, pre=None, alloc_only=False):
    nc = kb.nc
    w2d = cx.dram["w_in"][layer] if not alloc_only else None
    with ExitStack() as es_own:
        es = defer_es if defer_es is not None else es_own

        def P(name, shape, dt):
            if pre is not None and name in pre:
                return pre[name]
            t_ = kb.sb(name, shape, dt, es)
            if pre is not None:
                pre[name] = t_
            return t_
        QT = P("sb_QT", [128, 2, T], BF16)
        KT = P("sb_KT", [128, 2, T], BF16)
        V = P("sb_V", [128, 32, 256], BF16)
        if alloc_only:
            return None
        if dbg == "p0":
            kb.dma(yT.ap[0:128, 0:2048], mlt_b[:].rearrange("p o t -> p (o t)"), R=[mlt_b], Wr=[cx.xbuf(yT, 0)])
            return
        with ExitStack() as es2:
            ws = WStream(kb, es2, "sb_w", 8, 256, nbuf=(1 if defer_es is not None else 2))
            if dbg == "p1":
                wb = ws.load(wcols(w2d, SB_OFF, 256))
                kb.dma(yT.ap[0:128, 0:2048], wb[:].rearrange("p o t -> p (o t)"), R=[wb], Wr=[cx.xbuf(yT, 0)])
                return
            pp = [kb.ps("sb_pp%d" % i, [128, 512], F32, es2) for i in range(2)]
            n = 0
            for which in range(2 if dbg != "p3" else 0):
                wb = ws.load(wcols(w2d, SB_OFF + which * 256, 256))
                for fb in range(2):
                    for tg in range(8):
                        ps_ = pp[n % 2]
                        n += 1
                        for kc in range(8):
                            kb.op(kb.PE, lambda: nc.tensor.matmul(ps_[:], lhsT=wb[:, kc, fb * 128:(fb + 1) * 128],
                                                                  rhs=hT[:, kc, tg * 512:(tg + 1) * 512],
                                                                  start=(kc == 0), stop=(kc == 7)),
                                  R=[wb, hT], Wr=[ps_])
                        sl = slice(tg * 512, (tg + 1) * 512)
                        if which == 0:
                            kb.op(kb.ACT, lambda: nc.scalar.activation(out=QT[:, fb, sl], in_=ps_[:], func=AF.Copy, scale=0.125),
                                  R=[ps_], Wr=[QT])
                        else:
                            kb.op(kb.ACT, lambda: nc.scalar.activation(out=KT[:, fb, sl], in_=ps_[:], func=AF.Copy),
                                  R=[ps_], Wr=[KT])
            wb = ws.load(wcols(w2d, SB_OFF + 512, 256))
            for tt in range(32 if dbg not in ("p2", "p2a") else 0):
                ps_ = pp[n % 2]
                n += 1
                for kc in range(8):
                    kb.op(kb.PE, lambda: nc.tensor.matmul(ps_[:, 0:256], lhsT=hT[:, kc, tt * 128:(tt + 1) * 128],
                                                          rhs=wb[:, kc, :], start=(kc == 0), stop=(kc == 7)),
                          R=[wb, hT], Wr=[ps_])
                kb.op(kb.ACT, lambda: nc.scalar.activation(out=V[:, tt, :], in_=ps_[:, 0:256], func=AF.Copy), R=[ps_], Wr=[V])
            kb.barrier()
        if dbg == "p3":
            kb.dma(yT.ap[0:128, :], V[:, 0:16, :].rearrange("p o t -> p (o t)"), R=[V], Wr=[cx.xbuf(yT, 0)])
            return
        if dbg in ("proj", "p2", "p2a"):
            for b2 in range(2):
                kb.dma(yT.ap[b2 * 128:(b2 + 1) * 128, :], KT[:, b2, :], R=[KT], Wr=[cx.xbuf(yT, 0)])
            return
        def attn(es2):
            NB = 3
            mlt_b = kb.sb("sb_mltb", [128, 4, 512], BF16, es2)
            kb.dma(mlt_b[:], cx.dram["c_mask_lt_b"].rearrange("o s t -> s o t"), Wr=[mlt_b])
            pz = [kb.ps("sb_pz%d" % i, [128, 512], F32, es2) for i in range(2)]
            pc = [kb.ps("sb_pc%d" % i, [128, 512], F32, es2) for i in range(2)]
            po = [kb.ps("sb_po%d" % i, [64, 512], F32, es2) for i in range(1 if defer_es is not None else 2)]
            ee = [kb.sb("sb_e%d" % i, [128, 512], F32, es2) for i in range(NB)]
            lk = [kb.sb("sb_lk%d" % i, [128, 512], BF16, es2) for i in range(NB)]
            arb = [kb.sb("sb_arb%d" % i, [128, 512], BF16, es2) for i in range(2)]
            tri_b = kb.sb("sb_trib", [128, 128], BF16, es2)
            kb.op(kb.DVE, lambda: nc.vector.tensor_copy(out=tri_b[:], in_=cx.tri_f[:]), R=[cx.tri_f], Wr=[tri_b])
            at = [kb.sb("sb_at%d" % i, [128, 512], BF16, es2) for i in range(NB)]
            arun = [kb.sb("sb_ar%d" % i, [128, 512], F32, es2) for i in range(2)]
            yst = [kb.sb("sb_y%d" % i, [64, 512], BF16, es2) for i in range(2)]
            units = []
            gi = 0
            for h in range(NH):
                for g in range(8):
                    for j in range(4 * g + 3, -1, -1):
                        units.append((h, g, j, gi))
                    gi += 1

            def stA(u, i):
                h, g, j, gi = u
                blk, po_ = h // 2, (h % 2) * 64
                pz_, e_, lk_ = pz[i % 2], ee[i % NB], lk[i % NB]
                kb.op(kb.PE, lambda: nc.tensor.matmul(pz_[:], lhsT=KT[po_:po_ + 64, blk, j * 128:(j + 1) * 128],
                                                      rhs=QT[po_:po_ + 64, blk, g * 512:(g + 1) * 512], start=True, stop=True),
                      R=[KT, QT], Wr=[pz_])
                kb.op(kb.ACT, lambda: nc.scalar.activation(out=e_[:], in_=pz_[:], func=AF.Exp), R=[pz_], Wr=[e_])
                kb.op(kb.ACT, lambda: nc.scalar.activation(out=lk_[:], in_=e_[:], func=AF.Ln, bias=cx.one_c[:, 0:1]),
                      R=[e_, cx.one_c], Wr=[lk_])
                o = j - 4 * g
                if o >= 0:
                    kb.op(kb.POOL, lambda: nc.gpsimd.tensor_tensor(out=lk_[:], in0=lk_[:], in1=mlt_b[:, o, :], op=ALU.mult),
                          R=[lk_, mlt_b], Wr=[lk_])

            def stB(u, i):
                h, g, j, gi = u
                blk, po_ = h // 2, (h % 2) * 64
                pc_, lk_, at_, ar_, arb_ = pc[i % 2], lk[i % NB], at[i % NB], arun[gi % 2], arb[i % 2]
                first = (j == 4 * g + 3)
                kb.op(kb.PE, lambda: nc.tensor.matmul(pc_[:], lhsT=tri_b[:], rhs=lk_[:], start=True, stop=first),
                      R=[tri_b, lk_], Wr=[pc_])
                if not first:
                    kb.op(kb.PE, lambda: nc.tensor.matmul(pc_[:], lhsT=cx.ones_b[:], rhs=arb_[:], start=False, stop=True),
                          R=[cx.ones_b, arb_], Wr=[pc_])
                e_ = ee[i % NB]
                kb.op(kb.ACT, lambda: nc.scalar.activation(out=at_[:], in_=pc_[:], func=AF.Exp, scale=-1.0), R=[pc_], Wr=[at_])
                kb.op(kb.POOL, lambda: nc.gpsimd.tensor_tensor(out=at_[:], in0=at_[:], in1=e_[:], op=ALU.mult), R=[at_, e_], Wr=[at_])
                o = j - 4 * g
                if o >= 0:
                    kb.op(kb.POOL, lambda: nc.gpsimd.tensor_tensor(out=at_[:], in0=at_[:], in1=mlt_b[:, o, :], op=ALU.mult),
                          R=[at_, mlt_b], Wr=[at_])
                if j > 0:
                    arn_ = arb[(i + 1) % 2]
                    if first:
                        kb.op(kb.DVE, lambda: nc.vector.tensor_copy(out=arn_[:], in_=lk_[:]), R=[lk_], Wr=[arn_])
                        if j > 1:
                            kb.op(kb.DVE, lambda: nc.vector.tensor_copy(out=ar_[:], in_=lk_[:]), R=[lk_], Wr=[ar_])
                    else:
                        kb.op(kb.DVE, lambda: nc.vector.tensor_tensor(out=arn_[:], in0=ar_[:], in1=lk_[:], op=ALU.add),
                              R=[lk_, ar_], Wr=[arn_])
                        if j > 1:
                            kb.op(kb.DVE, lambda: nc.vector.tensor_tensor(out=ar_[:], in0=ar_[:], in1=lk_[:], op=ALU.add),
                                  R=[lk_, ar_], Wr=[ar_])

            def stC(u, i):
                h, g, j, gi = u
                at_, po_t = at[i % NB], po[gi % len(po)]
                first = (j == 4 * g + 3)
                kb.op(kb.PE, lambda: nc.tensor.matmul(po_t[:], lhsT=V[:, j, h * 64:(h + 1) * 64], rhs=at_[:],
                                                      start=first, stop=(j == 0)),
                      R=[V, at_], Wr=[po_t])
                if j == 0:
                    y_ = yst[gi % 2]
                    kb.op(kb.DVE, lambda: nc.vector.tensor_copy(out=y_[:], in_=po_t[:]), R=[po_t], Wr=[y_])
                    kb.dma(yT.ap[h * 64:(h + 1) * 64, g * 512:(g + 1) * 512], y_[:], R=[y_], Wr=[cx.xbuf(yT, g)])

            if dbg is not None and dbg.startswith("n"):
                units = units[:int(dbg[1:])]
            return units, [stA, stB, stC]

        if defer_es is not None:
            return attn
        with ExitStack() as es2:
            units, stages = attn(es2)
            pipeline(units, stages)
            kb.barrier()


def proj_fm(kb, hT, wb, ncols, pp, cnt, epi):
    nc = kb.nc
    for fb in range(ncols // 128):
        for tg in range(T // 512):
            ps_ = pp[cnt[0] % len(pp)]
            cnt[0] += 1
            for kc in range(8):
                kb.op(kb.PE, lambda: nc.tensor.matmul(ps_[:], lhsT=wb[:, kc, fb * 128:(fb + 1) * 128],
                                                      rhs=hT[:, kc, tg * 512:(tg + 1) * 512],
                                                      start=(kc == 0), stop=(kc == 7)),
                      R=[wb, hT], Wr=[ps_])
            epi(fb, tg, ps_)


def proj_tm(kb, hT, wb, ncols, pp, cnt, epi):
    nc = kb.nc
    for tt in range(T // 128):
        ps_ = pp[cnt[0] % len(pp)]
        cnt[0] += 1
        for kc in range(8):
            kb.op(kb.PE, lambda: nc.tensor.matmul(ps_[:, 0:ncols], lhsT=hT[:, kc, tt * 128:(tt + 1) * 128],
                                                  rhs=wb[:, kc, 0:ncols], start=(kc == 0), stop=(kc == 7)),
                  R=[wb, hT], Wr=[ps_])
        epi(tt, ps_)


def phase_moba(kb, cx, layer, hT, yT, dbg=None, defer_es=None, pre=None, alloc_only=False):
    nc = kb.nc
    w2d = cx.dram["w_in"][layer] if not alloc_only else None
    with ExitStack() as es_own:
        es = defer_es if defer_es is not None else es_own

        def P(name, shape, dt):
            if pre is not None and name in pre:
                return pre[name]
            t_ = kb.sb(name, shape, dt, es)
            if pre is not None:
                pre[name] = t_
            return t_
        QT = P("mb_QT", [128, 2, T], BF16)
        KT = P("mb_KT", [128, 2, T], BF16)
        V = P("mb_V", [128, 32, 4, 65], BF16)
        esel = P("mb_esel", [128, 64], F32)
        selbT = P("mb_selbT", [128, 2, T], BF16)
        E_b = P("mb_Eb", [128, 16, 128], BF16)
        kmT = P("mb_kmT", [128, 2, 16], BF16)
        if alloc_only:
            return None
        kb.op(kb.POOL, lambda: nc.gpsimd.memset(V[:, :, :, 64:65], 1.0), Wr=[V])
        kb.op(kb.POOL, lambda: nc.gpsimd.memset(esel[:], 0.0), Wr=[esel])
        kb.op(kb.POOL, lambda: nc.gpsimd.memset(esel[64:65, :], 1.0), Wr=[esel])
        kb.op(kb.POOL, lambda: nc.gpsimd.memset(selbT[:], 0.0), Wr=[selbT])
        kb.dma(E_b[:], cx.dram["c_E_b"][:, :, :], Wr=[E_b])
        with ExitStack() as es2:
            nb_ = 1 if defer_es is not None else 2
            cosS = [kb.sb("mb_cos%d" % i, [128, 512], F32, es2) for i in range(nb_)]
            sinS = [kb.sb("mb_sin%d" % i, [128, 512], F32, es2) for i in range(nb_)]
            ws = WStream(kb, es2, "mb_w", 8, 256, nbuf=(1 if defer_es is not None else 2))
            wp = kb.sb("mb_wp", [128, 8, 256], BF16, es2)
            t1 = [kb.sb("mb_t1%d" % i, [128, 512], F32, es2) for i in range(nb_)]
            t2 = [kb.sb("mb_t2%d" % i, [128, 512], F32, es2) for i in range(nb_)]
            pp = [kb.ps("mb_pp%d" % i, [128, 512], F32, es2) for i in range(2)]
            pq = [kb.ps("mb_pq%d" % i, [128, 512], F32, es2) for i in range(2)]
            cnt = [0]
            cnt2 = [0]
            for which in range(2):
                wb = ws.load(wcols(w2d, MOBA_OFF + which * 256, 256))
                wb4 = wb[:].rearrange("p k (h d) -> p k h d", h=4)
                wp4 = wp[:].rearrange("p k (h d) -> p k h d", h=4)
                kb.op(kb.POOL, lambda: nc.gpsimd.tensor_copy(out=wp4[:, :, :, 0:8], in_=wb4[:, :, :, 8:16]), R=[wb], Wr=[wp])
                kb.op(kb.POOL, lambda: nc.gpsimd.tensor_copy(out=wp4[:, :, :, 8:16], in_=wb4[:, :, :, 0:8]), R=[wb], Wr=[wp])
                kb.op(kb.POOL, lambda: nc.gpsimd.tensor_copy(out=wp4[:, :, :, 16:64], in_=wb4[:, :, :, 16:64]), R=[wb], Wr=[wp])
                dst = QT if which == 0 else KT
                sc = 0.125 if which == 0 else 1.0
                for fb in range(2):
                    for tg in range(8):
                        sl = slice(tg * 512, (tg + 1) * 512)
                        pa = pp[cnt[0] % 2]
                        pb = pq[cnt[0] % 2]
                        a1, a2 = t1[cnt[0] % nb_], t2[cnt[0] % nb_]
                        cosF, sinF = cosS[cnt[0] % nb_], sinS[cnt[0] % nb_]
                        cnt[0] += 1
                        kb.dma(cosF[:], cx.dram["c_cosF"][:, sl], Wr=[cosF])
                        kb.dma(sinF[:], cx.dram["c_sinF"][:, sl], Wr=[sinF])
                        for kc in range(8):
                            kb.op(kb.PE, lambda: nc.tensor.matmul(pa[:], lhsT=wb[:, kc, fb * 128:(fb + 1) * 128], rhs=hT[:, kc, sl],
                                                                  start=(kc == 0), stop=(kc == 7)), R=[wb, hT], Wr=[pa])
                        for kc in range(8):
                            kb.op(kb.PE, lambda: nc.tensor.matmul(pb[:], lhsT=wp[:, kc, fb * 128:(fb + 1) * 128], rhs=hT[:, kc, sl],
                                                                  start=(kc == 0), stop=(kc == 7)), R=[wp, hT], Wr=[pb])
                        kb.op(kb.DVE, lambda: nc.vector.tensor_tensor(out=a1[:], in0=pa[:], in1=cosF[:], op=ALU.mult),
                              R=[pa, cosF], Wr=[a1])
                        kb.op(kb.DVE, lambda: nc.vector.tensor_tensor(out=a2[:], in0=pb[:], in1=sinF[:], op=ALU.mult),
                              R=[pb, sinF], Wr=[a2])
                        if sc == 1.0:
                            kb.op(kb.POOL, lambda: nc.gpsimd.tensor_tensor(out=dst[:, fb, sl], in0=a1[:], in1=a2[:], op=ALU.add),
                                  R=[a1, a2], Wr=[dst])
                        else:
                            kb.op(kb.POOL, lambda: nc.gpsimd.tensor_tensor(out=a1[:], in0=a1[:], in1=a2[:], op=ALU.add),
                                  R=[a1, a2], Wr=[a1])
                        if sc != 1.0:
                            kb.op(kb.ACT, lambda: nc.scalar.activation(out=dst[:, fb, sl], in_=a1[:], func=AF.Copy, scale=sc),
                                  R=[a1], Wr=[dst])
            wb = ws.load(wcols(w2d, MOBA_OFF + 512, 256))
            proj_tm(kb, hT, wb, 256, pp, cnt2,
                    lambda tt, ps_: kb.op(kb.ACT, lambda: nc.scalar.activation(out=V[:, tt, :, 0:64],
                                                                               in_=ps_[:, 0:256].rearrange("p (h d) -> p h d", h=4), func=AF.Copy),
                                          R=[ps_], Wr=[V]))
            kb.barrier()
        if dbg == "mproj":
            for b2 in range(2):
                kb.dma(yT.ap[b2 * 128:(b2 + 1) * 128, :], KT[:, b2, :], R=[KT], Wr=[cx.xbuf(yT, 0)])
            return
        with ExitStack() as es2:
            kmf = kb.sb("mb_kmf", [128, 2, 16], F32, es2)
            cpn = kb.sb("mb_cpn", [128, 16, 4, 16], F32, es2)
            cpb = kb.sb("mb_cpb", [128, 16, 4, 16], F32, es2)
            com = kb.sb("mb_com", [128, 16, 4, 16], F32, es2)
            kb.dma(cpn[:], cx.dram["c_pastneg"][:, :, :, :], Wr=[cpn])
            kb.dma(cpb[:], cx.dram["c_pastbig"][:, :, :, :], Wr=[cpb])
            kb.dma(com[:], cx.dram["c_ownm"][:, :, :, :], Wr=[com])
            for b2 in range(2):
                kb.op(kb.DVE, lambda: nc.vector.tensor_reduce(out=kmf[:, b2, :], in_=KT[:, b2, :].rearrange("p (n s) -> p n s", s=256),
                                                              axis=AX.X, op=ALU.add), R=[KT], Wr=[kmf])
            kb.op(kb.DVE, lambda: nc.vector.tensor_scalar(out=kmT[:], in0=kmf[:], scalar1=1.0 / 256, scalar2=None, op0=ALU.mult),
                  R=[kmf], Wr=[kmT])
            pg = [kb.ps("mb_pg%d" % i, [128, 512], F32, es2) for i in range(4)]
            pt = [kb.ps("mb_pt%d" % i, [128, 2, 128], BF16, es2) for i in range(2)]

            gm = [kb.sb("mb_gm%d" % i, [128, 4, 16], F32, es2) for i in range(2)]
            m8 = [kb.sb("mb_m8%d" % i, [128, 4, 8], F32, es2) for i in range(2)]
            sel = [kb.sb("mb_sel%d" % i, [128, 4, 16], F32, es2) for i in range(2)]
            selb = [kb.sb("mb_selb%d" % i, [128, 4, 16], BF16, es2) for i in range(2)]
            lvl = int(dbg[3:]) if (dbg or "").startswith("sel") and len(dbg) > 3 else 99
            for tt in range(32 if lvl > 0 else 0):
                i = tt % 2
                qb = tt // 2
                pt_, gm_, m8_, sel_, selb_ = pt[i], gm[i], m8[i], sel[i], selb[i]
                for r in range(2):
                    pg_ = pg[2 * i + r]
                    for q in range(2):
                        kb.op(kb.PE, lambda: nc.tensor.matmul(pg_[:, q * 16:(q + 1) * 16], lhsT=QT[r * 64:r * 64 + 64, q, tt * 128:(tt + 1) * 128],
                                                              rhs=kmT[r * 64:r * 64 + 64, q, :], start=True, stop=True),
                              R=[QT, kmT], Wr=[pg_])
                    kb.op(kb.DVE, lambda: nc.vector.tensor_tensor(out=gm_[:, 2 * r:2 * r + 2, :],
                                                                  in0=pg_[:, 0:32].rearrange("p (q n) -> p q n", q=2),
                                                                  in1=cpn[:, qb, 2 * r:2 * r + 2, :], op=ALU.add),
                          R=[pg_, cpn], Wr=[gm_])
                if lvl < 2:
                    continue
                for h in range(4):
                    kb.op(kb.DVE, lambda: nc.vector.max(out=m8_[:, h, :], in_=gm_[:, h, :]), R=[gm_], Wr=[m8_])
                if lvl < 3:
                    continue
                for h in range(4):
                    kb.op(kb.DVE, lambda: nc.vector.tensor_scalar(out=sel_[:, h, :], in0=gm_[:, h, :], scalar1=m8_[:, h, 2:3], scalar2=None,
                                                                  op0=ALU.is_ge), R=[gm_, m8_], Wr=[sel_])
                kb.op(kb.DVE, lambda: nc.vector.tensor_tensor(out=sel_[:], in0=sel_[:], in1=cpb[:, qb, :, :], op=ALU.mult),
                      R=[sel_, cpb], Wr=[sel_])
                kb.op(kb.DVE, lambda: nc.vector.tensor_tensor(out=selb_[:], in0=sel_[:], in1=com[:, qb, :, :], op=ALU.add),
                      R=[sel_, com], Wr=[selb_])
                if lvl < 4:
                    continue
                for h in range(4):
                    hh = (h % 2) * 2 + h // 2
                    kb.op(kb.PE, lambda: nc.tensor.transpose(pt_[64 * (h % 2):64 * (h % 2) + 16, h // 2, :], selb_[:, hh, :], cx.ident_b[:]),
                          R=[selb_, cx.ident_b], Wr=[pt_])
                if lvl < 5:
                    continue
                for pb_ in (0, 64):
                    kb.op(kb.ACT, lambda: nc.scalar.activation(out=selbT[pb_:pb_ + 16, :, tt * 128:(tt + 1) * 128],
                                                               in_=pt_[pb_:pb_ + 16, :, :], func=AF.Copy),
                          R=[pt_], Wr=[selbT])
            kb.barrier()
        if (dbg or "").startswith("sel"):
            kb.dma(yT.ap[0:128, :], selbT[:, 0, :], R=[selbT], Wr=[cx.xbuf(yT, 0)])
            kb.dma(yT.ap[128:256, :], selbT[:, 1, :], R=[selbT], Wr=[cx.xbuf(yT, 0)])
            return
        def attn(es2):
            NB = 3
            mle_b = kb.sb("mb_mleb", [128, 4, 512], BF16, es2)
            kb.dma(mle_b[:], cx.dram["c_mask_le_b"].rearrange("o s t -> s o t"), Wr=[mle_b])
            pz = [kb.ps("mb_pz%d" % i, [128, 512], F32, es2) for i in range(2)]
            po = [kb.ps("mb_po%d" % i, [128, 512], F32, es2) for i in range(1 if defer_es is not None else 2)]
            pd = [kb.ps("mb_pd%d" % i, [64, 512], F32, es2) for i in range(2)] if defer_es is None else None
            pT = [kb.sb("mb_pT%d" % i, [128, 512], BF16, es2) for i in range(NB)]
            osb = [kb.sb("mb_osb%d" % i, [128, 512], F32, es2) for i in range(2)]
            rec = [kb.sb("mb_rec%d" % i, [64, 512], F32, es2) for i in range(2)]
            yst = [kb.sb("mb_y%d" % i, [64, 512], BF16, es2) for i in range(2)]
            units = []
            gi = 0
            for h in range(NH):
                for g in range(8):
                    for j in range(4 * g + 4):
                        units.append((h, g, j, gi))
                    gi += 1
            if dbg is not None and dbg.startswith("n"):
                units = units[:int(dbg[1:])]

            def stA(u, i):
                h, g, j, gi = u
                blk, po_ = h // 2, (h % 2) * 64
                pz_, p_ = pz[i % 2], pT[i % NB]
                kb.op(kb.PE, lambda: nc.tensor.matmul(pz_[:], lhsT=KT[po_:po_ + 64, blk, j * 128:(j + 1) * 128],
                                                      rhs=QT[po_:po_ + 64, blk, g * 512:(g + 1) * 512], start=True, stop=False),
                      R=[KT, QT], Wr=[pz_])
                kb.op(kb.PE, lambda: nc.tensor.matmul(pz_[:], lhsT=E_b[po_:po_ + 64, j // 2, :],
                                                      rhs=selbT[po_:po_ + 64, h // 2, g * 512:(g + 1) * 512],
                                                      start=False, stop=True), R=[E_b, selbT], Wr=[pz_])
                kb.op(kb.ACT, lambda: nc.scalar.activation(out=p_[:], in_=pz_[:], func=AF.Exp), R=[pz_], Wr=[p_])
                o = j - 4 * g
                if o >= 0:
                    kb.op(kb.POOL, lambda: nc.gpsimd.tensor_tensor(out=p_[:], in0=p_[:], in1=mle_b[:, o, :], op=ALU.mult),
                          R=[p_, mle_b], Wr=[p_])

            def stB(u, i):
                h, g, j, gi = u
                p_, po_t = pT[i % NB], po[gi % len(po)]
                pd_t = pd[gi % 2] if pd is not None else pz[i % 2]
                first, last = (j == 0), (j == 4 * g + 3)
                kb.op(kb.PE, lambda: nc.tensor.matmul(po_t[0:65, :], lhsT=V[:, j, h, :], rhs=p_[:], start=first, stop=last),
                      R=[V, p_], Wr=[po_t])
                if last:
                    r_, y_, o_ = rec[gi % 2], yst[gi % 2], osb[gi % 2]
                    kb.op(kb.ACT, lambda: nc.scalar.activation(out=o_[0:65, :], in_=po_t[0:65, :], func=AF.Copy), R=[po_t], Wr=[o_])
                    kb.op(kb.PE, lambda: nc.tensor.matmul(pd_t[0:64, :], lhsT=esel[0:65, :], rhs=o_[0:65, :], start=True, stop=True),
                          R=[esel, o_], Wr=[pd_t])
                    kb.op(kb.DVE, lambda: nc.vector.reciprocal(out=r_[:], in_=pd_t[0:64, :]), R=[pd_t], Wr=[r_])
                    kb.op(kb.DVE, lambda: nc.vector.tensor_tensor(out=y_[:], in0=o_[0:64, :], in1=r_[:], op=ALU.mult), R=[o_, r_], Wr=[y_])
                    kb.dma(yT.ap[h * 64:(h + 1) * 64, g * 512:(g + 1) * 512], y_[:], R=[y_], Wr=[cx.xbuf(yT, g)])

            return units, [stA, stB]

        if defer_es is not None:
            return attn
        with ExitStack() as es2:
            units, stages = attn(es2)
            pipeline(units, stages)
            kb.barrier()


def phase_hgrn(kb, cx, layer, hT, yT, dbg=None):
    nc = kb.nc
    w2d = cx.dram["w_in"][layer]
    with ExitStack() as es:
        qd = kb.sb("hg_qd", [128, 2, T], BF16, es)
        kt = kb.sb("hg_kt", [128, 2, T], BF16, es)
        ke = kb.sb("hg_ke", [128, 2, T], BF16, es)
        sg = kb.sb("hg_sg", [128, 2, T], BF16, es)
        iv = kb.sb("hg_iv", [128, 32, 256], BF16, es)
        dec = kb.sb("hg_dec", [128, 2, 128], F32, es)
        lbl = kb.sb("hg_lbl", [128, 2, 2], F32, es)
        lb = kb.sb("hg_lb", [128, 2], F32, es)
        oml = kb.sb("hg_oml", [128, 2], F32, es)
        noml = kb.sb("hg_noml", [128, 2], F32, es)
        gng = kb.sb("hg_gng", [128, 2], F32, es)
        hgm = kb.sb("hg_mask", [128, 128], F32, es)
        scm = kb.sb("hg_scm", [128, 512], F32, es)
        eps_c = kb.sb("hg_eps", [128, 1], F32, es)
        kb.op(kb.POOL, lambda: nc.gpsimd.memset(eps_c[:], NORM_EPS), Wr=[eps_c])
        kb.dma(hgm[:], cx.dram["c_hgmask"][:, :], Wr=[hgm])
        kb.dma(scm[:], cx.dram["c_scanmask"][:, :], Wr=[scm])
        kb.dma(lbl[:], cx.dram["hgrn_lb_logits"].rearrange("l (b p) -> p l b", p=128), Wr=[lbl])
        kb.dma(gng[:], cx.dram["hgrn_gn_g"][layer].rearrange("(b p) -> p b", p=128), Wr=[gng])
        if layer == 0:
            kb.op(kb.DVE, lambda: nc.vector.memset(lb[:], 0.0), Wr=[lb])
        else:
            kb.op(kb.DVE, lambda: nc.vector.tensor_tensor(out=lb[:], in0=lbl[:, 1, :], in1=lbl[:, 0, :], op=ALU.subtract),
                  R=[lbl], Wr=[lb])
            kb.op(kb.ACT, lambda: nc.scalar.activation(out=lb[:], in_=lb[:], func=AF.Sigmoid), R=[lb], Wr=[lb])
        kb.op(kb.DVE, lambda: nc.vector.tensor_scalar(out=oml[:], in0=lb[:], scalar1=-1.0, scalar2=1.0, op0=ALU.mult, op1=ALU.add),
              R=[lb], Wr=[oml])
        kb.op(kb.DVE, lambda: nc.vector.tensor_scalar(out=noml[:], in0=oml[:], scalar1=-1.0, scalar2=None, op0=ALU.mult),
              R=[oml], Wr=[noml])
        with ExitStack() as es2:
            ws = WStream(kb, es2, "hg_w", 8, 256, nbuf=2, alloc_wb=False)
            wq = kb.sb("hg_wq", [128, 8, 256], BF16, es2)
            wf = kb.sb("hg_wf", [128, 8, 256], BF16, es2)
            wg = kb.sb("hg_wg", [128, 8, 256], BF16, es2)
            wload(kb, ws, wq, wcols(w2d, HGRN_OFF, 256))
            wload(kb, ws, wf, wcols(w2d, HGRN_OFF + 256, 256))
            wload(kb, ws, wg, wcols(w2d, HGRN_OFF + 768, 256))
            pq = [kb.ps("hg_pq%d" % i, [128, 512], F32, es2) for i in range(2)]
            pf = [kb.ps("hg_pf%d" % i, [128, 512], F32, es2) for i in range(2)]
            pg = [kb.ps("hg_pg%d" % i, [128, 512], F32, es2) for i in range(2)]
            NW = 2
            sig = [kb.sb("hg_sig%d" % i, [128, 512], F32, es2) for i in range(NW)]
            fg = [kb.sb("hg_fg%d" % i, [128, 512], F32, es2) for i in range(NW)]
            key = [kb.sb("hg_key%d" % i, [128, 512], F32, es2) for i in range(NW)]
            bb = [kb.sb("hg_bb%d" % i, [128, 512], F32, es2) for i in range(NW)]
            eb = [kb.sb("hg_eb%d" % i, [128, 512], F32, es2) for i in range(NW)]
            enb = [kb.sb("hg_enb%d" % i, [128, 512], F32, es2) for i in range(NW)]
            dd = [kb.sb("hg_dd%d" % i, [128, 512], F32, es2) for i in range(NW)]
            n = 0
            for b2 in range(2):
                for tg in range(8):
                    i = n % 2
                    n += 1
                    sl = slice(tg * 512, (tg + 1) * 512)
                    fsl = slice(b2 * 128, (b2 + 1) * 128)
                    for (wt_, ps_) in ((wq, pq[i]), (wf, pf[i]), (wg, pg[i])):
                        for kc in range(8):
                            kb.op(kb.PE, lambda: nc.tensor.matmul(ps_[:], lhsT=wt_[:, kc, fsl], rhs=hT[:, kc, sl],
                                                                  start=(kc == 0), stop=(kc == 7)), R=[wt_, hT], Wr=[ps_])
                    sig_, fg_, key_, bb_, eb_, enb_, dd_ = sig[i], fg[i], key[i], bb[i], eb[i], enb[i], dd[i]
                    kb.op(kb.ACT, lambda: nc.scalar.activation(out=sig_[:], in_=pf[i][:], func=AF.Sigmoid), R=[pf[i]], Wr=[sig_])
                    kb.op(kb.ACT, lambda: nc.scalar.activation(out=sg[:, b2, sl], in_=pg[i][:], func=AF.Silu), R=[pg[i]], Wr=[sg])
                    kb.op(kb.DVE, lambda: nc.vector.tensor_scalar(out=fg_[:], in0=sig_[:], scalar1=oml[:, b2:b2 + 1], scalar2=lb[:, b2:b2 + 1],
                                                                  op0=ALU.mult, op1=ALU.add), R=[sig_, oml, lb], Wr=[fg_])
                    kb.op(kb.ACT, lambda: nc.scalar.activation(out=fg_[:], in_=fg_[:], func=AF.Ln), R=[fg_], Wr=[fg_])
                    kb.op(kb.DVE, lambda: nc.vector.tensor_scalar(out=key_[:], in0=sig_[:], scalar1=-1.0, scalar2=noml[:, b2:b2 + 1],
                                                                  op0=ALU.add, op1=ALU.mult), R=[sig_, noml], Wr=[key_])
                    kb.op(kb.DVE, lambda: nc.vector.tensor_tensor_scan(out=bb_[:], data0=scm[:], data1=fg_[:], initial=0.0,
                                                                       op0=ALU.mult, op1=ALU.add), R=[scm, fg_], Wr=[bb_])
                    kb.op(kb.ACT, lambda: nc.scalar.activation(out=eb_[:], in_=bb_[:], func=AF.Exp), R=[bb_], Wr=[eb_])
                    kb.op(kb.ACT, lambda: nc.scalar.activation(out=enb_[:], in_=bb_[:], func=AF.Exp, scale=-1.0), R=[bb_], Wr=[enb_])
                    b3 = bb_[:].rearrange("p (n c) -> p n c", c=32)
                    kb.op(kb.DVE, lambda: nc.vector.tensor_tensor(out=dd_[:].rearrange("p (n c) -> p n c", c=32),
                                                                  in0=b3[:, :, 31:32].to_broadcast([128, 16, 32]), in1=b3, op=ALU.subtract),
                          R=[bb_], Wr=[dd_])
                    kb.op(kb.ACT, lambda: nc.scalar.activation(out=dd_[:], in_=dd_[:], func=AF.Exp), R=[dd_], Wr=[dd_])
                    kb.op(kb.DVE, lambda: nc.vector.tensor_tensor(out=qd[:, b2, sl], in0=pq[i][:], in1=eb_[:], op=ALU.mult),
                          R=[pq[i], eb_], Wr=[qd])
                    kb.op(kb.POOL, lambda: nc.gpsimd.tensor_tensor(out=kt[:, b2, sl], in0=key_[:], in1=enb_[:], op=ALU.mult),
                          R=[key_, enb_], Wr=[kt])
                    kb.op(kb.POOL, lambda: nc.gpsimd.tensor_tensor(out=ke[:, b2, sl], in0=key_[:], in1=dd_[:], op=ALU.mult),
                          R=[key_, dd_], Wr=[ke])
                    kb.op(kb.POOL, lambda: nc.gpsimd.tensor_copy(out=dec[:, b2, tg * 16:(tg + 1) * 16],
                                                                 in_=eb_[:].rearrange("p (n c) -> p n c", c=32)[:, :, 31]),
                          R=[eb_], Wr=[dec])
            wi = wq
            wload(kb, ws, wi, wcols(w2d, HGRN_OFF + 512, 256))
            cnt = [0]
            proj_tm(kb, hT, wi, 256, pq, cnt,
                    lambda tt, ps_: kb.op(kb.ACT, lambda: nc.scalar.activation(out=iv[:, tt, :], in_=ps_[:, 0:256], func=AF.Copy),
                                          R=[ps_], Wr=[iv]))
            kb.barrier()
        if dbg == "hproj":
            for b2 in range(2):
                kb.dma(yT.ap[b2 * 128:(b2 + 1) * 128, :], ke[:, b2, :], R=[ke], Wr=[cx.xbuf(yT, 0)])
            return
        with ExitStack() as es2:
            ketok = kb.sb("hg_ketok", [128, 32, 256], BF16, es2)
            ketok3 = kb.sb("hg_ketok3", [128, 32, 256], BF16, es2)
            m3 = kb.sb("hg_m3", [128, 1], F32, es2)
            kb.op(kb.POOL, lambda: nc.gpsimd.memset(m3[:], 1.0), Wr=[m3])
            kb.op(kb.POOL, lambda: nc.gpsimd.memset(m3[64:96, :], 0.0), Wr=[m3])
            with ExitStack() as es3:
                ptr = [kb.ps("hg_ptr%d" % i, [128, 4, 128], BF16, es3) for i in range(2)]
                n = 0
                for b2 in range(2):
                    for t4 in range(8):
                        p_ = ptr[n % 2]
                        n += 1
                        for q in range(4):
                            tt = t4 * 4 + q
                            kb.op(kb.PE, lambda: nc.tensor.transpose(p_[:, q, :], ke[:, b2, tt * 128:(tt + 1) * 128], cx.ident_b[:]),
                                  R=[ke, cx.ident_b], Wr=[p_])
                        kb.op(kb.ACT, lambda: nc.scalar.activation(out=ketok[:, t4 * 4:(t4 + 1) * 4, b2 * 128:(b2 + 1) * 128], in_=p_[:], func=AF.Copy),
                              R=[p_], Wr=[ketok])
                        kb.op(kb.ACT, lambda: nc.scalar.activation(out=ketok3[64:128, t4 * 4:(t4 + 1) * 4, b2 * 128:(b2 + 1) * 128], in_=p_[64:128, :, :],
                                                                   func=AF.Copy, scale=m3[64:128, 0:1]),
                              R=[p_, m3], Wr=[ketok3])
                kb.barrier()
            psc = [kb.ps("hg_psc%d" % i, [128, 512], F32, es2) for i in range(2)]
            pkv = [kb.ps("hg_pkv%d" % i, [128, 512], F32, es2) for i in range(4)]
            pO = [kb.ps("hg_pO%d" % i, [128, 512], F32, es2) for i in range(2)]
            S = [kb.sb("hg_S%d" % i, [128, 128], F32, es2) for i in range(2)]
            Sb = [kb.sb("hg_Sb%d" % i, [128, 128], BF16, es2) for i in range(2)]
            scT = [[kb.sb("hg_scT%d%d" % (i, r), [128, 128], BF16, es2) for r in range(2)] for i in range(2)]
            osb = [kb.sb("hg_osb%d" % i, [128, 512], F32, es2) for i in range(2)]
            osq = [kb.sb("hg_osq%d" % i, [128, 512], F32, es2) for i in range(2)]
            rt = [kb.sb("hg_rt%d" % i, [128, 512], F32, es2) for i in range(2)]
            yst = [kb.sb("hg_y%d" % i, [128, 512], BF16, es2) for i in range(2)]
            for b2 in range(2):
                kb.op(kb.DVE, lambda: nc.vector.memset(S[b2][:], 0.0), Wr=[S[b2]])
                kb.op(kb.DVE, lambda: nc.vector.memset(Sb[b2][:], 0.0), Wr=[Sb[b2]])
            ntt = 32 if not (dbg or "").startswith("ht") else int(dbg[2:])
            for tt in range(ntt):
                tg, q4 = tt // 4, tt % 4
                for b2 in range(2):
                    O_ = pO[b2]
                    csl = slice(q4 * 128, (q4 + 1) * 128)
                    tsl = slice(tt * 128, (tt + 1) * 128)
                    for r in range(2):
                        kb.op(kb.PE, lambda: nc.tensor.matmul(psc[r][:, 0:128], lhsT=kt[r * 64:(r + 1) * 64, b2, tsl],
                                                              rhs=qd[r * 64:(r + 1) * 64, b2, tsl], start=True, stop=True),
                              R=[kt, qd], Wr=[psc[r]])
                        sc_ = scT[b2][r]
                        kb.op(kb.DVE, lambda: nc.vector.tensor_tensor(out=sc_[:], in0=psc[r][:, 0:128], in1=hgm[:], op=ALU.mult),
                              R=[psc[r], hgm], Wr=[sc_])
                    for r in range(2):
                        sc_ = scT[b2][r]
                        h = b2 * 2 + r
                        kb.op(kb.PE, lambda: nc.tensor.matmul(O_[r * 64:(r + 1) * 64, csl], lhsT=iv[:, tt, h * 64:(h + 1) * 64], rhs=sc_[:],
                                                              start=True, stop=False, skip_group_check=True),
                              R=[iv, sc_], Wr=[O_])
                    for c4 in range(4):
                        nchunk = tt * 4 + c4
                        c0 = tt * 128 + c4 * 32
                        kb.op(kb.PE, lambda: nc.tensor.matmul(O_[:, q4 * 128 + c4 * 32:q4 * 128 + (c4 + 1) * 32], lhsT=Sb[b2][:],
                                                              rhs=qd[:, b2, c0:c0 + 32], start=False, stop=(c4 == 3), skip_group_check=True),
                              R=[Sb[b2], qd], Wr=[O_])
                        kv_ = pkv[c4]
                        if c4 < 3:
                            kb.op(kb.PE, lambda: nc.tensor.matmul(kv_[:, 0:128], lhsT=ketok[c4 * 32:(c4 + 1) * 32, tt, b2 * 128:(b2 + 1) * 128],
                                                                  rhs=iv[c4 * 32:(c4 + 1) * 32, tt, b2 * 128:(b2 + 1) * 128], start=True, stop=True),
                                  R=[ketok, iv], Wr=[kv_])
                        else:
                            kb.op(kb.PE, lambda: nc.tensor.matmul(kv_[:, 0:128], lhsT=ketok3[64:128, tt, b2 * 128:(b2 + 1) * 128],
                                                                  rhs=iv[64:128, tt, b2 * 128:(b2 + 1) * 128], start=True, stop=True),
                                  R=[ketok3, iv], Wr=[kv_])
                        kb.op(kb.DVE, lambda: nc.vector.scalar_tensor_tensor(out=S[b2][:], in0=S[b2][:], scalar=dec[:, b2, nchunk:nchunk + 1],
                                                                             in1=kv_[:, 0:128], op0=ALU.mult, op1=ALU.add),
                              R=[S[b2], dec, kv_], Wr=[S[b2]])
                        kb.op(kb.POOL, lambda: nc.gpsimd.tensor_tensor(out=Sb[b2][:], in0=S[b2][:], in1=cx.blk_f[:], op=ALU.mult),
                              R=[S[b2], cx.blk_f], Wr=[Sb[b2]])
                    if q4 == 3:
                        sl = slice(tg * 512, (tg + 1) * 512)
                        i = b2
                        kb.op(kb.ACT, lambda: nc.scalar.activation(out=osb[i][:], in_=O_[:], func=AF.Copy), R=[O_], Wr=[osb[i]])
                        kb.op(kb.ACT, lambda: nc.scalar.activation(out=osq[i][:], in_=O_[:], func=AF.Square), R=[O_], Wr=[osq[i]])
                        kb.op(kb.PE, lambda: nc.tensor.matmul(psc[0][:], lhsT=cx.blk_f[:], rhs=osq[i][:], start=True, stop=True),
                              R=[cx.blk_f, osq[i]], Wr=[psc[0]])
                        kb.op(kb.ACT, lambda: nc.scalar.activation(out=rt[i][:], in_=psc[0][:], func=AF.Sqrt, scale=1.0 / 64, bias=eps_c[:, 0:1]),
                              R=[psc[0], eps_c], Wr=[rt[i]])
                        kb.op(kb.DVE, lambda: nc.vector.reciprocal(out=rt[i][:], in_=rt[i][:]), R=[rt[i]], Wr=[rt[i]])
                        kb.op(kb.DVE, lambda: nc.vector.tensor_tensor(out=osb[i][:], in0=osb[i][:], in1=rt[i][:], op=ALU.mult),
                              R=[osb[i], rt[i]], Wr=[osb[i]])
                        kb.op(kb.DVE, lambda: nc.vector.scalar_tensor_tensor(out=yst[i][:], in0=osb[i][:], scalar=gng[:, b2:b2 + 1], in1=sg[:, b2, sl],
                                                                             op0=ALU.mult, op1=ALU.mult), R=[osb[i], gng, sg], Wr=[yst[i]])
                        kb.dma(yT.ap[b2 * 128:(b2 + 1) * 128, sl], yst[i][:], R=[yst[i]], Wr=[cx.xbuf(yT, tg)])
            kb.barrier()


C0 = 0.6065306597126334


class OpsH:
    def __init__(self, kb):
        self.kb = kb
        self.nc = kb.nc

    def act(self, out, in_, func, R, Wr, scale=1.0, bias=None):
        kb, nc = self.kb, self.nc
        if bias is None:
            return kb.op(kb.ACT, lambda: nc.scalar.activation(out=out, in_=in_, func=func, scale=scale), R=R, Wr=Wr)
        return kb.op(kb.ACT, lambda: nc.scalar.activation(out=out, in_=in_, func=func, scale=scale, bias=bias), R=R, Wr=Wr)

    def tt(self, E, out, a, b, op, R, Wr):
        kb = self.kb
        return kb.op(E, lambda: E.eng.tensor_tensor(out=out, in0=a, in1=b, op=op), R=R, Wr=Wr)

    def ts(self, E, out, a, s1, s2, op0, op1, R, Wr):
        kb = self.kb
        if s2 is None:
            return kb.op(E, lambda: E.eng.tensor_scalar(out=out, in0=a, scalar1=s1, scalar2=None, op0=op0), R=R, Wr=Wr)
        return kb.op(E, lambda: E.eng.tensor_scalar(out=out, in0=a, scalar1=s1, scalar2=s2, op0=op0, op1=op1), R=R, Wr=Wr)

    def stt(self, out, a, sc, b, op0, op1, R, Wr):
        kb, nc = self.kb, self.nc
        return kb.op(kb.DVE, lambda: nc.vector.scalar_tensor_tensor(out=out, in0=a, scalar=sc, in1=b, op0=op0, op1=op1), R=R, Wr=Wr)

    def mm(self, out, lhsT, rhs, R, Wr, start=True, stop=True, skip=False):
        kb, nc = self.kb, self.nc
        return kb.op(kb.PE, lambda: nc.tensor.matmul(out, lhsT=lhsT, rhs=rhs, start=start, stop=stop, skip_group_check=skip), R=R, Wr=Wr)


def phase_rwkv(kb, cx, layer, hT, yT, dbg=None):
    nc = kb.nc
    o = OpsH(kb)
    w2d = cx.dram["w_in"][layer]
    MUL, ADD, SUB = ALU.mult, ALU.add, ALU.subtract
    DVE, POOL = kb.DVE, kb.POOL
    with ExitStack() as es:
        def col2(name, src, es_=es):
            t_ = kb.sb(name, [128, 2], F32, es_)
            kb.dma(t_[:], src.rearrange("(b p) -> p b", p=128), Wr=[t_])
            return t_
        mu = kb.sb("rw_mu", [128, 8], F32, es)
        kb.dma(mu[:], cx.dram["rwkv_mu"][layer].rearrange("(b p) -> p b", p=128), Wr=[mu])
        w0 = col2("rw_w0", cx.dram["rwkv_w0"][layer])
        a0 = col2("rw_a0", cx.dram["rwkv_a0"][layer])
        k_k = col2("rw_kk", cx.dram["rwkv_k_k"][layer])
        k_a = col2("rw_ka", cx.dram["rwkv_k_a"][layer])
        gnw = col2("rw_gnw", cx.dram["rwkv_gn_w"][layer])
        gnb = col2("rw_gnb", cx.dram["rwkv_gn_b"][layer])
        r_k = col2("rw_rk", cx.dram["rwkv_r_k"][layer].rearrange("h d -> (h d)"))
        gneps = kb.sb("rw_gneps", [128, 1], F32, es)
        kb.op(POOL, lambda: nc.gpsimd.memset(gneps[:], GN_EPS), Wr=[gneps])
        if layer > 0:
            v0 = col2("rw_v0", cx.dram["rwkv_v0"][layer - 1])
        stg = kb.sb("rw_stg", [128, 256], F32, es)

        def small_w(name, src_ap, rows, r0=0, ncols=256):
            t_ = kb.sb(name, [128, ncols], BF16, es)
            kb.dma(stg[r0:r0 + rows, 0:ncols], src_ap, Wr=[stg])
            kb.op(DVE, lambda: nc.vector.tensor_copy(out=t_[r0:r0 + rows, :], in_=stg[r0:r0 + rows, 0:ncols]), R=[stg], Wr=[t_])
            return t_
        w2b = small_w("rw_w2b", cx.dram["rwkv_w2"][layer], 64, 0)
        a2b = small_w("rw_a2b", cx.dram["rwkv_a2"][layer], 64, 64)
        g2b = small_w("rw_g2b", cx.dram["rwkv_g2"][layer], 128, 0)
        if layer > 0:
            v2b = small_w("rw_v2b", cx.dram["rwkv_v2"][layer - 1], 32, 0)
            v1b = kb.sb("rw_v1b", [128, 2, 32], BF16, es)
            for b2 in range(2):
                kb.dma(stg[:, 0:32], cx.dram["rwkv_v1"][layer - 1][b2 * 128:(b2 + 1) * 128, :], Wr=[stg])
                kb.op(DVE, lambda: nc.vector.tensor_copy(out=v1b[:, b2, :], in_=stg[:, 0:32]), R=[stg], Wr=[v1b])
        scm = kb.sb("rw_scm", [128, 512], F32, es)
        kb.dma(scm[:], cx.dram["c_scanmask128"][:, :], Wr=[scm])
        LT = kb.sb("rw_LT", [128, T], BF16, es)
        SG = kb.sb("rw_SG", [128, T], BF16, es)
        VL = kb.sb("rw_VL", [32, T], BF16, es) if layer > 0 else None

        class Shifter:
            def __init__(self, name, es_, d=None):
                self.pb = [kb.sb("%s_pb%d" % (name, i), [128, 513], F32, es_) for i in range(2)]
                self.d = d if d is not None else kb.sb("%s_d" % name, [128, 512], F32, es_)
                self.n = 0

            def run(self, ps_, mucol, out, out_tl=None):
                cur = self.pb[self.n % 2]
                prv = self.pb[(self.n + 1) % 2]
                first = (self.n % 8 == 0)
                self.n += 1
                o.act(cur[:, 1:513], ps_[:], AF.Copy, [ps_], [cur])
                if first:
                    kb.op(DVE, lambda: nc.vector.memset(cur[:, 0:1], 0.0), Wr=[cur])
                else:
                    kb.op(DVE, lambda: nc.vector.tensor_copy(out=cur[:, 0:1], in_=prv[:, 512:513]), R=[prv], Wr=[cur])
                o.tt(DVE, self.d[:], cur[:, 0:512], cur[:, 1:513], SUB, [cur], [self.d])
                o.stt(out, self.d[:], mucol, cur[:, 1:513], MUL, ADD, [self.d, cur, mu], [out_tl])

        with ExitStack() as es2:
            ws = WStream(kb, es2, "rw0_w", 8, 128, nbuf=2, alloc_wb=False)
            wl_ = kb.sb("rw0_wl", [128, 8, 128], BF16, es2)
            wg_ = kb.sb("rw0_wg", [128, 8, 128], BF16, es2)
            wload(kb, ws, wl_, wcols(w2d, RWKV_OFF + 768, 128))
            wload(kb, ws, wg_, wcols(w2d, RWKV_OFF + 896, 128))
            nv = 2 if layer > 0 else 0
            wv_ = [kb.sb("rw0_wv%d" % i, [128, 8, 128], BF16, es2) for i in range(nv)]
            for i in range(nv):
                wload(kb, ws, wv_[i], wcols(w2d, RWKV_OFF + 512 + i * 128, 128))
            pp = [kb.ps("rw0_pp%d" % i, [128, 512], F32, es2) for i in range(4)]
            pvl = kb.ps("rw0_pvl", [128, 512], F32, es2)
            shl = Shifter("rw0_sl", es2)
            shg = Shifter("rw0_sg", es2)
            shv = [Shifter("rw0_sv%d" % i, es2) for i in range(nv)]
            tl = kb.sb("rw0_tl", [128, 512], F32, es2)
            tg_ = kb.sb("rw0_tg", [128, 512], F32, es2)
            tv = [kb.sb("rw0_tv%d" % i, [128, 512], F32, es2) for i in range(nv)]
            tvb = [kb.sb("rw0_tvb%d" % i, [128, 512], BF16, es2) for i in range(nv)]
            for tg in range(8):
                sl = slice(tg * 512, (tg + 1) * 512)
                for (wt_, ps_) in [(wl_, pp[0]), (wg_, pp[1])] + [(wv_[i], pp[2 + i]) for i in range(nv)]:
                    for kc in range(8):
                        o.mm(ps_[:], wt_[:, kc, :], hT[:, kc, sl], [wt_, hT], [ps_], start=(kc == 0), stop=(kc == 7))
                shl.run(pp[0], mu[:, 6:7], tl[:], tl)
                o.act(LT[0:64, sl], tl[0:64, :], AF.Tanh, [tl], [LT])
                o.act(LT[64:128, sl], tl[64:128, :], AF.Copy, [tl], [LT])
                shg.run(pp[1], mu[:, 7:8], tg_[:], tg_)
                o.act(SG[:, sl], tg_[:], AF.Sigmoid, [tg_], [SG])
                for i in range(nv):
                    shv[i].run(pp[2 + i], mu[:, 4 + i:5 + i], tv[i][:], tv[i])
                    kb.op(DVE, lambda: nc.vector.tensor_copy(out=tvb[i][:], in_=tv[i][:]), R=[tv[i]], Wr=[tvb[i]])
                if nv:
                    for i in range(2):
                        o.mm(pvl[0:32, :], v1b[:, i, :], tvb[i][:], [v1b, tvb[i]], [pvl], start=(i == 0), stop=(i == 1))
                    o.act(VL[0:32, sl], pvl[0:32, :], AF.Copy, [pvl], [VL])
            kb.barrier()
        if dbg == "r0":
            kb.dma(yT.ap[0:128, :], LT[:, :], R=[LT], Wr=[cx.xbuf(yT, 0)])
            kb.dma(yT.ap[128:256, :], SG[:, :], R=[SG], Wr=[cx.xbuf(yT, 0)])
            return

        for blk in range(2):
            with ExitStack() as esb:
                at = kb.sb("rw_at", [128, T], BF16, esb)
                bt = kb.sb("rw_bt", [128, T], BF16, esb)
                ktl = kb.sb("rw_ktl", [128, T], BF16, esb)
                rt = kb.sb("rw_rt", [128, T], BF16, esb)
                bh = kb.sb("rw_bh", [128, T], BF16, esb)
                kh = kb.sb("rw_kh", [128, T], BF16, esb)
                vT = kb.sb("rw_vT", [128, T], BF16, esb)
                gend = kb.sb("rw_gend", [128, 32], F32, esb)
                with ExitStack() as es2:
                    ws = WStream(kb, es2, "rwp_w", 8, 128, nbuf=2, alloc_wb=False)
                    wr_ = kb.sb("rwp_wr", [128, 8, 128], BF16, es2)
                    wk_ = kb.sb("rwp_wk", [128, 8, 128], BF16, es2)
                    wv_ = kb.sb("rwp_wv", [128, 8, 128], BF16, es2)
                    wload(kb, ws, wr_, wcols(w2d, RWKV_OFF + blk * 128, 128))
                    wload(kb, ws, wk_, wcols(w2d, RWKV_OFF + 256 + blk * 128, 128))
                    wload(kb, ws, wv_, wcols(w2d, RWKV_OFF + 512 + blk * 128, 128))
                    pr = kb.ps("rwp_pr", [128, 512], F32, es2)
                    pk = kb.ps("rwp_pk", [128, 512], F32, es2)
                    pv = kb.ps("rwp_pv", [128, 512], F32, es2)
                    pl = kb.ps("rwp_pl", [128, 512], F32, es2)
                    pa = kb.ps("rwp_pa", [128, 512], F32, es2)
                    pg2 = kb.ps("rwp_pg2", [128, 512], F32, es2)
                    pss = kb.ps("rwp_pss", [128, 512], F32, es2)
                    pvm = kb.ps("rwp_pvm", [128, 512], F32, es2)
                    shr = Shifter("rwp_sr", es2)
                    shk, shv = Shifter("rwp_sk", es2, shr.d), Shifter("rwp_sv", es2, shr.d)
                    names = ["rs", "ks", "vs", "sgw", "av", "cum", "e1", "e2", "e3", "e4", "kk", "t1", "t2"]
                    wt = {n_: kb.sb("rwp_" + n_, [128, 512], F32, es2) for n_ in names}
                    wt["kkn"] = wt["sgw"]
                    wt["kmod"] = wt["cum"]
                    gst = kb.sb("rwp_gst", [128, 512], BF16, es2)
                    bc = slice(blk * 128, (blk + 1) * 128)
                    b1 = slice(blk, blk + 1)
                    for tg in range(8):
                        sl = slice(tg * 512, (tg + 1) * 512)
                        for (wt_, ps_) in ((wr_, pr), (wk_, pk), (wv_, pv)):
                            for kc in range(8):
                                o.mm(ps_[:], wt_[:, kc, :], hT[:, kc, sl], [wt_, hT], [ps_], start=(kc == 0), stop=(kc == 7))
                        rs, ks, vs = wt["rs"], wt["ks"], wt["vs"]
                        for (sh_, ps_, mc, dst) in ((shr, pr, blk, rs), (shk, pk, 2 + blk, ks), (shv, pv, 4 + blk, vs)):
                            sh_.run(ps_, mu[:, mc:mc + 1], dst[:], dst)
                        t1, t2 = wt["t1"], wt["t2"]
                        if layer == 0:
                            kb.dma(cx.vfirst.ap[bc, sl], vs[:], R=[vs], Wr=[cx.vfirst.buf((blk, tg))])
                        else:
                            o.mm(pvm[:], v2b[0:32, bc], VL[0:32, sl], [v2b, VL], [pvm])
                            o.act(t1[:], pvm[:], AF.Sigmoid, [pvm, v0], [t1], bias=v0[:, b1])
                            kb.dma(t2[:], cx.vfirst.ap[bc, sl], R=[cx.vfirst.buf((blk, tg))], Wr=[t2])
                            o.tt(DVE, t2[:], t2[:], vs[:], SUB, [t2, vs], [t2])
                            o.tt(DVE, t2[:], t2[:], t1[:], MUL, [t2, t1], [t2])
                            o.tt(DVE, vs[:], vs[:], t2[:], ADD, [vs, t2], [vs])
                        sgw, av, cum = wt["sgw"], wt["av"], wt["cum"]
                        o.mm(pl[:], w2b[0:64, bc], LT[0:64, sl], [w2b, LT], [pl])
                        o.act(sgw[:], pl[:], AF.Sigmoid, [pl, w0], [sgw], bias=w0[:, b1])
                        o.mm(pa[:], a2b[64:128, bc], LT[64:128, sl], [a2b, LT], [pa])
                        o.act(av[:], pa[:], AF.Sigmoid, [pa, a0], [av], bias=a0[:, b1])
                        o.mm(pg2[:], g2b[:, bc], SG[:, sl], [g2b, SG], [pg2])
                        o.act(gst[:], pg2[:], AF.Copy, [pg2], [gst])
                        kb.dma(cx.gscr.ap[bc, sl], gst[:], R=[gst], Wr=[cx.gscr.buf((blk, tg))])
                        kb.op(DVE, lambda: nc.vector.tensor_tensor_scan(out=cum[:], data0=scm[:], data1=sgw[:], initial=0.0, op0=MUL, op1=ADD),
                              R=[scm, sgw], Wr=[cum])
                        e1, e2, e3, e4 = wt["e1"], wt["e2"], wt["e3"], wt["e4"]
                        o.act(e1[:], cum[:], AF.Exp, [cum], [e1], scale=-C0)
                        o.act(e2[:], cum[:], AF.Exp, [cum], [e2], scale=C0)
                        c3 = cum[:].rearrange("p (n c) -> p n c", c=128)
                        o.tt(DVE, e3[:].rearrange("p (n c) -> p n c", c=128), c3[:, :, 127:128].to_broadcast([128, 4, 128]), c3, SUB, [cum], [e3])
                        o.act(e3[:], e3[:], AF.Exp, [e3], [e3], scale=-C0)
                        o.tt(DVE, e4[:], cum[:], sgw[:], SUB, [cum, sgw], [e4])
                        o.act(e4[:], e4[:], AF.Exp, [e4], [e4], scale=-C0)
                        kb.op(POOL, lambda: nc.gpsimd.tensor_copy(out=gend[:, tg * 4:(tg + 1) * 4], in_=e1[:].rearrange("p (n c) -> p n c", c=128)[:, :, 127]),
                              R=[e1], Wr=[gend])
                        kk, kkn = wt["kk"], wt["kkn"]
                        o.ts(DVE, kk[:], ks[:], k_k[:, b1], None, MUL, None, [ks, k_k], [kk])
                        o.act(t1[:], kk[:], AF.Square, [kk], [t1])
                        o.mm(pss[:], cx.blk_f[:], t1[:], [cx.blk_f, t1], [pss])
                        o.act(t1[:], pss[:], AF.Sqrt, [pss], [t1])
                        o.ts(DVE, t1[:], t1[:], 1e-12, None, ALU.max, None, [t1], [t1])
                        kb.op(DVE, lambda: nc.vector.reciprocal(out=t1[:], in_=t1[:]), R=[t1], Wr=[t1])
                        o.tt(DVE, kkn[:], kk[:], t1[:], MUL, [kk, t1], [kkn])
                        kmod, ka = wt["kmod"], wt["kk"]
                        o.ts(DVE, t2[:], av[:], -1.0, k_a[:, b1], ADD, MUL, [av, k_a], [t2])
                        o.stt(kmod[:], t2[:], 1.0, ks[:], ADD, MUL, [t2, ks], [kmod])
                        o.stt(t2[:], rs[:], r_k[:, b1], kmod[:], MUL, MUL, [rs, r_k, kmod], [t2])
                        o.mm(pss[:], cx.blk_f[:], t2[:], [cx.blk_f, t2], [pss])
                        o.tt(DVE, t1[:], pss[:], vs[:], MUL, [pss, vs], [t1])
                        kb.dma(cx.bscr.ap[bc, sl], t1[:], R=[t1], Wr=[cx.bscr.buf((blk, tg))])
                        o.tt(POOL, ka[:], kkn[:], av[:], MUL, [kkn, av], [ka])
                        o.stt(at[:, sl], kkn[:], -1.0, e4[:], MUL, MUL, [kkn, e4], [at])
                        o.tt(POOL, bt[:, sl], ka[:], e2[:], MUL, [ka, e2], [bt])
                        o.tt(POOL, ktl[:, sl], kmod[:], e2[:], MUL, [kmod, e2], [ktl])
                        o.tt(POOL, rt[:, sl], rs[:], e1[:], MUL, [rs, e1], [rt])
                        o.tt(POOL, bh[:, sl], ka[:], e3[:], MUL, [ka, e3], [bh])
                        o.tt(POOL, kh[:, sl], kmod[:], e3[:], MUL, [kmod, e3], [kh])
                        o.act(vT[:, sl], vs[:], AF.Copy, [vs], [vT])
                    kb.barrier()
                if dbg == "prep":
                    kb.dma(yT.ap[0:128, :], at[:, :], R=[at], Wr=[cx.xbuf(yT, 0)])
                    kb.dma(yT.ap[128:256, :], kh[:, :], R=[kh], Wr=[cx.xbuf(yT, 0)])
                    return
                with ExitStack() as es2:
                    rw_recur(kb, cx, o, es2, layer, blk, at, bt, ktl, rt, bh, kh, vT, gend, gnw, gnb, gneps, yT, dbg)
                    kb.barrier()


def rw_recur(kb, cx, o, es, layer, blk, at, bt, ktl, rt, bh, kh, vT, gend, gnw, gnb, gneps, yT, dbg):
    nc = kb.nc
    MUL, ADD, SUB = ALU.mult, ALU.add, ALU.subtract
    DVE, POOL = kb.DVE, kb.POOL
    bc = slice(blk * 128, (blk + 1) * 128)
    b1 = slice(blk, blk + 1)

    def cst(name, key):
        t_ = kb.sb(name, [128, 128], F32, es)
        kb.dma(t_[:], cx.dram[key][:, :], Wr=[t_])
        return t_
    mlow, mupS, mupI = cst("rr_mlow", "c_rw_low"), cst("rr_mupS", "c_rw_upS"), cst("rr_mupI", "c_rw_upI")
    A = [kb.ps("rr_A%d" % r, [128, 512], F32, es) for r in range(2)]
    B = [kb.ps("rr_B%d" % r, [128, 512], F32, es) for r in range(2)]
    N = [kb.ps("rr_N%d" % r, [128, 512], F32, es) for r in range(2)]
    YS = kb.ps("rr_YS", [128, 512], F32, es)
    TR = kb.ps("rr_TR", [128, 4, 128], BF16, es)
    NT = 3
    tok = [kb.sb("rr_tok%d" % i, [128, 4, 128], BF16, es) for i in range(NT)]
    AakT = [[kb.sb("rr_aak%d%d" % (i, r), [128, 128], BF16, es) for r in range(2)] for i in range(NT)]
    ArbT = [[kb.sb("rr_arb%d%d" % (i, r), [128, 128], BF16, es) for r in range(2)] for i in range(NT)]
    ArkT = [[kb.sb("rr_ark%d%d" % (i, r), [128, 128], BF16, es) for r in range(2)] for i in range(NT)]
    P0 = [[kb.sb("rr_P0%d%d" % (i, r), [128, 128], BF16, es) for r in range(2)] for i in range(2)]
    PT0 = [[kb.sb("rr_PT0%d%d" % (i, r), [128, 128], BF16, es) for r in range(2)] for i in range(2)]
    Gb0 = [[kb.sb("rr_Gb0%d%d" % (i, r), [128, 128], BF16, es) for r in range(2)] for i in range(2)]
    X0sb = [[kb.sb("rr_X0%d%d" % (i, r), [128, 64], BF16, es) for r in range(2)] for i in range(2)]
    Pa = [[kb.sb("rr_P%d%d" % (r, i), [128, 128], BF16, es) for i in range(2)] for r in range(2)]
    PTa = [[kb.sb("rr_PT%d%d" % (r, i), [128, 128], BF16, es) for i in range(2)] for r in range(2)]
    Gv = [[kb.sb("rr_G%d%d" % (r, i), [128, 128], BF16, es) for i in range(2)] for r in range(2)]
    Zb = [kb.sb("rr_Z%d" % i, [128, 128], F32, es) for i in range(2)]
    WmT = [kb.sb("rr_WmT%d" % i, [128, 128], BF16, es) for i in range(2)]
    M = kb.sb("rr_M", [128, 128], F32, es)
    Mb = kb.sb("rr_Mb", [128, 128], BF16, es)
    Ub = kb.sb("rr_Ub", [128, 128], BF16, es)
    ysb = kb.sb("rr_ysb", [128, 256], F32, es)
    yc = kb.sb("rr_yc", [128, 256], F32, es)
    sq = kb.sb("rr_sq", [128, 256], F32, es)
    bon = kb.sb("rr_bon", [128, 256], F32, es)
    gin = kb.sb("rr_gin", [128, 256], BF16, es)
    yo = kb.sb("rr_yo", [128, 256], BF16, es)
    kb.op(DVE, lambda: nc.vector.memset(M[:], 0.0), Wr=[M])
    kb.op(DVE, lambda: nc.vector.memset(Mb[:], 0.0), Wr=[Mb])
    ntile = 32 if not (dbg or "").startswith("rt") else int(dbg[2:])

    def s1a(tt):
        t3 = tt % NT
        cs = slice(tt * 128, (tt + 1) * 128)
        for i, src in enumerate((vT, at, bh, kh)):
            kb.op(kb.PE, lambda: nc.tensor.transpose(TR[:, i, :], src[:, cs], cx.ident_b[:]), R=[src, cx.ident_b], Wr=[TR])
        o.act(tok[t3][:], TR[:], AF.Copy, [TR], [tok[t3]])

    def s1m(tt):
        t3, tp = tt % NT, tt % 2
        cs = slice(tt * 128, (tt + 1) * 128)
        for r in range(2):
            ps_ = slice(r * 64, (r + 1) * 64)
            jobs = ((A[r][:, 0:128], A[r], at, bt, P0[tp][r], mlow),
                    (A[r][:, 128:256], A[r], bt, at, PT0[tp][r], mupS),
                    (A[r][:, 256:384], A[r], ktl, at, AakT[t3][r], mupS),
                    (A[r][:, 384:512], A[r], bt, rt, ArbT[t3][r], mupI),
                    (B[r][:, 0:128], B[r], ktl, rt, ArkT[t3][r], mupI))
            for (out_, ob_, l_, r_, dst, msk) in jobs:
                o.mm(out_, l_[ps_, cs], r_[ps_, cs], [l_, r_], [ob_])
            for (out_, ob_, l_, r_, dst, msk) in jobs:
                o.tt(DVE, dst[:], out_, msk[:], MUL, [ob_, msk], [dst])
            o.tt(POOL, Gb0[tp][r][:], PT0[tp][r][:], cx.ident_f[:], ADD, [PT0[tp][r], cx.ident_f], [Gb0[tp][r]])

    def s1x(tt):
        t3, tp = tt % NT, tt % 2
        for r in range(2):
            o.mm(B[r][:, 128:192], AakT[t3][r][:], tok[t3][:, 0, r * 64:(r + 1) * 64], [AakT[t3][r], tok[t3]], [B[r]])
            o.act(X0sb[tp][r][:], B[r][:, 128:192], AF.Copy, [B[r]], [X0sb[tp][r]])

    def evac(r, dst, src_ap, bank):
        if r == 0:
            o.act(dst[:], src_ap, AF.Copy, [bank], [dst])
        else:
            kb.op(DVE, lambda: nc.vector.tensor_copy(out=dst[:], in_=src_ap), R=[bank], Wr=[dst])

    def s2(tt, j):
        tp = tt % 2
        for r in range(2):
            Pc = P0[tp][r] if j == 0 else Pa[r][j % 2]
            PTc = PT0[tp][r] if j == 0 else PTa[r][j % 2]
            if j <= 5:
                o.mm(N[r][:, 0:128], PTc[:], Pc[:], [PTc, Pc], [N[r]])
            if j < 5:
                o.mm(N[r][:, 128:256], Pc[:], PTc[:], [PTc, Pc], [N[r]])
            if j >= 1:
                Gc = Gb0[tp][r] if j == 1 else Gv[r][(j - 1) % 2]
                o.mm(N[r][:, 256:384], cx.ident_b[:], Gc[:], [cx.ident_b, Gc], [N[r]], start=True, stop=False)
                o.mm(N[r][:, 256:384], Pc[:], Gc[:], [Pc, Gc], [N[r]], start=False, stop=True)
        for r in range(2):
            if j <= 5:
                evac(r, Pa[r][(j + 1) % 2], N[r][:, 0:128], N[r])
            if j < 5:
                evac(r, PTa[r][(j + 1) % 2], N[r][:, 128:256], N[r])
            if j >= 1:
                evac(r, Gv[r][j % 2], N[r][:, 256:384], N[r])

    def s3(tt):
        t3, tp = tt % NT, tt % 2
        for r in range(2):
            ps_ = slice(r * 64, (r + 1) * 64)
            Gf = Gv[r][0]
            o.mm(B[r][ps_, 256:384], tok[t3][:, 1, r * 64:(r + 1) * 64], Gf[:], [tok[t3], Gf], [B[r]])
            o.mm(B[r][:, 192:256], Gf[:], X0sb[tp][r][:], [Gf, X0sb[tp][r]], [B[r]])
            o.act(WmT[tp][ps_, :], B[r][ps_, 256:384], AF.Copy, [B[r]], [WmT[tp]])
            o.act(Zb[tp][:, r * 64:(r + 1) * 64], B[r][:, 192:256], AF.Copy, [B[r]], [Zb[tp]])

    def seqU(tt):
        tp = tt % 2
        o.mm(YS[:, 256:384], WmT[tp][:], Mb[:], [WmT[tp], Mb], [YS])
        o.tt(DVE, Ub[:], YS[:, 256:384], Zb[tp][:], ADD, [YS, Zb[tp]], [Ub])

    def seqY(tt):
        t3 = tt % NT
        cs = slice(tt * 128, (tt + 1) * 128)
        yc_ = slice((tt % 2) * 128, (tt % 2) * 128 + 128)
        o.mm(YS[:, yc_], Mb[:], rt[:, cs], [Mb, rt], [YS], start=True, stop=False, skip=True)
        for r in range(2):
            ps_ = slice(r * 64, (r + 1) * 64)
            o.mm(YS[ps_, yc_], Ub[:, r * 64:(r + 1) * 64], ArbT[t3][r][:], [Ub, ArbT[t3][r]], [YS], start=False, stop=False, skip=True)
            o.mm(YS[ps_, yc_], tok[t3][:, 0, r * 64:(r + 1) * 64], ArkT[t3][r][:], [tok[t3], ArkT[t3][r]], [YS], start=False, stop=(r == 1), skip=True)

    def seqM(tt):
        t3 = tt % NT
        o.mm(YS[:, 384:512], tok[t3][:, 2, :], Ub[:], [tok[t3], Ub], [YS], start=True, stop=False)
        o.mm(YS[:, 384:512], tok[t3][:, 3, :], tok[t3][:, 0, :], [tok[t3]], [YS], start=False, stop=True)
        o.stt(M[:], M[:], gend[:, tt:tt + 1], YS[:, 384:512], MUL, ADD, [M, gend, YS], [M])
        o.tt(POOL, Mb[:], M[:], cx.blk_f[:], MUL, [M, cx.blk_f], [Mb])

    def outp(tt):
        t2 = tt // 2
        sl = slice(t2 * 256, (t2 + 1) * 256)
        kb.op(DVE, lambda: nc.vector.tensor_copy(out=ysb[:], in_=YS[:, 0:256]), R=[YS], Wr=[ysb])
        kb.dma(bon[:], cx.bscr.ap[bc, sl], Wr=[bon])
        kb.dma(gin[:], cx.gscr.ap[bc, sl], Wr=[gin])
        o.mm(N[0][:, 0:256], cx.blk_f[:], ysb[:], [cx.blk_f, ysb], [N[0]])
        o.stt(yc[:], N[0][:, 0:256], -1.0 / 64, ysb[:], MUL, ADD, [N[0], ysb], [yc])
        o.act(sq[:], yc[:], AF.Square, [yc], [sq])
        o.mm(N[0][:, 0:256], cx.blk_f[:], sq[:], [cx.blk_f, sq], [N[0]])
        o.act(sq[:], N[0][:, 0:256], AF.Sqrt, [N[0], gneps], [sq], scale=1.0 / 64, bias=gneps[:, 0:1])
        kb.op(DVE, lambda: nc.vector.reciprocal(out=sq[:], in_=sq[:]), R=[sq], Wr=[sq])
        o.tt(DVE, yc[:], yc[:], sq[:], MUL, [yc, sq], [yc])
        o.ts(DVE, yc[:], yc[:], gnw[:, b1], gnb[:, b1], MUL, ADD, [yc, gnw, gnb], [yc])
        o.tt(DVE, yc[:], yc[:], bon[:], ADD, [yc, bon], [yc])
        o.tt(DVE, yo[:], yc[:], gin[:], MUL, [yc, gin], [yo])
        kb.dma(yT.ap[bc, sl], yo[:], R=[yo])

    s1a(0)
    s1m(0)
    s1x(0)
    for j in range(7):
        s2(0, j)
    s3(0)
    if ntile > 1:
        s1a(1)
        s1m(1)
        s1x(1)
    for t in range(ntile):
        has1 = t + 1 < ntile
        has2 = t + 2 < ntile
        seqU(t)
        if has1:
            s2(t + 1, 0)
        if has2:
            s1a(t + 2)
            s1m(t + 2)
        if has1:
            s2(t + 1, 1)
        seqY(t)
        seqM(t)
        if has1:
            s2(t + 1, 2)
        if has2:
            s1x(t + 2)
        if has1:
            s2(t + 1, 3)
            s2(t + 1, 4)
            s2(t + 1, 5)
            s2(t + 1, 6)
            s3(t + 1)
        if t % 2 == 1:
            outp(t)


def phase_merge(kb, cx, layer, hT, ys, mergedT):
    nc = kb.nc
    o = OpsH(kb)
    w2d = cx.dram["w_in"][layer]
    with ExitStack() as es:
        yS = [kb.sb("mg_y%d" % n, [128, 2, T], BF16, es) for n in range(4)]
        for n in range(4):
            kb.dma(yS[n][:], ys[n].ap.rearrange("(c p) t -> p c t", p=128), Wr=[yS[n]])
        ws = WStream(kb, es, "mg_w", 8, 128, nbuf=2, alloc_wb=False)
        wg = [[kb.sb("mg_wg%d%d" % (i, n), [128, 8, 128], BF16, es) for n in range(4)] for i in range(2)]
        wbr = [[kb.sb("mg_wb%d%d" % (i, n), [128, 2, 128], BF16, es) for n in range(4)] for i in range(2)]
        pg = [kb.ps("mg_pg%d" % i, [128, 512], F32, es) for i in range(3)]
        pu = [kb.ps("mg_pu%d" % i, [128, 512], F32, es) for i in range(3)]
        sg = [kb.sb("mg_sg%d" % i, [128, 512], F32, es) for i in range(3)]
        acc = [kb.sb("mg_acc%d" % i, [128, 512], F32, es) for i in range(2)]
        mst = [kb.sb("mg_mst%d" % i, [128, 512], BF16, es) for i in range(2)]
        cnt = 0
        for fb in range(8):
            i = fb % 2
            for n in range(4):
                wload(kb, ws, wg[i][n], wcols(w2d, GATE_OFF + n * 1024 + fb * 128, 128))
                wload(kb, ws, wbr[i][n], cx.dram["w_branch"][layer, n].rearrange("(c p) f -> p c f", p=128)[:, :, fb * 128:(fb + 1) * 128])
            for tg in range(8):
                sl = slice(tg * 512, (tg + 1) * 512)
                a_ = acc[(fb * 8 + tg) % 2]
                for n in range(4):
                    j = cnt % 3
                    cnt += 1
                    for kc in range(8):
                        o.mm(pg[j][:], wg[i][n][:, kc, :], hT[:, kc, sl], [wg[i][n], hT], [pg[j]], start=(kc == 0), stop=(kc == 7))
                    for c2 in range(2):
                        o.mm(pu[j][:], wbr[i][n][:, c2, :], yS[n][:, c2, sl], [wbr[i][n], yS[n]], [pu[j]], start=(c2 == 0), stop=(c2 == 1))
                    o.act(sg[j][:], pg[j][:], AF.Sigmoid, [pg[j]], [sg[j]])
                    if n == 0:
                        o.tt(kb.DVE, a_[:], pu[j][:], sg[j][:], ALU.mult, [pu[j], sg[j]], [a_])
                    else:
                        o.tt(kb.DVE, sg[j][:], pu[j][:], sg[j][:], ALU.mult, [pu[j], sg[j]], [sg[j]])
                        if n < 3:
                            o.tt(kb.POOL, a_[:], a_[:], sg[j][:], ALU.add, [a_, sg[j]], [a_])
                        else:
                            m_ = mst[(fb * 8 + tg) % 2]
                            o.tt(kb.POOL, m_[:], a_[:], sg[j][:], ALU.add, [a_, sg[j]], [m_])
                            kb.dma(mergedT.ap[fb * 128:(fb + 1) * 128, sl], m_[:], R=[m_])
        kb.barrier()


def phase_outproj(kb, cx, w2d, kch, inT, x_in, x_out):
    nc = kb.nc
    o = OpsH(kb)
    with ExitStack() as es:
        act = kb.sb("op_in", [128, kch, T], BF16, es)
        kb.dma(act[:], inT.ap.rearrange("(c p) t -> p c t", p=128), Wr=[act])
        ws = WStream(kb, es, "op_w", kch, 128, nbuf=2, alloc_wb=True)
        pp = [kb.ps("op_pp%d" % i, [128, 512], F32, es) for i in range(3)]
        xt = [kb.sb("op_x%d" % i, [128, 512], F32, es) for i in range(3)]
        n = 0
        for fb in range(8):
            wb = ws.load(wcols(w2d, fb * 128, 128))
            for tg in range(8):
                sl = slice(tg * 512, (tg + 1) * 512)
                j = n % 3
                n += 1
                kb.dma(xt[j][:], x_in.ap[fb * 128:(fb + 1) * 128, sl], Wr=[xt[j]])
                for kc in range(kch):
                    o.mm(pp[j][:], wb[:, kc, :], act[:, kc, sl], [wb, act], [pp[j]], start=(kc == 0), stop=(kc == kch - 1))
                o.tt(kb.DVE, xt[j][:], pp[j][:], xt[j][:], ALU.add, [pp[j], xt[j]], [xt[j]])
                kb.dma(x_out.ap[fb * 128:(fb + 1) * 128, sl], xt[j][:], R=[xt[j]])
        kb.barrier()


def phase_ffn_up(kb, cx, layer, hT, actT):
    nc = kb.nc
    o = OpsH(kb)
    wg2d = cx.dram["w_ffn_gate"][layer]
    wu2d = cx.dram["w_ffn_up"][layer]
    with ExitStack() as es:
        ws = WStream(kb, es, "fu_w", 8, 128, nbuf=2, alloc_wb=False)
        wg = [kb.sb("fu_wg%d" % i, [128, 8, 128], BF16, es) for i in range(2)]
        wu = [kb.sb("fu_wu%d" % i, [128, 8, 128], BF16, es) for i in range(2)]
        pg = [kb.ps("fu_pg%d" % i, [128, 512], F32, es) for i in range(3)]
        pu = [kb.ps("fu_pu%d" % i, [128, 512], F32, es) for i in range(3)]
        sg = [kb.sb("fu_sg%d" % i, [128, 512], F32, es) for i in range(3)]
        ast = [kb.sb("fu_a%d" % i, [128, 512], BF16, es) for i in range(3)]
        n = 0
        for fb in range(DFF // 128):
            i = fb % 2
            wload(kb, ws, wg[i], wcols(wg2d, fb * 128, 128))
            wload(kb, ws, wu[i], wcols(wu2d, fb * 128, 128))
            for tg in range(8):
                sl = slice(tg * 512, (tg + 1) * 512)
                j = n % 3
                n += 1
                for kc in range(8):
                    o.mm(pg[j][:], wg[i][:, kc, :], hT[:, kc, sl], [wg[i], hT], [pg[j]], start=(kc == 0), stop=(kc == 7))
                for kc in range(8):
                    o.mm(pu[j][:], wu[i][:, kc, :], hT[:, kc, sl], [wu[i], hT], [pu[j]], start=(kc == 0), stop=(kc == 7))
                o.act(sg[j][:], pg[j][:], AF.Silu, [pg[j]], [sg[j]])
                o.tt(kb.DVE, ast[j][:], pu[j][:], sg[j][:], ALU.mult, [pu[j], sg[j]], [ast[j]])
                kb.dma(actT.ap[fb * 128:(fb + 1) * 128, sl], ast[j][:], R=[ast[j]])
        kb.barrier()


def phase_ffn_down(kb, cx, layer, actT, x_in, x_out):
    nc = kb.nc
    o = OpsH(kb)
    w2d = cx.dram["w_ffn_down"][layer]
    KC = DFF // 128
    with ExitStack() as es:
        wd = kb.sb("fd_wd", [128, KC, 1024], BF16, es)
        stg = [kb.sb("fd_stg%d" % i, [128, 1024], F32, es) for i in range(2)]
        wv = w2d.rearrange("(c p) f -> p c f", p=128)
        for c in range(KC):
            s_ = stg[c % 2]
            kb.dma(s_[:], wv[:, c, :], Wr=[s_])
            kb.op(kb.DVE if c % 2 == 0 else kb.POOL,
                  lambda: (nc.vector if c % 2 == 0 else nc.gpsimd).tensor_copy(out=wd[:, c, :], in_=s_[:]), R=[s_], Wr=[wd])
        ain = [kb.sb("fd_a%d" % i, [128, KC, 512], BF16, es) for i in range(2)]
        pp = [kb.ps("fd_pp%d" % i, [128, 512], F32, es) for i in range(3)]
        xt = [kb.sb("fd_x%d" % i, [128, 512], F32, es) for i in range(3)]
        n = 0
        av = actT.ap.rearrange("(c p) t -> p c t", p=128)
        for tg in range(8):
            sl = slice(tg * 512, (tg + 1) * 512)
            a_ = ain[tg % 2]
            kb.dma(a_[:], av[:, :, sl], Wr=[a_])
            for fb in range(8):
                j = n % 3
                n += 1
                kb.dma(xt[j][:], x_in.ap[fb * 128:(fb + 1) * 128, sl], Wr=[xt[j]])
                for kc in range(KC):
                    o.mm(pp[j][:], wd[:, kc, fb * 128:(fb + 1) * 128], a_[:, kc, :], [wd, a_], [pp[j]], start=(kc == 0), stop=(kc == KC - 1))
                o.tt(kb.DVE, xt[j][:], pp[j][:], xt[j][:], ALU.add, [pp[j], xt[j]], [xt[j]])
                kb.dma(x_out.ap[fb * 128:(fb + 1) * 128, sl], xt[j][:], R=[xt[j]])
        kb.barrier()


def phase_sb_moba(kb, cx, layer, x_cur, ysb, ymoba):
    with ExitStack() as es:
        pre_mb, pre_sb = {}, {}
        phase_moba(kb, cx, layer, None, ymoba, defer_es=es, pre=pre_mb, alloc_only=True)
        phase_sb(kb, cx, layer, None, ysb, defer_es=es, pre=pre_sb, alloc_only=True)
        with ExitStack() as esh:
            hT = kb.sb("hT_a", [128, 8, T], BF16, esh)
            phase_norm(kb, cx, x_cur, cx.dram["norm1_g"][layer], hT, tgw=128, nbuf=1)
            at_mb = phase_moba(kb, cx, layer, hT, ymoba, defer_es=es, pre=pre_mb)
            at_sb = phase_sb(kb, cx, layer, hT, ysb, defer_es=es, pre=pre_sb)
            kb.barrier()
        u_sb, st_sb = at_sb(es)
        u_mb, st_mb = at_mb(es)
        assert len(u_sb) == len(u_mb)
        units = list(zip(u_sb, u_mb))
        stages = [lambda u, i: (st_sb[0](u[0], i), st_mb[0](u[1], i)),
                  lambda u, i: (st_sb[1](u[0], i), st_mb[1](u[1], i)),
                  lambda u, i: st_sb[2](u[0], i)]
        pipeline(units, stages)
        kb.barrier()


def build_program(first_layer=0, n_layers=DEPTH, final=True):
    nc = bass.Bass("TRN2", target_bir_lowering=False)
    cx = Ctx()
    for k, shp in CONST_SHAPES.items():
        cx.dram[k] = nc.dram_tensor(k, shp, const_dtype(k), kind="ExternalInput").ap()
    for k, shp in WEIGHT_SHAPES.items():
        cx.dram[k] = nc.dram_tensor(k, shp, F32, kind="ExternalInput").ap()
    xT = DT(nc.dram_tensor("xT", [D, T], F32, kind="ExternalInput").ap())
    outT = DT(nc.dram_tensor("outT", [D, T], F32, kind="ExternalOutput").ap())
    xa = DT(nc.dram_tensor("x_a", [D, T], F32).ap())
    xb = DT(nc.dram_tensor("x_b", [D, T], F32).ap())
    ys = [DT(nc.dram_tensor("y_mix%d" % n, [W, T], BF16).ap()) for n in range(4)]
    mergedT = DT(nc.dram_tensor("mergedT", [D, T], BF16).ap())
    actT = DT(nc.dram_tensor("actT", [DFF, T], BF16).ap())
    cx.vfirst = DT(nc.dram_tensor("vfirst", [W, T], F32).ap())
    cx.gscr = DT(nc.dram_tensor("gscr", [W, T], BF16).ap())
    cx.bscr = DT(nc.dram_tensor("bscr", [W, T], F32).ap())
    with ExitStack() as es:
        es.enter_context(nc.allow_non_contiguous_dma(reason="small parameter loads"))
        kb = KB(nc, es)
        load_consts(kb, cx)
        kb.barrier()
        x_cur = xT
        for layer in range(first_layer, first_layer + n_layers):
            phase_sb_moba(kb, cx, layer, x_cur, ys[0], ys[2])
            with ExitStack() as esl:
                hT = kb.sb("hT", [128, 8, T], BF16, esl)
                phase_norm(kb, cx, x_cur, cx.dram["norm1_g"][layer], hT)
                phase_rwkv(kb, cx, layer, hT, ys[1])
                phase_hgrn(kb, cx, layer, hT, ys[3])
                phase_merge(kb, cx, layer, hT, ys, mergedT)
                kb.barrier()
            phase_outproj(kb, cx, cx.dram["w_out"][layer], 8, mergedT, x_cur, xa)
            with ExitStack() as esl:
                hT = kb.sb("h2T", [128, 8, T], BF16, esl)
                phase_norm(kb, cx, xa, cx.dram["norm2_g"][layer], hT)
                phase_ffn_up(kb, cx, layer, hT, actT)
                kb.barrier()
            phase_ffn_down(kb, cx, layer, actT, xa, xb)
            x_cur = xb
        if final:
            phase_norm(kb, cx, x_cur, cx.dram["final_g"], None, out_dram=outT)
        kb.barrier()
        print("program: n_ins", kb.n_ins, {e.name: (e.total, e.n_ep) for e in [kb.PE, kb.ACT, kb.DVE, kb.POOL]})
    return nc


_CACHE = {}


def kernel(**inputs):
    x = np.asarray(inputs["x"], dtype=np.float32)
    consts = host_consts()
    if "nc" not in _CACHE:
        _CACHE["nc"] = build_program()
    nc = _CACHE["nc"]
    shared = {k: np.ascontiguousarray(np.asarray(inputs[k], dtype=np.float32)) for k in WEIGHT_SHAPES}
    shared.update(consts)
    in_maps = []
    for b in range(NCORES):
        m = dict(shared)
        m["xT"] = np.ascontiguousarray(x[b].T)
        in_maps.append(m)
    res = run_bass_kernel_spmd(nc, in_maps, core_ids=list(range(NCORES)))
    out = np.stack([np.asarray(res.results[b]["outT"], dtype=np.float32).T for b in range(NCORES)], axis=0)
    return np.ascontiguousarray(out)
```

```python
import numpy as np
import ml_dtypes
from contextlib import ExitStack
import concourse.bass as bass
import concourse.mybir as mybir
from concourse.bass_utils import run_bass_kernel_spmd

F32 = mybir.dt.float32
BF16 = mybir.dt.bfloat16
AF = mybir.ActivationFunctionType
ALU = mybir.AluOpType
AX = mybir.AxisListType

D = 1024
T = 4096
DEPTH = 2
HD = 64
NH = 4
W = 256
DFF = 2816
SB_OFF = 0
RWKV_OFF = 768
MOBA_OFF = 1792
HGRN_OFF = 2560
GATE_OFF = 3584
IN_COLS = 7680
NEG_BIG = -30000.0
NORM_EPS = 1e-6
GN_EPS = 64e-5
NCORES = 8


class Buf:
    __slots__ = ("w", "r", "name", "psum")

    def __init__(self, name=""):
        self.w = None
        self.r = []
        self.name = name
        self.psum = False


class Tl:
    def __init__(self, t, name=""):
        self.t = t
        self.b = Buf(name)

    def __getitem__(self, idx):
        return self.t[idx]


def _bufs(lst):
    out = []
    for x in lst:
        if x is None:
            continue
        out.append(x.b if isinstance(x, Tl) else x)
    return out


class SemEpoch:
    def __init__(self, kb, name):
        self.sem = kb.es.enter_context(kb.nc.semaphore(name))
        self.cnt = 0


class Eng:
    def __init__(self, kb, name, eng):
        self.kb = kb
        self.name = name
        self.eng = eng
        self.n_ep = 0
        self.ep = SemEpoch(kb, "s_%s_0" % name)
        self.seen = {}
        self.total = 0

    @property
    def sem(self):
        return self.ep.sem

    @property
    def cnt(self):
        return self.ep.cnt

    def new_epoch(self):
        self.n_ep += 1
        self.ep = SemEpoch(self.kb, "s_%s_%d" % (self.name, self.n_ep))

    def wait(self, src, val):
        if self.seen.get(id(src), 0) >= val:
            return
        self.eng.wait_ge(src.sem, val)
        self.seen[id(src)] = val


class DmaSlot:
    def __init__(self, kb, i):
        self.sem = kb.es.enter_context(kb.nc.semaphore("s_dma%d" % i))
        self.cnt = 0


class KB:
    def __init__(self, nc, es, ndma=24):
        self.nc = nc
        self.es = es
        self.PE = Eng(self, "pe", nc.tensor)
        self.ACT = Eng(self, "act", nc.scalar)
        self.DVE = Eng(self, "dve", nc.vector)
        self.POOL = Eng(self, "pool", nc.gpsimd)
        self.SP = Eng(self, "sp", nc.sync)
        self.slots = [DmaSlot(self, i) for i in range(ndma)]
        self.slot_i = 0
        self.n_ins = 0

    def _deps(self, E, R, Wr):
        deps = []
        mine = E.ep
        for b in R:
            if b.w is not None:
                deps.append(b.w)
            if b.psum:
                for ev in b.r:
                    if ev[0] is not mine:
                        deps.append(ev)
        for b in Wr:
            if b.w is not None and (b.w[0] is not mine or E is not self.PE):
                deps.append(b.w)
            for ev in b.r:
                if ev[0] is not mine:
                    deps.append(ev)
        for src, val in deps:
            E.wait(src, val)

    def op(self, E, fn, R=(), Wr=()):
        R = _bufs(R)
        Wr = _bufs(Wr)
        self._deps(E, R, Wr)
        ins = fn()
        E.ep.cnt += 1
        E.total += 1
        ins.then_inc(E.ep.sem, 1)
        ev = (E.ep, E.ep.cnt)
        for b in R:
            b.r.append(ev)
        for b in Wr:
            b.w = ev
            b.r = []
        self.n_ins += 1
        return ins

    def dma(self, out, in_, R=(), Wr=(), q=None):
        q = q or self.SP
        R = _bufs(R)
        Wr = _bufs(Wr)
        self._deps(q, R, Wr)
        slot = self.slots[self.slot_i]
        self.slot_i = (self.slot_i + 1) % len(self.slots)
        if slot.cnt:
            q.wait(slot, slot.cnt)
        ins = q.eng.dma_start(out=out, in_=in_)
        slot.cnt += 16
        ins.then_inc(slot.sem, 16)
        ev = (slot, slot.cnt)
        for b in R:
            b.r.append(ev)
        for b in Wr:
            b.w = ev
            b.r = []
        self.n_ins += 1
        return ev

    def _uniq(self, name):
        self.n_names = getattr(self, "n_names", 0) + 1
        return "%s_%d" % (name, self.n_names)

    def sb(self, name, shape, dt, es=None):
        es = es or self.es
        name = self._uniq(name)
        return Tl(es.enter_context(self.nc.sbuf_tensor(name, list(shape), dt)), name)

    def ps(self, name, shape, dt=F32, es=None):
        es = es or self.es
        name = self._uniq(name)
        t_ = Tl(es.enter_context(self.nc.psum_tensor(name, list(shape), dt)), name)
        t_.b.psum = True
        return t_

    def barrier(self):
        engs = [self.PE, self.ACT, self.DVE, self.POOL, self.SP]
        for E in engs:
            for F in engs:
                if F is not E and F.ep.cnt:
                    E.wait(F.ep, F.ep.cnt)
            for sl in self.slots:
                if sl.cnt:
                    E.wait(sl, sl.cnt)
        for E in engs:
            if E.ep.cnt > 12000:
                E.new_epoch()

    def finish(self, bufs):
        for b in _bufs(bufs):
            if b.w is not None:
                self.SP.wait(b.w[0], b.w[1])
        self.barrier()


def pipeline(units, stages):
    n = len(units)
    ns = len(stages)
    for step in range(n + ns - 1):
        for s, st in enumerate(stages):
            i = step - s
            if 0 <= i < n:
                st(units[i], i)


def host_consts():
    c = {}
    s = np.arange(128)[:, None]
    t = np.arange(512)[None, :]
    sbm = np.stack([((128 * o + s) < t) for o in range(4)]).astype(np.float32)
    c["c_mask_lt"] = sbm
    c["c_mask_le"] = np.stack([((128 * o + s) <= t) for o in range(4)]).astype(np.float32)
    a = np.arange(128)
    c["c_tri_ge"] = (a[:, None] >= a[None, :]).astype(np.float32)
    c["c_ones"] = np.ones((128, 128), np.float32)
    c["c_ident"] = np.eye(128, dtype=np.float32)
    c["c_blk64"] = (a[:, None] // 64 == a[None, :] // 64).astype(np.float32)
    half = 8
    inv_freq = (np.float32(500000.0) ** (-np.arange(0, 16, 2, dtype=np.float32) / np.float32(16))).astype(np.float32)
    ang = (np.arange(T, dtype=np.float32)[:, None] * inv_freq[None, :]).astype(np.float32)
    cos, sin = np.cos(ang).astype(np.float32), np.sin(ang).astype(np.float32)
    cosF = np.ones((64, T), np.float32)
    sinF = np.zeros((64, T), np.float32)
    cosF[0:8] = cos.T
    cosF[8:16] = cos.T
    sinF[0:8] = -sin.T
    sinF[8:16] = sin.T
    c["c_cosF"] = np.concatenate([cosF, cosF], 0)
    c["c_sinF"] = np.concatenate([sinF, sinF], 0)
    n = np.arange(16)
    qb = np.arange(16)[:, None]
    past = (n[None, :] < qb)
    own = (n[None, :] == qb)
    BIG = -NEG_BIG
    def rep(m):
        return np.ascontiguousarray(np.broadcast_to(m[None, :, None, :], (128, 16, 4, 16))).astype(np.float32)
    c["c_pastneg"] = rep(np.where(past, 0.0, -1e30))
    c["c_pastbig"] = rep(np.where(past, BIG, 0.0))
    c["c_ownm"] = rep(np.where(own, 0.0, -BIG))
    E = np.zeros((128, 16, 128), np.float32)
    for h in range(2):
        for i in range(16):
            E[64 * h + i, i, :] = 1.0
    c["c_E_b"] = E.astype(ml_dtypes.bfloat16)
    c["c_hgmask"] = ((a[:, None] // 32 == a[None, :] // 32) & (a[:, None] <= a[None, :])).astype(np.float32)
    c["c_scanmask"] = (np.broadcast_to((np.arange(512) % 32 != 0)[None, :], (128, 512))).astype(np.float32).copy()
    c["c_rw_low"] = (a[:, None] > a[None, :]).astype(np.float32)
    c["c_rw_upS"] = (a[:, None] < a[None, :]).astype(np.float32)
    c["c_rw_upI"] = (a[:, None] <= a[None, :]).astype(np.float32)
    c["c_scanmask128"] = (np.broadcast_to((np.arange(512) % 128 != 0)[None, :], (128, 512))).astype(np.float32).copy()
    c["c_mask_le_b"] = c["c_mask_le"].astype(ml_dtypes.bfloat16)
    c["c_mask_lt_b"] = c["c_mask_lt"].astype(ml_dtypes.bfloat16)
    return c


CONST_SHAPES = {
    "c_mask_lt": [4, 128, 512],
    "c_mask_le": [4, 128, 512],
    "c_tri_ge": [128, 128],
    "c_ones": [128, 128],
    "c_ident": [128, 128],
    "c_blk64": [128, 128],
    "c_cosF": [128, 4096],
    "c_sinF": [128, 4096],
    "c_pastneg": [128, 16, 4, 16],
    "c_pastbig": [128, 16, 4, 16],
    "c_ownm": [128, 16, 4, 16],
    "c_E_b": [128, 16, 128],
    "c_hgmask": [128, 128],
    "c_scanmask": [128, 512],
    "c_rw_low": [128, 128],
    "c_rw_upS": [128, 128],
    "c_rw_upI": [128, 128],
    "c_scanmask128": [128, 512],
    "c_mask_le_b": [4, 128, 512],
    "c_mask_lt_b": [4, 128, 512],
}


def const_dtype(k):
    return BF16 if k.endswith("_b") else F32


WEIGHT_SHAPES = {
    "norm1_g": [2, 1024], "w_in": [2, 1024, 7680], "rwkv_mu": [2, 1024], "rwkv_w0": [2, 256],
    "rwkv_w2": [2, 64, 256], "rwkv_a0": [2, 256], "rwkv_a2": [2, 64, 256], "rwkv_g2": [2, 128, 256],
    "rwkv_k_k": [2, 256], "rwkv_k_a": [2, 256], "rwkv_r_k": [2, 4, 64], "rwkv_gn_w": [2, 256],
    "rwkv_gn_b": [2, 256], "rwkv_v0": [1, 256], "rwkv_v1": [1, 256, 32], "rwkv_v2": [1, 32, 256],
    "hgrn_lb_logits": [2, 256], "hgrn_gn_g": [2, 256], "w_branch": [2, 4, 256, 1024],
    "w_out": [2, 1024, 1024], "norm2_g": [2, 1024], "w_ffn_gate": [2, 1024, 2816],
    "w_ffn_up": [2, 1024, 2816], "w_ffn_down": [2, 2816, 1024], "final_g": [1024],
}


class DT:
    def __init__(self, ap):
        self.ap = ap
        self.bufs = {}

    def buf(self, i):
        if i not in self.bufs:
            self.bufs[i] = Buf()
        return self.bufs[i]

    def all(self):
        return list(self.bufs.values())


class Ctx:
    def __init__(self):
        self.dram = {}

    def xbuf(self, dt, i):
        return dt.buf(i)


def load_consts(kb, cx):
    nc = kb.nc
    cx.ones_f = kb.sb("ones_f", [128, 128], F32)
    cx.tri_f = kb.sb("tri_f", [128, 128], F32)
    cx.ident_f = kb.sb("ident_f", [128, 128], F32)
    cx.blk_f = kb.sb("blk_f", [128, 128], F32)
    cx.ones_b = kb.sb("ones_b", [128, 128], BF16)
    cx.ident_b = kb.sb("ident_b", [128, 128], BF16)
    kb.dma(cx.ones_f[:], cx.dram["c_ones"][:, :], Wr=[cx.ones_f])
    kb.dma(cx.tri_f[:], cx.dram["c_tri_ge"][:, :], Wr=[cx.tri_f])
    kb.dma(cx.ident_f[:], cx.dram["c_ident"][:, :], Wr=[cx.ident_f])
    kb.dma(cx.blk_f[:], cx.dram["c_blk64"][:, :], Wr=[cx.blk_f])
    cx.eps_norm = kb.sb("eps_norm", [128, 1], F32)
    cx.one_c = kb.sb("one_c", [128, 1], F32)
    kb.op(kb.POOL, lambda: nc.gpsimd.memset(cx.eps_norm[:], NORM_EPS), Wr=[cx.eps_norm])
    kb.op(kb.POOL, lambda: nc.gpsimd.memset(cx.one_c[:], 1.0), Wr=[cx.one_c])
    kb.op(kb.DVE, lambda: nc.vector.tensor_copy(out=cx.ones_b[:], in_=cx.ones_f[:]), R=[cx.ones_f], Wr=[cx.ones_b])
    kb.op(kb.DVE, lambda: nc.vector.tensor_copy(out=cx.ident_b[:], in_=cx.ident_f[:]), R=[cx.ident_f], Wr=[cx.ident_b])


class WStream:
    def __init__(self, kb, es, name, kch, ncols, nbuf=2, alloc_wb=True):
        self.kb = kb
        self.kch = kch
        self.ncols = ncols
        self.stg = [kb.sb("%s_stg%d" % (name, i), [128, kch, ncols], F32, es) for i in range(nbuf)]
        self.wb = [kb.sb("%s_wb%d" % (name, i), [128, kch, ncols], BF16, es) for i in range(nbuf)] if alloc_wb else []
        self.i = 0

    def load(self, src_ap, eng=None, kch=None, ncols=None):
        kb = self.kb
        nc = kb.nc
        kch = kch or self.kch
        ncols = ncols or self.ncols
        i = self.i
        self.i = (self.i + 1) % len(self.stg)
        stg, wb = self.stg[i], self.wb[i]
        kb.dma(stg[:, :kch, :ncols], src_ap, Wr=[stg])
        eng = eng or kb.DVE
        kb.op(eng, lambda: eng.eng.tensor_copy(out=wb[:, :kch, :ncols], in_=stg[:, :kch, :ncols]), R=[stg], Wr=[wb])
        return wb


def wload(kb, ws, dst, src_ap, eng=None):
    nc = kb.nc
    i = ws.i
    ws.i = (ws.i + 1) % len(ws.stg)
    stg = ws.stg[i]
    kch, ncols = dst.t.shape[1], dst.t.shape[2]
    kb.dma(stg[:, :kch, :ncols], src_ap, Wr=[stg])
    eng = eng or kb.DVE
    kb.op(eng, lambda: eng.eng.tensor_copy(out=dst[:], in_=stg[:, :kch, :ncols]), R=[stg], Wr=[dst])
    return dst


def wcols(w2d, c0, ncols):
    return w2d.rearrange("(kc p) f -> p kc f", p=128)[:, :, c0:c0 + ncols]


def phase_norm(kb, cx, xT, g_ap, hT, out_dram=None, tgw=512, nbuf=2):
    nc = kb.nc
    with ExitStack() as es:
        gT = kb.sb("n_gT", [128, 8], F32, es)
        kb.dma(gT[:], g_ap.rearrange("(kc p) -> p kc", p=128), Wr=[gT])
        xt = [kb.sb("n_x%d" % i, [128, 8, tgw], F32, es) for i in range(nbuf)]
        sq = [kb.sb("n_sq%d" % i, [128, 8, tgw], F32, es) for i in range(nbuf)]
        rs = [kb.sb("n_rs%d" % i, [128, tgw], F32, es) for i in range(nbuf)]
        ob = [kb.sb("n_ob%d" % i, [128, 8, tgw], F32, es) for i in range(nbuf)] if out_dram is not None else None
        pss = [kb.ps("n_ps%d" % i, [128, tgw], F32, es) for i in range(nbuf)]
        xv = xT.ap.rearrange("(kc p) t -> p kc t", p=128)
        for tg in range(T // tgw):
            i = tg % nbuf
            x_, sq_, rs_, ps_ = xt[i], sq[i], rs[i], pss[i]
            kb.dma(x_[:], xv[:, :, tg * tgw:(tg + 1) * tgw], R=[cx.xbuf(xT, tg)], Wr=[x_])
            kb.op(kb.ACT, lambda: nc.scalar.activation(out=sq_[:], in_=x_[:], func=AF.Square), R=[x_], Wr=[sq_])
            for kc in range(8):
                kb.op(kb.PE, lambda: nc.tensor.matmul(ps_[:], lhsT=cx.ones_f[:], rhs=sq_[:, kc, :],
                                                      start=(kc == 0), stop=(kc == 7)),
                      R=[cx.ones_f, sq_], Wr=[ps_])
            kb.op(kb.ACT, lambda: nc.scalar.activation(out=rs_[:], in_=ps_[:], func=AF.Sqrt, scale=1.0 / D,
                                                       bias=cx.eps_norm[:, 0:1]),
                  R=[ps_, cx.eps_norm], Wr=[rs_])
            kb.op(kb.DVE, lambda: nc.vector.reciprocal(out=rs_[:], in_=rs_[:]), R=[rs_], Wr=[rs_])
            for kc in range(8):
                if out_dram is None:
                    o_ap = hT[:, kc, tg * tgw:(tg + 1) * tgw]
                    wr = [hT]
                else:
                    o_ap = ob[i][:, kc, :]
                    wr = [ob[i]]
                kb.op(kb.DVE, lambda: nc.vector.scalar_tensor_tensor(out=o_ap, in0=x_[:, kc, :], scalar=gT[:, kc:kc + 1],
                                                                     in1=rs_[:], op0=ALU.mult, op1=ALU.mult),
                      R=[x_, gT, rs_], Wr=wr)
            if out_dram is not None:
                kb.dma(out_dram.ap.rearrange("(kc p) t -> p kc t", p=128)[:, :, tg * tgw:(tg + 1) * tgw], ob[i][:],
                       R=[ob[i]], Wr=[cx.xbuf(out_dram, tg)])
        kb.barrier()


def phase_sb(kb, cx, layer, hT, yT, dbg=None, defer_es=None, pre=None, alloc_only=False):
    nc = kb.nc
    w2d = cx.dram["w_in"][layer] if not alloc_only else None
    with ExitStack() as es_own:
        es = defer_es if defer_es is not None else es_own

        def P(name, shape, dt):
            if pre is not None and name in pre:
                return pre[name]
            t_ = kb.sb(name, shape, dt, es)
            if pre is not None:
                pre[name] = t_
            return t_
        QT = P("sb_QT", [128, 2, T], BF16)
        KT = P("sb_KT", [128, 2, T], BF16)
        V = P("sb_V", [128, 32, 256], BF16)
        if alloc_only:
            return None
        if dbg == "p0":
            kb.dma(yT.ap[0:128, 0:2048], mlt_b[:].rearrange("p o t -> p (o t)"), R=[mlt_b], Wr=[cx.xbuf(yT, 0)])
            return
        with ExitStack() as es2:
            ws = WStream(kb, es2, "sb_w", 8, 256, nbuf=(1 if defer_es is not None else 2))
            if dbg == "p1":
                wb = ws.load(wcols(w2d, SB_OFF, 256))
                kb.dma(yT.ap[0:128, 0:2048], wb[:].rearrange("p o t -> p (o t)"), R=[wb], Wr=[cx.xbuf(yT, 0)])
                return
            pp = [kb.ps("sb_pp%d" % i, [128, 512], F32, es2) for i in range(2)]
            n = 0
            for which in range(2 if dbg != "p3" else 0):
                wb = ws.load(wcols(w2d, SB_OFF + which * 256, 256))
                for fb in range(2):
                    for tg in range(8):
                        ps_ = pp[n % 2]
                        n += 1
                        for kc in range(8):
                            kb.op(kb.PE, lambda: nc.tensor.matmul(ps_[:], lhsT=wb[:, kc, fb * 128:(fb + 1) * 128],
                                                                  rhs=hT[:, kc, tg * 512:(tg + 1) * 512],
                                                                  start=(kc == 0), stop=(kc == 7)),
                                  R=[wb, hT], Wr=[ps_])
                        sl = slice(tg * 512, (tg + 1) * 512)
                        if which == 0:
                            kb.op(kb.ACT, lambda: nc.scalar.activation(out=QT[:, fb, sl], in_=ps_[:], func=AF.Copy, scale=0.125),
                                  R=[ps_], Wr=[QT])
                        else:
                            kb.op(kb.ACT, lambda: nc.scalar.activation(out=KT[:, fb, sl], in_=ps_[:], func=AF.Copy),
                                  R=[ps_], Wr=[KT])
            wb = ws.load(wcols(w2d, SB_OFF + 512, 256))
            for tt in range(32 if dbg not in ("p2", "p2a") else 0):
                ps_ = pp[n % 2]
                n += 1
                for kc in range(8):
                    kb.op(kb.PE, lambda: nc.tensor.matmul(ps_[:, 0:256], lhsT=hT[:, kc, tt * 128:(tt + 1) * 128],
                                                          rhs=wb[:, kc, :], start=(kc == 0), stop=(kc == 7)),
                          R=[wb, hT], Wr=[ps_])
                kb.op(kb.ACT, lambda: nc.scalar.activation(out=V[:, tt, :], in_=ps_[:, 0:256], func=AF.Copy), R=[ps_], Wr=[V])
            kb.barrier()
        if dbg == "p3":
            kb.dma(yT.ap[0:128, :], V[:, 0:16, :].rearrange("p o t -> p (o t)"), R=[V], Wr=[cx.xbuf(yT, 0)])
            return
        if dbg in ("proj", "p2", "p2a"):
            for b2 in range(2):
                kb.dma(yT.ap[b2 * 128:(b2 + 1) * 128, :], KT[:, b2, :], R=[KT], Wr=[cx.xbuf(yT, 0)])
            return
        def attn(es2):
            NB = 3
            mlt_b = kb.sb("sb_mltb", [128, 4, 512], BF16, es2)
            kb.dma(mlt_b[:], cx.dram["c_mask_lt_b"].rearrange("o s t -> s o t"), Wr=[mlt_b])
            pz = [kb.ps("sb_pz%d" % i, [128, 512], F32, es2) for i in range(2)]
            pc = [kb.ps("sb_pc%d" % i, [128, 512], F32, es2) for i in range(2)]
            po = [kb.ps("sb_po%d" % i, [64, 512], F32, es2) for i in range(1 if defer_es is not None else 2)]
            ee = [kb.sb("sb_e%d" % i, [128, 512], F32, es2) for i in range(NB)]
            lk = [kb.sb("sb_lk%d" % i, [128, 512], BF16, es2) for i in range(NB)]
            arb = [kb.sb("sb_arb%d" % i, [128, 512], BF16, es2) for i in range(2)]
            tri_b = kb.sb("sb_trib", [128, 128], BF16, es2)
            kb.op(kb.DVE, lambda: nc.vector.tensor_copy(out=tri_b[:], in_=cx.tri_f[:]), R=[cx.tri_f], Wr=[tri_b])
            at = [kb.sb("sb_at%d" % i, [128, 512], BF16, es2) for i in range(NB)]
            arun = [kb.sb("sb_ar%d" % i, [128, 512], F32, es2) for i in range(2)]
            yst = [kb.sb("sb_y%d" % i, [64, 512], BF16, es2) for i in range(2)]
            units = []
            gi = 0
            for h in range(NH):
                for g in range(8):
                    for j in range(4 * g + 3, -1, -1):
                        units.append((h, g, j, gi))
                    gi += 1

            def stA(u, i):
                h, g, j, gi = u
                blk, po_ = h // 2, (h % 2) * 64
                pz_, e_, lk_ = pz[i % 2], ee[i % NB], lk[i % NB]
                kb.op(kb.PE, lambda: nc.tensor.matmul(pz_[:], lhsT=KT[po_:po_ + 64, blk, j * 128:(j + 1) * 128],
                                                      rhs=QT[po_:po_ + 64, blk, g * 512:(g + 1) * 512], start=True, stop=True),
                      R=[KT, QT], Wr=[pz_])
                kb.op(kb.ACT, lambda: nc.scalar.activation(out=e_[:], in_=pz_[:], func=AF.Exp), R=[pz_], Wr=[e_])
                kb.op(kb.ACT, lambda: nc.scalar.activation(out=lk_[:], in_=e_[:], func=AF.Ln, bias=cx.one_c[:, 0:1]),
                      R=[e_, cx.one_c], Wr=[lk_])
                o = j - 4 * g
                if o >= 0:
                    kb.op(kb.POOL, lambda: nc.gpsimd.tensor_tensor(out=lk_[:], in0=lk_[:], in1=mlt_b[:, o, :], op=ALU.mult),
                          R=[lk_, mlt_b], Wr=[lk_])

            def stB(u, i):
                h, g, j, gi = u
                blk, po_ = h // 2, (h % 2) * 64
                pc_, lk_, at_, ar_, arb_ = pc[i % 2], lk[i % NB], at[i % NB], arun[gi % 2], arb[i % 2]
                first = (j == 4 * g + 3)
                kb.op(kb.PE, lambda: nc.tensor.matmul(pc_[:], lhsT=tri_b[:], rhs=lk_[:], start=True, stop=first),
                      R=[tri_b, lk_], Wr=[pc_])
                if not first:
                    kb.op(kb.PE, lambda: nc.tensor.matmul(pc_[:], lhsT=cx.ones_b[:], rhs=arb_[:], start=False, stop=True),
                          R=[cx.ones_b, arb_], Wr=[pc_])
                e_ = ee[i % NB]
                kb.op(kb.ACT, lambda: nc.scalar.activation(out=at_[:], in_=pc_[:], func=AF.Exp, scale=-1.0), R=[pc_], Wr=[at_])
                kb.op(kb.POOL, lambda: nc.gpsimd.tensor_tensor(out=at_[:], in0=at_[:], in1=e_[:], op=ALU.mult), R=[at_, e_], Wr=[at_])
                o = j - 4 * g
                if o >= 0:
                    kb.op(kb.POOL, lambda: nc.gpsimd.tensor_tensor(out=at_[:], in0=at_[:], in1=mlt_b[:, o, :], op=ALU.mult),
                          R=[at_, mlt_b], Wr=[at_])
                if j > 0:
                    arn_ = arb[(i + 1) % 2]
                    if first:
                        kb.op(kb.DVE, lambda: nc.vector.tensor_copy(out=arn_[:], in_=lk_[:]), R=[lk_], Wr=[arn_])
                        if j > 1:
                            kb.op(kb.DVE, lambda: nc.vector.tensor_copy(out=ar_[:], in_=lk_[:]), R=[lk_], Wr=[ar_])
                    else:
                        kb.op(kb.DVE, lambda: nc.vector.tensor_tensor(out=arn_[:], in0=ar_[:], in1=lk_[:], op=ALU.add),
                              R=[lk_, ar_], Wr=[arn_])
                        if j > 1:
                            kb.op(kb.DVE, lambda: nc.vector.tensor_tensor(out=ar_[:], in0=ar_[:], in1=lk_[:], op=ALU.add),
                                  R=[lk_, ar_], Wr=[ar_])

            def stC(u, i):
                h, g, j, gi = u
                at_, po_t = at[i % NB], po[gi % len(po)]
                first = (j == 4 * g + 3)
                kb.op(kb.PE, lambda: nc.tensor.matmul(po_t[:], lhsT=V[:, j, h * 64:(h + 1) * 64], rhs=at_[:],
                                                      start=first, stop=(j == 0)),
                      R=[V, at_], Wr=[po_t])
                if j == 0:
                    y_ = yst[gi % 2]
                    kb.op(kb.DVE, lambda: nc.vector.tensor_copy(out=y_[:], in_=po_t[:]), R=[po_t], Wr=[y_])
                    kb.dma(yT.ap[h * 64:(h + 1) * 64, g * 512:(g + 1) * 512], y_[:], R=[y_], Wr=[cx.xbuf(yT, g)])

            if dbg is not None and dbg.startswith("n"):
                units = units[:int(dbg[1:])]
            return units, [stA, stB, stC]

        if defer_es is not None:
            return attn
        with ExitStack() as es2:
            units, stages = attn(es2)
            pipeline(units, stages)
            kb.barrier()


def proj_fm(kb, hT, wb, ncols, pp, cnt, epi):
    nc = kb.nc
    for fb in range(ncols // 128):
        for tg in range(T // 512):
            ps_ = pp[cnt[0] % len(pp)]
            cnt[0] += 1
            for kc in range(8):
                kb.op(kb.PE, lambda: nc.tensor.matmul(ps_[:], lhsT=wb[:, kc, fb * 128:(fb + 1) * 128],
                                                      rhs=hT[:, kc, tg * 512:(tg + 1) * 512],
                                                      start=(kc == 0), stop=(kc == 7)),
                      R=[wb, hT], Wr=[ps_])
            epi(fb, tg, ps_)


def proj_tm(kb, hT, wb, ncols, pp, cnt, epi):
    nc = kb.nc
    for tt in range(T // 128):
        ps_ = pp[cnt[0] % len(pp)]
        cnt[0] += 1
        for kc in range(8):
            kb.op(kb.PE, lambda: nc.tensor.matmul(ps_[:, 0:ncols], lhsT=hT[:, kc, tt * 128:(tt + 1) * 128],
                                                  rhs=wb[:, kc, 0:ncols], start=(kc == 0), stop=(kc == 7)),
                  R=[wb, hT], Wr=[ps_])
        epi(tt, ps_)


def phase_moba(kb, cx, layer, hT, yT, dbg=None, defer_es=None, pre=None, alloc_only=False):
    nc = kb.nc
    w2d = cx.dram["w_in"][layer] if not alloc_only else None
    with ExitStack() as es_own:
        es = defer_es if defer_es is not None else es_own

        def P(name, shape, dt):
            if pre is not None and name in pre:
                return pre[name]
            t_ = kb.sb(name, shape, dt, es)
            if pre is not None:
                pre[name] = t_
            return t_
        QT = P("mb_QT", [128, 2, T], BF16)
        KT = P("mb_KT", [128, 2, T], BF16)
        V = P("mb_V", [128, 32, 4, 65], BF16)
        esel = P("mb_esel", [128, 64], F32)
        selbT = P("mb_selbT", [128, 2, T], BF16)
        E_b = P("mb_Eb", [128, 16, 128], BF16)
        kmT = P("mb_kmT", [128, 2, 16], BF16)
        if alloc_only:
            return None
        kb.op(kb.POOL, lambda: nc.gpsimd.memset(V[:, :, :, 64:65], 1.0), Wr=[V])
        kb.op(kb.POOL, lambda: nc.gpsimd.memset(esel[:], 0.0), Wr=[esel])
        kb.op(kb.POOL, lambda: nc.gpsimd.memset(esel[64:65, :], 1.0), Wr=[esel])
        kb.op(kb.POOL, lambda: nc.gpsimd.memset(selbT[:], 0.0), Wr=[selbT])
        kb.dma(E_b[:], cx.dram["c_E_b"][:, :, :], Wr=[E_b])
        with ExitStack() as es2:
            nb_ = 1 if defer_es is not None else 2
            cosS = [kb.sb("mb_cos%d" % i, [128, 512], F32, es2) for i in range(nb_)]
            sinS = [kb.sb("mb_sin%d" % i, [128, 512], F32, es2) for i in range(nb_)]
            ws = WStream(kb, es2, "mb_w", 8, 256, nbuf=(1 if defer_es is not None else 2))
            wp = kb.sb("mb_wp", [128, 8, 256], BF16, es2)
            t1 = [kb.sb("mb_t1%d" % i, [128, 512], F32, es2) for i in range(nb_)]
            t2 = [kb.sb("mb_t2%d" % i, [128, 512], F32, es2) for i in range(nb_)]
            pp = [kb.ps("mb_pp%d" % i, [128, 512], F32, es2) for i in range(2)]
            pq = [kb.ps("mb_pq%d" % i, [128, 512], F32, es2) for i in range(2)]
            cnt = [0]
            cnt2 = [0]
            for which in range(2):
                wb = ws.load(wcols(w2d, MOBA_OFF + which * 256, 256))
                wb4 = wb[:].rearrange("p k (h d) -> p k h d", h=4)
                wp4 = wp[:].rearrange("p k (h d) -> p k h d", h=4)
                kb.op(kb.POOL, lambda: nc.gpsimd.tensor_copy(out=wp4[:, :, :, 0:8], in_=wb4[:, :, :, 8:16]), R=[wb], Wr=[wp])
                kb.op(kb.POOL, lambda: nc.gpsimd.tensor_copy(out=wp4[:, :, :, 8:16], in_=wb4[:, :, :, 0:8]), R=[wb], Wr=[wp])
                kb.op(kb.POOL, lambda: nc.gpsimd.tensor_copy(out=wp4[:, :, :, 16:64], in_=wb4[:, :, :, 16:64]), R=[wb], Wr=[wp])
                dst = QT if which == 0 else KT
                sc = 0.125 if which == 0 else 1.0
                for fb in range(2):
                    for tg in range(8):
                        sl = slice(tg * 512, (tg + 1) * 512)
                        pa = pp[cnt[0] % 2]
                        pb = pq[cnt[0] % 2]
                        a1, a2 = t1[cnt[0] % nb_], t2[cnt[0] % nb_]
                        cosF, sinF = cosS[cnt[0] % nb_], sinS[cnt[0] % nb_]
                        cnt[0] += 1
                        kb.dma(cosF[:], cx.dram["c_cosF"][:, sl], Wr=[cosF])
                        kb.dma(sinF[:], cx.dram["c_sinF"][:, sl], Wr=[sinF])
                        for kc in range(8):
                            kb.op(kb.PE, lambda: nc.tensor.matmul(pa[:], lhsT=wb[:, kc, fb * 128:(fb + 1) * 128], rhs=hT[:, kc, sl],
                                                                  start=(kc == 0), stop=(kc == 7)), R=[wb, hT], Wr=[pa])
                        for kc in range(8):
                            kb.op(kb.PE, lambda: nc.tensor.matmul(pb[:], lhsT=wp[:, kc, fb * 128:(fb + 1) * 128], rhs=hT[:, kc, sl],
                                                                  start=(kc == 0), stop=(kc == 7)), R=[wp, hT], Wr=[pb])
                        kb.op(kb.DVE, lambda: nc.vector.tensor_tensor(out=a1[:], in0=pa[:], in1=cosF[:], op=ALU.mult),
                              R=[pa, cosF], Wr=[a1])
                        kb.op(kb.DVE, lambda: nc.vector.tensor_tensor(out=a2[:], in0=pb[:], in1=sinF[:], op=ALU.mult),
                              R=[pb, sinF], Wr=[a2])
                        if sc == 1.0:
                            kb.op(kb.POOL, lambda: nc.gpsimd.tensor_tensor(out=dst[:, fb, sl], in0=a1[:], in1=a2[:], op=ALU.add),
                                  R=[a1, a2], Wr=[dst])
                        else:
                            kb.op(kb.POOL, lambda: nc.gpsimd.tensor_tensor(out=a1[:], in0=a1[:], in1=a2[:], op=ALU.add),
                                  R=[a1, a2], Wr=[a1])
                        if sc != 1.0:
                            kb.op(kb.ACT, lambda: nc.scalar.activation(out=dst[:, fb, sl], in_=a1[:], func=AF.Copy, scale=sc),
                                  R=[a1], Wr=[dst])
            wb = ws.load(wcols(w2d, MOBA_OFF + 512, 256))
            proj_tm(kb, hT, wb, 256, pp, cnt2,
                    lambda tt, ps_: kb.op(kb.ACT, lambda: nc.scalar.activation(out=V[:, tt, :, 0:64],
                                                                               in_=ps_[:, 0:256].rearrange("p (h d) -> p h d", h=4), func=AF.Copy),
                                          R=[ps_], Wr=[V]))
            kb.barrier()
        if dbg == "mproj":
            for b2 in range(2):
                kb.dma(yT.ap[b2 * 128:(b2 + 1) * 128, :], KT[:, b2, :], R=[KT], Wr=[cx.xbuf(yT, 0)])
            return
        with ExitStack() as es2:
            kmf = kb.sb("mb_kmf", [128, 2, 16], F32, es2)
            cpn = kb.sb("mb_cpn", [128, 16, 4, 16], F32, es2)
            cpb = kb.sb("mb_cpb", [128, 16, 4, 16], F32, es2)
            com = kb.sb("mb_com", [128, 16, 4, 16], F32, es2)
            kb.dma(cpn[:], cx.dram["c_pastneg"][:, :, :, :], Wr=[cpn])
            kb.dma(cpb[:], cx.dram["c_pastbig"][:, :, :, :], Wr=[cpb])
            kb.dma(com[:], cx.dram["c_ownm"][:, :, :, :], Wr=[com])
            for b2 in range(2):
                kb.op(kb.DVE, lambda: nc.vector.tensor_reduce(out=kmf[:, b2, :], in_=KT[:, b2, :].rearrange("p (n s) -> p n s", s=256),
                                                              axis=AX.X, op=ALU.add), R=[KT], Wr=[kmf])
            kb.op(kb.DVE, lambda: nc.vector.tensor_scalar(out=kmT[:], in0=kmf[:], scalar1=1.0 / 256, scalar2=None, op0=ALU.mult),
                  R=[kmf], Wr=[kmT])
            pg = [kb.ps("mb_pg%d" % i, [128, 512], F32, es2) for i in range(4)]
            pt = [kb.ps("mb_pt%d" % i, [128, 2, 128], BF16, es2) for i in range(2)]

            gm = [kb.sb("mb_gm%d" % i, [128, 4, 16], F32, es2) for i in range(2)]
            m8 = [kb.sb("mb_m8%d" % i, [128, 4, 8], F32, es2) for i in range(2)]
            sel = [kb.sb("mb_sel%d" % i, [128, 4, 16], F32, es2) for i in range(2)]
            selb = [kb.sb("mb_selb%d" % i, [128, 4, 16], BF16, es2) for i in range(2)]
            lvl = int(dbg[3:]) if (dbg or "").startswith("sel") and len(dbg) > 3 else 99
            for tt in range(32 if lvl > 0 else 0):
                i = tt % 2
                qb = tt // 2
                pt_, gm_, m8_, sel_, selb_ = pt[i], gm[i], m8[i], sel[i], selb[i]
                for r in range(2):
                    pg_ = pg[2 * i + r]
                    for q in range(2):
                        kb.op(kb.PE, lambda: nc.tensor.matmul(pg_[:, q * 16:(q + 1) * 16], lhsT=QT[r * 64:r * 64 + 64, q, tt * 128:(tt + 1) * 128],
                                                              rhs=kmT[r * 64:r * 64 + 64, q, :], start=True, stop=True),
                              R=[QT, kmT], Wr=[pg_])
                    kb.op(kb.DVE, lambda: nc.vector.tensor_tensor(out=gm_[:, 2 * r:2 * r + 2, :],
                                                                  in0=pg_[:, 0:32].rearrange("p (q n) -> p q n", q=2),
                                                                  in1=cpn[:, qb, 2 * r:2 * r + 2, :], op=ALU.add),
                          R=[pg_, cpn], Wr=[gm_])
                if lvl < 2:
                    continue
                for h in range(4):
                    kb.op(kb.DVE, lambda: nc.vector.max(out=m8_[:, h, :], in_=gm_[:, h, :]), R=[gm_], Wr=[m8_])
                if lvl < 3:
                    continue
                for h in range(4):
                    kb.op(kb.DVE, lambda: nc.vector.tensor_scalar(out=sel_[:, h, :], in0=gm_[:, h, :], scalar1=m8_[:, h, 2:3], scalar2=None,
                                                                  op0=ALU.is_ge), R=[gm_, m8_], Wr=[sel_])
                kb.op(kb.DVE, lambda: nc.vector.tensor_tensor(out=sel_[:], in0=sel_[:], in1=cpb[:, qb, :, :], op=ALU.mult),
                      R=[sel_, cpb], Wr=[sel_])
                kb.op(kb.DVE, lambda: nc.vector.tensor_tensor(out=selb_[:], in0=sel_[:], in1=com[:, qb, :, :], op=ALU.add),
                      R=[sel_, com], Wr=[selb_])
                if lvl < 4:
                    continue
                for h in range(4):
                    hh = (h % 2) * 2 + h // 2
                    kb.op(kb.PE, lambda: nc.tensor.transpose(pt_[64 * (h % 2):64 * (h % 2) + 16, h // 2, :], selb_[:, hh, :], cx.ident_b[:]),
                          R=[selb_, cx.ident_b], Wr=[pt_])
                if lvl < 5:
                    continue
                for pb_ in (0, 64):
                    kb.op(kb.ACT, lambda: nc.scalar.activation(out=selbT[pb_:pb_ + 16, :, tt * 128:(tt + 1) * 128],
                                                               in_=pt_[pb_:pb_ + 16, :, :], func=AF.Copy),
                          R=[pt_], Wr=[selbT])
            kb.barrier()
        if (dbg or "").startswith("sel"):
            kb.dma(yT.ap[0:128, :], selbT[:, 0, :], R=[selbT], Wr=[cx.xbuf(yT, 0)])
            kb.dma(yT.ap[128:256, :], selbT[:, 1, :], R=[selbT], Wr=[cx.xbuf(yT, 0)])
            return
        def attn(es2):
            NB = 3
            mle_b = kb.sb("mb_mleb", [128, 4, 512], BF16, es2)
            kb.dma(mle_b[:], cx.dram["c_mask_le_b"].rearrange("o s t -> s o t"), Wr=[mle_b])
            pz = [kb.ps("mb_pz%d" % i, [128, 512], F32, es2) for i in range(2)]
            po = [kb.ps("mb_po%d" % i, [128, 512], F32, es2) for i in range(1 if defer_es is not None else 2)]
            pd = [kb.ps("mb_pd%d" % i, [64, 512], F32, es2) for i in range(2)] if defer_es is None else None
            pT = [kb.sb("mb_pT%d" % i, [128, 512], BF16, es2) for i in range(NB)]
            osb = [kb.sb("mb_osb%d" % i, [128, 512], F32, es2) for i in range(2)]
            rec = [kb.sb("mb_rec%d" % i, [64, 512], F32, es2) for i in range(2)]
            yst = [kb.sb("mb_y%d" % i, [64, 512], BF16, es2) for i in range(2)]
            units = []
            gi = 0
            for h in range(NH):
                for g in range(8):
                    for j in range(4 * g + 4):
                        units.append((h, g, j, gi))
                    gi += 1
            if dbg is not None and dbg.startswith("n"):
                units = units[:int(dbg[1:])]

            def stA(u, i):
                h, g, j, gi = u
                blk, po_ = h // 2, (h % 2) * 64
                pz_, p_ = pz[i % 2], pT[i % NB]
                kb.op(kb.PE, lambda: nc.tensor.matmul(pz_[:], lhsT=KT[po_:po_ + 64, blk, j * 128:(j + 1) * 128],
                                                      rhs=QT[po_:po_ + 64, blk, g * 512:(g + 1) * 512], start=True, stop=False),
                      R=[KT, QT], Wr=[pz_])
                kb.op(kb.PE, lambda: nc.tensor.matmul(pz_[:], lhsT=E_b[po_:po_ + 64, j // 2, :],
                                                      rhs=selbT[po_:po_ + 64, h // 2, g * 512:(g + 1) * 512],
                                                      start=False, stop=True), R=[E_b, selbT], Wr=[pz_])
                kb.op(kb.ACT, lambda: nc.scalar.activation(out=p_[:], in_=pz_[:], func=AF.Exp), R=[pz_], Wr=[p_])
                o = j - 4 * g
                if o >= 0:
                    kb.op(kb.POOL, lambda: nc.gpsimd.tensor_tensor(out=p_[:], in0=p_[:], in1=mle_b[:, o, :], op=ALU.mult),
                          R=[p_, mle_b], Wr=[p_])

            def stB(u, i):
                h, g, j, gi = u
                p_, po_t = pT[i % NB], po[gi % len(po)]
                pd_t = pd[gi % 2] if pd is not None else pz[i % 2]
                first, last = (j == 0), (j == 4 * g + 3)
                kb.op(kb.PE, lambda: nc.tensor.matmul(po_t[0:65, :], lhsT=V[:, j, h, :], rhs=p_[:], start=first, stop=last),
                      R=[V, p_], Wr=[po_t])
                if last:
                    r_, y_, o_ = rec[gi % 2], yst[gi % 2], osb[gi % 2]
                    kb.op(kb.ACT, lambda: nc.scalar.activation(out=o_[0:65, :], in_=po_t[0:65, :], func=AF.Copy), R=[po_t], Wr=[o_])
                    kb.op(kb.PE, lambda: nc.tensor.matmul(pd_t[0:64, :], lhsT=esel[0:65, :], rhs=o_[0:65, :], start=True, stop=True),
                          R=[esel, o_], Wr=[pd_t])
                    kb.op(kb.DVE, lambda: nc.vector.reciprocal(out=r_[:], in_=pd_t[0:64, :]), R=[pd_t], Wr=[r_])
                    kb.op(kb.DVE, lambda: nc.vector.tensor_tensor(out=y_[:], in0=o_[0:64, :], in1=r_[:], op=ALU.mult), R=[o_, r_], Wr=[y_])
                    kb.dma(yT.ap[h * 64:(h + 1) * 64, g * 512:(g + 1) * 512], y_[:], R=[y_], Wr=[cx.xbuf(yT, g)])

            return units, [stA, stB]

        if defer_es is not None:
            return attn
        with ExitStack() as es2:
            units, stages = attn(es2)
            pipeline(units, stages)
            kb.barrier()


def phase_hgrn(kb, cx, layer, hT, yT, dbg=None):
    nc = kb.nc
    w2d = cx.dram["w_in"][layer]
    with ExitStack() as es:
        qd = kb.sb("hg_qd", [128, 2, T], BF16, es)
        kt = kb.sb("hg_kt", [128, 2, T], BF16, es)
        ke = kb.sb("hg_ke", [128, 2, T], BF16, es)
        sg = kb.sb("hg_sg", [128, 2, T], BF16, es)
        iv = kb.sb("hg_iv", [128, 32, 256], BF16, es)
        dec = kb.sb("hg_dec", [128, 2, 128], F32, es)
        lbl = kb.sb("hg_lbl", [128, 2, 2], F32, es)
        lb = kb.sb("hg_lb", [128, 2], F32, es)
        oml = kb.sb("hg_oml", [128, 2], F32, es)
        noml = kb.sb("hg_noml", [128, 2], F32, es)
        gng = kb.sb("hg_gng", [128, 2], F32, es)
        hgm = kb.sb("hg_mask", [128, 128], F32, es)
        scm = kb.sb("hg_scm", [128, 512], F32, es)
        eps_c = kb.sb("hg_eps", [128, 1], F32, es)
        kb.op(kb.POOL, lambda: nc.gpsimd.memset(eps_c[:], NORM_EPS), Wr=[eps_c])
        kb.dma(hgm[:], cx.dram["c_hgmask"][:, :], Wr=[hgm])
        kb.dma(scm[:], cx.dram["c_scanmask"][:, :], Wr=[scm])
        kb.dma(lbl[:], cx.dram["hgrn_lb_logits"].rearrange("l (b p) -> p l b", p=128), Wr=[lbl])
        kb.dma(gng[:], cx.dram["hgrn_gn_g"][layer].rearrange("(b p) -> p b", p=128), Wr=[gng])
        if layer == 0:
            kb.op(kb.DVE, lambda: nc.vector.memset(lb[:], 0.0), Wr=[lb])
        else:
            kb.op(kb.DVE, lambda: nc.vector.tensor_tensor(out=lb[:], in0=lbl[:, 1, :], in1=lbl[:, 0, :], op=ALU.subtract),
                  R=[lbl], Wr=[lb])
            kb.op(kb.ACT, lambda: nc.scalar.activation(out=lb[:], in_=lb[:], func=AF.Sigmoid), R=[lb], Wr=[lb])
        kb.op(kb.DVE, lambda: nc.vector.tensor_scalar(out=oml[:], in0=lb[:], scalar1=-1.0, scalar2=1.0, op0=ALU.mult, op1=ALU.add),
              R=[lb], Wr=[oml])
        kb.op(kb.DVE, lambda: nc.vector.tensor_scalar(out=noml[:], in0=oml[:], scalar1=-1.0, scalar2=None, op0=ALU.mult),
              R=[oml], Wr=[noml])
        with ExitStack() as es2:
            ws = WStream(kb, es2, "hg_w", 8, 256, nbuf=2, alloc_wb=False)
            wq = kb.sb("hg_wq", [128, 8, 256], BF16, es2)
            wf = kb.sb("hg_wf", [128, 8, 256], BF16, es2)
            wg = kb.sb("hg_wg", [128, 8, 256], BF16, es2)
            wload(kb, ws, wq, wcols(w2d, HGRN_OFF, 256))
            wload(kb, ws, wf, wcols(w2d, HGRN_OFF + 256, 256))
            wload(kb, ws, wg, wcols(w2d, HGRN_OFF + 768, 256))
            pq = [kb.ps("hg_pq%d" % i, [128, 512], F32, es2) for i in range(2)]
            pf = [kb.ps("hg_pf%d" % i, [128, 512], F32, es2) for i in range(2)]
            pg = [kb.ps("hg_pg%d" % i, [128, 512], F32, es2) for i in range(2)]
            NW = 2
            sig = [kb.sb("hg_sig%d" % i, [128, 512], F32, es2) for i in range(NW)]
            fg = [kb.sb("hg_fg%d" % i, [128, 512], F32, es2) for i in range(NW)]
            key = [kb.sb("hg_key%d" % i, [128, 512], F32, es2) for i in range(NW)]
            bb = [kb.sb("hg_bb%d" % i, [128, 512], F32, es2) for i in range(NW)]
            eb = [kb.sb("hg_eb%d" % i, [128, 512], F32, es2) for i in range(NW)]
            enb = [kb.sb("hg_enb%d" % i, [128, 512], F32, es2) for i in range(NW)]
            dd = [kb.sb("hg_dd%d" % i, [128, 512], F32, es2) for i in range(NW)]
            n = 0
            for b2 in range(2):
                for tg in range(8):
                    i = n % 2
                    n += 1
                    sl = slice(tg * 512, (tg + 1) * 512)
                    fsl = slice(b2 * 128, (b2 + 1) * 128)
                    for (wt_, ps_) in ((wq, pq[i]), (wf, pf[i]), (wg, pg[i])):
                        for kc in range(8):
                            kb.op(kb.PE, lambda: nc.tensor.matmul(ps_[:], lhsT=wt_[:, kc, fsl], rhs=hT[:, kc, sl],
                                                                  start=(kc == 0), stop=(kc == 7)), R=[wt_, hT], Wr=[ps_])
                    sig_, fg_, key_, bb_, eb_, enb_, dd_ = sig[i], fg[i], key[i], bb[i], eb[i], enb[i], dd[i]
                    kb.op(kb.ACT, lambda: nc.scalar.activation(out=sig_[:], in_=pf[i][:], func=AF.Sigmoid), R=[pf[i]], Wr=[sig_])
                    kb.op(kb.ACT, lambda: nc.scalar.activation(out=sg[:, b2, sl], in_=pg[i][:], func=AF.Silu), R=[pg[i]], Wr=[sg])
                    kb.op(kb.DVE, lambda: nc.vector.tensor_scalar(out=fg_[:], in0=sig_[:], scalar1=oml[:, b2:b2 + 1], scalar2=lb[:, b2:b2 + 1],
                                                                  op0=ALU.mult, op1=ALU.add), R=[sig_, oml, lb], Wr=[fg_])
                    kb.op(kb.ACT, lambda: nc.scalar.activation(out=fg_[:], in_=fg_[:], func=AF.Ln), R=[fg_], Wr=[fg_])
                    kb.op(kb.DVE, lambda: nc.vector.tensor_scalar(out=key_[:], in0=sig_[:], scalar1=-1.0, scalar2=noml[:, b2:b2 + 1],
                                                                  op0=ALU.add, op1=ALU.mult), R=[sig_, noml], Wr=[key_])
                    kb.op(kb.DVE, lambda: nc.vector.tensor_tensor_scan(out=bb_[:], data0=scm[:], data1=fg_[:], initial=0.0,
                                                                       op0=ALU.mult, op1=ALU.add), R=[scm, fg_], Wr=[bb_])
                    kb.op(kb.ACT, lambda: nc.scalar.activation(out=eb_[:], in_=bb_[:], func=AF.Exp), R=[bb_], Wr=[eb_])
                    kb.op(kb.ACT, lambda: nc.scalar.activation(out=enb_[:], in_=bb_[:], func=AF.Exp, scale=-1.0), R=[bb_], Wr=[enb_])
                    b3 = bb_[:].rearrange("p (n c) -> p n c", c=32)
                    kb.op(kb.DVE, lambda: nc.vector.tensor_tensor(out=dd_[:].rearrange("p (n c) -> p n c", c=32),
                                                                  in0=b3[:, :, 31:32].to_broadcast([128, 16, 32]), in1=b3, op=ALU.subtract),
                          R=[bb_], Wr=[dd_])
                    kb.op(kb.ACT, lambda: nc.scalar.activation(out=dd_[:], in_=dd_[:], func=AF.Exp), R=[dd_], Wr=[dd_])
                    kb.op(kb.DVE, lambda: nc.vector.tensor_tensor(out=qd[:, b2, sl], in0=pq[i][:], in1=eb_[:], op=ALU.mult),
                          R=[pq[i], eb_], Wr=[qd])
                    kb.op(kb.POOL, lambda: nc.gpsimd.tensor_tensor(out=kt[:, b2, sl], in0=key_[:], in1=enb_[:], op=ALU.mult),
                          R=[key_, enb_], Wr=[kt])
                    kb.op(kb.POOL, lambda: nc.gpsimd.tensor_tensor(out=ke[:, b2, sl], in0=key_[:], in1=dd_[:], op=ALU.mult),
                          R=[key_, dd_], Wr=[ke])
                    kb.op(kb.POOL, lambda: nc.gpsimd.tensor_copy(out=dec[:, b2, tg * 16:(tg + 1) * 16],
                                                                 in_=eb_[:].rearrange("p (n c) -> p n c", c=32)[:, :, 31]),
                          R=[eb_], Wr=[dec])
            wi = wq
            wload(kb, ws, wi, wcols(w2d, HGRN_OFF + 512, 256))
            cnt = [0]
            proj_tm(kb, hT, wi, 256, pq, cnt,
                    lambda tt, ps_: kb.op(kb.ACT, lambda: nc.scalar.activation(out=iv[:, tt, :], in_=ps_[:, 0:256], func=AF.Copy),
                                          R=[ps_], Wr=[iv]))
            kb.barrier()
        if dbg == "hproj":
            for b2 in range(2):
                kb.dma(yT.ap[b2 * 128:(b2 + 1) * 128, :], ke[:, b2, :], R=[ke], Wr=[cx.xbuf(yT, 0)])
            return
        with ExitStack() as es2:
            ketok = kb.sb("hg_ketok", [128, 32, 256], BF16, es2)
            ketok3 = kb.sb("hg_ketok3", [128, 32, 256], BF16, es2)
            m3 = kb.sb("hg_m3", [128, 1], F32, es2)
            kb.op(kb.POOL, lambda: nc.gpsimd.memset(m3[:], 1.0), Wr=[m3])
            kb.op(kb.POOL, lambda: nc.gpsimd.memset(m3[64:96, :], 0.0), Wr=[m3])
            with ExitStack() as es3:
                ptr = [kb.ps("hg_ptr%d" % i, [128, 4, 128], BF16, es3) for i in range(2)]
                n = 0
                for b2 in range(2):
                    for t4 in range(8):
                        p_ = ptr[n % 2]
                        n += 1
                        for q in range(4):
                            tt = t4 * 4 + q
                            kb.op(kb.PE, lambda: nc.tensor.transpose(p_[:, q, :], ke[:, b2, tt * 128:(tt + 1) * 128], cx.ident_b[:]),
                                  R=[ke, cx.ident_b], Wr=[p_])
                        kb.op(kb.ACT, lambda: nc.scalar.activation(out=ketok[:, t4 * 4:(t4 + 1) * 4, b2 * 128:(b2 + 1) * 128], in_=p_[:], func=AF.Copy),
                              R=[p_], Wr=[ketok])
                        kb.op(kb.ACT, lambda: nc.scalar.activation(out=ketok3[64:128, t4 * 4:(t4 + 1) * 4, b2 * 128:(b2 + 1) * 128], in_=p_[64:128, :, :],
                                                                   func=AF.Copy, scale=m3[64:128, 0:1]),
                              R=[p_, m3], Wr=[ketok3])
                kb.barrier()
            psc = [kb.ps("hg_psc%d" % i, [128, 512], F32, es2) for i in range(2)]
            pkv = [kb.ps("hg_pkv%d" % i, [128, 512], F32, es2) for i in range(4)]
            pO = [kb.ps("hg_pO%d" % i, [128, 512], F32, es2) for i in range(2)]
            S = [kb.sb("hg_S%d" % i, [128, 128], F32, es2) for i in range(2)]
            Sb = [kb.sb("hg_Sb%d" % i, [128, 128], BF16, es2) for i in range(2)]
            scT = [[kb.sb("hg_scT%d%d" % (i, r), [128, 128], BF16, es2) for r in range(2)] for i in range(2)]
            osb = [kb.sb("hg_osb%d" % i, [128, 512], F32, es2) for i in range(2)]
            osq = [kb.sb("hg_osq%d" % i, [128, 512], F32, es2) for i in range(2)]
            rt = [kb.sb("hg_rt%d" % i, [128, 512], F32, es2) for i in range(2)]
            yst = [kb.sb("hg_y%d" % i, [128, 512], BF16, es2) for i in range(2)]
            for b2 in range(2):
                kb.op(kb.DVE, lambda: nc.vector.memset(S[b2][:], 0.0), Wr=[S[b2]])
                kb.op(kb.DVE, lambda: nc.vector.memset(Sb[b2][:], 0.0), Wr=[Sb[b2]])
            ntt = 32 if not (dbg or "").startswith("ht") else int(dbg[2:])
            for tt in range(ntt):
                tg, q4 = tt // 4, tt % 4
                csl = slice(q4 * 128, (q4 + 1) * 128)
                tsl = slice(tt * 128, (tt + 1) * 128)
                for b2 in range(2):
                    O_ = pO[b2]
                    for r in range(2):
                        kb.op(kb.PE, lambda: nc.tensor.matmul(psc[r][:, 0:128], lhsT=kt[r * 64:(r + 1) * 64, b2, tsl],
                                                              rhs=qd[r * 64:(r + 1) * 64, b2, tsl], start=True, stop=True),
                              R=[kt, qd], Wr=[psc[r]])
                        sc_ = scT[b2][r]
                        kb.op(kb.DVE, lambda: nc.vector.tensor_tensor(out=sc_[:], in0=psc[r][:, 0:128], in1=hgm[:], op=ALU.mult),
                              R=[psc[r], hgm], Wr=[sc_])
                for b2 in range(2):
                    O_ = pO[b2]
                    for r in range(2):
                        sc_ = scT[b2][r]
                        h = b2 * 2 + r
                        kb.op(kb.PE, lambda: nc.tensor.matmul(O_[r * 64:(r + 1) * 64, csl], lhsT=iv[:, tt, h * 64:(h + 1) * 64], rhs=sc_[:],
                                                              start=True, stop=False, skip_group_check=True),
                              R=[iv, sc_], Wr=[O_])
                for c4 in range(4):
                    for b2 in range(2):
                        O_ = pO[b2]
                        nchunk = tt * 4 + c4
                        c0 = tt * 128 + c4 * 32
                        kb.op(kb.PE, lambda: nc.tensor.matmul(O_[:, q4 * 128 + c4 * 32:q4 * 128 + (c4 + 1) * 32], lhsT=Sb[b2][:],
                                                              rhs=qd[:, b2, c0:c0 + 32], start=False, stop=(c4 == 3), skip_group_check=True),
                              R=[Sb[b2], qd], Wr=[O_])
                        kv_ = pkv[c4]
                        if c4 < 3:
                            kb.op(kb.PE, lambda: nc.tensor.matmul(kv_[:, 0:128], lhsT=ketok[c4 * 32:(c4 + 1) * 32, tt, b2 * 128:(b2 + 1) * 128],
                                                                  rhs=iv[c4 * 32:(c4 + 1) * 32, tt, b2 * 128:(b2 + 1) * 128], start=True, stop=True),
                                  R=[ketok, iv], Wr=[kv_])
                        else:
                            kb.op(kb.PE, lambda: nc.tensor.matmul(kv_[:, 0:128], lhsT=ketok3[64:128, tt, b2 * 128:(b2 + 1) * 128],
                                                                  rhs=iv[64:128, tt, b2 * 128:(b2 + 1) * 128], start=True, stop=True),
                                  R=[ketok3, iv], Wr=[kv_])
                        kb.op(kb.DVE, lambda: nc.vector.scalar_tensor_tensor(out=S[b2][:], in0=S[b2][:], scalar=dec[:, b2, nchunk:nchunk + 1],
                                                                             in1=kv_[:, 0:128], op0=ALU.mult, op1=ALU.add),
                              R=[S[b2], dec, kv_], Wr=[S[b2]])
                        kb.op(kb.POOL, lambda: nc.gpsimd.tensor_tensor(out=Sb[b2][:], in0=S[b2][:], in1=cx.blk_f[:], op=ALU.mult),
                              R=[S[b2], cx.blk_f], Wr=[Sb[b2]])
                for b2 in range(2):
                    O_ = pO[b2]
                    if q4 == 3:
                        sl = slice(tg * 512, (tg + 1) * 512)
                        i = b2
                        kb.op(kb.ACT, lambda: nc.scalar.activation(out=osb[i][:], in_=O_[:], func=AF.Copy), R=[O_], Wr=[osb[i]])
                        kb.op(kb.ACT, lambda: nc.scalar.activation(out=osq[i][:], in_=O_[:], func=AF.Square), R=[O_], Wr=[osq[i]])
                        kb.op(kb.PE, lambda: nc.tensor.matmul(psc[0][:], lhsT=cx.blk_f[:], rhs=osq[i][:], start=True, stop=True),
                              R=[cx.blk_f, osq[i]], Wr=[psc[0]])
                        kb.op(kb.ACT, lambda: nc.scalar.activation(out=rt[i][:], in_=psc[0][:], func=AF.Sqrt, scale=1.0 / 64, bias=eps_c[:, 0:1]),
                              R=[psc[0], eps_c], Wr=[rt[i]])
                        kb.op(kb.DVE, lambda: nc.vector.reciprocal(out=rt[i][:], in_=rt[i][:]), R=[rt[i]], Wr=[rt[i]])
                        kb.op(kb.DVE, lambda: nc.vector.tensor_tensor(out=osb[i][:], in0=osb[i][:], in1=rt[i][:], op=ALU.mult),
                              R=[osb[i], rt[i]], Wr=[osb[i]])
                        kb.op(kb.DVE, lambda: nc.vector.scalar_tensor_tensor(out=yst[i][:], in0=osb[i][:], scalar=gng[:, b2:b2 + 1], in1=sg[:, b2, sl],
                                                                             op0=ALU.mult, op1=ALU.mult), R=[osb[i], gng, sg], Wr=[yst[i]])
                        kb.dma(yT.ap[b2 * 128:(b2 + 1) * 128, sl], yst[i][:], R=[yst[i]], Wr=[cx.xbuf(yT, tg)])
            kb.barrier()


C0 = 0.6065306597126334


class OpsH:
    def __init__(self, kb):
        self.kb = kb
        self.nc = kb.nc

    def act(self, out, in_, func, R, Wr, scale=1.0, bias=None):
        kb, nc = self.kb, self.nc
        if bias is None:
            return kb.op(kb.ACT, lambda: nc.scalar.activation(out=out, in_=in_, func=func, scale=scale), R=R, Wr=Wr)
        return kb.op(kb.ACT, lambda: nc.scalar.activation(out=out, in_=in_, func=func, scale=scale, bias=bias), R=R, Wr=Wr)

    def tt(self, E, out, a, b, op, R, Wr):
        kb = self.kb
        return kb.op(E, lambda: E.eng.tensor_tensor(out=out, in0=a, in1=b, op=op), R=R, Wr=Wr)

    def ts(self, E, out, a, s1, s2, op0, op1, R, Wr):
        kb = self.kb
        if s2 is None:
            return kb.op(E, lambda: E.eng.tensor_scalar(out=out, in0=a, scalar1=s1, scalar2=None, op0=op0), R=R, Wr=Wr)
        return kb.op(E, lambda: E.eng.tensor_scalar(out=out, in0=a, scalar1=s1, scalar2=s2, op0=op0, op1=op1), R=R, Wr=Wr)

    def stt(self, out, a, sc, b, op0, op1, R, Wr):
        kb, nc = self.kb, self.nc
        return kb.op(kb.DVE, lambda: nc.vector.scalar_tensor_tensor(out=out, in0=a, scalar=sc, in1=b, op0=op0, op1=op1), R=R, Wr=Wr)

    def mm(self, out, lhsT, rhs, R, Wr, start=True, stop=True, skip=False):
        kb, nc = self.kb, self.nc
        return kb.op(kb.PE, lambda: nc.tensor.matmul(out, lhsT=lhsT, rhs=rhs, start=start, stop=stop, skip_group_check=skip), R=R, Wr=Wr)


def phase_rwkv(kb, cx, layer, hT, yT, dbg=None):
    nc = kb.nc
    o = OpsH(kb)
    w2d = cx.dram["w_in"][layer]
    MUL, ADD, SUB = ALU.mult, ALU.add, ALU.subtract
    DVE, POOL = kb.DVE, kb.POOL
    with ExitStack() as es:
        def col2(name, src, es_=es):
            t_ = kb.sb(name, [128, 2], F32, es_)
            kb.dma(t_[:], src.rearrange("(b p) -> p b", p=128), Wr=[t_])
            return t_
        mu = kb.sb("rw_mu", [128, 8], F32, es)
        kb.dma(mu[:], cx.dram["rwkv_mu"][layer].rearrange("(b p) -> p b", p=128), Wr=[mu])
        w0 = col2("rw_w0", cx.dram["rwkv_w0"][layer])
        a0 = col2("rw_a0", cx.dram["rwkv_a0"][layer])
        k_k = col2("rw_kk", cx.dram["rwkv_k_k"][layer])
        k_a = col2("rw_ka", cx.dram["rwkv_k_a"][layer])
        gnw = col2("rw_gnw", cx.dram["rwkv_gn_w"][layer])
        gnb = col2("rw_gnb", cx.dram["rwkv_gn_b"][layer])
        r_k = col2("rw_rk", cx.dram["rwkv_r_k"][layer].rearrange("h d -> (h d)"))
        gneps = kb.sb("rw_gneps", [128, 1], F32, es)
        kb.op(POOL, lambda: nc.gpsimd.memset(gneps[:], GN_EPS), Wr=[gneps])
        if layer > 0:
            v0 = col2("rw_v0", cx.dram["rwkv_v0"][layer - 1])
        stg = kb.sb("rw_stg", [128, 256], F32, es)

        def small_w(name, src_ap, rows, r0=0, ncols=256):
            t_ = kb.sb(name, [128, ncols], BF16, es)
            kb.dma(stg[r0:r0 + rows, 0:ncols], src_ap, Wr=[stg])
            kb.op(DVE, lambda: nc.vector.tensor_copy(out=t_[r0:r0 + rows, :], in_=stg[r0:r0 + rows, 0:ncols]), R=[stg], Wr=[t_])
            return t_
        w2b = small_w("rw_w2b", cx.dram["rwkv_w2"][layer], 64, 0)
        a2b = small_w("rw_a2b", cx.dram["rwkv_a2"][layer], 64, 64)
        g2b = small_w("rw_g2b", cx.dram["rwkv_g2"][layer], 128, 0)
        if layer > 0:
            v2b = small_w("rw_v2b", cx.dram["rwkv_v2"][layer - 1], 32, 0)
            v1b = kb.sb("rw_v1b", [128, 2, 32], BF16, es)
            for b2 in range(2):
                kb.dma(stg[:, 0:32], cx.dram["rwkv_v1"][layer - 1][b2 * 128:(b2 + 1) * 128, :], Wr=[stg])
                kb.op(DVE, lambda: nc.vector.tensor_copy(out=v1b[:, b2, :], in_=stg[:, 0:32]), R=[stg], Wr=[v1b])
        scm = kb.sb("rw_scm", [128, 512], F32, es)
        kb.dma(scm[:], cx.dram["c_scanmask128"][:, :], Wr=[scm])
        LT = kb.sb("rw_LT", [128, T], BF16, es)
        SG = kb.sb("rw_SG", [128, T], BF16, es)
        VL = kb.sb("rw_VL", [32, T], BF16, es) if layer > 0 else None

        class Shifter:
            def __init__(self, name, es_, d=None):
                self.pb = [kb.sb("%s_pb%d" % (name, i), [128, 513], F32, es_) for i in range(2)]
                self.d = d if d is not None else kb.sb("%s_d" % name, [128, 512], F32, es_)
                self.n = 0

            def run(self, ps_, mucol, out, out_tl=None):
                cur = self.pb[self.n % 2]
                prv = self.pb[(self.n + 1) % 2]
                first = (self.n % 8 == 0)
                self.n += 1
                o.act(cur[:, 1:513], ps_[:], AF.Copy, [ps_], [cur])
                if first:
                    kb.op(DVE, lambda: nc.vector.memset(cur[:, 0:1], 0.0), Wr=[cur])
                else:
                    kb.op(DVE, lambda: nc.vector.tensor_copy(out=cur[:, 0:1], in_=prv[:, 512:513]), R=[prv], Wr=[cur])
                o.tt(DVE, self.d[:], cur[:, 0:512], cur[:, 1:513], SUB, [cur], [self.d])
                o.stt(out, self.d[:], mucol, cur[:, 1:513], MUL, ADD, [self.d, cur, mu], [out_tl])

        with ExitStack() as es2:
            ws = WStream(kb, es2, "rw0_w", 8, 128, nbuf=2, alloc_wb=False)
            wl_ = kb.sb("rw0_wl", [128, 8, 128], BF16, es2)
            wg_ = kb.sb("rw0_wg", [128, 8, 128], BF16, es2)
            wload(kb, ws, wl_, wcols(w2d, RWKV_OFF + 768, 128))
            wload(kb, ws, wg_, wcols(w2d, RWKV_OFF + 896, 128))
            nv = 2 if layer > 0 else 0
            wv_ = [kb.sb("rw0_wv%d" % i, [128, 8, 128], BF16, es2) for i in range(nv)]
            for i in range(nv):
                wload(kb, ws, wv_[i], wcols(w2d, RWKV_OFF + 512 + i * 128, 128))
            pp = [kb.ps("rw0_pp%d" % i, [128, 512], F32, es2) for i in range(4)]
            pvl = kb.ps("rw0_pvl", [128, 512], F32, es2)
            shl = Shifter("rw0_sl", es2)
            shg = Shifter("rw0_sg", es2)
            shv = [Shifter("rw0_sv%d" % i, es2) for i in range(nv)]
            tl = kb.sb("rw0_tl", [128, 512], F32, es2)
            tg_ = kb.sb("rw0_tg", [128, 512], F32, es2)
            tv = [kb.sb("rw0_tv%d" % i, [128, 512], F32, es2) for i in range(nv)]
            tvb = [kb.sb("rw0_tvb%d" % i, [128, 512], BF16, es2) for i in range(nv)]
            for tg in range(8):
                sl = slice(tg * 512, (tg + 1) * 512)
                for (wt_, ps_) in [(wl_, pp[0]), (wg_, pp[1])] + [(wv_[i], pp[2 + i]) for i in range(nv)]:
                    for kc in range(8):
                        o.mm(ps_[:], wt_[:, kc, :], hT[:, kc, sl], [wt_, hT], [ps_], start=(kc == 0), stop=(kc == 7))
                shl.run(pp[0], mu[:, 6:7], tl[:], tl)
                o.act(LT[0:64, sl], tl[0:64, :], AF.Tanh, [tl], [LT])
                o.act(LT[64:128, sl], tl[64:128, :], AF.Copy, [tl], [LT])
                shg.run(pp[1], mu[:, 7:8], tg_[:], tg_)
                o.act(SG[:, sl], tg_[:], AF.Sigmoid, [tg_], [SG])
                for i in range(nv):
                    shv[i].run(pp[2 + i], mu[:, 4 + i:5 + i], tv[i][:], tv[i])
                    kb.op(DVE, lambda: nc.vector.tensor_copy(out=tvb[i][:], in_=tv[i][:]), R=[tv[i]], Wr=[tvb[i]])
                if nv:
                    for i in range(2):
                        o.mm(pvl[0:32, :], v1b[:, i, :], tvb[i][:], [v1b, tvb[i]], [pvl], start=(i == 0), stop=(i == 1))
                    o.act(VL[0:32, sl], pvl[0:32, :], AF.Copy, [pvl], [VL])
            kb.barrier()
        if dbg == "r0":
            kb.dma(yT.ap[0:128, :], LT[:, :], R=[LT], Wr=[cx.xbuf(yT, 0)])
            kb.dma(yT.ap[128:256, :], SG[:, :], R=[SG], Wr=[cx.xbuf(yT, 0)])
            return

        for blk in range(2):
            with ExitStack() as esb:
                at = kb.sb("rw_at", [128, T], BF16, esb)
                bt = kb.sb("rw_bt", [128, T], BF16, esb)
                ktl = kb.sb("rw_ktl", [128, T], BF16, esb)
                rt = kb.sb("rw_rt", [128, T], BF16, esb)
                bh = kb.sb("rw_bh", [128, T], BF16, esb)
                kh = kb.sb("rw_kh", [128, T], BF16, esb)
                vT = kb.sb("rw_vT", [128, T], BF16, esb)
                gend = kb.sb("rw_gend", [128, 32], F32, esb)
                with ExitStack() as es2:
                    ws = WStream(kb, es2, "rwp_w", 8, 128, nbuf=2, alloc_wb=False)
                    wr_ = kb.sb("rwp_wr", [128, 8, 128], BF16, es2)
                    wk_ = kb.sb("rwp_wk", [128, 8, 128], BF16, es2)
                    wv_ = kb.sb("rwp_wv", [128, 8, 128], BF16, es2)
                    wload(kb, ws, wr_, wcols(w2d, RWKV_OFF + blk * 128, 128))
                    wload(kb, ws, wk_, wcols(w2d, RWKV_OFF + 256 + blk * 128, 128))
                    wload(kb, ws, wv_, wcols(w2d, RWKV_OFF + 512 + blk * 128, 128))
                    pr = kb.ps("rwp_pr", [128, 512], F32, es2)
                    pk = kb.ps("rwp_pk", [128, 512], F32, es2)
                    pv = kb.ps("rwp_pv", [128, 512], F32, es2)
                    pl = kb.ps("rwp_pl", [128, 512], F32, es2)
                    pa = kb.ps("rwp_pa", [128, 512], F32, es2)
                    pg2 = kb.ps("rwp_pg2", [128, 512], F32, es2)
                    pss = kb.ps("rwp_pss", [128, 512], F32, es2)
                    pvm = kb.ps("rwp_pvm", [128, 512], F32, es2)
                    shr = Shifter("rwp_sr", es2)
                    shk, shv = Shifter("rwp_sk", es2, shr.d), Shifter("rwp_sv", es2, shr.d)
                    names = ["rs", "ks", "vs", "sgw", "av", "cum", "e1", "e2", "e3", "e4", "kk", "t1", "t2"]
                    wt = {n_: kb.sb("rwp_" + n_, [128, 512], F32, es2) for n_ in names}
                    wt["kkn"] = wt["sgw"]
                    wt["kmod"] = wt["cum"]
                    gst = kb.sb("rwp_gst", [128, 512], BF16, es2)
                    bc = slice(blk * 128, (blk + 1) * 128)
                    b1 = slice(blk, blk + 1)
                    for tg in range(8):
                        sl = slice(tg * 512, (tg + 1) * 512)
                        for (wt_, ps_) in ((wr_, pr), (wk_, pk), (wv_, pv)):
                            for kc in range(8):
                                o.mm(ps_[:], wt_[:, kc, :], hT[:, kc, sl], [wt_, hT], [ps_], start=(kc == 0), stop=(kc == 7))
                        rs, ks, vs = wt["rs"], wt["ks"], wt["vs"]
                        for (sh_, ps_, mc, dst) in ((shr, pr, blk, rs), (shk, pk, 2 + blk, ks), (shv, pv, 4 + blk, vs)):
                            sh_.run(ps_, mu[:, mc:mc + 1], dst[:], dst)
                        t1, t2 = wt["t1"], wt["t2"]
                        if layer == 0:
                            kb.dma(cx.vfirst.ap[bc, sl], vs[:], R=[vs], Wr=[cx.vfirst.buf((blk, tg))])
                        else:
                            o.mm(pvm[:], v2b[0:32, bc], VL[0:32, sl], [v2b, VL], [pvm])
                            o.act(t1[:], pvm[:], AF.Sigmoid, [pvm, v0], [t1], bias=v0[:, b1])
                            kb.dma(t2[:], cx.vfirst.ap[bc, sl], R=[cx.vfirst.buf((blk, tg))], Wr=[t2])
                            o.tt(DVE, t2[:], t2[:], vs[:], SUB, [t2, vs], [t2])
                            o.tt(DVE, t2[:], t2[:], t1[:], MUL, [t2, t1], [t2])
                            o.tt(DVE, vs[:], vs[:], t2[:], ADD, [vs, t2], [vs])
                        sgw, av, cum = wt["sgw"], wt["av"], wt["cum"]
                        o.mm(pl[:], w2b[0:64, bc], LT[0:64, sl], [w2b, LT], [pl])
                        o.act(sgw[:], pl[:], AF.Sigmoid, [pl, w0], [sgw], bias=w0[:, b1])
                        o.mm(pa[:], a2b[64:128, bc], LT[64:128, sl], [a2b, LT], [pa])
                        o.act(av[:], pa[:], AF.Sigmoid, [pa, a0], [av], bias=a0[:, b1])
                        o.mm(pg2[:], g2b[:, bc], SG[:, sl], [g2b, SG], [pg2])
                        o.act(gst[:], pg2[:], AF.Copy, [pg2], [gst])
                        kb.dma(cx.gscr.ap[bc, sl], gst[:], R=[gst], Wr=[cx.gscr.buf((blk, tg))])
                        kb.op(DVE, lambda: nc.vector.tensor_tensor_scan(out=cum[:], data0=scm[:], data1=sgw[:], initial=0.0, op0=MUL, op1=ADD),
                              R=[scm, sgw], Wr=[cum])
                        e1, e2, e3, e4 = wt["e1"], wt["e2"], wt["e3"], wt["e4"]
                        o.act(e1[:], cum[:], AF.Exp, [cum], [e1], scale=-C0)
                        o.act(e2[:], cum[:], AF.Exp, [cum], [e2], scale=C0)
                        c3 = cum[:].rearrange("p (n c) -> p n c", c=128)
                        o.tt(DVE, e3[:].rearrange("p (n c) -> p n c", c=128), c3[:, :, 127:128].to_broadcast([128, 4, 128]), c3, SUB, [cum], [e3])
                        o.act(e3[:], e3[:], AF.Exp, [e3], [e3], scale=-C0)
                        o.tt(DVE, e4[:], cum[:], sgw[:], SUB, [cum, sgw], [e4])
                        o.act(e4[:], e4[:], AF.Exp, [e4], [e4], scale=-C0)
                        kb.op(POOL, lambda: nc.gpsimd.tensor_copy(out=gend[:, tg * 4:(tg + 1) * 4], in_=e1[:].rearrange("p (n c) -> p n c", c=128)[:, :, 127]),
                              R=[e1], Wr=[gend])
                        kk, kkn = wt["kk"], wt["kkn"]
                        o.ts(DVE, kk[:], ks[:], k_k[:, b1], None, MUL, None, [ks, k_k], [kk])
                        o.act(t1[:], kk[:], AF.Square, [kk], [t1])
                        o.mm(pss[:], cx.blk_f[:], t1[:], [cx.blk_f, t1], [pss])
                        o.act(t1[:], pss[:], AF.Sqrt, [pss], [t1])
                        o.ts(DVE, t1[:], t1[:], 1e-12, None, ALU.max, None, [t1], [t1])
                        kb.op(DVE, lambda: nc.vector.reciprocal(out=t1[:], in_=t1[:]), R=[t1], Wr=[t1])
                        o.tt(DVE, kkn[:], kk[:], t1[:], MUL, [kk, t1], [kkn])
                        kmod, ka = wt["kmod"], wt["kk"]
                        o.ts(DVE, t2[:], av[:], -1.0, k_a[:, b1], ADD, MUL, [av, k_a], [t2])
                        o.stt(kmod[:], t2[:], 1.0, ks[:], ADD, MUL, [t2, ks], [kmod])
                        o.stt(t2[:], rs[:], r_k[:, b1], kmod[:], MUL, MUL, [rs, r_k, kmod], [t2])
                        o.mm(pss[:], cx.blk_f[:], t2[:], [cx.blk_f, t2], [pss])
                        o.tt(DVE, t1[:], pss[:], vs[:], MUL, [pss, vs], [t1])
                        kb.dma(cx.bscr.ap[bc, sl], t1[:], R=[t1], Wr=[cx.bscr.buf((blk, tg))])
                        o.tt(POOL, ka[:], kkn[:], av[:], MUL, [kkn, av], [ka])
                        o.stt(at[:, sl], kkn[:], -1.0, e4[:], MUL, MUL, [kkn, e4], [at])
                        o.tt(POOL, bt[:, sl], ka[:], e2[:], MUL, [ka, e2], [bt])
                        o.tt(POOL, ktl[:, sl], kmod[:], e2[:], MUL, [kmod, e2], [ktl])
                        o.tt(POOL, rt[:, sl], rs[:], e1[:], MUL, [rs, e1], [rt])
                        o.tt(POOL, bh[:, sl], ka[:], e3[:], MUL, [ka, e3], [bh])
                        o.tt(POOL, kh[:, sl], kmod[:], e3[:], MUL, [kmod, e3], [kh])
                        o.act(vT[:, sl], vs[:], AF.Copy, [vs], [vT])
                    kb.barrier()
                if dbg == "prep":
                    kb.dma(yT.ap[0:128, :], at[:, :], R=[at], Wr=[cx.xbuf(yT, 0)])
                    kb.dma(yT.ap[128:256, :], kh[:, :], R=[kh], Wr=[cx.xbuf(yT, 0)])
                    return
                with ExitStack() as es2:
                    rw_recur(kb, cx, o, es2, layer, blk, at, bt, ktl, rt, bh, kh, vT, gend, gnw, gnb, gneps, yT, dbg)
                    kb.barrier()


def rw_recur(kb, cx, o, es, layer, blk, at, bt, ktl, rt, bh, kh, vT, gend, gnw, gnb, gneps, yT, dbg):
    nc = kb.nc
    MUL, ADD, SUB = ALU.mult, ALU.add, ALU.subtract
    DVE, POOL = kb.DVE, kb.POOL
    bc = slice(blk * 128, (blk + 1) * 128)
    b1 = slice(blk, blk + 1)

    def cst(name, key):
        t_ = kb.sb(name, [128, 128], F32, es)
        kb.dma(t_[:], cx.dram[key][:, :], Wr=[t_])
        return t_
    mlow, mupS, mupI = cst("rr_mlow", "c_rw_low"), cst("rr_mupS", "c_rw_upS"), cst("rr_mupI", "c_rw_upI")
    A = [kb.ps("rr_A%d" % r, [128, 512], F32, es) for r in range(2)]
    B = [kb.ps("rr_B%d" % r, [128, 512], F32, es) for r in range(2)]
    N = [kb.ps("rr_N%d" % r, [128, 512], F32, es) for r in range(2)]
    YS = kb.ps("rr_YS", [128, 512], F32, es)
    TR = kb.ps("rr_TR", [128, 4, 128], BF16, es)
    NT = 3
    tok = [kb.sb("rr_tok%d" % i, [128, 4, 128], BF16, es) for i in range(NT)]
    AakT = [[kb.sb("rr_aak%d%d" % (i, r), [128, 128], BF16, es) for r in range(2)] for i in range(NT)]
    ArbT = [[kb.sb("rr_arb%d%d" % (i, r), [128, 128], BF16, es) for r in range(2)] for i in range(NT)]
    ArkT = [[kb.sb("rr_ark%d%d" % (i, r), [128, 128], BF16, es) for r in range(2)] for i in range(NT)]
    P0 = [[kb.sb("rr_P0%d%d" % (i, r), [128, 128], BF16, es) for r in range(2)] for i in range(2)]
    PT0 = [[kb.sb("rr_PT0%d%d" % (i, r), [128, 128], BF16, es) for r in range(2)] for i in range(2)]
    Gb0 = [[kb.sb("rr_Gb0%d%d" % (i, r), [128, 128], BF16, es) for r in range(2)] for i in range(2)]
    X0sb = [[kb.sb("rr_X0%d%d" % (i, r), [128, 64], BF16, es) for r in range(2)] for i in range(2)]
    Pa = [[kb.sb("rr_P%d%d" % (r, i), [128, 128], BF16, es) for i in range(2)] for r in range(2)]
    PTa = [[kb.sb("rr_PT%d%d" % (r, i), [128, 128], BF16, es) for i in range(2)] for r in range(2)]
    Gv = [[kb.sb("rr_G%d%d" % (r, i), [128, 128], BF16, es) for i in range(2)] for r in range(2)]
    Zb = [kb.sb("rr_Z%d" % i, [128, 128], F32, es) for i in range(2)]
    WmT = [kb.sb("rr_WmT%d" % i, [128, 128], BF16, es) for i in range(2)]
    M = kb.sb("rr_M", [128, 128], F32, es)
    Mb = kb.sb("rr_Mb", [128, 128], BF16, es)
    Ub = kb.sb("rr_Ub", [128, 128], BF16, es)
    ysb = kb.sb("rr_ysb", [128, 256], F32, es)
    yc = kb.sb("rr_yc", [128, 256], F32, es)
    sq = kb.sb("rr_sq", [128, 256], F32, es)
    bon = kb.sb("rr_bon", [128, 256], F32, es)
    gin = kb.sb("rr_gin", [128, 256], BF16, es)
    yo = kb.sb("rr_yo", [128, 256], BF16, es)
    kb.op(DVE, lambda: nc.vector.memset(M[:], 0.0), Wr=[M])
    kb.op(DVE, lambda: nc.vector.memset(Mb[:], 0.0), Wr=[Mb])
    ntile = 32 if not (dbg or "").startswith("rt") else int(dbg[2:])

    def s1a(tt):
        t3 = tt % NT
        cs = slice(tt * 128, (tt + 1) * 128)
        for i, src in enumerate((vT, at, bh, kh)):
            kb.op(kb.PE, lambda: nc.tensor.transpose(TR[:, i, :], src[:, cs], cx.ident_b[:]), R=[src, cx.ident_b], Wr=[TR])
        o.act(tok[t3][:], TR[:], AF.Copy, [TR], [tok[t3]])

    def s1m(tt):
        t3, tp = tt % NT, tt % 2
        cs = slice(tt * 128, (tt + 1) * 128)
        for r in range(2):
            ps_ = slice(r * 64, (r + 1) * 64)
            jobs = ((A[r][:, 0:128], A[r], at, bt, P0[tp][r], mlow),
                    (A[r][:, 128:256], A[r], bt, at, PT0[tp][r], mupS),
                    (A[r][:, 256:384], A[r], ktl, at, AakT[t3][r], mupS),
                    (A[r][:, 384:512], A[r], bt, rt, ArbT[t3][r], mupI),
                    (B[r][:, 0:128], B[r], ktl, rt, ArkT[t3][r], mupI))
            for (out_, ob_, l_, r_, dst, msk) in jobs:
                o.mm(out_, l_[ps_, cs], r_[ps_, cs], [l_, r_], [ob_])
            for (out_, ob_, l_, r_, dst, msk) in jobs:
                o.tt(DVE, dst[:], out_, msk[:], MUL, [ob_, msk], [dst])
            o.tt(POOL, Gb0[tp][r][:], PT0[tp][r][:], cx.ident_f[:], ADD, [PT0[tp][r], cx.ident_f], [Gb0[tp][r]])

    def s1x(tt):
        t3, tp = tt % NT, tt % 2
        for r in range(2):
            o.mm(B[r][:, 128:192], AakT[t3][r][:], tok[t3][:, 0, r * 64:(r + 1) * 64], [AakT[t3][r], tok[t3]], [B[r]])
            o.act(X0sb[tp][r][:], B[r][:, 128:192], AF.Copy, [B[r]], [X0sb[tp][r]])

    def evac(r, dst, src_ap, bank):
        if r == 0:
            o.act(dst[:], src_ap, AF.Copy, [bank], [dst])
        else:
            kb.op(DVE, lambda: nc.vector.tensor_copy(out=dst[:], in_=src_ap), R=[bank], Wr=[dst])

    def s2(tt, j):
        tp = tt % 2
        for r in range(2):
            Pc = P0[tp][r] if j == 0 else Pa[r][j % 2]
            PTc = PT0[tp][r] if j == 0 else PTa[r][j % 2]
            if j <= 5:
                o.mm(N[r][:, 0:128], PTc[:], Pc[:], [PTc, Pc], [N[r]])
            if j < 5:
                o.mm(N[r][:, 128:256], Pc[:], PTc[:], [PTc, Pc], [N[r]])
            if j >= 1:
                Gc = Gb0[tp][r] if j == 1 else Gv[r][(j - 1) % 2]
                o.mm(N[r][:, 256:384], cx.ident_b[:], Gc[:], [cx.ident_b, Gc], [N[r]], start=True, stop=False)
                o.mm(N[r][:, 256:384], Pc[:], Gc[:], [Pc, Gc], [N[r]], start=False, stop=True)
        for r in range(2):
            if j <= 5:
                evac(r, Pa[r][(j + 1) % 2], N[r][:, 0:128], N[r])
            if j < 5:
                evac(r, PTa[r][(j + 1) % 2], N[r][:, 128:256], N[r])
            if j >= 1:
                evac(r, Gv[r][j % 2], N[r][:, 256:384], N[r])

    def s3(tt):
        t3, tp = tt % NT, tt % 2
        for r in range(2):
            ps_ = slice(r * 64, (r + 1) * 64)
            Gf = Gv[r][0]
            o.mm(B[r][ps_, 256:384], tok[t3][:, 1, r * 64:(r + 1) * 64], Gf[:], [tok[t3], Gf], [B[r]])
            o.mm(B[r][:, 192:256], Gf[:], X0sb[tp][r][:], [Gf, X0sb[tp][r]], [B[r]])
            o.act(WmT[tp][ps_, :], B[r][ps_, 256:384], AF.Copy, [B[r]], [WmT[tp]])
            o.act(Zb[tp][:, r * 64:(r + 1) * 64], B[r][:, 192:256], AF.Copy, [B[r]], [Zb[tp]])

    def seqU(tt):
        tp = tt % 2
        o.mm(YS[:, 256:384], WmT[tp][:], Mb[:], [WmT[tp], Mb], [YS])
        o.tt(DVE, Ub[:], YS[:, 256:384], Zb[tp][:], ADD, [YS, Zb[tp]], [Ub])

    def seqY(tt):
        t3 = tt % NT
        cs = slice(tt * 128, (tt + 1) * 128)
        yc_ = slice((tt % 2) * 128, (tt % 2) * 128 + 128)
        o.mm(YS[:, yc_], Mb[:], rt[:, cs], [Mb, rt], [YS], start=True, stop=False, skip=True)
        for r in range(2):
            ps_ = slice(r * 64, (r + 1) * 64)
            o.mm(YS[ps_, yc_], Ub[:, r * 64:(r + 1) * 64], ArbT[t3][r][:], [Ub, ArbT[t3][r]], [YS], start=False, stop=False, skip=True)
            o.mm(YS[ps_, yc_], tok[t3][:, 0, r * 64:(r + 1) * 64], ArkT[t3][r][:], [tok[t3], ArkT[t3][r]], [YS], start=False, stop=(r == 1), skip=True)

    def seqM(tt):
        t3 = tt % NT
        o.mm(YS[:, 384:512], tok[t3][:, 2, :], Ub[:], [tok[t3], Ub], [YS], start=True, stop=False)
        o.mm(YS[:, 384:512], tok[t3][:, 3, :], tok[t3][:, 0, :], [tok[t3]], [YS], start=False, stop=True)
        o.stt(M[:], M[:], gend[:, tt:tt + 1], YS[:, 384:512], MUL, ADD, [M, gend, YS], [M])
        o.tt(POOL, Mb[:], M[:], cx.blk_f[:], MUL, [M, cx.blk_f], [Mb])

    def outp(tt):
        t2 = tt // 2
        sl = slice(t2 * 256, (t2 + 1) * 256)
        kb.op(DVE, lambda: nc.vector.tensor_copy(out=ysb[:], in_=YS[:, 0:256]), R=[YS], Wr=[ysb])
        kb.dma(bon[:], cx.bscr.ap[bc, sl], Wr=[bon])
        kb.dma(gin[:], cx.gscr.ap[bc, sl], Wr=[gin])
        o.mm(N[0][:, 0:256], cx.blk_f[:], ysb[:], [cx.blk_f, ysb], [N[0]])
        o.stt(yc[:], N[0][:, 0:256], -1.0 / 64, ysb[:], MUL, ADD, [N[0], ysb], [yc])
        o.act(sq[:], yc[:], AF.Square, [yc], [sq])
        o.mm(N[0][:, 0:256], cx.blk_f[:], sq[:], [cx.blk_f, sq], [N[0]])
        o.act(sq[:], N[0][:, 0:256], AF.Sqrt, [N[0], gneps], [sq], scale=1.0 / 64, bias=gneps[:, 0:1])
        kb.op(DVE, lambda: nc.vector.reciprocal(out=sq[:], in_=sq[:]), R=[sq], Wr=[sq])
        o.tt(DVE, yc[:], yc[:], sq[:], MUL, [yc, sq], [yc])
        o.ts(DVE, yc[:], yc[:], gnw[:, b1], gnb[:, b1], MUL, ADD, [yc, gnw, gnb], [yc])
        o.tt(DVE, yc[:], yc[:], bon[:], ADD, [yc, bon], [yc])
        o.tt(DVE, yo[:], yc[:], gin[:], MUL, [yc, gin], [yo])
        kb.dma(yT.ap[bc, sl], yo[:], R=[yo])

    s1a(0)
    s1m(0)
    s1x(0)
    for j in range(7):
        s2(0, j)
    s3(0)
    if ntile > 1:
        s1a(1)
        s1m(1)
        s1x(1)
    for t in range(ntile):
        has1 = t + 1 < ntile
        has2 = t + 2 < ntile
        seqU(t)
        if has1:
            s2(t + 1, 0)
        if has2:
            s1a(t + 2)
            s1m(t + 2)
        if has1:
            s2(t + 1, 1)
        seqY(t)
        seqM(t)
        if has1:
            s2(t + 1, 2)
        if has2:
            s1x(t + 2)
        if has1:
            s2(t + 1, 3)
            s2(t + 1, 4)
            s2(t + 1, 5)
            s2(t + 1, 6)
            s3(t + 1)
        if t % 2 == 1:
            outp(t)


def phase_merge(kb, cx, layer, hT, ys, mergedT):
    nc = kb.nc
    o = OpsH(kb)
    w2d = cx.dram["w_in"][layer]
    with ExitStack() as es:
        yS = [kb.sb("mg_y%d" % n, [128, 2, T], BF16, es) for n in range(4)]
        for n in range(4):
            kb.dma(yS[n][:], ys[n].ap.rearrange("(c p) t -> p c t", p=128), Wr=[yS[n]])
        ws = WStream(kb, es, "mg_w", 8, 128, nbuf=2, alloc_wb=False)
        wg = [[kb.sb("mg_wg%d%d" % (i, n), [128, 8, 128], BF16, es) for n in range(4)] for i in range(2)]
        wbr = [[kb.sb("mg_wb%d%d" % (i, n), [128, 2, 128], BF16, es) for n in range(4)] for i in range(2)]
        pg = [kb.ps("mg_pg%d" % i, [128, 512], F32, es) for i in range(3)]
        pu = [kb.ps("mg_pu%d" % i, [128, 512], F32, es) for i in range(3)]
        sg = [kb.sb("mg_sg%d" % i, [128, 512], F32, es) for i in range(3)]
        acc = [kb.sb("mg_acc%d" % i, [128, 512], F32, es) for i in range(2)]
        mst = [kb.sb("mg_mst%d" % i, [128, 512], BF16, es) for i in range(2)]
        cnt = 0
        for fb in range(8):
            i = fb % 2
            for n in range(4):
                wload(kb, ws, wg[i][n], wcols(w2d, GATE_OFF + n * 1024 + fb * 128, 128))
                wload(kb, ws, wbr[i][n], cx.dram["w_branch"][layer, n].rearrange("(c p) f -> p c f", p=128)[:, :, fb * 128:(fb + 1) * 128])
            for tg in range(8):
                sl = slice(tg * 512, (tg + 1) * 512)
                a_ = acc[(fb * 8 + tg) % 2]
                for n in range(4):
                    j = cnt % 3
                    cnt += 1
                    for kc in range(8):
                        o.mm(pg[j][:], wg[i][n][:, kc, :], hT[:, kc, sl], [wg[i][n], hT], [pg[j]], start=(kc == 0), stop=(kc == 7))
                    for c2 in range(2):
                        o.mm(pu[j][:], wbr[i][n][:, c2, :], yS[n][:, c2, sl], [wbr[i][n], yS[n]], [pu[j]], start=(c2 == 0), stop=(c2 == 1))
                    o.act(sg[j][:], pg[j][:], AF.Sigmoid, [pg[j]], [sg[j]])
                    if n == 0:
                        o.tt(kb.DVE, a_[:], pu[j][:], sg[j][:], ALU.mult, [pu[j], sg[j]], [a_])
                    else:
                        o.tt(kb.DVE, sg[j][:], pu[j][:], sg[j][:], ALU.mult, [pu[j], sg[j]], [sg[j]])
                        if n < 3:
                            o.tt(kb.POOL, a_[:], a_[:], sg[j][:], ALU.add, [a_, sg[j]], [a_])
                        else:
                            m_ = mst[(fb * 8 + tg) % 2]
                            o.tt(kb.POOL, m_[:], a_[:], sg[j][:], ALU.add, [a_, sg[j]], [m_])
                            kb.dma(mergedT.ap[fb * 128:(fb + 1) * 128, sl], m_[:], R=[m_])
        kb.barrier()


def phase_outproj(kb, cx, w2d, kch, inT, x_in, x_out):
    nc = kb.nc
    o = OpsH(kb)
    with ExitStack() as es:
        act = kb.sb("op_in", [128, kch, T], BF16, es)
        kb.dma(act[:], inT.ap.rearrange("(c p) t -> p c t", p=128), Wr=[act])
        ws = WStream(kb, es, "op_w", kch, 128, nbuf=2, alloc_wb=True)
        pp = [kb.ps("op_pp%d" % i, [128, 512], F32, es) for i in range(3)]
        xt = [kb.sb("op_x%d" % i, [128, 512], F32, es) for i in range(3)]
        n = 0
        for fb in range(8):
            wb = ws.load(wcols(w2d, fb * 128, 128))
            for tg in range(8):
                sl = slice(tg * 512, (tg + 1) * 512)
                j = n % 3
                n += 1
                kb.dma(xt[j][:], x_in.ap[fb * 128:(fb + 1) * 128, sl], Wr=[xt[j]])
                for kc in range(kch):
                    o.mm(pp[j][:], wb[:, kc, :], act[:, kc, sl], [wb, act], [pp[j]], start=(kc == 0), stop=(kc == kch - 1))
                o.tt(kb.DVE, xt[j][:], pp[j][:], xt[j][:], ALU.add, [pp[j], xt[j]], [xt[j]])
                kb.dma(x_out.ap[fb * 128:(fb + 1) * 128, sl], xt[j][:], R=[xt[j]])
        kb.barrier()


def phase_ffn_up(kb, cx, layer, hT, actT):
    nc = kb.nc
    o = OpsH(kb)
    wg2d = cx.dram["w_ffn_gate"][layer]
    wu2d = cx.dram["w_ffn_up"][layer]
    with ExitStack() as es:
        ws = WStream(kb, es, "fu_w", 8, 128, nbuf=2, alloc_wb=False)
        wg = [kb.sb("fu_wg%d" % i, [128, 8, 128], BF16, es) for i in range(2)]
        wu = [kb.sb("fu_wu%d" % i, [128, 8, 128], BF16, es) for i in range(2)]
        pg = [kb.ps("fu_pg%d" % i, [128, 512], F32, es) for i in range(3)]
        pu = [kb.ps("fu_pu%d" % i, [128, 512], F32, es) for i in range(3)]
        sg = [kb.sb("fu_sg%d" % i, [128, 512], F32, es) for i in range(3)]
        ast = [kb.sb("fu_a%d" % i, [128, 512], BF16, es) for i in range(3)]
        n = 0
        for fb in range(DFF // 128):
            i = fb % 2
            wload(kb, ws, wg[i], wcols(wg2d, fb * 128, 128))
            wload(kb, ws, wu[i], wcols(wu2d, fb * 128, 128))
            for tg in range(8):
                sl = slice(tg * 512, (tg + 1) * 512)
                j = n % 3
                n += 1
                for kc in range(8):
                    o.mm(pg[j][:], wg[i][:, kc, :], hT[:, kc, sl], [wg[i], hT], [pg[j]], start=(kc == 0), stop=(kc == 7))
                for kc in range(8):
                    o.mm(pu[j][:], wu[i][:, kc, :], hT[:, kc, sl], [wu[i], hT], [pu[j]], start=(kc == 0), stop=(kc == 7))
                o.act(sg[j][:], pg[j][:], AF.Silu, [pg[j]], [sg[j]])
                o.tt(kb.DVE, ast[j][:], pu[j][:], sg[j][:], ALU.mult, [pu[j], sg[j]], [ast[j]])
                kb.dma(actT.ap[fb * 128:(fb + 1) * 128, sl], ast[j][:], R=[ast[j]])
        kb.barrier()


def phase_ffn_down(kb, cx, layer, actT, x_in, x_out):
    nc = kb.nc
    o = OpsH(kb)
    w2d = cx.dram["w_ffn_down"][layer]
    KC = DFF // 128
    with ExitStack() as es:
        wd = kb.sb("fd_wd", [128, KC, 1024], BF16, es)
        stg = [kb.sb("fd_stg%d" % i, [128, 1024], F32, es) for i in range(2)]
        wv = w2d.rearrange("(c p) f -> p c f", p=128)
        for c in range(KC):
            s_ = stg[c % 2]
            kb.dma(s_[:], wv[:, c, :], Wr=[s_])
            kb.op(kb.DVE if c % 2 == 0 else kb.POOL,
                  lambda: (nc.vector if c % 2 == 0 else nc.gpsimd).tensor_copy(out=wd[:, c, :], in_=s_[:]), R=[s_], Wr=[wd])
        ain = [kb.sb("fd_a%d" % i, [128, KC, 512], BF16, es) for i in range(2)]
        pp = [kb.ps("fd_pp%d" % i, [128, 512], F32, es) for i in range(3)]
        xt = [kb.sb("fd_x%d" % i, [128, 512], F32, es) for i in range(3)]
        n = 0
        av = actT.ap.rearrange("(c p) t -> p c t", p=128)
        for tg in range(8):
            sl = slice(tg * 512, (tg + 1) * 512)
            a_ = ain[tg % 2]
            kb.dma(a_[:], av[:, :, sl], Wr=[a_])
            for fb in range(8):
                j = n % 3
                n += 1
                kb.dma(xt[j][:], x_in.ap[fb * 128:(fb + 1) * 128, sl], Wr=[xt[j]])
                for kc in range(KC):
                    o.mm(pp[j][:], wd[:, kc, fb * 128:(fb + 1) * 128], a_[:, kc, :], [wd, a_], [pp[j]], start=(kc == 0), stop=(kc == KC - 1))
                o.tt(kb.DVE, xt[j][:], pp[j][:], xt[j][:], ALU.add, [pp[j], xt[j]], [xt[j]])
                kb.dma(x_out.ap[fb * 128:(fb + 1) * 128, sl], xt[j][:], R=[xt[j]])
        kb.barrier()


def phase_sb_moba(kb, cx, layer, x_cur, ysb, ymoba, hscr=None):
    with ExitStack() as es:
        pre_mb, pre_sb = {}, {}
        phase_moba(kb, cx, layer, None, ymoba, defer_es=es, pre=pre_mb, alloc_only=True)
        phase_sb(kb, cx, layer, None, ysb, defer_es=es, pre=pre_sb, alloc_only=True)
        with ExitStack() as esh:
            hT = kb.sb("hT_a", [128, 8, T], BF16, esh)
            phase_norm(kb, cx, x_cur, cx.dram["norm1_g"][layer], hT, tgw=256, nbuf=1)
            if hscr is not None:
                for kc in range(8):
                    kb.dma(hscr.ap[kc * 128:(kc + 1) * 128, :], hT[:, kc, :], R=[hT])
            at_mb = phase_moba(kb, cx, layer, hT, ymoba, defer_es=es, pre=pre_mb)
            at_sb = phase_sb(kb, cx, layer, hT, ysb, defer_es=es, pre=pre_sb)
            kb.barrier()
        u_sb, st_sb = at_sb(es)
        u_mb, st_mb = at_mb(es)
        assert len(u_sb) == len(u_mb)
        units = list(zip(u_sb, u_mb))
        stages = [lambda u, i: (st_sb[0](u[0], i), st_mb[0](u[1], i)),
                  lambda u, i: (st_sb[1](u[0], i), st_mb[1](u[1], i)),
                  lambda u, i: st_sb[2](u[0], i)]
        pipeline(units, stages)
        kb.barrier()


def build_program(first_layer=0, n_layers=DEPTH, final=True):
    nc = bass.Bass("TRN2", target_bir_lowering=False)
    cx = Ctx()
    for k, shp in CONST_SHAPES.items():
        cx.dram[k] = nc.dram_tensor(k, shp, const_dtype(k), kind="ExternalInput").ap()
    for k, shp in WEIGHT_SHAPES.items():
        cx.dram[k] = nc.dram_tensor(k, shp, F32, kind="ExternalInput").ap()
    xT = DT(nc.dram_tensor("xT", [D, T], F32, kind="ExternalInput").ap())
    outT = DT(nc.dram_tensor("outT", [D, T], F32, kind="ExternalOutput").ap())
    xa = DT(nc.dram_tensor("x_a", [D, T], F32).ap())
    xb = DT(nc.dram_tensor("x_b", [D, T], F32).ap())
    ys = [DT(nc.dram_tensor("y_mix%d" % n, [W, T], BF16).ap()) for n in range(4)]
    mergedT = DT(nc.dram_tensor("mergedT", [D, T], BF16).ap())
    actT = DT(nc.dram_tensor("actT", [DFF, T], BF16).ap())
    cx.vfirst = DT(nc.dram_tensor("vfirst", [W, T], F32).ap())
    cx.gscr = DT(nc.dram_tensor("gscr", [W, T], BF16).ap())
    cx.bscr = DT(nc.dram_tensor("bscr", [W, T], F32).ap())
    hscr = DT(nc.dram_tensor("hT_scr", [D, T], BF16).ap())
    with ExitStack() as es:
        es.enter_context(nc.allow_non_contiguous_dma(reason="small parameter loads"))
        kb = KB(nc, es)
        load_consts(kb, cx)
        kb.barrier()
        x_cur = xT
        for layer in range(first_layer, first_layer + n_layers):
            phase_sb_moba(kb, cx, layer, x_cur, ys[0], ys[2], hscr)
            with ExitStack() as esl:
                hT = kb.sb("hT", [128, 8, T], BF16, esl)
                for kc in range(8):
                    kb.dma(hT[:, kc, :], hscr.ap[kc * 128:(kc + 1) * 128, :], Wr=[hT])
                phase_rwkv(kb, cx, layer, hT, ys[1])
                phase_hgrn(kb, cx, layer, hT, ys[3])
                phase_merge(kb, cx, layer, hT, ys, mergedT)
                kb.barrier()
            phase_outproj(kb, cx, cx.dram["w_out"][layer], 8, mergedT, x_cur, xa)
            with ExitStack() as esl:
                hT = kb.sb("h2T", [128, 8, T], BF16, esl)
                phase_norm(kb, cx, xa, cx.dram["norm2_g"][layer], hT)
                phase_ffn_up(kb, cx, layer, hT, actT)
                kb.barrier()
            phase_ffn_down(kb, cx, layer, actT, xa, xb)
            x_cur = xb
        if final:
            phase_norm(kb, cx, x_cur, cx.dram["final_g"], None, out_dram=outT)
        kb.barrier()
        print("program: n_ins", kb.n_ins, {e.name: (e.total, e.n_ep) for e in [kb.PE, kb.ACT, kb.DVE, kb.POOL]})
    return nc


_CACHE = {}


def kernel(**inputs):
    x = np.asarray(inputs["x"], dtype=np.float32)
    consts = host_consts()
    if "nc" not in _CACHE:
        _CACHE["nc"] = build_program()
    nc = _CACHE["nc"]
    shared = {k: np.ascontiguousarray(np.asarray(inputs[k], dtype=np.float32)) for k in WEIGHT_SHAPES}
    shared.update(consts)
    in_maps = []
    for b in range(NCORES):
        m = dict(shared)
        m["xT"] = np.ascontiguousarray(x[b].T)
        in_maps.append(m)
    res = run_bass_kernel_spmd(nc, in_maps, core_ids=list(range(NCORES)))
    out = np.stack([np.asarray(res.results[b]["outT"], dtype=np.float32).T for b in range(NCORES)], axis=0)
    return np.ascontiguousarray(out)
```

```python
import numpy as np
import ml_dtypes
from contextlib import ExitStack
import concourse.bass as bass
import concourse.mybir as mybir
from concourse.bass_utils import run_bass_kernel_spmd

F32 = mybir.dt.float32
BF16 = mybir.dt.bfloat16
AF = mybir.ActivationFunctionType
ALU = mybir.AluOpType
AX = mybir.AxisListType

D = 1024
T = 4096
DEPTH = 2
HD = 64
NH = 4
W = 256
DFF = 2816
SB_OFF = 0
RWKV_OFF = 768
MOBA_OFF = 1792
HGRN_OFF = 2560
GATE_OFF = 3584
IN_COLS = 7680
NEG_BIG = -30000.0
NORM_EPS = 1e-6
GN_EPS = 64e-5
NCORES = 8


class Buf:
    __slots__ = ("w", "r", "name", "psum")

    def __init__(self, name=""):
        self.w = None
        self.r = []
        self.name = name
        self.psum = False


class Tl:
    def __init__(self, t, name=""):
        self.t = t
        self.b = Buf(name)

    def __getitem__(self, idx):
        return self.t[idx]


def _bufs(lst):
    out = []
    for x in lst:
        if x is None:
            continue
        out.append(x.b if isinstance(x, Tl) else x)
    return out


class SemEpoch:
    def __init__(self, kb, name):
        self.sem = kb.es.enter_context(kb.nc.semaphore(name))
        self.cnt = 0


class Eng:
    def __init__(self, kb, name, eng):
        self.kb = kb
        self.name = name
        self.eng = eng
        self.n_ep = 0
        self.ep = SemEpoch(kb, "s_%s_0" % name)
        self.seen = {}
        self.total = 0

    @property
    def sem(self):
        return self.ep.sem

    @property
    def cnt(self):
        return self.ep.cnt

    def new_epoch(self):
        self.n_ep += 1
        self.ep = SemEpoch(self.kb, "s_%s_%d" % (self.name, self.n_ep))

    def wait(self, src, val):
        if self.seen.get(id(src), 0) >= val:
            return
        self.eng.wait_ge(src.sem, val)
        self.seen[id(src)] = val


class DmaSlot:
    def __init__(self, kb, i):
        self.sem = kb.es.enter_context(kb.nc.semaphore("s_dma%d" % i))
        self.cnt = 0


class KB:
    def __init__(self, nc, es, ndma=24):
        self.nc = nc
        self.es = es
        self.PE = Eng(self, "pe", nc.tensor)
        self.ACT = Eng(self, "act", nc.scalar)
        self.DVE = Eng(self, "dve", nc.vector)
        self.POOL = Eng(self, "pool", nc.gpsimd)
        self.SP = Eng(self, "sp", nc.sync)
        self.slots = [DmaSlot(self, i) for i in range(ndma)]
        self.slot_i = 0
        self.n_ins = 0

    def _deps(self, E, R, Wr):
        deps = []
        mine = E.ep
        for b in R:
            if b.w is not None:
                deps.append(b.w)
            if b.psum:
                for ev in b.r:
                    if ev[0] is not mine:
                        deps.append(ev)
        for b in Wr:
            if b.w is not None and (b.w[0] is not mine or E is not self.PE):
                deps.append(b.w)
            for ev in b.r:
                if ev[0] is not mine:
                    deps.append(ev)
        for src, val in deps:
            E.wait(src, val)

    def op(self, E, fn, R=(), Wr=()):
        R = _bufs(R)
        Wr = _bufs(Wr)
        self._deps(E, R, Wr)
        ins = fn()
        E.ep.cnt += 1
        E.total += 1
        ins.then_inc(E.ep.sem, 1)
        ev = (E.ep, E.ep.cnt)
        for b in R:
            b.r.append(ev)
        for b in Wr:
            b.w = ev
            b.r = []
        self.n_ins += 1
        return ins

    def dma(self, out, in_, R=(), Wr=(), q=None):
        q = q or self.SP
        R = _bufs(R)
        Wr = _bufs(Wr)
        self._deps(q, R, Wr)
        slot = self.slots[self.slot_i]
        self.slot_i = (self.slot_i + 1) % len(self.slots)
        if slot.cnt:
            q.wait(slot, slot.cnt)
        ins = q.eng.dma_start(out=out, in_=in_)
        slot.cnt += 16
        ins.then_inc(slot.sem, 16)
        ev = (slot, slot.cnt)
        for b in R:
            b.r.append(ev)
        for b in Wr:
            b.w = ev
            b.r = []
        self.n_ins += 1
        return ev

    def _uniq(self, name):
        self.n_names = getattr(self, "n_names", 0) + 1
        return "%s_%d" % (name, self.n_names)

    def sb(self, name, shape, dt, es=None):
        es = es or self.es
        name = self._uniq(name)
        return Tl(es.enter_context(self.nc.sbuf_tensor(name, list(shape), dt)), name)

    def ps(self, name, shape, dt=F32, es=None):
        es = es or self.es
        name = self._uniq(name)
        t_ = Tl(es.enter_context(self.nc.psum_tensor(name, list(shape), dt)), name)
        t_.b.psum = True
        return t_

    def barrier(self):
        engs = [self.PE, self.ACT, self.DVE, self.POOL, self.SP]
        for E in engs:
            for F in engs:
                if F is not E and F.ep.cnt:
                    E.wait(F.ep, F.ep.cnt)
            for sl in self.slots:
                if sl.cnt:
                    E.wait(sl, sl.cnt)
        for E in engs:
            if E.ep.cnt > 12000:
                E.new_epoch()

    def finish(self, bufs):
        for b in _bufs(bufs):
            if b.w is not None:
                self.SP.wait(b.w[0], b.w[1])
        self.barrier()


def pipeline(units, stages):
    n = len(units)
    ns = len(stages)
    for step in range(n + ns - 1):
        for s, st in enumerate(stages):
            i = step - s
            if 0 <= i < n:
                st(units[i], i)


def host_consts():
    c = {}
    s = np.arange(128)[:, None]
    t = np.arange(512)[None, :]
    sbm = np.stack([((128 * o + s) < t) for o in range(4)]).astype(np.float32)
    c["c_mask_lt"] = sbm
    c["c_mask_le"] = np.stack([((128 * o + s) <= t) for o in range(4)]).astype(np.float32)
    a = np.arange(128)
    c["c_tri_ge"] = (a[:, None] >= a[None, :]).astype(np.float32)
    c["c_ones"] = np.ones((128, 128), np.float32)
    c["c_ident"] = np.eye(128, dtype=np.float32)
    c["c_blk64"] = (a[:, None] // 64 == a[None, :] // 64).astype(np.float32)
    half = 8
    inv_freq = (np.float32(500000.0) ** (-np.arange(0, 16, 2, dtype=np.float32) / np.float32(16))).astype(np.float32)
    ang = (np.arange(T, dtype=np.float32)[:, None] * inv_freq[None, :]).astype(np.float32)
    cos, sin = np.cos(ang).astype(np.float32), np.sin(ang).astype(np.float32)
    cosF = np.ones((64, T), np.float32)
    sinF = np.zeros((64, T), np.float32)
    cosF[0:8] = cos.T
    cosF[8:16] = cos.T
    sinF[0:8] = -sin.T
    sinF[8:16] = sin.T
    c["c_cosF"] = np.concatenate([cosF, cosF], 0)
    c["c_sinF"] = np.concatenate([sinF, sinF], 0)
    n = np.arange(16)
    qb = np.arange(16)[:, None]
    past = (n[None, :] < qb)
    own = (n[None, :] == qb)
    BIG = -NEG_BIG
    def rep(m):
        return np.ascontiguousarray(np.broadcast_to(m[None, :, None, :], (128, 16, 4, 16))).astype(np.float32)
    c["c_pastneg"] = rep(np.where(past, 0.0, -1e30))
    c["c_pastbig"] = rep(np.where(past, BIG, 0.0))
    c["c_ownm"] = rep(np.where(own, 0.0, -BIG))
    E = np.zeros((128, 16, 128), np.float32)
    for h in range(2):
        for i in range(16):
            E[64 * h + i, i, :] = 1.0
    c["c_E_b"] = E.astype(ml_dtypes.bfloat16)
    c["c_hgmask"] = ((a[:, None] // 32 == a[None, :] // 32) & (a[:, None] <= a[None, :])).astype(np.float32)
    c["c_scanmask"] = (np.broadcast_to((np.arange(512) % 32 != 0)[None, :], (128, 512))).astype(np.float32).copy()
    c["c_rw_low"] = (a[:, None] > a[None, :]).astype(np.float32)
    c["c_rw_upS"] = (a[:, None] < a[None, :]).astype(np.float32)
    c["c_rw_upI"] = (a[:, None] <= a[None, :]).astype(np.float32)
    c["c_scanmask128"] = (np.broadcast_to((np.arange(512) % 128 != 0)[None, :], (128, 512))).astype(np.float32).copy()
    c["c_mask_le_b"] = c["c_mask_le"].astype(ml_dtypes.bfloat16)
    c["c_mask_lt_b"] = c["c_mask_lt"].astype(ml_dtypes.bfloat16)
    return c


CONST_SHAPES = {
    "c_mask_lt": [4, 128, 512],
    "c_mask_le": [4, 128, 512],
    "c_tri_ge": [128, 128],
    "c_ones": [128, 128],
    "c_ident": [128, 128],
    "c_blk64": [128, 128],
    "c_cosF": [128, 4096],
    "c_sinF": [128, 4096],
    "c_pastneg": [128, 16, 4, 16],
    "c_pastbig": [128, 16, 4, 16],
    "c_ownm": [128, 16, 4, 16],
    "c_E_b": [128, 16, 128],
    "c_hgmask": [128, 128],
    "c_scanmask": [128, 512],
    "c_rw_low": [128, 128],
    "c_rw_upS": [128, 128],
    "c_rw_upI": [128, 128],
    "c_scanmask128": [128, 512],
    "c_mask_le_b": [4, 128, 512],
    "c_mask_lt_b": [4, 128, 512],
}


def const_dtype(k):
    return BF16 if k.endswith("_b") else F32


WEIGHT_SHAPES = {
    "norm1_g": [2, 1024], "w_in": [2, 1024, 7680], "rwkv_mu": [2, 1024], "rwkv_w0": [2, 256],
    "rwkv_w2": [2, 64, 256], "rwkv_a0": [2, 256], "rwkv_a2": [2, 64, 256], "rwkv_g2": [2, 128, 256],
    "rwkv_k_k": [2, 256], "rwkv_k_a": [2, 256], "rwkv_r_k": [2, 4, 64], "rwkv_gn_w": [2, 256],
    "rwkv_gn_b": [2, 256], "rwkv_v0": [1, 256], "rwkv_v1": [1, 256, 32], "rwkv_v2": [1, 32, 256],
    "hgrn_lb_logits": [2, 256], "hgrn_gn_g": [2, 256], "w_branch": [2, 4, 256, 1024],
    "w_out": [2, 1024, 1024], "norm2_g": [2, 1024], "w_ffn_gate": [2, 1024, 2816],
    "w_ffn_up": [2, 1024, 2816], "w_ffn_down": [2, 2816, 1024], "final_g": [1024],
}


class DT:
    def __init__(self, ap):
        self.ap = ap
        self.bufs = {}

    def buf(self, i):
        if i not in self.bufs:
            self.bufs[i] = Buf()
        return self.bufs[i]

    def all(self):
        return list(self.bufs.values())


class Ctx:
    def __init__(self):
        self.dram = {}

    def xbuf(self, dt, i):
        return dt.buf(i)


def load_consts(kb, cx):
    nc = kb.nc
    cx.ones_f = kb.sb("ones_f", [128, 128], F32)
    cx.tri_f = kb.sb("tri_f", [128, 128], F32)
    cx.ident_f = kb.sb("ident_f", [128, 128], F32)
    cx.blk_f = kb.sb("blk_f", [128, 128], F32)
    cx.ones_b = kb.sb("ones_b", [128, 128], BF16)
    cx.ident_b = kb.sb("ident_b", [128, 128], BF16)
    kb.dma(cx.ones_f[:], cx.dram["c_ones"][:, :], Wr=[cx.ones_f])
    kb.dma(cx.tri_f[:], cx.dram["c_tri_ge"][:, :], Wr=[cx.tri_f])
    kb.dma(cx.ident_f[:], cx.dram["c_ident"][:, :], Wr=[cx.ident_f])
    kb.dma(cx.blk_f[:], cx.dram["c_blk64"][:, :], Wr=[cx.blk_f])
    cx.eps_norm = kb.sb("eps_norm", [128, 1], F32)
    cx.one_c = kb.sb("one_c", [128, 1], F32)
    kb.op(kb.POOL, lambda: nc.gpsimd.memset(cx.eps_norm[:], NORM_EPS), Wr=[cx.eps_norm])
    kb.op(kb.POOL, lambda: nc.gpsimd.memset(cx.one_c[:], 1.0), Wr=[cx.one_c])
    kb.op(kb.DVE, lambda: nc.vector.tensor_copy(out=cx.ones_b[:], in_=cx.ones_f[:]), R=[cx.ones_f], Wr=[cx.ones_b])
    kb.op(kb.DVE, lambda: nc.vector.tensor_copy(out=cx.ident_b[:], in_=cx.ident_f[:]), R=[cx.ident_f], Wr=[cx.ident_b])


class WStream:
    def __init__(self, kb, es, name, kch, ncols, nbuf=2, alloc_wb=True):
        self.kb = kb
        self.kch = kch
        self.ncols = ncols
        self.stg = [kb.sb("%s_stg%d" % (name, i), [128, kch, ncols], F32, es) for i in range(nbuf)]
        self.wb = [kb.sb("%s_wb%d" % (name, i), [128, kch, ncols], BF16, es) for i in range(nbuf)] if alloc_wb else []
        self.i = 0

    def load(self, src_ap, eng=None, kch=None, ncols=None):
        kb = self.kb
        nc = kb.nc
        kch = kch or self.kch
        ncols = ncols or self.ncols
        i = self.i
        self.i = (self.i + 1) % len(self.stg)
        stg, wb = self.stg[i], self.wb[i]
        kb.dma(stg[:, :kch, :ncols], src_ap, Wr=[stg])
        eng = eng or kb.DVE
        kb.op(eng, lambda: eng.eng.tensor_copy(out=wb[:, :kch, :ncols], in_=stg[:, :kch, :ncols]), R=[stg], Wr=[wb])
        return wb


def wload(kb, ws, dst, src_ap, eng=None):
    nc = kb.nc
    i = ws.i
    ws.i = (ws.i + 1) % len(ws.stg)
    stg = ws.stg[i]
    kch, ncols = dst.t.shape[1], dst.t.shape[2]
    kb.dma(stg[:, :kch, :ncols], src_ap, Wr=[stg])
    eng = eng or kb.DVE
    kb.op(eng, lambda: eng.eng.tensor_copy(out=dst[:], in_=stg[:, :kch, :ncols]), R=[stg], Wr=[dst])
    return dst


def wcols(w2d, c0, ncols):
    return w2d.rearrange("(kc p) f -> p kc f", p=128)[:, :, c0:c0 + ncols]


def phase_norm(kb, cx, xT, g_ap, hT, out_dram=None, tgw=512, nbuf=2):
    nc = kb.nc
    with ExitStack() as es:
        gT = kb.sb("n_gT", [128, 8], F32, es)
        kb.dma(gT[:], g_ap.rearrange("(kc p) -> p kc", p=128), Wr=[gT])
        xt = [kb.sb("n_x%d" % i, [128, 8, tgw], F32, es) for i in range(nbuf)]
        sq = [kb.sb("n_sq%d" % i, [128, 8, tgw], BF16, es) for i in range(nbuf)]
        rs = [kb.sb("n_rs%d" % i, [128, tgw], F32, es) for i in range(nbuf)]
        ob = [kb.sb("n_ob%d" % i, [128, 8, tgw], F32, es) for i in range(nbuf)] if out_dram is not None else None
        pss = [kb.ps("n_ps%d" % i, [128, tgw], F32, es) for i in range(nbuf)]
        xv = xT.ap.rearrange("(kc p) t -> p kc t", p=128)
        for tg in range(T // tgw):
            i = tg % nbuf
            x_, sq_, rs_, ps_ = xt[i], sq[i], rs[i], pss[i]
            kb.dma(x_[:], xv[:, :, tg * tgw:(tg + 1) * tgw], R=[cx.xbuf(xT, tg)], Wr=[x_])
            kb.op(kb.ACT, lambda: nc.scalar.activation(out=sq_[:], in_=x_[:], func=AF.Square), R=[x_], Wr=[sq_])
            for kc in range(8):
                kb.op(kb.PE, lambda: nc.tensor.matmul(ps_[:], lhsT=cx.ones_b[:], rhs=sq_[:, kc, :],
                                                      start=(kc == 0), stop=(kc == 7)),
                      R=[cx.ones_b, sq_], Wr=[ps_])
            kb.op(kb.ACT, lambda: nc.scalar.activation(out=rs_[:], in_=ps_[:], func=AF.Sqrt, scale=1.0 / D,
                                                       bias=cx.eps_norm[:, 0:1]),
                  R=[ps_, cx.eps_norm], Wr=[rs_])
            kb.op(kb.DVE, lambda: nc.vector.reciprocal(out=rs_[:], in_=rs_[:]), R=[rs_], Wr=[rs_])
            for kc in range(8):
                if out_dram is None:
                    o_ap = hT[:, kc, tg * tgw:(tg + 1) * tgw]
                    wr = [hT]
                else:
                    o_ap = ob[i][:, kc, :]
                    wr = [ob[i]]
                kb.op(kb.DVE, lambda: nc.vector.scalar_tensor_tensor(out=o_ap, in0=x_[:, kc, :], scalar=gT[:, kc:kc + 1],
                                                                     in1=rs_[:], op0=ALU.mult, op1=ALU.mult),
                      R=[x_, gT, rs_], Wr=wr)
            if out_dram is not None:
                kb.dma(out_dram.ap.rearrange("(kc p) t -> p kc t", p=128)[:, :, tg * tgw:(tg + 1) * tgw], ob[i][:],
                       R=[ob[i]], Wr=[cx.xbuf(out_dram, tg)])
        kb.barrier()


def phase_sb(kb, cx, layer, hT, yT, dbg=None, defer_es=None, pre=None, alloc_only=False):
    nc = kb.nc
    w2d = cx.dram["w_in"][layer] if not alloc_only else None
    with ExitStack() as es_own:
        es = defer_es if defer_es is not None else es_own

        def P(name, shape, dt):
            if pre is not None and name in pre:
                return pre[name]
            t_ = kb.sb(name, shape, dt, es)
            if pre is not None:
                pre[name] = t_
            return t_
        QT = P("sb_QT", [128, 2, T], BF16)
        KT = P("sb_KT", [128, 2, T], BF16)
        V = P("sb_V", [128, 32, 256], BF16)
        if alloc_only:
            return None
        if dbg == "p0":
            kb.dma(yT.ap[0:128, 0:2048], mlt_b[:].rearrange("p o t -> p (o t)"), R=[mlt_b], Wr=[cx.xbuf(yT, 0)])
            return
        with ExitStack() as es2:
            ws = WStream(kb, es2, "sb_w", 8, 256, nbuf=(1 if defer_es is not None else 2))
            if dbg == "p1":
                wb = ws.load(wcols(w2d, SB_OFF, 256))
                kb.dma(yT.ap[0:128, 0:2048], wb[:].rearrange("p o t -> p (o t)"), R=[wb], Wr=[cx.xbuf(yT, 0)])
                return
            pp = [kb.ps("sb_pp%d" % i, [128, 512], F32, es2) for i in range(2)]
            n = 0
            for which in range(2 if dbg != "p3" else 0):
                wb = ws.load(wcols(w2d, SB_OFF + which * 256, 256))
                for fb in range(2):
                    for tg in range(8):
                        ps_ = pp[n % 2]
                        n += 1
                        for kc in range(8):
                            kb.op(kb.PE, lambda: nc.tensor.matmul(ps_[:], lhsT=wb[:, kc, fb * 128:(fb + 1) * 128],
                                                                  rhs=hT[:, kc, tg * 512:(tg + 1) * 512],
                                                                  start=(kc == 0), stop=(kc == 7)),
                                  R=[wb, hT], Wr=[ps_])
                        sl = slice(tg * 512, (tg + 1) * 512)
                        if which == 0:
                            kb.op(kb.ACT, lambda: nc.scalar.activation(out=QT[:, fb, sl], in_=ps_[:], func=AF.Copy, scale=0.125),
                                  R=[ps_], Wr=[QT])
                        else:
                            kb.op(kb.ACT, lambda: nc.scalar.activation(out=KT[:, fb, sl], in_=ps_[:], func=AF.Copy),
                                  R=[ps_], Wr=[KT])
            wb = ws.load(wcols(w2d, SB_OFF + 512, 256))
            for tt in range(32 if dbg not in ("p2", "p2a") else 0):
                ps_ = pp[n % 2]
                n += 1
                for kc in range(8):
                    kb.op(kb.PE, lambda: nc.tensor.matmul(ps_[:, 0:256], lhsT=hT[:, kc, tt * 128:(tt + 1) * 128],
                                                          rhs=wb[:, kc, :], start=(kc == 0), stop=(kc == 7)),
                          R=[wb, hT], Wr=[ps_])
                kb.op(kb.ACT, lambda: nc.scalar.activation(out=V[:, tt, :], in_=ps_[:, 0:256], func=AF.Copy), R=[ps_], Wr=[V])
            kb.barrier()
        if dbg == "p3":
            kb.dma(yT.ap[0:128, :], V[:, 0:16, :].rearrange("p o t -> p (o t)"), R=[V], Wr=[cx.xbuf(yT, 0)])
            return
        if dbg in ("proj", "p2", "p2a"):
            for b2 in range(2):
                kb.dma(yT.ap[b2 * 128:(b2 + 1) * 128, :], KT[:, b2, :], R=[KT], Wr=[cx.xbuf(yT, 0)])
            return
        def attn(es2):
            NB = 3
            mlt_b = kb.sb("sb_mltb", [128, 4, 512], BF16, es2)
            kb.dma(mlt_b[:], cx.dram["c_mask_lt_b"].rearrange("o s t -> s o t"), Wr=[mlt_b])
            pz = [kb.ps("sb_pz%d" % i, [128, 512], F32, es2) for i in range(2)]
            pc = [kb.ps("sb_pc%d" % i, [128, 512], F32, es2) for i in range(2)]
            po = [kb.ps("sb_po%d" % i, [64, 512], F32, es2) for i in range(1 if defer_es is not None else 2)]
            ee = [kb.sb("sb_e%d" % i, [128, 512], F32, es2) for i in range(NB)]
            lk = [kb.sb("sb_lk%d" % i, [128, 512], BF16, es2) for i in range(NB)]
            arb = [kb.sb("sb_arb%d" % i, [128, 512], BF16, es2) for i in range(2)]
            tri_b = kb.sb("sb_trib", [128, 128], BF16, es2)
            kb.op(kb.DVE, lambda: nc.vector.tensor_copy(out=tri_b[:], in_=cx.tri_f[:]), R=[cx.tri_f], Wr=[tri_b])
            at = [kb.sb("sb_at%d" % i, [128, 512], BF16, es2) for i in range(NB)]
            arun = [kb.sb("sb_ar%d" % i, [128, 512], F32, es2) for i in range(2)]
            yst = [kb.sb("sb_y%d" % i, [64, 512], BF16, es2) for i in range(2)]
            units = []
            gi = 0
            for h in range(NH):
                for g in range(8):
                    for j in range(4 * g + 3, -1, -1):
                        units.append((h, g, j, gi))
                    gi += 1

            def stA(u, i):
                h, g, j, gi = u
                blk, po_ = h // 2, (h % 2) * 64
                pz_, e_, lk_ = pz[i % 2], ee[i % NB], lk[i % NB]
                kb.op(kb.PE, lambda: nc.tensor.matmul(pz_[:], lhsT=KT[po_:po_ + 64, blk, j * 128:(j + 1) * 128],
                                                      rhs=QT[po_:po_ + 64, blk, g * 512:(g + 1) * 512], start=True, stop=True),
                      R=[KT, QT], Wr=[pz_])
                kb.op(kb.ACT, lambda: nc.scalar.activation(out=e_[:], in_=pz_[:], func=AF.Exp), R=[pz_], Wr=[e_])
                kb.op(kb.ACT, lambda: nc.scalar.activation(out=lk_[:], in_=e_[:], func=AF.Ln, bias=cx.one_c[:, 0:1]),
                      R=[e_, cx.one_c], Wr=[lk_])
                o = j - 4 * g
                if o >= 0:
                    kb.op(kb.POOL, lambda: nc.gpsimd.tensor_tensor(out=lk_[:], in0=lk_[:], in1=mlt_b[:, o, :], op=ALU.mult),
                          R=[lk_, mlt_b], Wr=[lk_])

            def stB(u, i):
                h, g, j, gi = u
                blk, po_ = h // 2, (h % 2) * 64
                pc_, lk_, at_, ar_, arb_ = pc[i % 2], lk[i % NB], at[i % NB], arun[gi % 2], arb[i % 2]
                first = (j == 4 * g + 3)
                kb.op(kb.PE, lambda: nc.tensor.matmul(pc_[:], lhsT=tri_b[:], rhs=lk_[:], start=True, stop=first),
                      R=[tri_b, lk_], Wr=[pc_])
                if not first:
                    kb.op(kb.PE, lambda: nc.tensor.matmul(pc_[:], lhsT=cx.ones_b[:], rhs=arb_[:], start=False, stop=True),
                          R=[cx.ones_b, arb_], Wr=[pc_])
                e_ = ee[i % NB]
                kb.op(kb.ACT, lambda: nc.scalar.activation(out=at_[:], in_=pc_[:], func=AF.Exp, scale=-1.0), R=[pc_], Wr=[at_])
                kb.op(kb.POOL, lambda: nc.gpsimd.tensor_tensor(out=at_[:], in0=at_[:], in1=e_[:], op=ALU.mult), R=[at_, e_], Wr=[at_])
                o = j - 4 * g
                if o >= 0:
                    kb.op(kb.POOL, lambda: nc.gpsimd.tensor_tensor(out=at_[:], in0=at_[:], in1=mlt_b[:, o, :], op=ALU.mult),
                          R=[at_, mlt_b], Wr=[at_])
                if j > 0:
                    arn_ = arb[(i + 1) % 2]
                    if first:
                        kb.op(kb.DVE, lambda: nc.vector.tensor_copy(out=arn_[:], in_=lk_[:]), R=[lk_], Wr=[arn_])
                        if j > 1:
                            kb.op(kb.DVE, lambda: nc.vector.tensor_copy(out=ar_[:], in_=lk_[:]), R=[lk_], Wr=[ar_])
                    else:
                        kb.op(kb.DVE, lambda: nc.vector.tensor_tensor(out=arn_[:], in0=ar_[:], in1=lk_[:], op=ALU.add),
                              R=[lk_, ar_], Wr=[arn_])
                        if j > 1:
                            kb.op(kb.DVE, lambda: nc.vector.tensor_tensor(out=ar_[:], in0=ar_[:], in1=lk_[:], op=ALU.add),
                                  R=[lk_, ar_], Wr=[ar_])

            def stC(u, i):
                h, g, j, gi = u
                at_, po_t = at[i % NB], po[gi % len(po)]
                first = (j == 4 * g + 3)
                kb.op(kb.PE, lambda: nc.tensor.matmul(po_t[:], lhsT=V[:, j, h * 64:(h + 1) * 64], rhs=at_[:],
                                                      start=first, stop=(j == 0)),
                      R=[V, at_], Wr=[po_t])
                if j == 0:
                    y_ = yst[gi % 2]
                    kb.op(kb.DVE, lambda: nc.vector.tensor_copy(out=y_[:], in_=po_t[:]), R=[po_t], Wr=[y_])
                    kb.dma(yT.ap[h * 64:(h + 1) * 64, g * 512:(g + 1) * 512], y_[:], R=[y_], Wr=[cx.xbuf(yT, g)])

            if dbg is not None and dbg.startswith("n"):
                units = units[:int(dbg[1:])]
            return units, [stA, stB, stC]

        if defer_es is not None:
            return attn
        with ExitStack() as es2:
            units, stages = attn(es2)
            pipeline(units, stages)
            kb.barrier()


def proj_fm(kb, hT, wb, ncols, pp, cnt, epi):
    nc = kb.nc
    for fb in range(ncols // 128):
        for tg in range(T // 512):
            ps_ = pp[cnt[0] % len(pp)]
            cnt[0] += 1
            for kc in range(8):
                kb.op(kb.PE, lambda: nc.tensor.matmul(ps_[:], lhsT=wb[:, kc, fb * 128:(fb + 1) * 128],
                                                      rhs=hT[:, kc, tg * 512:(tg + 1) * 512],
                                                      start=(kc == 0), stop=(kc == 7)),
                      R=[wb, hT], Wr=[ps_])
            epi(fb, tg, ps_)


def proj_tm(kb, hT, wb, ncols, pp, cnt, epi):
    nc = kb.nc
    for tt in range(T // 128):
        ps_ = pp[cnt[0] % len(pp)]
        cnt[0] += 1
        for kc in range(8):
            kb.op(kb.PE, lambda: nc.tensor.matmul(ps_[:, 0:ncols], lhsT=hT[:, kc, tt * 128:(tt + 1) * 128],
                                                  rhs=wb[:, kc, 0:ncols], start=(kc == 0), stop=(kc == 7)),
                  R=[wb, hT], Wr=[ps_])
        epi(tt, ps_)


def phase_moba(kb, cx, layer, hT, yT, dbg=None, defer_es=None, pre=None, alloc_only=False):
    nc = kb.nc
    w2d = cx.dram["w_in"][layer] if not alloc_only else None
    with ExitStack() as es_own:
        es = defer_es if defer_es is not None else es_own

        def P(name, shape, dt):
            if pre is not None and name in pre:
                return pre[name]
            t_ = kb.sb(name, shape, dt, es)
            if pre is not None:
                pre[name] = t_
            return t_
        QT = P("mb_QT", [128, 2, T], BF16)
        KT = P("mb_KT", [128, 2, T], BF16)
        V = P("mb_V", [128, 32, 4, 65], BF16)
        esel = P("mb_esel", [128, 64], F32)
        selbT = P("mb_selbT", [128, 2, T], BF16)
        E_b = P("mb_Eb", [128, 16, 128], BF16)
        kmT = P("mb_kmT", [128, 2, 16], BF16)
        if alloc_only:
            return None
        kb.op(kb.POOL, lambda: nc.gpsimd.memset(V[:, :, :, 64:65], 1.0), Wr=[V])
        kb.op(kb.POOL, lambda: nc.gpsimd.memset(esel[:], 0.0), Wr=[esel])
        kb.op(kb.POOL, lambda: nc.gpsimd.memset(esel[64:65, :], 1.0), Wr=[esel])
        kb.op(kb.POOL, lambda: nc.gpsimd.memset(selbT[:], 0.0), Wr=[selbT])
        kb.dma(E_b[:], cx.dram["c_E_b"][:, :, :], Wr=[E_b])
        with ExitStack() as es2:
            nb_ = 1 if defer_es is not None else 2
            cosS = [kb.sb("mb_cos%d" % i, [128, 512], F32, es2) for i in range(nb_)]
            sinS = [kb.sb("mb_sin%d" % i, [128, 512], F32, es2) for i in range(nb_)]
            ws = WStream(kb, es2, "mb_w", 8, 256, nbuf=(1 if defer_es is not None else 2))
            wp = kb.sb("mb_wp", [128, 8, 256], BF16, es2)
            t1 = [kb.sb("mb_t1%d" % i, [128, 512], F32, es2) for i in range(nb_)]
            t2 = [kb.sb("mb_t2%d" % i, [128, 512], F32, es2) for i in range(nb_)]
            pp = [kb.ps("mb_pp%d" % i, [128, 512], F32, es2) for i in range(2)]
            pq = [kb.ps("mb_pq%d" % i, [128, 512], F32, es2) for i in range(2)]
            cnt = [0]
            cnt2 = [0]
            for which in range(2):
                wb = ws.load(wcols(w2d, MOBA_OFF + which * 256, 256))
                wb4 = wb[:].rearrange("p k (h d) -> p k h d", h=4)
                wp4 = wp[:].rearrange("p k (h d) -> p k h d", h=4)
                kb.op(kb.POOL, lambda: nc.gpsimd.tensor_copy(out=wp4[:, :, :, 0:8], in_=wb4[:, :, :, 8:16]), R=[wb], Wr=[wp])
                kb.op(kb.POOL, lambda: nc.gpsimd.tensor_copy(out=wp4[:, :, :, 8:16], in_=wb4[:, :, :, 0:8]), R=[wb], Wr=[wp])
                kb.op(kb.POOL, lambda: nc.gpsimd.tensor_copy(out=wp4[:, :, :, 16:64], in_=wb4[:, :, :, 16:64]), R=[wb], Wr=[wp])
                dst = QT if which == 0 else KT
                sc = 0.125 if which == 0 else 1.0
                for fb in range(2):
                    for tg in range(8):
                        sl = slice(tg * 512, (tg + 1) * 512)
                        pa = pp[cnt[0] % 2]
                        pb = pq[cnt[0] % 2]
                        a1, a2 = t1[cnt[0] % nb_], t2[cnt[0] % nb_]
                        cosF, sinF = cosS[cnt[0] % nb_], sinS[cnt[0] % nb_]
                        cnt[0] += 1
                        kb.dma(cosF[:], cx.dram["c_cosF"][:, sl], Wr=[cosF])
                        kb.dma(sinF[:], cx.dram["c_sinF"][:, sl], Wr=[sinF])
                        for kc in range(8):
                            kb.op(kb.PE, lambda: nc.tensor.matmul(pa[:], lhsT=wb[:, kc, fb * 128:(fb + 1) * 128], rhs=hT[:, kc, sl],
                                                                  start=(kc == 0), stop=(kc == 7)), R=[wb, hT], Wr=[pa])
                        for kc in range(8):
                            kb.op(kb.PE, lambda: nc.tensor.matmul(pb[:], lhsT=wp[:, kc, fb * 128:(fb + 1) * 128], rhs=hT[:, kc, sl],
                                                                  start=(kc == 0), stop=(kc == 7)), R=[wp, hT], Wr=[pb])
                        kb.op(kb.DVE, lambda: nc.vector.tensor_tensor(out=a1[:], in0=pa[:], in1=cosF[:], op=ALU.mult),
                              R=[pa, cosF], Wr=[a1])
                        kb.op(kb.DVE, lambda: nc.vector.tensor_tensor(out=a2[:], in0=pb[:], in1=sinF[:], op=ALU.mult),
                              R=[pb, sinF], Wr=[a2])
                        if sc == 1.0:
                            kb.op(kb.POOL, lambda: nc.gpsimd.tensor_tensor(out=dst[:, fb, sl], in0=a1[:], in1=a2[:], op=ALU.add),
                                  R=[a1, a2], Wr=[dst])
                        else:
                            kb.op(kb.POOL, lambda: nc.gpsimd.tensor_tensor(out=a1[:], in0=a1[:], in1=a2[:], op=ALU.add),
                                  R=[a1, a2], Wr=[a1])
                        if sc != 1.0:
                            kb.op(kb.ACT, lambda: nc.scalar.activation(out=dst[:, fb, sl], in_=a1[:], func=AF.Copy, scale=sc),
                                  R=[a1], Wr=[dst])
            wb = ws.load(wcols(w2d, MOBA_OFF + 512, 256))
            proj_tm(kb, hT, wb, 256, pp, cnt2,
                    lambda tt, ps_: kb.op(kb.ACT, lambda: nc.scalar.activation(out=V[:, tt, :, 0:64],
                                                                               in_=ps_[:, 0:256].rearrange("p (h d) -> p h d", h=4), func=AF.Copy),
                                          R=[ps_], Wr=[V]))
            kb.barrier()
        if dbg == "mproj":
            for b2 in range(2):
                kb.dma(yT.ap[b2 * 128:(b2 + 1) * 128, :], KT[:, b2, :], R=[KT], Wr=[cx.xbuf(yT, 0)])
            return
        with ExitStack() as es2:
            kmf = kb.sb("mb_kmf", [128, 2, 16], F32, es2)
            cpn = kb.sb("mb_cpn", [128, 16, 4, 16], F32, es2)
            cpb = kb.sb("mb_cpb", [128, 16, 4, 16], F32, es2)
            com = kb.sb("mb_com", [128, 16, 4, 16], F32, es2)
            kb.dma(cpn[:], cx.dram["c_pastneg"][:, :, :, :], Wr=[cpn])
            kb.dma(cpb[:], cx.dram["c_pastbig"][:, :, :, :], Wr=[cpb])
            kb.dma(com[:], cx.dram["c_ownm"][:, :, :, :], Wr=[com])
            for b2 in range(2):
                kb.op(kb.DVE, lambda: nc.vector.tensor_reduce(out=kmf[:, b2, :], in_=KT[:, b2, :].rearrange("p (n s) -> p n s", s=256),
                                                              axis=AX.X, op=ALU.add), R=[KT], Wr=[kmf])
            kb.op(kb.DVE, lambda: nc.vector.tensor_scalar(out=kmT[:], in0=kmf[:], scalar1=1.0 / 256, scalar2=None, op0=ALU.mult),
                  R=[kmf], Wr=[kmT])
            pg = [kb.ps("mb_pg%d" % i, [128, 512], F32, es2) for i in range(4)]
            pt = [kb.ps("mb_pt%d" % i, [128, 2, 128], BF16, es2) for i in range(2)]

            gm = [kb.sb("mb_gm%d" % i, [128, 4, 16], F32, es2) for i in range(2)]
            m8 = [kb.sb("mb_m8%d" % i, [128, 4, 8], F32, es2) for i in range(2)]
            sel = [kb.sb("mb_sel%d" % i, [128, 4, 16], F32, es2) for i in range(2)]
            selb = [kb.sb("mb_selb%d" % i, [128, 4, 16], BF16, es2) for i in range(2)]
            lvl = int(dbg[3:]) if (dbg or "").startswith("sel") and len(dbg) > 3 else 99
            for tt in range(32 if lvl > 0 else 0):
                i = tt % 2
                qb = tt // 2
                pt_, gm_, m8_, sel_, selb_ = pt[i], gm[i], m8[i], sel[i], selb[i]
                for r in range(2):
                    pg_ = pg[2 * i + r]
                    for q in range(2):
                        kb.op(kb.PE, lambda: nc.tensor.matmul(pg_[:, q * 16:(q + 1) * 16], lhsT=QT[r * 64:r * 64 + 64, q, tt * 128:(tt + 1) * 128],
                                                              rhs=kmT[r * 64:r * 64 + 64, q, :], start=True, stop=True),
                              R=[QT, kmT], Wr=[pg_])
                    kb.op(kb.DVE, lambda: nc.vector.tensor_tensor(out=gm_[:, 2 * r:2 * r + 2, :],
                                                                  in0=pg_[:, 0:32].rearrange("p (q n) -> p q n", q=2),
                                                                  in1=cpn[:, qb, 2 * r:2 * r + 2, :], op=ALU.add),
                          R=[pg_, cpn], Wr=[gm_])
                if lvl < 2:
                    continue
                for h in range(4):
                    kb.op(kb.DVE, lambda: nc.vector.max(out=m8_[:, h, :], in_=gm_[:, h, :]), R=[gm_], Wr=[m8_])
                if lvl < 3:
                    continue
                for h in range(4):
                    kb.op(kb.DVE, lambda: nc.vector.tensor_scalar(out=sel_[:, h, :], in0=gm_[:, h, :], scalar1=m8_[:, h, 2:3], scalar2=None,
                                                                  op0=ALU.is_ge), R=[gm_, m8_], Wr=[sel_])
                kb.op(kb.DVE, lambda: nc.vector.tensor_tensor(out=sel_[:], in0=sel_[:], in1=cpb[:, qb, :, :], op=ALU.mult),
                      R=[sel_, cpb], Wr=[sel_])
                kb.op(kb.DVE, lambda: nc.vector.tensor_tensor(out=selb_[:], in0=sel_[:], in1=com[:, qb, :, :], op=ALU.add),
                      R=[sel_, com], Wr=[selb_])
                if lvl < 4:
                    continue
                for h in range(4):
                    hh = (h % 2) * 2 + h // 2
                    kb.op(kb.PE, lambda: nc.tensor.transpose(pt_[64 * (h % 2):64 * (h % 2) + 16, h // 2, :], selb_[:, hh, :], cx.ident_b[:]),
                          R=[selb_, cx.ident_b], Wr=[pt_])
                if lvl < 5:
                    continue
                for pb_ in (0, 64):
                    kb.op(kb.ACT, lambda: nc.scalar.activation(out=selbT[pb_:pb_ + 16, :, tt * 128:(tt + 1) * 128],
                                                               in_=pt_[pb_:pb_ + 16, :, :], func=AF.Copy),
                          R=[pt_], Wr=[selbT])
            kb.barrier()
        if (dbg or "").startswith("sel"):
            kb.dma(yT.ap[0:128, :], selbT[:, 0, :], R=[selbT], Wr=[cx.xbuf(yT, 0)])
            kb.dma(yT.ap[128:256, :], selbT[:, 1, :], R=[selbT], Wr=[cx.xbuf(yT, 0)])
            return
        def attn(es2):
            NB = 3
            mle_b = kb.sb("mb_mleb", [128, 4, 512], BF16, es2)
            kb.dma(mle_b[:], cx.dram["c_mask_le_b"].rearrange("o s t -> s o t"), Wr=[mle_b])
            pz = [kb.ps("mb_pz%d" % i, [128, 512], F32, es2) for i in range(2)]
            po = [kb.ps("mb_po%d" % i, [128, 512], F32, es2) for i in range(1 if defer_es is not None else 2)]
            pd = [kb.ps("mb_pd%d" % i, [64, 512], F32, es2) for i in range(2)] if defer_es is None else None
            pT = [kb.sb("mb_pT%d" % i, [128, 512], BF16, es2) for i in range(NB)]
            osb = [kb.sb("mb_osb%d" % i, [128, 512], F32, es2) for i in range(2)]
            rec = [kb.sb("mb_rec%d" % i, [64, 512], F32, es2) for i in range(2)]
            yst = [kb.sb("mb_y%d" % i, [64, 512], BF16, es2) for i in range(2)]
            units = []
            gi = 0
            for h in range(NH):
                for g in range(8):
                    for j in range(4 * g + 4):
                        units.append((h, g, j, gi))
                    gi += 1
            if dbg is not None and dbg.startswith("n"):
                units = units[:int(dbg[1:])]

            def stA(u, i):
                h, g, j, gi = u
                blk, po_ = h // 2, (h % 2) * 64
                pz_, p_ = pz[i % 2], pT[i % NB]
                kb.op(kb.PE, lambda: nc.tensor.matmul(pz_[:], lhsT=KT[po_:po_ + 64, blk, j * 128:(j + 1) * 128],
                                                      rhs=QT[po_:po_ + 64, blk, g * 512:(g + 1) * 512], start=True, stop=False),
                      R=[KT, QT], Wr=[pz_])
                kb.op(kb.PE, lambda: nc.tensor.matmul(pz_[:], lhsT=E_b[po_:po_ + 64, j // 2, :],
                                                      rhs=selbT[po_:po_ + 64, h // 2, g * 512:(g + 1) * 512],
                                                      start=False, stop=True), R=[E_b, selbT], Wr=[pz_])
                kb.op(kb.ACT, lambda: nc.scalar.activation(out=p_[:], in_=pz_[:], func=AF.Exp), R=[pz_], Wr=[p_])
                o = j - 4 * g
                if o >= 0:
                    kb.op(kb.POOL, lambda: nc.gpsimd.tensor_tensor(out=p_[:], in0=p_[:], in1=mle_b[:, o, :], op=ALU.mult),
                          R=[p_, mle_b], Wr=[p_])

            def stB(u, i):
                h, g, j, gi = u
                p_, po_t = pT[i % NB], po[gi % len(po)]
                pd_t = pd[gi % 2] if pd is not None else pz[i % 2]
                first, last = (j == 0), (j == 4 * g + 3)
                kb.op(kb.PE, lambda: nc.tensor.matmul(po_t[0:65, :], lhsT=V[:, j, h, :], rhs=p_[:], start=first, stop=last),
                      R=[V, p_], Wr=[po_t])
                if last:
                    r_, y_, o_ = rec[gi % 2], yst[gi % 2], osb[gi % 2]
                    kb.op(kb.ACT, lambda: nc.scalar.activation(out=o_[0:65, :], in_=po_t[0:65, :], func=AF.Copy), R=[po_t], Wr=[o_])
                    kb.op(kb.PE, lambda: nc.tensor.matmul(pd_t[0:64, :], lhsT=esel[0:65, :], rhs=o_[0:65, :], start=True, stop=True),
                          R=[esel, o_], Wr=[pd_t])
                    kb.op(kb.DVE, lambda: nc.vector.reciprocal(out=r_[:], in_=pd_t[0:64, :]), R=[pd_t], Wr=[r_])
                    kb.op(kb.DVE, lambda: nc.vector.tensor_tensor(out=y_[:], in0=o_[0:64, :], in1=r_[:], op=ALU.mult), R=[o_, r_], Wr=[y_])
                    kb.dma(yT.ap[h * 64:(h + 1) * 64, g * 512:(g + 1) * 512], y_[:], R=[y_], Wr=[cx.xbuf(yT, g)])

            return units, [stA, stB]

        if defer_es is not None:
            return attn
        with ExitStack() as es2:
            units, stages = attn(es2)
            pipeline(units, stages)
            kb.barrier()


def phase_hgrn(kb, cx, layer, hT, yT, dbg=None):
    nc = kb.nc
    w2d = cx.dram["w_in"][layer]
    with ExitStack() as es:
        qd = kb.sb("hg_qd", [128, 2, T], BF16, es)
        kt = kb.sb("hg_kt", [128, 2, T], BF16, es)
        ke = kb.sb("hg_ke", [128, 2, T], BF16, es)
        sg = kb.sb("hg_sg", [128, 2, T], BF16, es)
        iv = kb.sb("hg_iv", [128, 32, 256], BF16, es)
        dec = kb.sb("hg_dec", [128, 2, 128], F32, es)
        lbl = kb.sb("hg_lbl", [128, 2, 2], F32, es)
        lb = kb.sb("hg_lb", [128, 2], F32, es)
        oml = kb.sb("hg_oml", [128, 2], F32, es)
        noml = kb.sb("hg_noml", [128, 2], F32, es)
        gng = kb.sb("hg_gng", [128, 2], F32, es)
        hgm = kb.sb("hg_mask", [128, 128], F32, es)
        scm = kb.sb("hg_scm", [128, 512], F32, es)
        eps_c = kb.sb("hg_eps", [128, 1], F32, es)
        kb.op(kb.POOL, lambda: nc.gpsimd.memset(eps_c[:], NORM_EPS), Wr=[eps_c])
        kb.dma(hgm[:], cx.dram["c_hgmask"][:, :], Wr=[hgm])
        kb.dma(scm[:], cx.dram["c_scanmask"][:, :], Wr=[scm])
        kb.dma(lbl[:], cx.dram["hgrn_lb_logits"].rearrange("l (b p) -> p l b", p=128), Wr=[lbl])
        kb.dma(gng[:], cx.dram["hgrn_gn_g"][layer].rearrange("(b p) -> p b", p=128), Wr=[gng])
        if layer == 0:
            kb.op(kb.DVE, lambda: nc.vector.memset(lb[:], 0.0), Wr=[lb])
        else:
            kb.op(kb.DVE, lambda: nc.vector.tensor_tensor(out=lb[:], in0=lbl[:, 1, :], in1=lbl[:, 0, :], op=ALU.subtract),
                  R=[lbl], Wr=[lb])
            kb.op(kb.ACT, lambda: nc.scalar.activation(out=lb[:], in_=lb[:], func=AF.Sigmoid), R=[lb], Wr=[lb])
        kb.op(kb.DVE, lambda: nc.vector.tensor_scalar(out=oml[:], in0=lb[:], scalar1=-1.0, scalar2=1.0, op0=ALU.mult, op1=ALU.add),
              R=[lb], Wr=[oml])
        kb.op(kb.DVE, lambda: nc.vector.tensor_scalar(out=noml[:], in0=oml[:], scalar1=-1.0, scalar2=None, op0=ALU.mult),
              R=[oml], Wr=[noml])
        with ExitStack() as es2:
            ws = WStream(kb, es2, "hg_w", 8, 256, nbuf=2, alloc_wb=False)
            wq = kb.sb("hg_wq", [128, 8, 256], BF16, es2)
            wf = kb.sb("hg_wf", [128, 8, 256], BF16, es2)
            wg = kb.sb("hg_wg", [128, 8, 256], BF16, es2)
            wload(kb, ws, wq, wcols(w2d, HGRN_OFF, 256))
            wload(kb, ws, wf, wcols(w2d, HGRN_OFF + 256, 256))
            wload(kb, ws, wg, wcols(w2d, HGRN_OFF + 768, 256))
            pq = [kb.ps("hg_pq%d" % i, [128, 512], F32, es2) for i in range(2)]
            pf = [kb.ps("hg_pf%d" % i, [128, 512], F32, es2) for i in range(2)]
            pg = [kb.ps("hg_pg%d" % i, [128, 512], F32, es2) for i in range(2)]
            NW = 2
            sig = [kb.sb("hg_sig%d" % i, [128, 512], F32, es2) for i in range(NW)]
            fg = [kb.sb("hg_fg%d" % i, [128, 512], F32, es2) for i in range(NW)]
            key = [kb.sb("hg_key%d" % i, [128, 512], F32, es2) for i in range(NW)]
            bb = [kb.sb("hg_bb%d" % i, [128, 512], F32, es2) for i in range(NW)]
            eb = [kb.sb("hg_eb%d" % i, [128, 512], F32, es2) for i in range(NW)]
            enb = [kb.sb("hg_enb%d" % i, [128, 512], F32, es2) for i in range(NW)]
            dd = [kb.sb("hg_dd%d" % i, [128, 512], F32, es2) for i in range(NW)]
            n = 0
            for b2 in range(2):
                for tg in range(8):
                    i = n % 2
                    n += 1
                    sl = slice(tg * 512, (tg + 1) * 512)
                    fsl = slice(b2 * 128, (b2 + 1) * 128)
                    for (wt_, ps_) in ((wq, pq[i]), (wf, pf[i]), (wg, pg[i])):
                        for kc in range(8):
                            kb.op(kb.PE, lambda: nc.tensor.matmul(ps_[:], lhsT=wt_[:, kc, fsl], rhs=hT[:, kc, sl],
                                                                  start=(kc == 0), stop=(kc == 7)), R=[wt_, hT], Wr=[ps_])
                    sig_, fg_, key_, bb_, eb_, enb_, dd_ = sig[i], fg[i], key[i], bb[i], eb[i], enb[i], dd[i]
                    kb.op(kb.ACT, lambda: nc.scalar.activation(out=sig_[:], in_=pf[i][:], func=AF.Sigmoid), R=[pf[i]], Wr=[sig_])
                    kb.op(kb.ACT, lambda: nc.scalar.activation(out=sg[:, b2, sl], in_=pg[i][:], func=AF.Silu), R=[pg[i]], Wr=[sg])
                    kb.op(kb.DVE, lambda: nc.vector.tensor_scalar(out=fg_[:], in0=sig_[:], scalar1=oml[:, b2:b2 + 1], scalar2=lb[:, b2:b2 + 1],
                                                                  op0=ALU.mult, op1=ALU.add), R=[sig_, oml, lb], Wr=[fg_])
                    kb.op(kb.ACT, lambda: nc.scalar.activation(out=fg_[:], in_=fg_[:], func=AF.Ln), R=[fg_], Wr=[fg_])
                    kb.op(kb.DVE, lambda: nc.vector.tensor_scalar(out=key_[:], in0=sig_[:], scalar1=-1.0, scalar2=noml[:, b2:b2 + 1],
                                                                  op0=ALU.add, op1=ALU.mult), R=[sig_, noml], Wr=[key_])
                    kb.op(kb.DVE, lambda: nc.vector.tensor_tensor_scan(out=bb_[:], data0=scm[:], data1=fg_[:], initial=0.0,
                                                                       op0=ALU.mult, op1=ALU.add), R=[scm, fg_], Wr=[bb_])
                    kb.op(kb.ACT, lambda: nc.scalar.activation(out=eb_[:], in_=bb_[:], func=AF.Exp), R=[bb_], Wr=[eb_])
                    kb.op(kb.ACT, lambda: nc.scalar.activation(out=enb_[:], in_=bb_[:], func=AF.Exp, scale=-1.0), R=[bb_], Wr=[enb_])
                    b3 = bb_[:].rearrange("p (n c) -> p n c", c=32)
                    kb.op(kb.DVE, lambda: nc.vector.tensor_tensor(out=dd_[:].rearrange("p (n c) -> p n c", c=32),
                                                                  in0=b3[:, :, 31:32].to_broadcast([128, 16, 32]), in1=b3, op=ALU.subtract),
                          R=[bb_], Wr=[dd_])
                    kb.op(kb.ACT, lambda: nc.scalar.activation(out=dd_[:], in_=dd_[:], func=AF.Exp), R=[dd_], Wr=[dd_])
                    kb.op(kb.DVE, lambda: nc.vector.tensor_tensor(out=qd[:, b2, sl], in0=pq[i][:], in1=eb_[:], op=ALU.mult),
                          R=[pq[i], eb_], Wr=[qd])
                    kb.op(kb.POOL, lambda: nc.gpsimd.tensor_tensor(out=kt[:, b2, sl], in0=key_[:], in1=enb_[:], op=ALU.mult),
                          R=[key_, enb_], Wr=[kt])
                    kb.op(kb.POOL, lambda: nc.gpsimd.tensor_tensor(out=ke[:, b2, sl], in0=key_[:], in1=dd_[:], op=ALU.mult),
                          R=[key_, dd_], Wr=[ke])
                    kb.op(kb.POOL, lambda: nc.gpsimd.tensor_copy(out=dec[:, b2, tg * 16:(tg + 1) * 16],
                                                                 in_=eb_[:].rearrange("p (n c) -> p n c", c=32)[:, :, 31]),
                          R=[eb_], Wr=[dec])
            wi = wq
            wload(kb, ws, wi, wcols(w2d, HGRN_OFF + 512, 256))
            cnt = [0]
            proj_tm(kb, hT, wi, 256, pq, cnt,
                    lambda tt, ps_: kb.op(kb.ACT, lambda: nc.scalar.activation(out=iv[:, tt, :], in_=ps_[:, 0:256], func=AF.Copy),
                                          R=[ps_], Wr=[iv]))
            kb.barrier()
        if dbg == "hproj":
            for b2 in range(2):
                kb.dma(yT.ap[b2 * 128:(b2 + 1) * 128, :], ke[:, b2, :], R=[ke], Wr=[cx.xbuf(yT, 0)])
            return
        with ExitStack() as es2:
            ketok = kb.sb("hg_ketok", [128, 32, 256], BF16, es2)
            ketok3 = kb.sb("hg_ketok3", [128, 32, 256], BF16, es2)
            m3 = kb.sb("hg_m3", [128, 1], F32, es2)
            kb.op(kb.POOL, lambda: nc.gpsimd.memset(m3[:], 1.0), Wr=[m3])
            kb.op(kb.POOL, lambda: nc.gpsimd.memset(m3[64:96, :], 0.0), Wr=[m3])
            with ExitStack() as es3:
                ptr = [kb.ps("hg_ptr%d" % i, [128, 4, 128], BF16, es3) for i in range(2)]
                n = 0
                for b2 in range(2):
                    for t4 in range(8):
                        p_ = ptr[n % 2]
                        n += 1
                        for q in range(4):
                            tt = t4 * 4 + q
                            kb.op(kb.PE, lambda: nc.tensor.transpose(p_[:, q, :], ke[:, b2, tt * 128:(tt + 1) * 128], cx.ident_b[:]),
                                  R=[ke, cx.ident_b], Wr=[p_])
                        kb.op(kb.ACT, lambda: nc.scalar.activation(out=ketok[:, t4 * 4:(t4 + 1) * 4, b2 * 128:(b2 + 1) * 128], in_=p_[:], func=AF.Copy),
                              R=[p_], Wr=[ketok])
                        kb.op(kb.ACT, lambda: nc.scalar.activation(out=ketok3[64:128, t4 * 4:(t4 + 1) * 4, b2 * 128:(b2 + 1) * 128], in_=p_[64:128, :, :],
                                                                   func=AF.Copy, scale=m3[64:128, 0:1]),
                              R=[p_, m3], Wr=[ketok3])
                kb.barrier()
            psc = [kb.ps("hg_psc%d" % i, [128, 512], F32, es2) for i in range(2)]
            pkv = [kb.ps("hg_pkv%d" % i, [128, 512], F32, es2) for i in range(4)]
            pO = [kb.ps("hg_pO%d" % i, [128, 512], F32, es2) for i in range(2)]
            S = [kb.sb("hg_S%d" % i, [128, 128], F32, es2) for i in range(2)]
            Sb = [kb.sb("hg_Sb%d" % i, [128, 128], BF16, es2) for i in range(2)]
            scT = [[kb.sb("hg_scT%d%d" % (i, r), [128, 128], BF16, es2) for r in range(2)] for i in range(2)]
            osb = [kb.sb("hg_osb%d" % i, [128, 512], F32, es2) for i in range(2)]
            osq = [kb.sb("hg_osq%d" % i, [128, 512], BF16, es2) for i in range(2)]
            blk_b = kb.sb("hg_blkb", [128, 128], BF16, es2)
            kb.op(kb.DVE, lambda: nc.vector.tensor_copy(out=blk_b[:], in_=cx.blk_f[:]), R=[cx.blk_f], Wr=[blk_b])
            rt = [kb.sb("hg_rt%d" % i, [128, 512], F32, es2) for i in range(2)]
            yst = [kb.sb("hg_y%d" % i, [128, 512], BF16, es2) for i in range(2)]
            for b2 in range(2):
                kb.op(kb.DVE, lambda: nc.vector.memset(S[b2][:], 0.0), Wr=[S[b2]])
                kb.op(kb.DVE, lambda: nc.vector.memset(Sb[b2][:], 0.0), Wr=[Sb[b2]])
            ntt = 32 if not (dbg or "").startswith("ht") else int(dbg[2:])
            for tt in range(ntt):
                tg, q4 = tt // 4, tt % 4
                csl = slice(q4 * 128, (q4 + 1) * 128)
                tsl = slice(tt * 128, (tt + 1) * 128)
                for b2 in range(2):
                    O_ = pO[b2]
                    for r in range(2):
                        kb.op(kb.PE, lambda: nc.tensor.matmul(psc[r][:, 0:128], lhsT=kt[r * 64:(r + 1) * 64, b2, tsl],
                                                              rhs=qd[r * 64:(r + 1) * 64, b2, tsl], start=True, stop=True),
                              R=[kt, qd], Wr=[psc[r]])
                        sc_ = scT[b2][r]
                        kb.op(kb.DVE, lambda: nc.vector.tensor_tensor(out=sc_[:], in0=psc[r][:, 0:128], in1=hgm[:], op=ALU.mult),
                              R=[psc[r], hgm], Wr=[sc_])
                for b2 in range(2):
                    O_ = pO[b2]
                    for r in range(2):
                        sc_ = scT[b2][r]
                        h = b2 * 2 + r
                        kb.op(kb.PE, lambda: nc.tensor.matmul(O_[r * 64:(r + 1) * 64, csl], lhsT=iv[:, tt, h * 64:(h + 1) * 64], rhs=sc_[:],
                                                              start=True, stop=False, skip_group_check=True),
                              R=[iv, sc_], Wr=[O_])
                for c4 in range(4):
                    for b2 in range(2):
                        O_ = pO[b2]
                        nchunk = tt * 4 + c4
                        c0 = tt * 128 + c4 * 32
                        kb.op(kb.PE, lambda: nc.tensor.matmul(O_[:, q4 * 128 + c4 * 32:q4 * 128 + (c4 + 1) * 32], lhsT=Sb[b2][:],
                                                              rhs=qd[:, b2, c0:c0 + 32], start=False, stop=(c4 == 3), skip_group_check=True),
                              R=[Sb[b2], qd], Wr=[O_])
                        kv_ = pkv[c4]
                        if c4 < 3:
                            kb.op(kb.PE, lambda: nc.tensor.matmul(kv_[:, 0:128], lhsT=ketok[c4 * 32:(c4 + 1) * 32, tt, b2 * 128:(b2 + 1) * 128],
                                                                  rhs=iv[c4 * 32:(c4 + 1) * 32, tt, b2 * 128:(b2 + 1) * 128], start=True, stop=True),
                                  R=[ketok, iv], Wr=[kv_])
                        else:
                            kb.op(kb.PE, lambda: nc.tensor.matmul(kv_[:, 0:128], lhsT=ketok3[64:128, tt, b2 * 128:(b2 + 1) * 128],
                                                                  rhs=iv[64:128, tt, b2 * 128:(b2 + 1) * 128], start=True, stop=True),
                                  R=[ketok3, iv], Wr=[kv_])
                        kb.op(kb.DVE, lambda: nc.vector.scalar_tensor_tensor(out=S[b2][:], in0=S[b2][:], scalar=dec[:, b2, nchunk:nchunk + 1],
                                                                             in1=kv_[:, 0:128], op0=ALU.mult, op1=ALU.add),
                              R=[S[b2], dec, kv_], Wr=[S[b2]])
                        kb.op(kb.POOL, lambda: nc.gpsimd.tensor_tensor(out=Sb[b2][:], in0=S[b2][:], in1=cx.blk_f[:], op=ALU.mult),
                              R=[S[b2], cx.blk_f], Wr=[Sb[b2]])
                for b2 in range(2):
                    O_ = pO[b2]
                    if q4 == 3:
                        sl = slice(tg * 512, (tg + 1) * 512)
                        i = b2
                        kb.op(kb.ACT, lambda: nc.scalar.activation(out=osb[i][:], in_=O_[:], func=AF.Copy), R=[O_], Wr=[osb[i]])
                        kb.op(kb.ACT, lambda: nc.scalar.activation(out=osq[i][:], in_=O_[:], func=AF.Square), R=[O_], Wr=[osq[i]])
                        kb.op(kb.PE, lambda: nc.tensor.matmul(psc[0][:], lhsT=blk_b[:], rhs=osq[i][:], start=True, stop=True),
                              R=[blk_b, osq[i]], Wr=[psc[0]])
                        kb.op(kb.ACT, lambda: nc.scalar.activation(out=rt[i][:], in_=psc[0][:], func=AF.Sqrt, scale=1.0 / 64, bias=eps_c[:, 0:1]),
                              R=[psc[0], eps_c], Wr=[rt[i]])
                        kb.op(kb.DVE, lambda: nc.vector.reciprocal(out=rt[i][:], in_=rt[i][:]), R=[rt[i]], Wr=[rt[i]])
                        kb.op(kb.DVE, lambda: nc.vector.tensor_tensor(out=osb[i][:], in0=osb[i][:], in1=rt[i][:], op=ALU.mult),
                              R=[osb[i], rt[i]], Wr=[osb[i]])
                        kb.op(kb.DVE, lambda: nc.vector.scalar_tensor_tensor(out=yst[i][:], in0=osb[i][:], scalar=gng[:, b2:b2 + 1], in1=sg[:, b2, sl],
                                                                             op0=ALU.mult, op1=ALU.mult), R=[osb[i], gng, sg], Wr=[yst[i]])
                        kb.dma(yT.ap[b2 * 128:(b2 + 1) * 128, sl], yst[i][:], R=[yst[i]], Wr=[cx.xbuf(yT, tg)])
            kb.barrier()


C0 = 0.6065306597126334


class OpsH:
    def __init__(self, kb):
        self.kb = kb
        self.nc = kb.nc

    def act(self, out, in_, func, R, Wr, scale=1.0, bias=None):
        kb, nc = self.kb, self.nc
        if bias is None:
            return kb.op(kb.ACT, lambda: nc.scalar.activation(out=out, in_=in_, func=func, scale=scale), R=R, Wr=Wr)
        return kb.op(kb.ACT, lambda: nc.scalar.activation(out=out, in_=in_, func=func, scale=scale, bias=bias), R=R, Wr=Wr)

    def tt(self, E, out, a, b, op, R, Wr):
        kb = self.kb
        return kb.op(E, lambda: E.eng.tensor_tensor(out=out, in0=a, in1=b, op=op), R=R, Wr=Wr)

    def ts(self, E, out, a, s1, s2, op0, op1, R, Wr):
        kb = self.kb
        if s2 is None:
            return kb.op(E, lambda: E.eng.tensor_scalar(out=out, in0=a, scalar1=s1, scalar2=None, op0=op0), R=R, Wr=Wr)
        return kb.op(E, lambda: E.eng.tensor_scalar(out=out, in0=a, scalar1=s1, scalar2=s2, op0=op0, op1=op1), R=R, Wr=Wr)

    def stt(self, out, a, sc, b, op0, op1, R, Wr):
        kb, nc = self.kb, self.nc
        return kb.op(kb.DVE, lambda: nc.vector.scalar_tensor_tensor(out=out, in0=a, scalar=sc, in1=b, op0=op0, op1=op1), R=R, Wr=Wr)

    def mm(self, out, lhsT, rhs, R, Wr, start=True, stop=True, skip=False):
        kb, nc = self.kb, self.nc
        return kb.op(kb.PE, lambda: nc.tensor.matmul(out, lhsT=lhsT, rhs=rhs, start=start, stop=stop, skip_group_check=skip), R=R, Wr=Wr)


def phase_rwkv(kb, cx, layer, hT, yT, dbg=None):
    nc = kb.nc
    o = OpsH(kb)
    w2d = cx.dram["w_in"][layer]
    MUL, ADD, SUB = ALU.mult, ALU.add, ALU.subtract
    DVE, POOL = kb.DVE, kb.POOL
    with ExitStack() as es:
        def col2(name, src, es_=es):
            t_ = kb.sb(name, [128, 2], F32, es_)
            kb.dma(t_[:], src.rearrange("(b p) -> p b", p=128), Wr=[t_])
            return t_
        mu = kb.sb("rw_mu", [128, 8], F32, es)
        kb.dma(mu[:], cx.dram["rwkv_mu"][layer].rearrange("(b p) -> p b", p=128), Wr=[mu])
        w0 = col2("rw_w0", cx.dram["rwkv_w0"][layer])
        a0 = col2("rw_a0", cx.dram["rwkv_a0"][layer])
        k_k = col2("rw_kk", cx.dram["rwkv_k_k"][layer])
        k_a = col2("rw_ka", cx.dram["rwkv_k_a"][layer])
        gnw = col2("rw_gnw", cx.dram["rwkv_gn_w"][layer])
        gnb = col2("rw_gnb", cx.dram["rwkv_gn_b"][layer])
        r_k = col2("rw_rk", cx.dram["rwkv_r_k"][layer].rearrange("h d -> (h d)"))
        gneps = kb.sb("rw_gneps", [128, 1], F32, es)
        kb.op(POOL, lambda: nc.gpsimd.memset(gneps[:], GN_EPS), Wr=[gneps])
        if layer > 0:
            v0 = col2("rw_v0", cx.dram["rwkv_v0"][layer - 1])
        stg = kb.sb("rw_stg", [128, 256], F32, es)

        def small_w(name, src_ap, rows, r0=0, ncols=256):
            t_ = kb.sb(name, [128, ncols], BF16, es)
            kb.dma(stg[r0:r0 + rows, 0:ncols], src_ap, Wr=[stg])
            kb.op(DVE, lambda: nc.vector.tensor_copy(out=t_[r0:r0 + rows, :], in_=stg[r0:r0 + rows, 0:ncols]), R=[stg], Wr=[t_])
            return t_
        w2b = small_w("rw_w2b", cx.dram["rwkv_w2"][layer], 64, 0)
        a2b = small_w("rw_a2b", cx.dram["rwkv_a2"][layer], 64, 64)
        g2b = small_w("rw_g2b", cx.dram["rwkv_g2"][layer], 128, 0)
        if layer > 0:
            v2b = small_w("rw_v2b", cx.dram["rwkv_v2"][layer - 1], 32, 0)
            v1b = kb.sb("rw_v1b", [128, 2, 32], BF16, es)
            for b2 in range(2):
                kb.dma(stg[:, 0:32], cx.dram["rwkv_v1"][layer - 1][b2 * 128:(b2 + 1) * 128, :], Wr=[stg])
                kb.op(DVE, lambda: nc.vector.tensor_copy(out=v1b[:, b2, :], in_=stg[:, 0:32]), R=[stg], Wr=[v1b])
        scm = kb.sb("rw_scm", [128, 512], F32, es)
        kb.dma(scm[:], cx.dram["c_scanmask128"][:, :], Wr=[scm])
        LT = kb.sb("rw_LT", [128, T], BF16, es)
        SG = kb.sb("rw_SG", [128, T], BF16, es)
        VL = kb.sb("rw_VL", [32, T], BF16, es) if layer > 0 else None

        class Shifter:
            def __init__(self, name, es_, d=None):
                self.pb = [kb.sb("%s_pb%d" % (name, i), [128, 513], F32, es_) for i in range(2)]
                self.d = d if d is not None else kb.sb("%s_d" % name, [128, 512], F32, es_)
                self.n = 0

            def run(self, ps_, mucol, out, out_tl=None):
                cur = self.pb[self.n % 2]
                prv = self.pb[(self.n + 1) % 2]
                first = (self.n % 8 == 0)
                self.n += 1
                o.act(cur[:, 1:513], ps_[:], AF.Copy, [ps_], [cur])
                if first:
                    kb.op(DVE, lambda: nc.vector.memset(cur[:, 0:1], 0.0), Wr=[cur])
                else:
                    kb.op(DVE, lambda: nc.vector.tensor_copy(out=cur[:, 0:1], in_=prv[:, 512:513]), R=[prv], Wr=[cur])
                o.tt(DVE, self.d[:], cur[:, 0:512], cur[:, 1:513], SUB, [cur], [self.d])
                o.stt(out, self.d[:], mucol, cur[:, 1:513], MUL, ADD, [self.d, cur, mu], [out_tl])

        with ExitStack() as es2:
            ws = WStream(kb, es2, "rw0_w", 8, 128, nbuf=2, alloc_wb=False)
            wl_ = kb.sb("rw0_wl", [128, 8, 128], BF16, es2)
            wg_ = kb.sb("rw0_wg", [128, 8, 128], BF16, es2)
            wload(kb, ws, wl_, wcols(w2d, RWKV_OFF + 768, 128))
            wload(kb, ws, wg_, wcols(w2d, RWKV_OFF + 896, 128))
            nv = 2 if layer > 0 else 0
            wv_ = [kb.sb("rw0_wv%d" % i, [128, 8, 128], BF16, es2) for i in range(nv)]
            for i in range(nv):
                wload(kb, ws, wv_[i], wcols(w2d, RWKV_OFF + 512 + i * 128, 128))
            pp = [kb.ps("rw0_pp%d" % i, [128, 512], F32, es2) for i in range(4)]
            pvl = kb.ps("rw0_pvl", [128, 512], F32, es2)
            shl = Shifter("rw0_sl", es2)
            shg = Shifter("rw0_sg", es2)
            shv = [Shifter("rw0_sv%d" % i, es2) for i in range(nv)]
            tl = kb.sb("rw0_tl", [128, 512], F32, es2)
            tg_ = kb.sb("rw0_tg", [128, 512], F32, es2)
            tv = [kb.sb("rw0_tv%d" % i, [128, 512], F32, es2) for i in range(nv)]
            tvb = [kb.sb("rw0_tvb%d" % i, [128, 512], BF16, es2) for i in range(nv)]
            for tg in range(8):
                sl = slice(tg * 512, (tg + 1) * 512)
                for (wt_, ps_) in [(wl_, pp[0]), (wg_, pp[1])] + [(wv_[i], pp[2 + i]) for i in range(nv)]:
                    for kc in range(8):
                        o.mm(ps_[:], wt_[:, kc, :], hT[:, kc, sl], [wt_, hT], [ps_], start=(kc == 0), stop=(kc == 7))
                shl.run(pp[0], mu[:, 6:7], tl[:], tl)
                o.act(LT[0:64, sl], tl[0:64, :], AF.Tanh, [tl], [LT])
                o.act(LT[64:128, sl], tl[64:128, :], AF.Copy, [tl], [LT])
                shg.run(pp[1], mu[:, 7:8], tg_[:], tg_)
                o.act(SG[:, sl], tg_[:], AF.Sigmoid, [tg_], [SG])
                for i in range(nv):
                    shv[i].run(pp[2 + i], mu[:, 4 + i:5 + i], tv[i][:], tv[i])
                    kb.op(DVE, lambda: nc.vector.tensor_copy(out=tvb[i][:], in_=tv[i][:]), R=[tv[i]], Wr=[tvb[i]])
                if nv:
                    for i in range(2):
                        o.mm(pvl[0:32, :], v1b[:, i, :], tvb[i][:], [v1b, tvb[i]], [pvl], start=(i == 0), stop=(i == 1))
                    o.act(VL[0:32, sl], pvl[0:32, :], AF.Copy, [pvl], [VL])
            kb.barrier()
        if dbg == "r0":
            kb.dma(yT.ap[0:128, :], LT[:, :], R=[LT], Wr=[cx.xbuf(yT, 0)])
            kb.dma(yT.ap[128:256, :], SG[:, :], R=[SG], Wr=[cx.xbuf(yT, 0)])
            return

        for blk in range(2):
            with ExitStack() as esb:
                at = kb.sb("rw_at", [128, T], BF16, esb)
                bt = kb.sb("rw_bt", [128, T], BF16, esb)
                ktl = kb.sb("rw_ktl", [128, T], BF16, esb)
                rt = kb.sb("rw_rt", [128, T], BF16, esb)
                bh = kb.sb("rw_bh", [128, T], BF16, esb)
                kh = kb.sb("rw_kh", [128, T], BF16, esb)
                vT = kb.sb("rw_vT", [128, T], BF16, esb)
                gend = kb.sb("rw_gend", [128, 32], F32, esb)
                with ExitStack() as es2:
                    ws = WStream(kb, es2, "rwp_w", 8, 128, nbuf=2, alloc_wb=False)
                    wr_ = kb.sb("rwp_wr", [128, 8, 128], BF16, es2)
                    wk_ = kb.sb("rwp_wk", [128, 8, 128], BF16, es2)
                    wv_ = kb.sb("rwp_wv", [128, 8, 128], BF16, es2)
                    wload(kb, ws, wr_, wcols(w2d, RWKV_OFF + blk * 128, 128))
                    wload(kb, ws, wk_, wcols(w2d, RWKV_OFF + 256 + blk * 128, 128))
                    wload(kb, ws, wv_, wcols(w2d, RWKV_OFF + 512 + blk * 128, 128))
                    pr = kb.ps("rwp_pr", [128, 512], F32, es2)
                    pk = kb.ps("rwp_pk", [128, 512], F32, es2)
                    pv = kb.ps("rwp_pv", [128, 512], F32, es2)
                    pl = kb.ps("rwp_pl", [128, 512], F32, es2)
                    pa = kb.ps("rwp_pa", [128, 512], F32, es2)
                    pg2 = kb.ps("rwp_pg2", [128, 512], F32, es2)
                    pss = kb.ps("rwp_pss", [128, 512], F32, es2)
                    pvm = kb.ps("rwp_pvm", [128, 512], F32, es2)
                    shr = Shifter("rwp_sr", es2)
                    shk, shv = Shifter("rwp_sk", es2, shr.d), Shifter("rwp_sv", es2, shr.d)
                    names = ["rs", "ks", "vs", "sgw", "av", "cum", "e1", "e2", "e3", "e4", "kk", "t1", "t2"]
                    wt = {n_: kb.sb("rwp_" + n_, [128, 512], F32, es2) for n_ in names}
                    wt["kkn"] = wt["sgw"]
                    wt["kmod"] = wt["cum"]
                    gst = kb.sb("rwp_gst", [128, 512], BF16, es2)
                    bc = slice(blk * 128, (blk + 1) * 128)
                    b1 = slice(blk, blk + 1)
                    for tg in range(8):
                        sl = slice(tg * 512, (tg + 1) * 512)
                        for (wt_, ps_) in ((wr_, pr), (wk_, pk), (wv_, pv)):
                            for kc in range(8):
                                o.mm(ps_[:], wt_[:, kc, :], hT[:, kc, sl], [wt_, hT], [ps_], start=(kc == 0), stop=(kc == 7))
                        rs, ks, vs = wt["rs"], wt["ks"], wt["vs"]
                        for (sh_, ps_, mc, dst) in ((shr, pr, blk, rs), (shk, pk, 2 + blk, ks), (shv, pv, 4 + blk, vs)):
                            sh_.run(ps_, mu[:, mc:mc + 1], dst[:], dst)
                        t1, t2 = wt["t1"], wt["t2"]
                        if layer == 0:
                            kb.dma(cx.vfirst.ap[bc, sl], vs[:], R=[vs], Wr=[cx.vfirst.buf((blk, tg))])
                        else:
                            o.mm(pvm[:], v2b[0:32, bc], VL[0:32, sl], [v2b, VL], [pvm])
                            o.act(t1[:], pvm[:], AF.Sigmoid, [pvm, v0], [t1], bias=v0[:, b1])
                            kb.dma(t2[:], cx.vfirst.ap[bc, sl], R=[cx.vfirst.buf((blk, tg))], Wr=[t2])
                            o.tt(DVE, t2[:], t2[:], vs[:], SUB, [t2, vs], [t2])
                            o.tt(DVE, t2[:], t2[:], t1[:], MUL, [t2, t1], [t2])
                            o.tt(DVE, vs[:], vs[:], t2[:], ADD, [vs, t2], [vs])
                        sgw, av, cum = wt["sgw"], wt["av"], wt["cum"]
                        o.mm(pl[:], w2b[0:64, bc], LT[0:64, sl], [w2b, LT], [pl])
                        o.act(sgw[:], pl[:], AF.Sigmoid, [pl, w0], [sgw], bias=w0[:, b1])
                        o.mm(pa[:], a2b[64:128, bc], LT[64:128, sl], [a2b, LT], [pa])
                        o.act(av[:], pa[:], AF.Sigmoid, [pa, a0], [av], bias=a0[:, b1])
                        o.mm(pg2[:], g2b[:, bc], SG[:, sl], [g2b, SG], [pg2])
                        o.act(gst[:], pg2[:], AF.Copy, [pg2], [gst])
                        kb.dma(cx.gscr.ap[bc, sl], gst[:], R=[gst], Wr=[cx.gscr.buf((blk, tg))])
                        kb.op(DVE, lambda: nc.vector.tensor_tensor_scan(out=cum[:], data0=scm[:], data1=sgw[:], initial=0.0, op0=MUL, op1=ADD),
                              R=[scm, sgw], Wr=[cum])
                        e1, e2, e3, e4 = wt["e1"], wt["e2"], wt["e3"], wt["e4"]
                        o.act(e1[:], cum[:], AF.Exp, [cum], [e1], scale=-C0)
                        o.act(e2[:], cum[:], AF.Exp, [cum], [e2], scale=C0)
                        c3 = cum[:].rearrange("p (n c) -> p n c", c=128)
                        o.tt(DVE, e3[:].rearrange("p (n c) -> p n c", c=128), c3[:, :, 127:128].to_broadcast([128, 4, 128]), c3, SUB, [cum], [e3])
                        o.act(e3[:], e3[:], AF.Exp, [e3], [e3], scale=-C0)
                        o.tt(DVE, e4[:], cum[:], sgw[:], SUB, [cum, sgw], [e4])
                        o.act(e4[:], e4[:], AF.Exp, [e4], [e4], scale=-C0)
                        kb.op(POOL, lambda: nc.gpsimd.tensor_copy(out=gend[:, tg * 4:(tg + 1) * 4], in_=e1[:].rearrange("p (n c) -> p n c", c=128)[:, :, 127]),
                              R=[e1], Wr=[gend])
                        kk, kkn = wt["kk"], wt["kkn"]
                        o.ts(DVE, kk[:], ks[:], k_k[:, b1], None, MUL, None, [ks, k_k], [kk])
                        o.act(t1[:], kk[:], AF.Square, [kk], [t1])
                        o.mm(pss[:], cx.blk_f[:], t1[:], [cx.blk_f, t1], [pss])
                        o.act(t1[:], pss[:], AF.Sqrt, [pss], [t1])
                        o.ts(DVE, t1[:], t1[:], 1e-12, None, ALU.max, None, [t1], [t1])
                        kb.op(DVE, lambda: nc.vector.reciprocal(out=t1[:], in_=t1[:]), R=[t1], Wr=[t1])
                        o.tt(DVE, kkn[:], kk[:], t1[:], MUL, [kk, t1], [kkn])
                        kmod, ka = wt["kmod"], wt["kk"]
                        o.ts(DVE, t2[:], av[:], -1.0, k_a[:, b1], ADD, MUL, [av, k_a], [t2])
                        o.stt(kmod[:], t2[:], 1.0, ks[:], ADD, MUL, [t2, ks], [kmod])
                        o.stt(t2[:], rs[:], r_k[:, b1], kmod[:], MUL, MUL, [rs, r_k, kmod], [t2])
                        o.mm(pss[:], cx.blk_f[:], t2[:], [cx.blk_f, t2], [pss])
                        o.tt(DVE, t1[:], pss[:], vs[:], MUL, [pss, vs], [t1])
                        kb.dma(cx.bscr.ap[bc, sl], t1[:], R=[t1], Wr=[cx.bscr.buf((blk, tg))])
                        o.tt(POOL, ka[:], kkn[:], av[:], MUL, [kkn, av], [ka])
                        o.stt(at[:, sl], kkn[:], -1.0, e4[:], MUL, MUL, [kkn, e4], [at])
                        o.tt(POOL, bt[:, sl], ka[:], e2[:], MUL, [ka, e2], [bt])
                        o.tt(POOL, ktl[:, sl], kmod[:], e2[:], MUL, [kmod, e2], [ktl])
                        o.tt(POOL, rt[:, sl], rs[:], e1[:], MUL, [rs, e1], [rt])
                        o.tt(POOL, bh[:, sl], ka[:], e3[:], MUL, [ka, e3], [bh])
                        o.tt(POOL, kh[:, sl], kmod[:], e3[:], MUL, [kmod, e3], [kh])
                        o.act(vT[:, sl], vs[:], AF.Copy, [vs], [vT])
                    kb.barrier()
                if dbg == "prep":
                    kb.dma(yT.ap[0:128, :], at[:, :], R=[at], Wr=[cx.xbuf(yT, 0)])
                    kb.dma(yT.ap[128:256, :], kh[:, :], R=[kh], Wr=[cx.xbuf(yT, 0)])
                    return
                with ExitStack() as es2:
                    rw_recur(kb, cx, o, es2, layer, blk, at, bt, ktl, rt, bh, kh, vT, gend, gnw, gnb, gneps, yT, dbg)
                    kb.barrier()


def rw_recur(kb, cx, o, es, layer, blk, at, bt, ktl, rt, bh, kh, vT, gend, gnw, gnb, gneps, yT, dbg):
    nc = kb.nc
    MUL, ADD, SUB = ALU.mult, ALU.add, ALU.subtract
    DVE, POOL = kb.DVE, kb.POOL
    bc = slice(blk * 128, (blk + 1) * 128)
    b1 = slice(blk, blk + 1)

    def cst(name, key):
        t_ = kb.sb(name, [128, 128], F32, es)
        kb.dma(t_[:], cx.dram[key][:, :], Wr=[t_])
        return t_
    mlow, mupS, mupI = cst("rr_mlow", "c_rw_low"), cst("rr_mupS", "c_rw_upS"), cst("rr_mupI", "c_rw_upI")
    A = [kb.ps("rr_A%d" % r, [128, 512], F32, es) for r in range(2)]
    B = [kb.ps("rr_B%d" % r, [128, 512], F32, es) for r in range(2)]
    N = [kb.ps("rr_N%d" % r, [128, 512], F32, es) for r in range(2)]
    YS = kb.ps("rr_YS", [128, 512], F32, es)
    TR = kb.ps("rr_TR", [128, 4, 128], BF16, es)
    NT = 3
    tok = [kb.sb("rr_tok%d" % i, [128, 4, 128], BF16, es) for i in range(NT)]
    AakT = [[kb.sb("rr_aak%d%d" % (i, r), [128, 128], BF16, es) for r in range(2)] for i in range(NT)]
    ArbT = [[kb.sb("rr_arb%d%d" % (i, r), [128, 128], BF16, es) for r in range(2)] for i in range(NT)]
    ArkT = [[kb.sb("rr_ark%d%d" % (i, r), [128, 128], BF16, es) for r in range(2)] for i in range(NT)]
    P0 = [[kb.sb("rr_P0%d%d" % (i, r), [128, 128], BF16, es) for r in range(2)] for i in range(2)]
    PT0 = [[kb.sb("rr_PT0%d%d" % (i, r), [128, 128], BF16, es) for r in range(2)] for i in range(2)]
    Gb0 = [[kb.sb("rr_Gb0%d%d" % (i, r), [128, 128], BF16, es) for r in range(2)] for i in range(2)]
    X0sb = [[kb.sb("rr_X0%d%d" % (i, r), [128, 64], BF16, es) for r in range(2)] for i in range(2)]
    Pa = [[kb.sb("rr_P%d%d" % (r, i), [128, 128], BF16, es) for i in range(2)] for r in range(2)]
    PTa = [[kb.sb("rr_PT%d%d" % (r, i), [128, 128], BF16, es) for i in range(2)] for r in range(2)]
    Gv = [[kb.sb("rr_G%d%d" % (r, i), [128, 128], BF16, es) for i in range(2)] for r in range(2)]
    Zb = [kb.sb("rr_Z%d" % i, [128, 128], F32, es) for i in range(2)]
    WmT = [kb.sb("rr_WmT%d" % i, [128, 128], BF16, es) for i in range(2)]
    M = kb.sb("rr_M", [128, 128], F32, es)
    Mb = kb.sb("rr_Mb", [128, 128], BF16, es)
    Ub = kb.sb("rr_Ub", [128, 128], BF16, es)
    ysb = kb.sb("rr_ysb", [128, 256], F32, es)
    yc = kb.sb("rr_yc", [128, 256], F32, es)
    sq = kb.sb("rr_sq", [128, 256], F32, es)
    bon = kb.sb("rr_bon", [128, 256], F32, es)
    gin = kb.sb("rr_gin", [128, 256], BF16, es)
    yo = kb.sb("rr_yo", [128, 256], BF16, es)
    kb.op(DVE, lambda: nc.vector.memset(M[:], 0.0), Wr=[M])
    kb.op(DVE, lambda: nc.vector.memset(Mb[:], 0.0), Wr=[Mb])
    ntile = 32 if not (dbg or "").startswith("rt") else int(dbg[2:])

    def s1a(tt):
        t3 = tt % NT
        cs = slice(tt * 128, (tt + 1) * 128)
        for i, src in enumerate((vT, at, bh, kh)):
            kb.op(kb.PE, lambda: nc.tensor.transpose(TR[:, i, :], src[:, cs], cx.ident_b[:]), R=[src, cx.ident_b], Wr=[TR])
        o.act(tok[t3][:], TR[:], AF.Copy, [TR], [tok[t3]])

    def s1m(tt):
        t3, tp = tt % NT, tt % 2
        cs = slice(tt * 128, (tt + 1) * 128)
        for r in range(2):
            ps_ = slice(r * 64, (r + 1) * 64)
            jobs = ((A[r][:, 0:128], A[r], at, bt, P0[tp][r], mlow),
                    (A[r][:, 128:256], A[r], bt, at, PT0[tp][r], mupS),
                    (A[r][:, 256:384], A[r], ktl, at, AakT[t3][r], mupS),
                    (A[r][:, 384:512], A[r], bt, rt, ArbT[t3][r], mupI),
                    (B[r][:, 0:128], B[r], ktl, rt, ArkT[t3][r], mupI))
            for (out_, ob_, l_, r_, dst, msk) in jobs:
                o.mm(out_, l_[ps_, cs], r_[ps_, cs], [l_, r_], [ob_])
            for (out_, ob_, l_, r_, dst, msk) in jobs:
                o.tt(DVE, dst[:], out_, msk[:], MUL, [ob_, msk], [dst])
            o.tt(POOL, Gb0[tp][r][:], PT0[tp][r][:], cx.ident_f[:], ADD, [PT0[tp][r], cx.ident_f], [Gb0[tp][r]])

    def s1x(tt):
        t3, tp = tt % NT, tt % 2
        for r in range(2):
            o.mm(B[r][:, 128:192], AakT[t3][r][:], tok[t3][:, 0, r * 64:(r + 1) * 64], [AakT[t3][r], tok[t3]], [B[r]])
            o.act(X0sb[tp][r][:], B[r][:, 128:192], AF.Copy, [B[r]], [X0sb[tp][r]])

    def evac(r, dst, src_ap, bank):
        if r == 0:
            o.act(dst[:], src_ap, AF.Copy, [bank], [dst])
        else:
            kb.op(DVE, lambda: nc.vector.tensor_copy(out=dst[:], in_=src_ap), R=[bank], Wr=[dst])

    def s2(tt, j):
        tp = tt % 2
        for r in range(2):
            Pc = P0[tp][r] if j == 0 else Pa[r][j % 2]
            PTc = PT0[tp][r] if j == 0 else PTa[r][j % 2]
            if j <= 5:
                o.mm(N[r][:, 0:128], PTc[:], Pc[:], [PTc, Pc], [N[r]])
            if j < 5:
                o.mm(N[r][:, 128:256], Pc[:], PTc[:], [PTc, Pc], [N[r]])
            if j >= 1:
                Gc = Gb0[tp][r] if j == 1 else Gv[r][(j - 1) % 2]
                o.mm(N[r][:, 256:384], cx.ident_b[:], Gc[:], [cx.ident_b, Gc], [N[r]], start=True, stop=False)
                o.mm(N[r][:, 256:384], Pc[:], Gc[:], [Pc, Gc], [N[r]], start=False, stop=True)
        for r in range(2):
            if j <= 5:
                evac(r, Pa[r][(j + 1) % 2], N[r][:, 0:128], N[r])
            if j < 5:
                evac(r, PTa[r][(j + 1) % 2], N[r][:, 128:256], N[r])
            if j >= 1:
                evac(r, Gv[r][j % 2], N[r][:, 256:384], N[r])

    def s3(tt):
        t3, tp = tt % NT, tt % 2
        for r in range(2):
            ps_ = slice(r * 64, (r + 1) * 64)
            Gf = Gv[r][0]
            o.mm(B[r][ps_, 256:384], tok[t3][:, 1, r * 64:(r + 1) * 64], Gf[:], [tok[t3], Gf], [B[r]])
            o.mm(B[r][:, 192:256], Gf[:], X0sb[tp][r][:], [Gf, X0sb[tp][r]], [B[r]])
            o.act(WmT[tp][ps_, :], B[r][ps_, 256:384], AF.Copy, [B[r]], [WmT[tp]])
            o.act(Zb[tp][:, r * 64:(r + 1) * 64], B[r][:, 192:256], AF.Copy, [B[r]], [Zb[tp]])

    def seqU(tt):
        tp = tt % 2
        o.mm(YS[:, 256:384], WmT[tp][:], Mb[:], [WmT[tp], Mb], [YS])
        o.tt(DVE, Ub[:], YS[:, 256:384], Zb[tp][:], ADD, [YS, Zb[tp]], [Ub])

    def seqY(tt):
        t3 = tt % NT
        cs = slice(tt * 128, (tt + 1) * 128)
        yc_ = slice((tt % 2) * 128, (tt % 2) * 128 + 128)
        o.mm(YS[:, yc_], Mb[:], rt[:, cs], [Mb, rt], [YS], start=True, stop=False, skip=True)
        for r in range(2):
            ps_ = slice(r * 64, (r + 1) * 64)
            o.mm(YS[ps_, yc_], Ub[:, r * 64:(r + 1) * 64], ArbT[t3][r][:], [Ub, ArbT[t3][r]], [YS], start=False, stop=False, skip=True)
            o.mm(YS[ps_, yc_], tok[t3][:, 0, r * 64:(r + 1) * 64], ArkT[t3][r][:], [tok[t3], ArkT[t3][r]], [YS], start=False, stop=(r == 1), skip=True)

    def seqM(tt):
        t3 = tt % NT
        o.mm(YS[:, 384:512], tok[t3][:, 2, :], Ub[:], [tok[t3], Ub], [YS], start=True, stop=False)
        o.mm(YS[:, 384:512], tok[t3][:, 3, :], tok[t3][:, 0, :], [tok[t3]], [YS], start=False, stop=True)
        o.stt(M[:], M[:], gend[:, tt:tt + 1], YS[:, 384:512], MUL, ADD, [M, gend, YS], [M])
        o.tt(POOL, Mb[:], M[:], cx.blk_f[:], MUL, [M, cx.blk_f], [Mb])

    def outp(tt):
        t2 = tt // 2
        sl = slice(t2 * 256, (t2 + 1) * 256)
        kb.op(DVE, lambda: nc.vector.tensor_copy(out=ysb[:], in_=YS[:, 0:256]), R=[YS], Wr=[ysb])
        kb.dma(bon[:], cx.bscr.ap[bc, sl], Wr=[bon])
        kb.dma(gin[:], cx.gscr.ap[bc, sl], Wr=[gin])
        o.mm(N[0][:, 0:256], cx.blk_f[:], ysb[:], [cx.blk_f, ysb], [N[0]])
        o.stt(yc[:], N[0][:, 0:256], -1.0 / 64, ysb[:], MUL, ADD, [N[0], ysb], [yc])
        o.act(sq[:], yc[:], AF.Square, [yc], [sq])
        o.mm(N[0][:, 0:256], cx.blk_f[:], sq[:], [cx.blk_f, sq], [N[0]])
        o.act(sq[:], N[0][:, 0:256], AF.Sqrt, [N[0], gneps], [sq], scale=1.0 / 64, bias=gneps[:, 0:1])
        kb.op(DVE, lambda: nc.vector.reciprocal(out=sq[:], in_=sq[:]), R=[sq], Wr=[sq])
        o.tt(DVE, yc[:], yc[:], sq[:], MUL, [yc, sq], [yc])
        o.ts(DVE, yc[:], yc[:], gnw[:, b1], gnb[:, b1], MUL, ADD, [yc, gnw, gnb], [yc])
        o.tt(DVE, yc[:], yc[:], bon[:], ADD, [yc, bon], [yc])
        o.tt(DVE, yo[:], yc[:], gin[:], MUL, [yc, gin], [yo])
        kb.dma(yT.ap[bc, sl], yo[:], R=[yo])

    s1a(0)
    s1m(0)
    s1x(0)
    for j in range(7):
        s2(0, j)
    s3(0)
    if ntile > 1:
        s1a(1)
        s1m(1)
        s1x(1)
    for t in range(ntile):
        has1 = t + 1 < ntile
        has2 = t + 2 < ntile
        seqU(t)
        if has1:
            s2(t + 1, 0)
        if has2:
            s1a(t + 2)
            s1m(t + 2)
        if has1:
            s2(t + 1, 1)
        seqY(t)
        seqM(t)
        if has1:
            s2(t + 1, 2)
        if has2:
            s1x(t + 2)
        if has1:
            s2(t + 1, 3)
            s2(t + 1, 4)
            s2(t + 1, 5)
            s2(t + 1, 6)
            s3(t + 1)
        if t % 2 == 1:
            outp(t)


def phase_merge(kb, cx, layer, hT, ys, mergedT):
    nc = kb.nc
    o = OpsH(kb)
    w2d = cx.dram["w_in"][layer]
    with ExitStack() as es:
        yS = [kb.sb("mg_y%d" % n, [128, 2, T], BF16, es) for n in range(4)]
        for n in range(4):
            kb.dma(yS[n][:], ys[n].ap.rearrange("(c p) t -> p c t", p=128), Wr=[yS[n]])
        ws = WStream(kb, es, "mg_w", 8, 128, nbuf=2, alloc_wb=False)
        wg = [[kb.sb("mg_wg%d%d" % (i, n), [128, 8, 128], BF16, es) for n in range(4)] for i in range(2)]
        wbr = [[kb.sb("mg_wb%d%d" % (i, n), [128, 2, 128], BF16, es) for n in range(4)] for i in range(2)]
        pg = [kb.ps("mg_pg%d" % i, [128, 512], F32, es) for i in range(3)]
        pu = [kb.ps("mg_pu%d" % i, [128, 512], F32, es) for i in range(3)]
        sg = [kb.sb("mg_sg%d" % i, [128, 512], F32, es) for i in range(3)]
        acc = [kb.sb("mg_acc%d" % i, [128, 512], F32, es) for i in range(2)]
        mst = [kb.sb("mg_mst%d" % i, [128, 512], BF16, es) for i in range(2)]
        cnt = 0
        for fb in range(8):
            i = fb % 2
            for n in range(4):
                wload(kb, ws, wg[i][n], wcols(w2d, GATE_OFF + n * 1024 + fb * 128, 128))
                wload(kb, ws, wbr[i][n], cx.dram["w_branch"][layer, n].rearrange("(c p) f -> p c f", p=128)[:, :, fb * 128:(fb + 1) * 128])
            for tg in range(8):
                sl = slice(tg * 512, (tg + 1) * 512)
                a_ = acc[(fb * 8 + tg) % 2]
                for n in range(4):
                    j = cnt % 3
                    cnt += 1
                    for kc in range(8):
                        o.mm(pg[j][:], wg[i][n][:, kc, :], hT[:, kc, sl], [wg[i][n], hT], [pg[j]], start=(kc == 0), stop=(kc == 7))
                    for c2 in range(2):
                        o.mm(pu[j][:], wbr[i][n][:, c2, :], yS[n][:, c2, sl], [wbr[i][n], yS[n]], [pu[j]], start=(c2 == 0), stop=(c2 == 1))
                    o.act(sg[j][:], pg[j][:], AF.Sigmoid, [pg[j]], [sg[j]])
                    if n == 0:
                        o.tt(kb.DVE, a_[:], pu[j][:], sg[j][:], ALU.mult, [pu[j], sg[j]], [a_])
                    else:
                        o.tt(kb.DVE, sg[j][:], pu[j][:], sg[j][:], ALU.mult, [pu[j], sg[j]], [sg[j]])
                        if n < 3:
                            o.tt(kb.POOL, a_[:], a_[:], sg[j][:], ALU.add, [a_, sg[j]], [a_])
                        else:
                            m_ = mst[(fb * 8 + tg) % 2]
                            o.tt(kb.POOL, m_[:], a_[:], sg[j][:], ALU.add, [a_, sg[j]], [m_])
                            kb.dma(mergedT.ap[fb * 128:(fb + 1) * 128, sl], m_[:], R=[m_], q=kb.ACT)
        kb.barrier()


def phase_outproj(kb, cx, w2d, kch, inT, x_in, x_out):
    nc = kb.nc
    o = OpsH(kb)
    with ExitStack() as es:
        act = kb.sb("op_in", [128, kch, T], BF16, es)
        kb.dma(act[:], inT.ap.rearrange("(c p) t -> p c t", p=128), Wr=[act])
        ws = WStream(kb, es, "op_w", kch, 128, nbuf=2, alloc_wb=True)
        pp = [kb.ps("op_pp%d" % i, [128, 512], F32, es) for i in range(3)]
        xt = [kb.sb("op_x%d" % i, [128, 512], F32, es) for i in range(3)]
        n = 0
        for fb in range(8):
            wb = ws.load(wcols(w2d, fb * 128, 128))
            for tg in range(8):
                sl = slice(tg * 512, (tg + 1) * 512)
                j = n % 3
                n += 1
                kb.dma(xt[j][:], x_in.ap[fb * 128:(fb + 1) * 128, sl], Wr=[xt[j]])
                for kc in range(kch):
                    o.mm(pp[j][:], wb[:, kc, :], act[:, kc, sl], [wb, act], [pp[j]], start=(kc == 0), stop=(kc == kch - 1))
                o.tt(kb.DVE, xt[j][:], pp[j][:], xt[j][:], ALU.add, [pp[j], xt[j]], [xt[j]])
                kb.dma(x_out.ap[fb * 128:(fb + 1) * 128, sl], xt[j][:], R=[xt[j]], q=kb.ACT)
        kb.barrier()


def phase_ffn_up(kb, cx, layer, hT, actT):
    nc = kb.nc
    o = OpsH(kb)
    wg2d = cx.dram["w_ffn_gate"][layer]
    wu2d = cx.dram["w_ffn_up"][layer]
    with ExitStack() as es:
        ws = WStream(kb, es, "fu_w", 8, 128, nbuf=2, alloc_wb=False)
        wg = [kb.sb("fu_wg%d" % i, [128, 8, 128], BF16, es) for i in range(2)]
        wu = [kb.sb("fu_wu%d" % i, [128, 8, 128], BF16, es) for i in range(2)]
        pg = [kb.ps("fu_pg%d" % i, [128, 512], F32, es) for i in range(3)]
        pu = [kb.ps("fu_pu%d" % i, [128, 512], F32, es) for i in range(3)]
        sg = [kb.sb("fu_sg%d" % i, [128, 512], F32, es) for i in range(3)]
        ast = [kb.sb("fu_a%d" % i, [128, 512], BF16, es) for i in range(3)]
        n = 0
        for fb in range(DFF // 128):
            i = fb % 2
            wload(kb, ws, wg[i], wcols(wg2d, fb * 128, 128))
            wload(kb, ws, wu[i], wcols(wu2d, fb * 128, 128))
            for tg in range(8):
                sl = slice(tg * 512, (tg + 1) * 512)
                j = n % 3
                n += 1
                for kc in range(8):
                    o.mm(pg[j][:], wg[i][:, kc, :], hT[:, kc, sl], [wg[i], hT], [pg[j]], start=(kc == 0), stop=(kc == 7))
                for kc in range(8):
                    o.mm(pu[j][:], wu[i][:, kc, :], hT[:, kc, sl], [wu[i], hT], [pu[j]], start=(kc == 0), stop=(kc == 7))
                o.act(sg[j][:], pg[j][:], AF.Silu, [pg[j]], [sg[j]])
                o.tt(kb.DVE, ast[j][:], pu[j][:], sg[j][:], ALU.mult, [pu[j], sg[j]], [ast[j]])
                kb.dma(actT.ap[fb * 128:(fb + 1) * 128, sl], ast[j][:], R=[ast[j]], q=kb.ACT)
        kb.barrier()


def phase_ffn_down(kb, cx, layer, actT, x_in, x_out):
    nc = kb.nc
    o = OpsH(kb)
    w2d = cx.dram["w_ffn_down"][layer]
    KC = DFF // 128
    with ExitStack() as es:
        wd = kb.sb("fd_wd", [128, KC, 1024], BF16, es)
        stg = [kb.sb("fd_stg%d" % i, [128, 1024], F32, es) for i in range(2)]
        wv = w2d.rearrange("(c p) f -> p c f", p=128)
        for c in range(KC):
            s_ = stg[c % 2]
            kb.dma(s_[:], wv[:, c, :], Wr=[s_])
            kb.op(kb.DVE if c % 2 == 0 else kb.POOL,
                  lambda: (nc.vector if c % 2 == 0 else nc.gpsimd).tensor_copy(out=wd[:, c, :], in_=s_[:]), R=[s_], Wr=[wd])
        ain = [kb.sb("fd_a%d" % i, [128, KC, 512], BF16, es) for i in range(2)]
        pp = [kb.ps("fd_pp%d" % i, [128, 512], F32, es) for i in range(3)]
        xt = [kb.sb("fd_x%d" % i, [128, 512], F32, es) for i in range(3)]
        n = 0
        av = actT.ap.rearrange("(c p) t -> p c t", p=128)
        for tg in range(8):
            sl = slice(tg * 512, (tg + 1) * 512)
            a_ = ain[tg % 2]
            kb.dma(a_[:], av[:, :, sl], Wr=[a_])
            for fb in range(8):
                j = n % 3
                n += 1
                kb.dma(xt[j][:], x_in.ap[fb * 128:(fb + 1) * 128, sl], Wr=[xt[j]])
                for kc in range(KC):
                    o.mm(pp[j][:], wd[:, kc, fb * 128:(fb + 1) * 128], a_[:, kc, :], [wd, a_], [pp[j]], start=(kc == 0), stop=(kc == KC - 1))
                o.tt(kb.DVE, xt[j][:], pp[j][:], xt[j][:], ALU.add, [pp[j], xt[j]], [xt[j]])
                kb.dma(x_out.ap[fb * 128:(fb + 1) * 128, sl], xt[j][:], R=[xt[j]], q=kb.ACT)
        kb.barrier()


def phase_sb_moba(kb, cx, layer, x_cur, ysb, ymoba, hscr=None):
    with ExitStack() as es:
        pre_mb, pre_sb = {}, {}
        phase_moba(kb, cx, layer, None, ymoba, defer_es=es, pre=pre_mb, alloc_only=True)
        phase_sb(kb, cx, layer, None, ysb, defer_es=es, pre=pre_sb, alloc_only=True)
        with ExitStack() as esh:
            hT = kb.sb("hT_a", [128, 8, T], BF16, esh)
            phase_norm(kb, cx, x_cur, cx.dram["norm1_g"][layer], hT, tgw=256, nbuf=1)
            if hscr is not None:
                for kc in range(8):
                    kb.dma(hscr.ap[kc * 128:(kc + 1) * 128, :], hT[:, kc, :], R=[hT])
            at_mb = phase_moba(kb, cx, layer, hT, ymoba, defer_es=es, pre=pre_mb)
            at_sb = phase_sb(kb, cx, layer, hT, ysb, defer_es=es, pre=pre_sb)
            kb.barrier()
        u_sb, st_sb = at_sb(es)
        u_mb, st_mb = at_mb(es)
        assert len(u_sb) == len(u_mb)
        units = list(zip(u_sb, u_mb))
        stages = [lambda u, i: (st_sb[0](u[0], i), st_mb[0](u[1], i)),
                  lambda u, i: (st_sb[1](u[0], i), st_mb[1](u[1], i)),
                  lambda u, i: st_sb[2](u[0], i)]
        pipeline(units, stages)
        kb.barrier()


def build_program(first_layer=0, n_layers=DEPTH, final=True):
    nc = bass.Bass("TRN2", target_bir_lowering=False)
    cx = Ctx()
    for k, shp in CONST_SHAPES.items():
        cx.dram[k] = nc.dram_tensor(k, shp, const_dtype(k), kind="ExternalInput").ap()
    for k, shp in WEIGHT_SHAPES.items():
        cx.dram[k] = nc.dram_tensor(k, shp, F32, kind="ExternalInput").ap()
    xT = DT(nc.dram_tensor("xT", [D, T], F32, kind="ExternalInput").ap())
    outT = DT(nc.dram_tensor("outT", [D, T], F32, kind="ExternalOutput").ap())
    xa = DT(nc.dram_tensor("x_a", [D, T], F32).ap())
    xb = DT(nc.dram_tensor("x_b", [D, T], F32).ap())
    ys = [DT(nc.dram_tensor("y_mix%d" % n, [W, T], BF16).ap()) for n in range(4)]
    mergedT = DT(nc.dram_tensor("mergedT", [D, T], BF16).ap())
    actT = DT(nc.dram_tensor("actT", [DFF, T], BF16).ap())
    cx.vfirst = DT(nc.dram_tensor("vfirst", [W, T], F32).ap())
    cx.gscr = DT(nc.dram_tensor("gscr", [W, T], BF16).ap())
    cx.bscr = DT(nc.dram_tensor("bscr", [W, T], F32).ap())
    hscr = DT(nc.dram_tensor("hT_scr", [D, T], BF16).ap())
    with ExitStack() as es:
        es.enter_context(nc.allow_non_contiguous_dma(reason="small parameter loads"))
        kb = KB(nc, es)
        load_consts(kb, cx)
        kb.barrier()
        x_cur = xT
        for layer in range(first_layer, first_layer + n_layers):
            phase_sb_moba(kb, cx, layer, x_cur, ys[0], ys[2], hscr)
            with ExitStack() as esl:
                hT = kb.sb("hT", [128, 8, T], BF16, esl)
                for kc in range(8):
                    kb.dma(hT[:, kc, :], hscr.ap[kc * 128:(kc + 1) * 128, :], Wr=[hT])
                phase_rwkv(kb, cx, layer, hT, ys[1])
                phase_hgrn(kb, cx, layer, hT, ys[3])
                phase_merge(kb, cx, layer, hT, ys, mergedT)
                kb.barrier()
            phase_outproj(kb, cx, cx.dram["w_out"][layer], 8, mergedT, x_cur, xa)
            with ExitStack() as esl:
                hT = kb.sb("h2T", [128, 8, T], BF16, esl)
                phase_norm(kb, cx, xa, cx.dram["norm2_g"][layer], hT)
                phase_ffn_up(kb, cx, layer, hT, actT)
                kb.barrier()
            phase_ffn_down(kb, cx, layer, actT, xa, xb)
            x_cur = xb
        if final:
            phase_norm(kb, cx, x_cur, cx.dram["final_g"], None, out_dram=outT)
        kb.barrier()
        print("program: n_ins", kb.n_ins, {e.name: (e.total, e.n_ep) for e in [kb.PE, kb.ACT, kb.DVE, kb.POOL]})
    return nc


_CACHE = {}


def kernel(**inputs):
    x = np.asarray(inputs["x"], dtype=np.float32)
    consts = host_consts()
    if "nc" not in _CACHE:
        _CACHE["nc"] = build_program()
    nc = _CACHE["nc"]
    shared = {k: np.ascontiguousarray(np.asarray(inputs[k], dtype=np.float32)) for k in WEIGHT_SHAPES}
    shared.update(consts)
    in_maps = []
    for b in range(NCORES):
        m = dict(shared)
        m["xT"] = np.ascontiguousarray(x[b].T)
        in_maps.append(m)
    res = run_bass_kernel_spmd(nc, in_maps, core_ids=list(range(NCORES)))
    out = np.stack([np.asarray(res.results[b]["outT"], dtype=np.float32).T for b in range(NCORES)], axis=0)
    return np.ascontiguousarray(out)
```
